# Optimizing a Trainium2 kernel written in Bass

```python
import math
import jax, jax.numpy as jnp
from jax import lax
import numpy as np

D_MODEL = 1024
BATCH = 8
SEQ = 4096
DEPTH = 4

GRID_W = 64
CTX_LEN = 256
N_MIXERS = 4
BLOCK = 128
EPS = 1e-6
ROPE_BASE = 10000.0
ADALN_CHUNKS = 6

ATT_HEADS = 16
ATT_KV_HEADS = 4
ATT_GROUP = ATT_HEADS // ATT_KV_HEADS
ATT_HEAD_DIM = D_MODEL // ATT_HEADS
ATT_IN_DIM = (ATT_HEADS + 2 * ATT_KV_HEADS) * ATT_HEAD_DIM
WINDOW = 128

SSM_D_INNER = 2 * D_MODEL
SSM_HEAD_DIM = 64
SSM_HEADS = SSM_D_INNER // SSM_HEAD_DIM
SSM_GROUPS = 4
SSM_HPG = SSM_HEADS // SSM_GROUPS
SSM_STATE = 128
SSM_CONV = 5
SSM_CONV_DIM = SSM_D_INNER + 2 * SSM_GROUPS * SSM_STATE
SSM_IN_DIM = SSM_D_INNER + SSM_CONV_DIM + 2 * SSM_HEADS

ML_HEADS = 8
ML_QK_DIM = D_MODEL // (2 * ML_HEADS)
ML_V_DIM = D_MODEL // ML_HEADS
ML_IN_DIM = 2 * ML_HEADS * ML_QK_DIM + 2 * ML_HEADS * ML_V_DIM + 4 * ML_HEADS

MLA_HEADS = 16
MLA_Q_RANK = D_MODEL // 4
MLA_KV_RANK = D_MODEL // 4
MLA_NOPE = 64
MLA_ROPE = 32
MLA_V = 64
MLA_IN_DIM = MLA_Q_RANK + MLA_KV_RANK + MLA_ROPE

MOE_GROUPS = 4
MOE_PER_GROUP = 4
MOE_EXPERTS = MOE_GROUPS * MOE_PER_GROUP
MOE_FF = D_MODEL // 4
MOE_TOPK = 2

N_ATT = (DEPTH + 3) // 4
N_SSM = (DEPTH + 2) // 4
N_ML = (DEPTH + 1) // 4
N_MLA = DEPTH // 4

kernel_name = "hybrid_interleaved_diffusion_trunk"


def rmsnorm(x, g):
    xf = x.astype(jnp.float32)
    y = xf * lax.rsqrt(jnp.mean(xf * xf, axis=-1, keepdims=True) + EPS)
    return (y * g.astype(jnp.float32)).astype(x.dtype)


def group_rmsnorm(x, g, n_groups):
    shp = x.shape
    xf = x.astype(jnp.float32).reshape(*shp[:-1], n_groups, shp[-1] // n_groups)
    y = xf * lax.rsqrt(jnp.mean(xf * xf, axis=-1, keepdims=True) + EPS)
    return (y.reshape(shp) * g.astype(jnp.float32)).astype(x.dtype)


def axial_rope(n_tokens, rot_dim):
    rows = n_tokens // GRID_W
    row = jnp.repeat(jnp.arange(rows), GRID_W).astype(jnp.float32)
    col = jnp.tile(jnp.arange(GRID_W), rows).astype(jnp.float32)
    quarter = rot_dim // 4
    inv = ROPE_BASE ** (-jnp.arange(quarter, dtype=jnp.float32) / quarter)
    ang = jnp.concatenate([row[:, None] * inv, col[:, None] * inv], axis=-1)
    return jnp.cos(ang), jnp.sin(ang)


def apply_rope(x, cos, sin):
    shape = (1, cos.shape[0]) + (1,) * (x.ndim - 3) + (cos.shape[1],)
    cos = cos.reshape(shape).astype(x.dtype)
    sin = sin.reshape(shape).astype(x.dtype)
    x1, x2 = jnp.split(x, 2, axis=-1)
    return jnp.concatenate([x1 * cos - x2 * sin, x2 * cos + x1 * sin], axis=-1)


def softmax_with_sink(logits, sink):
    s = jnp.broadcast_to(sink[:, :, None, None], logits.shape[:-1] + (1,))
    p = jax.nn.softmax(jnp.concatenate([logits, s], axis=-1), axis=-1)
    return p[..., :-1]


def centred_dwconv(x, w, b):
    k_w = w.shape[0]
    pad = k_w // 2
    n = x.shape[1]
    xp = jnp.pad(x, ((0, 0), (pad, pad), (0, 0)))
    return b + sum(xp[:, t:t + n] * w[t] for t in range(k_w))


def to_chunks(t):
    bsz, n = t.shape[:2]
    return jnp.moveaxis(t.reshape(bsz, n // BLOCK, BLOCK, *t.shape[2:]), 1, 0)


def from_chunks(t):
    t = jnp.moveaxis(t, 0, 1)
    return t.reshape(t.shape[0], t.shape[1] * t.shape[2], *t.shape[3:])


def windowed_gqa(hc, hx, w_in, sink, w_out, need_ctx):
    bsz, n_lat, _ = hx.shape
    n_blk = n_lat // BLOCK
    scale = ATT_HEAD_DIM ** -0.5
    sink = sink.astype(jnp.float32).reshape(ATT_KV_HEADS, ATT_GROUP)

    def project(h):
        qkv = h @ w_in
        q, k, v = jnp.split(qkv, [ATT_HEADS * ATT_HEAD_DIM, (ATT_HEADS + ATT_KV_HEADS) * ATT_HEAD_DIM], axis=-1)
        lead = h.shape[:2]
        return (q.reshape(*lead, ATT_KV_HEADS, ATT_GROUP, ATT_HEAD_DIM),
                k.reshape(*lead, ATT_KV_HEADS, ATT_HEAD_DIM),
                v.reshape(*lead, ATT_KV_HEADS, ATT_HEAD_DIM))

    qc, kc, vc = project(hc)
    qx, kx, vx = project(hx)
    cos, sin = axial_rope(n_lat, ATT_HEAD_DIM)
    qx = apply_rope(qx, cos, sin)
    kx = apply_rope(kx, cos, sin)

    yc = None
    if need_ctx:
        lg = jnp.einsum('bqhgd,bkhd->bhgqk', qc, kc).astype(jnp.float32) * scale
        p = softmax_with_sink(lg, sink).astype(vc.dtype)
        oc = jnp.einsum('bhgqk,bkhd->bqhgd', p, vc)
        yc = oc.reshape(bsz, hc.shape[1], ATT_HEADS * ATT_HEAD_DIM) @ w_out

    def band(t):
        tp = jnp.pad(t, ((0, 0), (BLOCK, BLOCK), (0, 0), (0, 0)))
        tb = tp.reshape(bsz, n_blk + 2, BLOCK, ATT_KV_HEADS, ATT_HEAD_DIM)
        tb = jnp.concatenate([tb[:, :-2], tb[:, 1:-1], tb[:, 2:]], axis=2)
        return jnp.moveaxis(tb, 1, 0)

    kb, vb = band(kx), band(vx)
    qb = to_chunks(qx)
    qi = jnp.arange(BLOCK)[:, None]
    kj = jnp.arange(3 * BLOCK)[None, :]
    in_win = (kj >= qi + BLOCK - WINDOW) & (kj <= qi + BLOCK + WINDOW)
    kpos = (jnp.arange(n_blk)[:, None] - 1) * BLOCK + jnp.arange(3 * BLOCK)[None, :]
    valid = (kpos >= 0) & (kpos < n_lat)
    mask = in_win[None] & valid[:, None, :]

    def block_attn(args):
        q, k, v, m = args
        ll = jnp.einsum('bqhgd,bkhd->bhgqk', q, k).astype(jnp.float32) * scale
        ll = jnp.where(m, ll, -jnp.inf)
        lcx = jnp.einsum('bqhgd,bkhd->bhgqk', q, kc).astype(jnp.float32) * scale
        p = softmax_with_sink(jnp.concatenate([ll, lcx], axis=-1), sink).astype(v.dtype)
        return (jnp.einsum('bhgqk,bkhd->bqhgd', p[..., :3 * BLOCK], v)
                + jnp.einsum('bhgqk,bkhd->bqhgd', p[..., 3 * BLOCK:], vc))

    ob = lax.map(block_attn, (qb, kb, vb, mask))
    yx = from_chunks(ob).reshape(bsz, n_lat, ATT_HEADS * ATT_HEAD_DIM) @ w_out
    return yc, yx


def ssd_scan(u, a, bm, cm, state0):
    tri = jnp.tril(jnp.ones((BLOCK, BLOCK), bool))

    def step(state, inp):
        uq, aq, bq, cq = inp
        acum = jnp.cumsum(aq, axis=1)
        seg = acum[:, :, None] - acum[:, None, :]
        lmat = jnp.exp(jnp.where(tri[None, :, :, None, None], seg, -jnp.inf))
        cb = jnp.einsum('blgn,bsgn->blsg', cq, bq)
        y = jnp.einsum('blsg,blsge,bsgep->blgep', cb, lmat, uq)
        y = y + jnp.einsum('blgn,bgepn->blgep', cq, state) * jnp.exp(acum)[..., None]
        decay_end = jnp.exp(acum[:, -1:] - acum)
        state = (state * jnp.exp(acum[:, -1])[..., None, None]
                 + jnp.einsum('bsgn,bsge,bsgep->bgepn', bq, decay_end, uq))
        return state, y

    state, ys = lax.scan(step, state0, (to_chunks(u), to_chunks(a), to_chunks(bm), to_chunks(cm)))
    return from_chunks(ys), state


def mamba2_bidir(hc, hx, w_in, conv_w, conv_b, dt_bias, a_log, d_skip, norm_g, w_out, need_ctx):
    a_neg = -jnp.exp(a_log.astype(jnp.float32))

    def project(h):
        p = h @ w_in
        z, xbc, dt = jnp.split(p, [SSM_D_INNER, SSM_D_INNER + SSM_CONV_DIM], axis=-1)
        xbc = jax.nn.silu(centred_dwconv(xbc, conv_w, conv_b))
        xs, bm, cm = jnp.split(xbc, [SSM_D_INNER, SSM_D_INNER + SSM_GROUPS * SSM_STATE], axis=-1)
        return z, xs, bm, cm, dt.reshape(*h.shape[:2], 2, SSM_HEADS)

    def run(xs, bm, cm, dt, d, state0):
        bsz, n = xs.shape[:2]
        dtd = jax.nn.softplus(dt[:, :, d].astype(jnp.float32) + dt_bias[d].astype(jnp.float32))
        dtg = dtd.reshape(bsz, n, SSM_GROUPS, SSM_HPG)
        xh = xs.reshape(bsz, n, SSM_GROUPS, SSM_HPG, SSM_HEAD_DIM)
        u = xh * dtg[..., None]
        a = dtg * a_neg[d].reshape(SSM_GROUPS, SSM_HPG)
        bg = bm.reshape(bsz, n, SSM_GROUPS, SSM_STATE)
        cg = cm.reshape(bsz, n, SSM_GROUPS, SSM_STATE)
        if d == 1:
            u, a, bg, cg = (jnp.flip(t, axis=1) for t in (u, a, bg, cg))
        y, st = ssd_scan(u, a, bg, cg, state0)
        if d == 1:
            y = jnp.flip(y, axis=1)
        y = y + d_skip[d].reshape(SSM_GROUPS, SSM_HPG)[:, :, None] * xh
        return y.reshape(bsz, n, SSM_D_INNER), st

    zc, xc, bc, cc, dtc = project(hc)
    zx, xx, bx, cx, dtx = project(hx)
    bsz = hx.shape[0]
    zero = jnp.zeros((bsz, SSM_GROUPS, SSM_HPG, SSM_HEAD_DIM, SSM_STATE), jnp.float32)
    yc_f, st_f = run(xc, bc, cc, dtc, 0, zero)
    yc_b, st_b = run(xc, bc, cc, dtc, 1, zero)
    yx_f, _ = run(xx, bx, cx, dtx, 0, st_f)
    yx_b, _ = run(xx, bx, cx, dtx, 1, st_b)

    def out(y, z):
        return group_rmsnorm(y * jax.nn.silu(z), norm_g, SSM_GROUPS) @ w_out

    yc = out(yc_f + yc_b, zc) if need_ctx else None
    return yc, out(yx_f + yx_b, zx)


def mlstm_scan(q, k, v, ig, lf, state0):
    tri = jnp.tril(jnp.ones((BLOCK, BLOCK), bool))

    def step(carry, inp):
        cs, ns, ms = carry
        qq, kq, vq, iq, fq = inp
        bcum = jnp.cumsum(fq, axis=1)
        logd = bcum[:, :, None] - bcum[:, None, :] + iq[:, None, :]
        logd = jnp.where(tri[None, :, :, None], logd, -jnp.inf)
        inter = bcum + ms[:, None]
        mt = jnp.maximum(inter, jnp.max(logd, axis=2))
        dmat = jnp.exp(logd - mt[:, :, None])
        sc = jnp.einsum('bthd,bshd->btsh', qq, kq) * dmat
        w_int = jnp.exp(inter - mt)
        num = (jnp.einsum('btsh,bshv->bthv', sc, vq)
               + w_int[..., None] * jnp.einsum('bthd,bhdv->bthv', qq, cs))
        den = jnp.sum(sc, axis=2) + w_int * jnp.einsum('bthd,bhd->bth', qq, ns)
        h = num / jnp.maximum(jnp.abs(den), jnp.exp(-mt))[..., None]
        tot = bcum[:, -1]
        logw = tot[:, None] - bcum + iq
        m_new = jnp.maximum(tot + ms, jnp.max(logw, axis=1))
        ws = jnp.exp(logw - m_new[:, None])
        cw = jnp.exp(tot + ms - m_new)
        c_new = cw[..., None, None] * cs + jnp.einsum('bsh,bshd,bshv->bhdv', ws, kq, vq)
        n_new = cw[..., None] * ns + jnp.einsum('bsh,bshd->bhd', ws, kq)
        return (c_new, n_new, m_new), h

    state, hs = lax.scan(step, state0, tuple(to_chunks(t) for t in (q, k, v, ig, lf)))
    return from_chunks(hs), state


def mlstm_bidir(hc, hx, w_in, gate_b, norm_g, w_out, need_ctx):
    qk = ML_HEADS * ML_QK_DIM
    vd = ML_HEADS * ML_V_DIM

    def project(h):
        p = h @ w_in
        q, k, v, o, g = jnp.split(p, [qk, 2 * qk, 2 * qk + vd, 2 * qk + 2 * vd], axis=-1)
        lead = h.shape[:2]
        q = q.reshape(*lead, ML_HEADS, ML_QK_DIM)
        k = k.reshape(*lead, ML_HEADS, ML_QK_DIM) * (ML_QK_DIM ** -0.5)
        v = v.reshape(*lead, ML_HEADS, ML_V_DIM)
        g = g.reshape(*lead, 4, ML_HEADS).astype(jnp.float32) + gate_b.astype(jnp.float32)
        return q, k, v, o, g

    def run(q, k, v, g, d, state0):
        ig = g[:, :, 2 * d]
        lf = jax.nn.log_sigmoid(g[:, :, 2 * d + 1])
        if d == 1:
            q, k, v, ig, lf = (jnp.flip(t, axis=1) for t in (q, k, v, ig, lf))
        h, st = mlstm_scan(q, k, v, ig, lf, state0)
        if d == 1:
            h = jnp.flip(h, axis=1)
        return h, st

    qc, kc, vc, oc, gc = project(hc)
    qx, kx, vx, ox, gx = project(hx)
    bsz = hx.shape[0]
    zero = (jnp.zeros((bsz, ML_HEADS, ML_QK_DIM, ML_V_DIM), jnp.float32),
            jnp.zeros((bsz, ML_HEADS, ML_QK_DIM), jnp.float32),
            jnp.zeros((bsz, ML_HEADS), jnp.float32))
    hc_f, st_f = run(qc, kc, vc, gc, 0, zero)
    hc_b, st_b = run(qc, kc, vc, gc, 1, zero)
    hx_f, _ = run(qx, kx, vx, gx, 0, st_f)
    hx_b, _ = run(qx, kx, vx, gx, 1, st_b)

    def out(h, o):
        h = h.reshape(*h.shape[:2], vd)
        return (group_rmsnorm(h, norm_g, ML_HEADS) * jax.nn.sigmoid(o)) @ w_out

    yc = out(hc_f + hc_b, oc) if need_ctx else None
    return yc, out(hx_f + hx_b, ox)


def mla(hc, hx, w_in, q_norm_g, w_q_up, kv_norm_g, w_kv_up, w_out, need_ctx):
    bsz, n_lat, _ = hx.shape
    n_blk = n_lat // BLOCK
    scale = (MLA_NOPE + MLA_ROPE) ** -0.5

    def project(h, rope):
        p = h @ w_in
        cq, ckv, kr = jnp.split(p, [MLA_Q_RANK, MLA_Q_RANK + MLA_KV_RANK], axis=-1)
        lead = h.shape[:2]
        q = (rmsnorm(cq, q_norm_g) @ w_q_up).reshape(*lead, MLA_HEADS, MLA_NOPE + MLA_ROPE)
        kv = (rmsnorm(ckv, kv_norm_g) @ w_kv_up).reshape(*lead, MLA_HEADS, MLA_NOPE + MLA_V)
        qn, qr = jnp.split(q, [MLA_NOPE], axis=-1)
        kn, v = jnp.split(kv, [MLA_NOPE], axis=-1)
        if rope is not None:
            qr = apply_rope(qr, *rope)
            kr = apply_rope(kr, *rope)
        return qn, qr, kn, kr, v

    qn_c, qr_c, kn_c, kr_c, v_c = project(hc, None)
    qn_x, qr_x, kn_x, kr_x, v_x = project(hx, axial_rope(n_lat, MLA_ROPE))

    def scores(qn, qr, kn, kr):
        return (jnp.einsum('bqhd,bkhd->bhqk', qn, kn)
                + jnp.einsum('bqhd,bkd->bhqk', qr, kr)).astype(jnp.float32) * scale

    yc = None
    if need_ctx:
        p = jax.nn.softmax(scores(qn_c, qr_c, kn_c, kr_c), axis=-1).astype(v_c.dtype)
        oc = jnp.einsum('bhqk,bkhd->bqhd', p, v_c)
        yc = oc.reshape(bsz, hc.shape[1], MLA_HEADS * MLA_V) @ w_out

    def block_attn(args):
        qn, qr = args
        s = jnp.concatenate([scores(qn, qr, kn_x, kr_x), scores(qn, qr, kn_c, kr_c)], axis=-1)
        p = jax.nn.softmax(s, axis=-1).astype(v_x.dtype)
        return (jnp.einsum('bhqk,bkhd->bqhd', p[..., :n_lat], v_x)
                + jnp.einsum('bhqk,bkhd->bqhd', p[..., n_lat:], v_c))

    ob = lax.map(block_attn, (to_chunks(qn_x), to_chunks(qr_x)))
    yx = from_chunks(ob).reshape(bsz, n_lat, MLA_HEADS * MLA_V) @ w_out
    return yc, yx


def hier_moe(h, w_rg, b_rg, w_re, b_re, w_gate, w_up, w_down):
    lead = h.shape[:-1]
    lg = (h @ w_rg + b_rg).astype(jnp.float32)
    g_sel = jnp.argmax(lg, axis=-1)
    p_g = jnp.take_along_axis(jax.nn.softmax(lg, axis=-1), g_sel[..., None], axis=-1)
    le = (h @ w_re + b_re).astype(jnp.float32).reshape(*lead, MOE_GROUPS, MOE_PER_GROUP)
    le_sel = jnp.take_along_axis(le, g_sel[..., None, None], axis=-2)[..., 0, :]
    top_v, top_i = lax.top_k(le_sel, MOE_TOPK)
    w = jax.nn.softmax(top_v, axis=-1) * p_g
    eid = g_sel[..., None] * MOE_PER_GROUP + top_i
    combine = jnp.sum(jax.nn.one_hot(eid, MOE_EXPERTS, dtype=jnp.float32) * w[..., None], axis=-2)
    combine = combine.astype(h.dtype)
    out = jnp.zeros_like(h)
    for e in range(MOE_EXPERTS):
        act = jax.nn.silu(h @ w_gate[e]) * (h @ w_up[e])
        out = out + combine[..., e:e + 1] * (act @ w_down[e])
    return out


def setup_inputs(seed: int = 0) -> dict:
    key = jax.random.key(seed)
    ks = iter(jax.random.split(key, 64))
    D = D_MODEL

    def nrm(shape, scale):
        return jax.random.normal(next(ks), shape, jnp.float32) * scale

    def gain(shape):
        return 1.0 + nrm(shape, 0.02)

    def unif(shape, lo, hi):
        return jax.random.uniform(next(ks), shape, jnp.float32, lo, hi)

    dt0 = jnp.exp(unif((N_SSM, 2, SSM_HEADS), math.log(1e-3), math.log(1e-1)))
    gate_b = jnp.stack([nrm((N_ML, ML_HEADS), 0.1), 3.0 + unif((N_ML, ML_HEADS), 0.0, 3.0),
                        nrm((N_ML, ML_HEADS), 0.1), 3.0 + unif((N_ML, ML_HEADS), 0.0, 3.0)], axis=1)
    return {
        "x": nrm((BATCH, SEQ, D), 1.0),
        "c": nrm((BATCH, D), 1.0),
        "ctx": nrm((BATCH, CTX_LEN, D), 1.0),
        "c_ctx": nrm((D,), 1.0),
        "norm1_g": gain((DEPTH, D)),
        "norm2_g": gain((DEPTH, D)),
        "w_mod": nrm((DEPTH, D, ADALN_CHUNKS * D), 0.5 * D ** -0.5),
        "b_mod": nrm((DEPTH, ADALN_CHUNKS * D), 0.02),
        "moe_w_group": nrm((DEPTH, D, MOE_GROUPS), D ** -0.5),
        "moe_b_group": nrm((DEPTH, MOE_GROUPS), 0.01),
        "moe_w_expert": nrm((DEPTH, D, MOE_EXPERTS), D ** -0.5),
        "moe_b_expert": nrm((DEPTH, MOE_EXPERTS), 0.01),
        "moe_w_gate": nrm((DEPTH, MOE_EXPERTS, D, MOE_FF), D ** -0.5),
        "moe_w_up": nrm((DEPTH, MOE_EXPERTS, D, MOE_FF), D ** -0.5),
        "moe_w_down": nrm((DEPTH, MOE_EXPERTS, MOE_FF, D), MOE_FF ** -0.5),
        "attn_w_in": nrm((N_ATT, D, ATT_IN_DIM), D ** -0.5),
        "attn_sink": nrm((N_ATT, ATT_HEADS), 0.5),
        "attn_w_out": nrm((N_ATT, ATT_HEADS * ATT_HEAD_DIM, D), (ATT_HEADS * ATT_HEAD_DIM) ** -0.5),
        "ssm_w_in": nrm((N_SSM, D, SSM_IN_DIM), D ** -0.5),
        "ssm_conv_w": nrm((N_SSM, SSM_CONV, SSM_CONV_DIM), SSM_CONV ** -0.5),
        "ssm_conv_b": nrm((N_SSM, SSM_CONV_DIM), 0.02),
        "ssm_dt_bias": dt0 + jnp.log(-jnp.expm1(-dt0)),
        "ssm_a_log": jnp.log(unif((N_SSM, 2, SSM_HEADS), 1.0, 16.0)),
        "ssm_d": 1.0 + nrm((N_SSM, 2, SSM_HEADS), 0.1),
        "ssm_norm_g": gain((N_SSM, SSM_D_INNER)),
        "ssm_w_out": nrm((N_SSM, SSM_D_INNER, D), SSM_D_INNER ** -0.5),
        "mlstm_w_in": nrm((N_ML, D, ML_IN_DIM), D ** -0.5),
        "mlstm_gate_b": gate_b,
        "mlstm_norm_g": gain((N_ML, ML_HEADS * ML_V_DIM)),
        "mlstm_w_out": nrm((N_ML, ML_HEADS * ML_V_DIM, D), (ML_HEADS * ML_V_DIM) ** -0.5),
        "mla_w_in": nrm((N_MLA, D, MLA_IN_DIM), D ** -0.5),
        "mla_q_norm_g": gain((N_MLA, MLA_Q_RANK)),
        "mla_w_q_up": nrm((N_MLA, MLA_Q_RANK, MLA_HEADS * (MLA_NOPE + MLA_ROPE)), MLA_Q_RANK ** -0.5),
        "mla_kv_norm_g": gain((N_MLA, MLA_KV_RANK)),
        "mla_w_kv_up": nrm((N_MLA, MLA_KV_RANK, MLA_HEADS * (MLA_NOPE + MLA_V)), MLA_KV_RANK ** -0.5),
        "mla_w_out": nrm((N_MLA, MLA_HEADS * MLA_V, D), (MLA_HEADS * MLA_V) ** -0.5),
        "final_norm_g": gain((D,)),
    }


def reference(x, c, ctx, c_ctx, norm1_g, norm2_g, w_mod, b_mod,
              moe_w_group, moe_b_group, moe_w_expert, moe_b_expert, moe_w_gate, moe_w_up, moe_w_down,
              attn_w_in, attn_sink, attn_w_out,
              ssm_w_in, ssm_conv_w, ssm_conv_b, ssm_dt_bias, ssm_a_log, ssm_d, ssm_norm_g, ssm_w_out,
              mlstm_w_in, mlstm_gate_b, mlstm_norm_g, mlstm_w_out,
              mla_w_in, mla_q_norm_g, mla_w_q_up, mla_kv_norm_g, mla_w_kv_up, mla_w_out,
              final_norm_g):
    xs, cs = x, ctx
    for i in range(DEPTH):
        kind, j = i % N_MIXERS, i // N_MIXERS
        need_ctx = i < DEPTH - 1
        mod_x = jax.nn.silu(c) @ w_mod[i] + b_mod[i]
        mod_c = jax.nn.silu(c_ctx) @ w_mod[i] + b_mod[i]
        sh1x, sc1x, g1x, sh2x, sc2x, g2x = jnp.split(mod_x[:, None, :], ADALN_CHUNKS, axis=-1)
        sh1c, sc1c, g1c, sh2c, sc2c, g2c = jnp.split(mod_c, ADALN_CHUNKS, axis=-1)

        hx = rmsnorm(xs, norm1_g[i]) * (1.0 + sc1x) + sh1x
        hc = rmsnorm(cs, norm1_g[i]) * (1.0 + sc1c) + sh1c
        if kind == 0:
            yc, yx = windowed_gqa(hc, hx, attn_w_in[j], attn_sink[j], attn_w_out[j], need_ctx)
        elif kind == 1:
            yc, yx = mamba2_bidir(hc, hx, ssm_w_in[j], ssm_conv_w[j], ssm_conv_b[j], ssm_dt_bias[j],
                                  ssm_a_log[j], ssm_d[j], ssm_norm_g[j], ssm_w_out[j], need_ctx)
        elif kind == 2:
            yc, yx = mlstm_bidir(hc, hx, mlstm_w_in[j], mlstm_gate_b[j], mlstm_norm_g[j],
                                 mlstm_w_out[j], need_ctx)
        else:
            yc, yx = mla(hc, hx, mla_w_in[j], mla_q_norm_g[j], mla_w_q_up[j], mla_kv_norm_g[j],
                         mla_w_kv_up[j], mla_w_out[j], need_ctx)
        xs = xs + g1x * yx

        moe_args = (moe_w_group[i], moe_b_group[i], moe_w_expert[i], moe_b_expert[i],
                    moe_w_gate[i], moe_w_up[i], moe_w_down[i])
        hx = rmsnorm(xs, norm2_g[i]) * (1.0 + sc2x) + sh2x
        xs = xs + g2x * hier_moe(hx, *moe_args)
        if need_ctx:
            cs = cs + g1c * yc
            hc = rmsnorm(cs, norm2_g[i]) * (1.0 + sc2c) + sh2c
            cs = cs + g2c * hier_moe(hc, *moe_args)
    return rmsnorm(xs, final_norm_g)
```

```python
import numpy as np
import concourse.bass as bass
import concourse.mybir as mybir

F32 = mybir.dt.float32
BF16 = mybir.dt.bfloat16
AF = mybir.ActivationFunctionType
ALU = mybir.AluOpType
AX = mybir.AxisListType


class Buf:
    __slots__ = ("w", "r", "name")

    def __init__(self, name=""):
        self.w = None
        self.r = []
        self.name = name


class Ctx:
    EPOCH = 30000

    def __init__(self, nc, n_dma=None):
        self.nc = nc
        self.E = {"pe": nc.tensor, "act": nc.scalar, "dve": nc.vector, "pool": nc.gpsimd, "sp": nc.sync}
        self.csem = {}
        self.seen = {e: {} for e in self.E}
        self.semid = {}
        n_dma = n_dma or {"sp": 48, "act": 2, "pool": 30}
        self.dslots = {}
        self.drr = {}
        for q, n in n_dma.items():
            self.dslots[q] = [[self._new_sem(f"d{q}{i}"), 0] for i in range(n)]
            self.drr[q] = 0
        for e in ("pe", "act", "dve", "pool"):
            self.csem[e] = [self._new_sem(f"c{e}0"), 0, 0]
        self.sb_off = 0
        self.sb_base = 16512
        self.sb_cap = 229344 - 16512
        self.n_alloc = 0
        self.n_ins = 0
        self.n_wait = 0

    def _new_sem(self, name):
        s = self.nc.alloc_semaphore(name)
        self.semid[id(s)] = s
        return s

    def sb_mark(self):
        return self.sb_off

    def sb_release(self, mark):
        self.sb_off = mark

    def sb(self, shape, dtype, name=None):
        esz = 4 if dtype == F32 else 2
        if dtype in (mybir.dt.int32, mybir.dt.uint32):
            esz = 4
        n = 1
        for s in shape[1:]:
            n *= s
        nbytes = (n * esz + 63) // 64 * 64
        off = self.sb_off
        if off + nbytes > self.sb_cap:
            raise RuntimeError(f"SBUF overflow: want {nbytes} at {off} cap {self.sb_cap} ({name})")
        self.sb_off += nbytes
        self.n_alloc += 1
        t = self.nc.alloc_sbuf_tensor_at(f"{name or 't'}_{self.n_alloc}", list(shape), dtype, offset=self._abs(off))
        return t

    def _abs(self, off):
        return self.sb_base + off

    def _wait(self, eng, ev):
        if ev is None:
            return
        sem, val = ev
        k = id(sem)
        if self.seen[eng].get(k, 0) >= val:
            return
        self.E[eng].wait_ge(sem, val)
        self.n_wait += 1
        self.seen[eng][k] = val

    def _deps(self, eng, reads, writes):
        for b in reads:
            if b.w is not None and not (eng == "pe" and b.w[2] == "pe"):
                self._wait(eng, b.w[:2])
        for b in writes:
            if b.w is not None and not (eng == "pe" and b.w[2] == "pe"):
                self._wait(eng, b.w[:2])
            for r in b.r:
                if not (eng == "pe" and r[2] == "pe"):
                    self._wait(eng, r[:2])

    def _record(self, ev, reads, writes):
        for b in reads:
            b.r.append(ev)
            if len(b.r) > 64:
                b.r = b.r[-64:] if False else b.r
        for b in writes:
            b.w = ev
            b.r = []

    def _signal(self, eng, ins):
        st = self.csem[eng]
        if st[1] >= self.EPOCH:
            st = self.csem[eng] = [self._new_sem(f"c{eng}{st[2] + 1}"), 0, st[2] + 1]
        st[1] += 1
        ins.then_inc(st[0], 1)
        return (st[0], st[1], eng)

    def op(self, eng, fn, reads=(), writes=()):
        self._deps(eng, reads, writes)
        ins = fn(self.E[eng])
        self.n_ins += 1
        ev = self._signal(eng, ins)
        self._record(ev, reads, writes)
        return ev

    def mm(self, out, pairs, reads=(), writes=(), start=True, stop=True):
        self._deps("pe", reads, writes)
        n = len(pairs)
        ins = None
        for i, (l, r) in enumerate(pairs):
            ins = self.nc.tensor.matmul(out, l, r, start=(start and i == 0), stop=(stop and i == n - 1))
            self.n_ins += 1
        ev = self._signal("pe", ins)
        self._record(ev, reads, writes)
        return ev

    def mm_multi(self, groups, reads=(), writes=()):
        self._deps("pe", reads, writes)
        ins = None
        for out, pairs in groups:
            n = len(pairs)
            for i, (l, r) in enumerate(pairs):
                ins = self.nc.tensor.matmul(out, l, r, start=(i == 0), stop=(i == n - 1))
                self.n_ins += 1
        ev = self._signal("pe", ins)
        self._record(ev, reads, writes)
        return ev

    def tr(self, out, in_, ident, reads=(), writes=()):
        self._deps("pe", reads, writes)
        ins = self.nc.tensor.transpose(out, in_, ident)
        self.n_ins += 1
        ev = self._signal("pe", ins)
        self._record(ev, reads, writes)
        return ev

    def tr_multi(self, items, reads=(), writes=()):
        self._deps("pe", reads, writes)
        ins = None
        for out, in_, ident in items:
            ins = self.nc.tensor.transpose(out, in_, ident)
            self.n_ins += 1
        ev = self._signal("pe", ins)
        self._record(ev, reads, writes)
        return ev

    def dma(self, q, out, in_, reads=(), writes=()):
        self._deps(q, reads, writes)
        slots = self.dslots[q]
        i = self.drr[q]
        self.drr[q] = (i + 1) % len(slots)
        sl = slots[i]
        if sl[1] > 0:
            self._wait(q, (sl[0], sl[1]))
        ins = self.E[q].dma_start(out=out, in_=in_)
        self.n_ins += 1
        sl[1] += 16
        ins.then_inc(sl[0], 16)
        ev = (sl[0], sl[1], "dma")
        self._record(ev, reads, writes)
        return ev

    def barrier(self):
        evs = []
        for e, st in self.csem.items():
            if st[1] > 0:
                evs.append((st[0], st[1]))
        for q, slots in self.dslots.items():
            for sl in slots:
                if sl[1] > 0:
                    evs.append((sl[0], sl[1]))
        for eng in self.E:
            for ev in evs:
                self._wait(eng, ev)

    def finish(self, eng="sp"):
        evs = []
        for e, st in self.csem.items():
            if st[1] > 0:
                evs.append((st[0], st[1]))
        for q, slots in self.dslots.items():
            for sl in slots:
                if sl[1] > 0:
                    evs.append((sl[0], sl[1]))
        for ev in evs:
            self._wait(eng, ev)
from concourse.bass_utils import run_bass_kernel_spmd
D = 1024
EPS = 1e-6


def host_consts(n_lat):
    ident = np.eye(128, dtype=np.float32)
    ones = np.ones((128, 128), np.float32)
    j = np.arange(128)[:, None]
    i = np.arange(128)[None, :]
    tri_le = (j <= i).astype(np.float32)
    tri_ge = (j >= i).astype(np.float32)
    cm = np.stack([ident, ones, tri_le, tri_ge], axis=1)
    sel = np.zeros((32, 32, 128), np.float32)
    for h in range(32):
        sel[h, h, :] = 1.0
    rows = n_lat // 64
    row = np.repeat(np.arange(rows), 64).astype(np.float32)
    col = np.tile(np.arange(64), rows).astype(np.float32)

    def tab(rot_dim, nrep):
        q = rot_dim // 4
        inv = (10000.0 ** (-np.arange(q, dtype=np.float32) / q)).astype(np.float32)
        ang = np.concatenate([row[:, None] * inv, col[:, None] * inv], axis=-1).astype(np.float32)
        cs = np.cos(ang).astype(np.float32).T
        sn = np.sin(ang).astype(np.float32).T
        cs = np.concatenate([cs] * (2 * nrep), axis=0)
        sn = np.concatenate([sn] * (2 * nrep), axis=0)
        return np.ascontiguousarray(np.stack([cs, sn], axis=0))

    r32 = tab(32, 1)
    L = r32.shape[2]
    r96 = np.ascontiguousarray(np.concatenate([np.stack([np.ones((64, L), np.float32), np.zeros((64, L), np.float32)], axis=0), r32], axis=1))
    return {"cmat": np.ascontiguousarray(cm), "sel32": sel, "rope64": tab(64, 1), "rope32": r32, "rope96": r96}


class Model:
    def __init__(self, n_lat, n_ctx, kinds, debug=False):
        self.NL, self.NCX = n_lat, n_ctx
        self.T = n_lat + n_ctx
        self.NT = self.T // 128
        self.NCT = n_ctx // 128
        self.NLT = n_lat // 128
        self.kinds = kinds
        self.depth = len(kinds)
        self.nc = bass.Bass("TRN2", target_bir_lowering=False)
        self.c = Ctx(self.nc)
        self.w = {}

    def inp(self, name, shape, dtype=F32):
        t = self.nc.dram_tensor(name, list(shape), dtype, kind="ExternalInput")
        self.w[name] = t
        return t

    def scratch(self, name, shape, dtype):
        return self.nc.dram_tensor(name, list(shape), dtype)

    def declare(self, shapes):
        for k, s in shapes.items():
            self.inp(k, s)

    def groups(self, gsz, with_ctx=True):
        g = []
        if with_ctx:
            g.append((0, self.NCT, True))
        t = self.NCT
        while t < self.NT:
            n = min(gsz, self.NT - t)
            g.append((t, n, False))
            t += n
        return g

    def setup_consts(self):
        c, nc = self.c, self.nc
        self.cm32 = c.sb([128, 4, 128], F32, "cm32")
        self.cmb = c.sb([128, 4, 128], BF16, "cmb")
        self.b_const = Buf("const")
        c.dma("sp", self.cm32[:], self.w["cmat"].ap(), writes=[self.b_const])
        c.op("dve", lambda e: e.tensor_copy(out=self.cmb[:], in_=self.cm32[:]), reads=[self.b_const], writes=[self.b_const])
        self.ident32 = self.cm32[:, 0, :]
        self.ones32 = self.cm32[:, 1, :]
        self.trile32 = self.cm32[:, 2, :]
        self.trige32 = self.cm32[:, 3, :]
        self.identb = self.cmb[:, 0, :]
        self.onesb = self.cmb[:, 1, :]
        self.trileb = self.cmb[:, 2, :]
        self.trigeb = self.cmb[:, 3, :]
        self.ps = [nc.alloc_psum_tensor(f"psb{i}", [128, 512], F32) for i in range(8)]
        self.bps = [Buf(f"ps{i}") for i in range(8)]
        self.mark0 = c.sb_mark()

    def phase_end(self):
        self.c.barrier()
        self.c.sb_release(self.mark0)

    def prologue(self):
        c, nc = self.c, self.nc
        L = self.depth
        self.modv = self.scratch("modv", [L, 2, 6 * D], F32)
        self.xs = self.scratch("xs", [self.T, D], F32)
        cs = c.sb([128, 8, 2], F32, "cs")
        b_cs = Buf()
        c.dma("sp", cs[:], self.w["cT"].ap(), writes=[b_cs])
        c.op("act", lambda e: e.activation(out=cs[:], in_=cs[:], func=AF.Silu), reads=[b_cs], writes=[b_cs])
        b_xs = self.b_xs = [Buf(f"xs{t}") for t in range(self.NT)]
        c.dma("sp", self.xs.ap()[0:self.NCX, :], self.w["ctx"].ap(), writes=b_xs[0:self.NCT])
        c.dma("sp", self.xs.ap()[self.NCX:self.T, :], self.w["x"].ap(), writes=b_xs[self.NCT:])
        wm = [c.sb([128, 8, 512], F32, f"wm{i}") for i in range(2)]
        b_wm = [Buf(), Buf()]
        modsb = c.sb([2, 6 * D], F32, "modsb")
        b_mod = Buf()
        bmb = c.sb([2, 6 * D], F32, "bmb")
        gb = c.sb([2, 2, D], F32, "gb")
        b_misc = Buf()
        self.b_modv = [Buf(f"modv{i}") for i in range(L)]
        it = 0
        for li in range(L):
            c.dma("sp", bmb[:], self.w["b_mod"].ap()[li:li + 1, :].partition_broadcast(2), writes=[b_misc])
            c.dma("sp", gb[:, 0, :], self.w["norm1_g"].ap()[li:li + 1, :].partition_broadcast(2), writes=[b_misc])
            c.dma("sp", gb[:, 1, :], self.w["norm2_g"].ap()[li:li + 1, :].partition_broadcast(2), writes=[b_misc])
            for j in range(12):
                s = it % 2
                it += 1
                c.dma("sp", wm[s][:], self.w["w_mod"].ap()[li, :, j * 512:(j + 1) * 512].rearrange("(k p) n -> p k n", p=128), writes=[b_wm[s]])
                pb = it % 2
                c.mm(self.ps[pb][0:2, :], [(cs[:, k, :], wm[s][:, k, :]) for k in range(8)], reads=[b_cs, b_wm[s]], writes=[self.bps[pb]])
                c.op("dve", lambda e: e.tensor_tensor(out=modsb[:, j * 512:(j + 1) * 512], in0=self.ps[pb][0:2, :], in1=bmb[:, j * 512:(j + 1) * 512], op=ALU.add),
                     reads=[self.bps[pb], b_misc], writes=[b_mod])
            for (ch, gi) in ((1, 0), (4, 1)):
                c.op("dve", lambda e: e.scalar_tensor_tensor(out=modsb[:, ch * D:(ch + 1) * D], in0=modsb[:, ch * D:(ch + 1) * D], scalar=1.0, in1=gb[:, gi, :], op0=ALU.add, op1=ALU.mult),
                     reads=[b_mod, b_misc], writes=[b_mod])
            c.dma("sp", self.modv.ap()[li], modsb[:], reads=[b_mod], writes=[self.b_modv[li]])
        self.phase_end()

    def load_mod(self, li, chunk, row, name):
        c = self.c
        t = c.sb([128, D], F32, name)
        b = Buf(name)
        c.dma("sp", t[:], self.modv.ap()[li, row:row + 1, chunk * D:(chunk + 1) * D].partition_broadcast(128), reads=[self.b_modv[li]], writes=[b])
        return t, b

    def alloc_norm_bufs(self, nbuf=2):
        c = self.c
        if getattr(self, "want_precast", None) is not None:
            li_ = self.want_precast
            self.want_precast = None
            self.moe_precast(li_)
        self.nb = []
        for i in range(nbuf):
            d = dict(x=c.sb([128, D], F32, "nx"), bx=Buf(), junk=c.sb([128, D], BF16, "nj"), bj=Buf(),
                     st=c.sb([128, 2], F32, "nst"), bst=Buf(), tmp=c.sb([128, D], F32, "ntmp"), btmp=Buf(),
                     h=c.sb([128, D], BF16, "nh"), bh=Buf())
            self.nb.append(d)
        self.nbi = 0

    def norm_tile(self, tile, A, bA, B, bB, hT, b_hT, col0, psb, want_x=False):
        c = self.c
        d = self.nb[self.nbi % len(self.nb)]
        self.nbi += 1
        c.dma("sp", d["x"][:], self.xs.ap()[tile * 128:(tile + 1) * 128, :], reads=[self.b_xs[tile]], writes=[d["bx"]])
        c.op("dve", lambda e: e.memset(d["st"][:], 0.0), writes=[d["bst"]])
        c.op("act", lambda e: e.activation(out=d["junk"][:], in_=d["x"][:], func=AF.Square, accum_out=d["st"][:, 0:1]), reads=[d["bx"]], writes=[d["bj"], d["bst"]])
        c.op("dve", lambda e: e.tensor_scalar(out=d["st"][:, 1:2], in0=d["st"][:, 0:1], scalar1=1.0 / D, scalar2=EPS, op0=ALU.mult, op1=ALU.add), reads=[d["bst"]], writes=[d["bst"]])
        c.op("act", lambda e: e.activation(out=d["st"][:, 1:2], in_=d["st"][:, 1:2], func=AF.Sqrt), reads=[d["bst"]], writes=[d["bst"]])
        c.op("dve", lambda e: e.reciprocal(out=d["st"][:, 1:2], in_=d["st"][:, 1:2]), reads=[d["bst"]], writes=[d["bst"]])
        c.op("dve", lambda e: e.scalar_tensor_tensor(out=d["tmp"][:], in0=d["x"][:], scalar=d["st"][:, 1:2], in1=A[:], op0=ALU.mult, op1=ALU.mult),
             reads=[d["bx"], d["bst"], bA], writes=[d["btmp"]])
        c.op("pool", lambda e: e.tensor_tensor(out=d["h"][:], in0=d["tmp"][:], in1=B[:], op=ALU.add), reads=[d["btmp"], bB], writes=[d["bh"]])
        pT = self.ps[psb].ap().bitcast(BF16)
        c.tr_multi([(pT[:, k * 128:(k + 1) * 128], d["h"][:, k * 128:(k + 1) * 128], self.identb) for k in range(8)],
                   reads=[d["bh"], self.b_const], writes=[self.bps[psb]])
        c.op("act", lambda e: e.activation(out=hT[:, :, col0:col0 + 128], in_=pT.rearrange("p (k n) -> p k n", k=8), func=AF.Copy),
             reads=[self.bps[psb]], writes=[b_hT])
        return d

    def layer_gqa(self, li, j, need_ctx):
        c, nc = self.c, self.nc
        T, NT, NCT = self.T, self.NT, self.NCT
        w_in = self.w["attn_w_in"].ap()[j]
        w_out = self.w["attn_w_out"].ap()[j]
        QT = self.scratch(f"gqa_qt{li}", [4, NT, 64, 4, 128], BF16)
        b_QT = [Buf() for _ in range(NT)]
        b_w = Buf("gqa_w")
        Wq = c.sb([128, 8, 1024], BF16, "Wq")
        Wqr = c.sb([128, 8, 1024], BF16, "Wqr")
        Wk = c.sb([128, 8, 256], BF16, "Wk")
        Wkr = c.sb([128, 8, 256], BF16, "Wkr")
        Wv = c.sb([128, 8, 256], BF16, "Wv")
        c.dma("pool", Wq[:], w_in[:, 0:1024].rearrange("(k p) n -> p k n", p=128), writes=[b_w])
        c.dma("pool", Wk[:], w_in[:, 1024:1280].rearrange("(k p) n -> p k n", p=128), writes=[b_w])
        c.dma("pool", Wv[:], w_in[:, 1280:1536].rearrange("(k p) n -> p k n", p=128), writes=[b_w])
        b_wr = Buf("gqa_wr")
        for (W, Wr, nh) in ((Wq, Wqr, 16), (Wk, Wkr, 4)):
            for k in range(8):
                src = W[:, k, :].rearrange("p (h two i) -> p h two i", two=2, i=32)
                dst = Wr[:, k, :].rearrange("p (h two i) -> p h two i", two=2, i=32)
                c.op("act", lambda e: e.activation(out=dst[:, :, 0, :], in_=src[:, :, 1, :], func=AF.Copy, scale=-1.0), reads=[b_w], writes=[b_wr])
                c.op("dve", lambda e: e.tensor_copy(out=dst[:, :, 1, :], in_=src[:, :, 0, :]), reads=[b_w], writes=[b_wr])
        KT = c.sb([64, 4, T], BF16, "KT")
        b_KT = Buf("KT")
        Vs = c.sb([128, NT, 4, 65], BF16, "Vs")
        b_V = Buf("Vs")
        c.op("pool", lambda e: e.memset(Vs[:], 1.0), writes=[b_V])
        mark = c.sb_mark()
        A1x, bA1x = self.load_mod(li, 1, 0, "A1x")
        S1x, bS1x = self.load_mod(li, 0, 0, "S1x")
        A1c, bA1c = self.load_mod(li, 1, 1, "A1c")
        S1c, bS1c = self.load_mod(li, 0, 1, "S1c")
        self.alloc_norm_bufs(2)
        hTs = [c.sb([128, 8, 512], BF16, "hT") for _ in range(2)]
        b_hT = [Buf(), Buf()]
        rts = [c.sb([64, 2, 512], F32, "rt") for _ in range(2)]
        b_rt = [Buf(), Buf()]
        qst = [c.sb([64, 4, 512], BF16, "qst") for _ in range(2)]
        b_qst = [Buf(), Buf()]
        t1s = [c.sb([64, 512], F32, "t1") for _ in range(2)]
        t2s = [c.sb([64, 512], F32, "t2") for _ in range(2)]
        b_t1 = [Buf(), Buf()]
        b_t2 = [Buf(), Buf()]
        cnt = 0
        qcnt = 0
        for gi, (t0, n, is_ctx) in enumerate(self.groups(4)):
            ncols = n * 128
            hT, bh = hTs[gi % 2], b_hT[gi % 2]
            rt, brt = rts[gi % 2], b_rt[gi % 2]
            for s in range(n):
                if is_ctx:
                    self.norm_tile(t0 + s, A1c, bA1c, S1c, bS1c, hT, bh, s * 128, (t0 + s) % 2)
                else:
                    self.norm_tile(t0 + s, A1x, bA1x, S1x, bS1x, hT, bh, s * 128, (t0 + s) % 2)
            if not is_ctx:
                l0 = (t0 - NCT) * 128
                c.dma("sp", rt[:, :, :ncols], self.w["rope64"].ap()[:, :, l0:l0 + ncols].rearrange("two d l -> d two l"), writes=[brt])

            def proj_head(W, Wr, col, dst_ap, dst_buf):
                nonlocal cnt
                i = cnt % 2
                cnt += 1
                P1, bP1 = self.ps[2 + i], self.bps[2 + i]
                P2, bP2 = self.ps[4 + i], self.bps[4 + i]
                c.mm(P1[0:64, :ncols], [(W[:, k, col:col + 64], hT[:, k, :ncols]) for k in range(8)], reads=[b_w, bh], writes=[bP1])
                if is_ctx:
                    c.op("act", lambda e: e.activation(out=dst_ap, in_=P1[0:64, :ncols], func=AF.Copy), reads=[bP1], writes=[dst_buf])
                else:
                    c.mm(P2[0:64, :ncols], [(Wr[:, k, col:col + 64], hT[:, k, :ncols]) for k in range(8)], reads=[b_wr, bh], writes=[bP2])
                    c.op("dve", lambda e: e.tensor_tensor(out=t1s[i][:, :ncols], in0=P1[0:64, :ncols], in1=rt[:, 0, :ncols], op=ALU.mult), reads=[bP1, brt], writes=[b_t1[i]])
                    c.op("dve", lambda e: e.tensor_tensor(out=t2s[i][:, :ncols], in0=P2[0:64, :ncols], in1=rt[:, 1, :ncols], op=ALU.mult), reads=[bP2, brt], writes=[b_t2[i]])
                    c.op("pool", lambda e: e.tensor_tensor(out=dst_ap, in0=t1s[i][:, :ncols], in1=t2s[i][:, :ncols], op=ALU.add), reads=[b_t1[i], b_t2[i]], writes=[dst_buf])

            for kvh in range(4):
                qs, bq = qst[qcnt % 2], b_qst[qcnt % 2]
                qcnt += 1
                for g in range(4):
                    proj_head(Wq, Wqr, (kvh * 4 + g) * 64, qs[:, g, :ncols], bq)
                for qb_ in range(n):
                    c.dma("sp", QT.ap()[kvh, t0 + qb_], qs[:, :, qb_ * 128:(qb_ + 1) * 128], reads=[bq], writes=[b_QT[t0 + qb_]])
                proj_head(Wk, Wkr, kvh * 64, KT[:, kvh, t0 * 128:t0 * 128 + ncols], b_KT)
            for s in range(n):
                i = cnt % 2
                cnt += 1
                Pv, bPv = self.ps[6 + i], self.bps[6 + i]
                c.mm(Pv[:, 0:256], [(hT[:, k, s * 128:(s + 1) * 128], Wv[:, k, :]) for k in range(8)], reads=[b_w, bh], writes=[bPv])
                c.op("act", lambda e: e.activation(out=Vs[:, t0 + s, :, 0:64], in_=Pv[:, 0:256].rearrange("p (h d) -> p h d", h=4), func=AF.Copy), reads=[bPv], writes=[b_V])
        c.barrier()
        c.sb_release(mark)
        Wo = c.sb([64, 16, 1024], BF16, "Wo")
        b_wo = Buf()
        c.dma("pool", Wo[:], w_out.rearrange("(h d) n -> d h n", d=64), writes=[b_wo])
        sk = c.sb([65, 16], F32, "sk")
        b_sk = Buf()
        sinkrow = c.sb([65, 16, 128], F32, "sinkrow")
        c.dma("sp", sk[64:65, :], self.w["attn_sink"].ap()[j:j + 1, :], writes=[b_sk])
        c.op("act", lambda e: e.activation(out=sk[64:65, :], in_=sk[64:65, :], func=AF.Exp), reads=[b_sk], writes=[b_sk])
        c.op("dve", lambda e: e.tensor_copy(out=sinkrow[64:65, :, :], in_=sk[64:65, :].unsqueeze(2).to_broadcast([1, 16, 128])), reads=[b_sk], writes=[b_sk])
        G1x, bG1x = self.load_mod(li, 2, 0, "G1x")
        G1c, bG1c = self.load_mod(li, 2, 1, "G1c")
        Qts = [c.sb([64, 512], BF16, "Qt") for _ in range(4)]
        b_Qt = [Buf() for _ in range(4)]
        PTs = [c.sb([128, 512], BF16, "PT") for _ in range(4)]
        b_PT = [Buf() for _ in range(4)]
        osb = [c.sb([65, 512], F32, "osb") for _ in range(2)]
        b_osb = [Buf(), Buf()]
        rden = [c.sb([65, 512], F32, "rden") for _ in range(2)]
        b_rden = [Buf(), Buf()]
        yTs = [c.sb([64, 16, 128], BF16, "yT") for _ in range(2)]
        b_yT = [Buf(), Buf()]
        xts = [c.sb([128, D], F32, "xres") for _ in range(2)]
        b_xt = [Buf(), Buf()]
        tmps = [c.sb([128, D], F32, "rtmp") for _ in range(2)]
        b_tmp = [Buf(), Buf()]
        scale = 64 ** -0.5
        qi = 0
        pi = 0
        si = 0
        for bi, qb in enumerate(range(0 if need_ctx else NCT, NT)):
            is_ctx = qb < NCT
            if is_ctx:
                keys = [(t, None) for t in range(NCT)]
            else:
                keys = []
                if qb - 1 >= NCT:
                    keys.append((qb - 1, self.trigeb))
                keys.append((qb, None))
                if qb + 1 < NT:
                    keys.append((qb + 1, self.trileb))
                keys += [(t, None) for t in range(NCT)]
            xt, bxt = xts[bi % 2], b_xt[bi % 2]
            c.dma("sp", xt[:], self.xs.ap()[qb * 128:(qb + 1) * 128, :], reads=[self.b_xs[qb]], writes=[bxt])
            yT, byT = yTs[bi % 2], b_yT[bi % 2]
            for kvh in range(4):
                Qt, bQt = Qts[qi % 4], b_Qt[qi % 4]
                qi += 1
                c.dma("sp", Qt[:], QT.ap()[kvh, qb].rearrange("d g t -> d (g t)"), reads=[b_QT[qb]], writes=[bQt])
                oT, boT = self.ps[2 + kvh % 2], self.bps[2 + kvh % 2]
                SB = (0, 1, 7)
                LA = 2

                def issue_S(ki_):
                    kt_ = keys[ki_][0]
                    bk_ = SB[(si + ki_) % len(SB)]
                    c.mm(self.ps[bk_][:, :], [(KT[:, kvh, kt_ * 128:(kt_ + 1) * 128], Qt[:])], reads=[b_KT, bQt], writes=[self.bps[bk_]])
                for k0 in range(min(LA, len(keys))):
                    issue_S(k0)
                for ki, (kt, mask) in enumerate(keys):
                    bk = SB[(si + ki) % len(SB)]
                    sT, bsT = self.ps[bk], self.bps[bk]
                    if ki + LA < len(keys):
                        issue_S(ki + LA)
                    PT, bPT = PTs[pi % 4], b_PT[pi % 4]
                    pi += 1
                    c.op("act", lambda e: e.activation(out=PT[:], in_=sT[:, :], func=AF.Exp, scale=scale), reads=[bsT], writes=[bPT])
                    if mask is not None:
                        c.op("dve", lambda e: e.tensor_tensor(out=PT[:].rearrange("p (g t) -> p g t", g=4), in0=PT[:].rearrange("p (g t) -> p g t", g=4),
                                                              in1=mask.unsqueeze(1).to_broadcast([128, 4, 128]), op=ALU.mult), reads=[bPT, self.b_const], writes=[bPT])
                    c.mm(oT[0:65, :], [(Vs[:, kt, kvh, :], PT[:])], reads=[b_V, bPT], writes=[boT], start=(ki == 0), stop=(ki == len(keys) - 1))
                si += len(keys)
                o, bo = osb[kvh % 2], b_osb[kvh % 2]
                rd, brd = rden[kvh % 2], b_rden[kvh % 2]
                c.op("act", lambda e: e.activation(out=o[:], in_=oT[0:65, :], func=AF.Copy), reads=[boT], writes=[bo])
                c.op("dve", lambda e: e.tensor_tensor(out=rd[64:65, :], in0=o[64:65, :], in1=sinkrow[64:65, kvh * 4:(kvh + 1) * 4, :].rearrange("p g t -> p (g t)"), op=ALU.add),
                     reads=[bo, b_sk], writes=[brd])
                c.mm(self.ps[4][0:64, :], [(self.ones32[64:65, 0:64], rd[64:65, :])], reads=[self.b_const, brd], writes=[self.bps[4]])
                c.op("dve", lambda e: e.reciprocal(out=rd[0:64, :], in_=self.ps[4][0:64, :]), reads=[self.bps[4], brd], writes=[brd])
                c.op("dve", lambda e: e.tensor_tensor(out=yT[:, kvh * 4:(kvh + 1) * 4, :].rearrange("d g t -> d (g t)"), in0=o[0:64, :], in1=rd[0:64, :], op=ALU.mult),
                     reads=[bo, brd], writes=[byT])
            G1, bG1 = (G1c, bG1c) if is_ctx else (G1x, bG1x)
            tmp, btmp = tmps[bi % 2], b_tmp[bi % 2]
            for nn in range(2):
                z, bz = self.ps[5 + nn], self.bps[5 + nn]
                c.mm(z[:, :], [(yT[:, hq, :], Wo[:, hq, nn * 512:(nn + 1) * 512]) for hq in range(16)], reads=[byT, b_wo], writes=[bz])
                c.op("dve", lambda e: e.tensor_tensor(out=tmp[:, nn * 512:(nn + 1) * 512], in0=z[:, :], in1=G1[:, nn * 512:(nn + 1) * 512], op=ALU.mult), reads=[bz, bG1], writes=[btmp])
            c.op("pool", lambda e: e.tensor_tensor(out=xt[:], in0=xt[:], in1=tmp[:], op=ALU.add), reads=[btmp, bxt], writes=[bxt])
            c.dma("sp", self.xs.ap()[qb * 128:(qb + 1) * 128, :], xt[:], reads=[bxt], writes=[self.b_xs[qb]])
        self.phase_end()

    def moe_precast(self, li):
        c = self.c
        wg = self.w["moe_w_gate"].ap()[li]
        wu = self.w["moe_w_up"].ap()[li]
        wd = self.w["moe_w_down"].ap()[li]
        self.wgb = self.scratch(f"moe_wgb{li}", [16, 1024, 512], BF16)
        self.wdb = self.scratch(f"moe_wdb{li}", [16, 256, 1024], BF16)
        self.b_wgb = Buf(); self.b_wdb = Buf()
        for e2 in range(8):
            c.dma("pool", self.wgb.ap()[2 * e2:2 * e2 + 2, :, 0:256], wg[2 * e2:2 * e2 + 2], writes=[self.b_wgb])
            c.dma("pool", self.wgb.ap()[2 * e2:2 * e2 + 2, :, 256:512], wu[2 * e2:2 * e2 + 2], writes=[self.b_wgb])
            c.dma("pool", self.wdb.ap()[2 * e2:2 * e2 + 2], wd[2 * e2:2 * e2 + 2], writes=[self.b_wdb])
        self.precast_done = li

    def layer_moe(self, li, need_ctx):
        c, nc = self.c, self.nc
        NCT = self.NCT
        if getattr(self, "precast_done", -1) != li:
            self.moe_precast(li)
        wgb = self.wgb.ap()
        wdb = self.wdb.ap()
        b_w = Buf("moe_wr")
        Wr = c.sb([128, 8, 20], BF16, "Wr")
        c.dma("pool", Wr[:, :, 0:4], self.w["moe_w_group"].ap()[li].rearrange("(k p) n -> p k n", p=128), writes=[b_w])
        c.dma("pool", Wr[:, :, 4:20], self.w["moe_w_expert"].ap()[li].rearrange("(k p) n -> p k n", p=128), writes=[b_w])
        brow = c.sb([128, 20], F32, "brow")
        c.dma("sp", brow[:, 0:4], self.w["moe_b_group"].ap()[li:li + 1, :].partition_broadcast(128), writes=[b_w])
        c.dma("sp", brow[:, 4:20], self.w["moe_b_expert"].ap()[li:li + 1, :].partition_broadcast(128), writes=[b_w])
        sel16 = c.sb([16, 16, 128], BF16, "sel16")
        c.dma("pool", sel16[:], self.w["sel32"].ap()[0:16, 0:16, :], writes=[b_w])
        hT = c.sb([128, 8, 1024], BF16, "mhT")
        b_hT = Buf()
        act = c.sb([128, 16, 2, 1024], BF16, "mact")
        b_act = Buf()
        Wgu = [c.sb([128, 8, 512], BF16, "Wgu") for _ in range(2)]
        b_Wgu = [Buf(), Buf()]
        Wd = [c.sb([128, 16, 2, 256], BF16, "Wd") for _ in range(2)]
        b_Wd = [Buf(), Buf()]
        xq = [c.sb([128, 256], F32, "xq") for _ in range(4)]
        b_xq = [Buf() for _ in range(4)]
        xqi = 0
        self.alloc_norm_bufs(2)
        A2 = c.sb([128, D], F32, "A2"); S2 = c.sb([128, D], F32, "S2"); G2 = c.sb([128, D], F32, "G2")
        b_m = Buf()
        R = 8
        lg = c.sb([128, R, 20], F32, "lg"); le = c.sb([128, R, 16], F32, "le"); le2 = c.sb([128, R, 16], F32, "le2")
        gm = c.sb([128, R, 4], F32, "gm"); eg = c.sb([128, R, 4], F32, "eg"); pen = c.sb([128, R, 4], F32, "pen")
        mk1 = c.sb([128, R, 16], F32, "mk1"); mk2 = c.sb([128, R, 16], F32, "mk2"); cmb = c.sb([128, R, 16], F32, "cmb")
        sc = c.sb([128, 8, R], F32, "rsc")
        b_r = Buf("route")
        cmbT = c.sb([16, 1024], BF16, "cmbT")
        b_cT = Buf()
        s_sb = [c.sb([128, 512], F32, "msil") for _ in range(2)]
        b_s = [Buf(), Buf()]
        t_sb = [c.sb([128, 512], F32, "mt") for _ in range(2)]
        b_t = [Buf(), Buf()]
        tmpd = [c.sb([128, 256], F32, "mtd") for _ in range(2)]
        b_td = [Buf(), Buf()]
        wi = 0
        di = 0
        ui = 0
        for (t0, n, is_ctx) in self.groups(8, with_ctx=need_ctx):
            G = n * 128
            row = 1 if is_ctx else 0
            for (tl, ch) in ((S2, 3), (A2, 4), (G2, 5)):
                c.dma("sp", tl[:], self.modv.ap()[li, row:row + 1, ch * D:(ch + 1) * D].partition_broadcast(128), reads=[self.b_modv[li]], writes=[b_m])
            for s in range(n):
                self.norm_tile(t0 + s, A2, b_m, S2, b_m, hT, b_hT, s * 128, s % 2)
            lp, blp = self.ps[2], self.bps[2]
            c.mm_multi([(lp[:, s * 20:(s + 1) * 20], [(hT[:, k, s * 128:(s + 1) * 128], Wr[:, k, :]) for k in range(8)]) for s in range(n)],
                       reads=[b_hT, b_w], writes=[blp])
            V = lambda e: e
            lgn = lg[:, 0:n, :]
            c.op("dve", lambda e: e.tensor_tensor(out=lgn, in0=lp[:, 0:n * 20].rearrange("p (s j) -> p s j", j=20), in1=brow[:].unsqueeze(1).to_broadcast([128, n, 20]), op=ALU.add),
                 reads=[blp, b_w], writes=[b_r])
            R1 = [b_r]
            c.op("dve", lambda e: e.tensor_reduce(out=sc[:, 0, 0:n], in_=lgn[:, :, 0:4], axis=AX.X, op=ALU.max), reads=R1, writes=R1)
            c.op("dve", lambda e: e.tensor_tensor(out=gm[:, 0:n, :], in0=lgn[:, :, 0:4], in1=sc[:, 0, 0:n].unsqueeze(2).to_broadcast([128, n, 4]), op=ALU.is_equal), reads=R1, writes=R1)
            c.op("dve", lambda e: e.tensor_tensor(out=eg[:, 0:n, :], in0=lgn[:, :, 0:4], in1=sc[:, 0, 0:n].unsqueeze(2).to_broadcast([128, n, 4]), op=ALU.subtract), reads=R1, writes=R1)
            c.op("act", lambda e: e.activation(out=eg[:, 0:n, :], in_=eg[:, 0:n, :], func=AF.Exp), reads=R1, writes=R1)
            c.op("dve", lambda e: e.tensor_reduce(out=sc[:, 1, 0:n], in_=eg[:, 0:n, :], axis=AX.X, op=ALU.add), reads=R1, writes=R1)
            c.op("dve", lambda e: e.reciprocal(out=sc[:, 1, 0:n], in_=sc[:, 1, 0:n]), reads=R1, writes=R1)
            c.op("dve", lambda e: e.tensor_scalar(out=pen[:, 0:n, :], in0=gm[:, 0:n, :], scalar1=1.0, scalar2=1e30, op0=ALU.subtract, op1=ALU.mult), reads=R1, writes=R1)
            c.op("dve", lambda e: e.tensor_copy(out=le[:, 0:n, :], in_=lgn[:, :, 4:20]), reads=R1, writes=R1)
            lev = le[:, 0:n, :].rearrange("p s (g j) -> p (s g) j", g=4)
            c.op("dve", lambda e: e.tensor_tensor(out=lev, in0=lev, in1=pen[:, 0:n, :].rearrange("p s g -> p (s g)").unsqueeze(2).to_broadcast([128, n * 4, 4]), op=ALU.add), reads=R1, writes=R1)
            c.op("dve", lambda e: e.tensor_reduce(out=sc[:, 2, 0:n], in_=le[:, 0:n, :], axis=AX.X, op=ALU.max), reads=R1, writes=R1)
            c.op("dve", lambda e: e.tensor_tensor(out=mk1[:, 0:n, :], in0=le[:, 0:n, :], in1=sc[:, 2, 0:n].unsqueeze(2).to_broadcast([128, n, 16]), op=ALU.is_equal), reads=R1, writes=R1)
            c.op("dve", lambda e: e.scalar_tensor_tensor(out=le2[:, 0:n, :], in0=mk1[:, 0:n, :], scalar=-1e30, in1=le[:, 0:n, :], op0=ALU.mult, op1=ALU.add), reads=R1, writes=R1)
            c.op("dve", lambda e: e.tensor_reduce(out=sc[:, 3, 0:n], in_=le2[:, 0:n, :], axis=AX.X, op=ALU.max), reads=R1, writes=R1)
            c.op("dve", lambda e: e.tensor_tensor(out=mk2[:, 0:n, :], in0=le2[:, 0:n, :], in1=sc[:, 3, 0:n].unsqueeze(2).to_broadcast([128, n, 16]), op=ALU.is_equal), reads=R1, writes=R1)
            c.op("dve", lambda e: e.tensor_tensor(out=sc[:, 4, 0:n], in0=sc[:, 2, 0:n], in1=sc[:, 3, 0:n], op=ALU.subtract), reads=R1, writes=R1)
            c.op("act", lambda e: e.activation(out=sc[:, 4, 0:n], in_=sc[:, 4, 0:n], func=AF.Sigmoid), reads=R1, writes=R1)
            c.op("dve", lambda e: e.tensor_tensor(out=sc[:, 4, 0:n], in0=sc[:, 4, 0:n], in1=sc[:, 1, 0:n], op=ALU.mult), reads=R1, writes=R1)
            c.op("dve", lambda e: e.tensor_tensor(out=sc[:, 5, 0:n], in0=sc[:, 1, 0:n], in1=sc[:, 4, 0:n], op=ALU.subtract), reads=R1, writes=R1)
            c.op("dve", lambda e: e.tensor_tensor(out=mk1[:, 0:n, :], in0=mk1[:, 0:n, :], in1=sc[:, 4, 0:n].unsqueeze(2).to_broadcast([128, n, 16]), op=ALU.mult), reads=R1, writes=R1)
            c.op("dve", lambda e: e.tensor_tensor(out=mk2[:, 0:n, :], in0=mk2[:, 0:n, :], in1=sc[:, 5, 0:n].unsqueeze(2).to_broadcast([128, n, 16]), op=ALU.mult), reads=R1, writes=R1)
            c.op("dve", lambda e: e.tensor_tensor(out=cmb[:, 0:n, :], in0=mk1[:, 0:n, :], in1=mk2[:, 0:n, :], op=ALU.add), reads=R1, writes=R1)
            for hb in range((n + 3) // 4):
                s0, s1 = hb * 4, min(n, hb * 4 + 4)
                pc, bpc = self.ps[3 + hb], self.bps[3 + hb]
                c.tr_multi([(pc[0:16, (s - s0) * 128:(s - s0 + 1) * 128], cmb[:, s, :], self.ident32) for s in range(s0, s1)], reads=[b_r, self.b_const], writes=[bpc])
                c.op("act", lambda e: e.activation(out=cmbT[:, s0 * 128:s1 * 128], in_=pc[0:16, 0:(s1 - s0) * 128], func=AF.Copy), reads=[bpc], writes=[b_cT])
            ncb = (G + 511) // 512
            for ex in range(16):
                W, bW = Wgu[wi % 2], b_Wgu[wi % 2]
                wi += 1
                c.dma("sp", W[:], wgb[ex].rearrange("(k p) n -> p k n", p=128), reads=[self.b_wgb], writes=[bW])
                for cb in range(ncb):
                    c0 = cb * 512
                    cw = min(512, G - c0)
                    pbc, bpbc = self.ps[4 + (ui % 2)], self.bps[4 + (ui % 2)]
                    c.mm(pbc[:, :cw], [(sel16[:, ex, :], cmbT[:, c0:c0 + cw])], reads=[b_w, b_cT], writes=[bpbc])
                    for ffc in range(2):
                        i = ui % 2
                        ui += 1
                        pg_, bpg = self.ps[0 + i], self.bps[0 + i]
                        pu, bpu = self.ps[2 + i], self.bps[2 + i]
                        c.mm(pg_[:, :cw], [(W[:, k, ffc * 128:(ffc + 1) * 128], hT[:, k, c0:c0 + cw]) for k in range(8)], reads=[bW, b_hT], writes=[bpg])
                        c.mm(pu[:, :cw], [(W[:, k, 256 + ffc * 128:256 + (ffc + 1) * 128], hT[:, k, c0:c0 + cw]) for k in range(8)], reads=[bW, b_hT], writes=[bpu])
                        c.op("act", lambda e: e.activation(out=s_sb[i][:, :cw], in_=pg_[:, :cw], func=AF.Silu), reads=[bpg], writes=[b_s[i]])
                        c.op("dve", lambda e: e.tensor_tensor(out=t_sb[i][:, :cw], in0=s_sb[i][:, :cw], in1=pu[:, :cw], op=ALU.mult), reads=[b_s[i], bpu], writes=[b_t[i]])
                        c.op("dve", lambda e: e.tensor_tensor(out=act[:, ex, ffc, c0:c0 + cw], in0=t_sb[i][:, :cw], in1=pbc[:, :cw], op=ALU.mult), reads=[b_t[i], bpbc], writes=[b_act])
            for dq in range(4):
                Wdt, bWd = Wd[di % 2], b_Wd[di % 2]
                di += 1
                c.dma("sp", Wdt[:], wdb[:, :, dq * 256:(dq + 1) * 256].rearrange("e (f p) n -> p e f n", p=128), reads=[self.b_wdb], writes=[bWd])
                for s in range(n):
                    pa, bpa = self.ps[4 + s // 2], self.bps[4 + s // 2]
                    pav = pa[:, (s % 2) * 256:(s % 2 + 1) * 256]
                    c.mm(pav, [(act[:, ex, ffc, s * 128:(s + 1) * 128], Wdt[:, ex, ffc, :]) for ex in range(16) for ffc in range(2)], reads=[b_act, bWd], writes=[bpa])
                    i = s % 2
                    xq_, bxq = xq[xqi % 4], b_xq[xqi % 4]
                    xqi += 1
                    tl = t0 + s
                    c.dma("sp", xq_[:], self.xs.ap()[tl * 128:(tl + 1) * 128, dq * 256:(dq + 1) * 256], reads=[self.b_xs[tl]], writes=[bxq])
                    c.op("dve", lambda e: e.tensor_tensor(out=tmpd[i][:], in0=pav, in1=G2[:, dq * 256:(dq + 1) * 256], op=ALU.mult), reads=[bpa, b_m], writes=[b_td[i]])
                    c.op("pool", lambda e: e.tensor_tensor(out=xq_[:], in0=xq_[:], in1=tmpd[i][:], op=ALU.add), reads=[b_td[i], bxq], writes=[bxq])
                    c.dma("sp", self.xs.ap()[tl * 128:(tl + 1) * 128, dq * 256:(dq + 1) * 256], xq_[:], reads=[bxq], writes=[self.b_xs[tl]])
        self.phase_end()

    def final(self):
        c = self.c
        gf = c.sb([128, D], F32, "gfin")
        b_g = Buf()
        c.dma("sp", gf[:], self.w["final_norm_g"].ap().rearrange("(o n) -> o n", o=1).partition_broadcast(128), writes=[b_g])
        xs_ = [c.sb([128, D], F32, "fx") for _ in range(2)]
        js = [c.sb([128, D], BF16, "fj") for _ in range(2)]
        st = [c.sb([128, 2], F32, "fst") for _ in range(2)]
        ys = [c.sb([128, D], F32, "fy") for _ in range(2)]
        bx = [Buf(), Buf()]; bj = [Buf(), Buf()]; bs = [Buf(), Buf()]; by = [Buf(), Buf()]
        b_out = Buf()
        for t in range(self.NCT, self.NT):
            i = t % 2
            c.dma("sp", xs_[i][:], self.xs.ap()[t * 128:(t + 1) * 128, :], reads=[self.b_xs[t]], writes=[bx[i]])
            c.op("dve", lambda e: e.memset(st[i][:], 0.0), writes=[bs[i]])
            c.op("act", lambda e: e.activation(out=js[i][:], in_=xs_[i][:], func=AF.Square, accum_out=st[i][:, 0:1]), reads=[bx[i]], writes=[bj[i], bs[i]])
            c.op("dve", lambda e: e.tensor_scalar(out=st[i][:, 1:2], in0=st[i][:, 0:1], scalar1=1.0 / D, scalar2=EPS, op0=ALU.mult, op1=ALU.add), reads=[bs[i]], writes=[bs[i]])
            c.op("act", lambda e: e.activation(out=st[i][:, 1:2], in_=st[i][:, 1:2], func=AF.Sqrt), reads=[bs[i]], writes=[bs[i]])
            c.op("dve", lambda e: e.reciprocal(out=st[i][:, 1:2], in_=st[i][:, 1:2]), reads=[bs[i]], writes=[bs[i]])
            c.op("dve", lambda e: e.scalar_tensor_tensor(out=ys[i][:], in0=xs_[i][:], scalar=st[i][:, 1:2], in1=gf[:], op0=ALU.mult, op1=ALU.mult), reads=[bx[i], bs[i], b_g], writes=[by[i]])
            lt = t - self.NCT
            c.dma("sp", self.out.ap()[lt * 128:(lt + 1) * 128, :], ys[i][:], reads=[by[i]], writes=[b_out])
        self.c.finish("sp")

    def layer_mla(self, li, j, need_ctx):
        c, nc = self.c, self.nc
        T, NT, NCT = self.T, self.NT, self.NCT
        w_in = self.w["mla_w_in"].ap()[j]
        w_qup = self.w["mla_w_q_up"].ap()[j]
        w_kvup = self.w["mla_w_kv_up"].ap()[j]
        w_out = self.w["mla_w_out"].ap()[j]
        QT = self.scratch(f"mla_qt{li}", [16, 96, T], BF16)
        KTd = self.scratch(f"mla_kt{li}", [16, 96, T], BF16)
        Vd = self.scratch(f"mla_v{li}", [16, NT, 128, 65], BF16)
        YT = self.scratch(f"mla_yt{li}", [16, 64, T], BF16)
        b_QT = Buf(); b_KTd = Buf(); b_Vd = Buf(); b_YT = Buf()
        b_w = Buf("mla_w")
        Win = c.sb([128, 8, 544], BF16, "Win")
        c.dma("pool", Win[:], w_in.rearrange("(k p) n -> p k n", p=128), writes=[b_w])
        Wkr_rot = c.sb([128, 8, 32], BF16, "Wkrrot")
        Wq = c.sb([128, 2, 1536], BF16, "Wqup")
        c.dma("pool", Wq[:], w_qup.rearrange("(k p) n -> p k n", p=128), writes=[b_w])
        Wqr = c.sb([128, 2, 1536], BF16, "Wquprot")
        Wkn = c.sb([128, 2, 16, 64], BF16, "Wkn")
        Wv = c.sb([128, 2, 16, 64], BF16, "Wvv")
        kvv = w_kvup.rearrange("(k p) (h two d) -> p k h two d", p=128, two=2, d=64)
        for kc in range(2):
            c.dma("pool", Wkn[:, kc], kvv[:, kc, :, 0, :], writes=[b_w])
            c.dma("pool", Wv[:, kc], kvv[:, kc, :, 1, :], writes=[b_w])
        b_wr = Buf("mla_wr")
        c.op("pool", lambda e: e.memset(Wqr[:], 0.0), writes=[b_wr])
        for kc in range(2):
            src = Wq[:, kc, :].rearrange("p (h d) -> p h d", d=96)
            dst = Wqr[:, kc, :].rearrange("p (h d) -> p h d", d=96)
            c.op("act", lambda e: e.activation(out=dst[:, :, 64:80], in_=src[:, :, 80:96], func=AF.Copy, scale=-1.0), reads=[b_w], writes=[b_wr])
            c.op("dve", lambda e: e.tensor_copy(out=dst[:, :, 80:96], in_=src[:, :, 64:80]), reads=[b_w], writes=[b_wr])
        c.op("act", lambda e: e.activation(out=Wkr_rot[:, :, 0:16], in_=Win[:, :, 528:544], func=AF.Copy, scale=-1.0), reads=[b_w], writes=[b_wr])
        c.op("dve", lambda e: e.tensor_copy(out=Wkr_rot[:, :, 16:32], in_=Win[:, :, 512:528]), reads=[b_w], writes=[b_wr])
        gq = c.sb([128, 512], F32, "gqkv")
        c.dma("sp", gq[:, 0:256], self.w["mla_q_norm_g"].ap()[j:j + 1, :].partition_broadcast(128), writes=[b_w])
        c.dma("sp", gq[:, 256:512], self.w["mla_kv_norm_g"].ap()[j:j + 1, :].partition_broadcast(128), writes=[b_w])
        mark = c.sb_mark()
        A1x, bA1x = self.load_mod(li, 1, 0, "A1x")
        S1x, bS1x = self.load_mod(li, 0, 0, "S1x")
        A1c, bA1c = self.load_mod(li, 1, 1, "A1c")
        S1c, bS1c = self.load_mod(li, 0, 1, "S1c")
        self.alloc_norm_bufs(2)
        hTs = [c.sb([128, 8, 512], BF16, "hT") for _ in range(2)]
        b_hT = [Buf(), Buf()]
        cnT = [c.sb([128, 4, 512], BF16, "cnT") for _ in range(2)]
        b_cnT = [Buf(), Buf()]
        rts = [c.sb([96, 2, 512], F32, "rt96") for _ in range(2)]
        rks = [c.sb([32, 2, 512], F32, "rt32") for _ in range(2)]
        b_rt = [Buf(), Buf()]
        krT = [c.sb([32, 512], BF16, "krT") for _ in range(2)]
        b_krT = [Buf(), Buf()]
        st = [c.sb([128, 4], F32, "mst") for _ in range(2)]
        b_st = [Buf(), Buf()]
        jk = c.sb([128, 256], BF16, "mjunk"); b_jk = Buf()
        cn = [c.sb([128, 512], BF16, "cn") for _ in range(2)]
        b_cn = [Buf(), Buf()]
        qst = [c.sb([96, 512], BF16, "mqst") for _ in range(2)]
        b_qst = [Buf(), Buf()]
        kst = [c.sb([64, 512], BF16, "mkst") for _ in range(2)]
        b_kst = [Buf(), Buf()]
        t1s = [c.sb([96, 512], F32, "t1") for _ in range(2)]
        t2s = [c.sb([96, 512], F32, "t2") for _ in range(2)]
        b_t1 = [Buf(), Buf()]; b_t2 = [Buf(), Buf()]
        Vt = [c.sb([128, 16, 65], BF16, "Vt") for _ in range(2)]
        b_Vt = [Buf(), Buf()]
        for i in range(2):
            c.op("pool", lambda e: e.memset(Vt[i][:], 1.0), writes=[b_Vt[i]])
        cnt = 0
        ti = 0
        for gi, (t0, n, is_ctx) in enumerate(self.groups(4)):
            ncols = n * 128
            col0 = t0 * 128
            hT, bh = hTs[gi % 2], b_hT[gi % 2]
            cT_, bcT = cnT[gi % 2], b_cnT[gi % 2]
            rt, rk, brt = rts[gi % 2], rks[gi % 2], b_rt[gi % 2]
            for s in range(n):
                if is_ctx:
                    self.norm_tile(t0 + s, A1c, bA1c, S1c, bS1c, hT, bh, s * 128, (t0 + s) % 2)
                else:
                    self.norm_tile(t0 + s, A1x, bA1x, S1x, bS1x, hT, bh, s * 128, (t0 + s) % 2)
            if not is_ctx:
                l0 = (t0 - NCT) * 128
                c.dma("sp", rt[:, :, :ncols], self.w["rope96"].ap()[:, :, l0:l0 + ncols].rearrange("two d l -> d two l"), writes=[brt])
                c.dma("sp", rk[:, :, :ncols], self.w["rope32"].ap()[:, :, l0:l0 + ncols].rearrange("two d l -> d two l"), writes=[brt])
            for s in range(n):
                i = ti % 2
                ti += 1
                pA, bpA = self.ps[2 + i], self.bps[2 + i]
                c.mm(pA[:, :], [(hT[:, k, s * 128:(s + 1) * 128], Win[:, k, 0:512]) for k in range(8)], reads=[bh, b_w], writes=[bpA])
                c.op("dve", lambda e: e.memset(st[i][:], 0.0), writes=[b_st[i]])
                for u in range(2):
                    c.op("act", lambda e: e.activation(out=jk[:], in_=pA[:, u * 256:(u + 1) * 256], func=AF.Square, accum_out=st[i][:, u:u + 1]), reads=[bpA], writes=[b_jk, b_st[i]])
                c.op("dve", lambda e: e.tensor_scalar(out=st[i][:, 2:4], in0=st[i][:, 0:2], scalar1=1.0 / 256, scalar2=EPS, op0=ALU.mult, op1=ALU.add), reads=[b_st[i]], writes=[b_st[i]])
                c.op("act", lambda e: e.activation(out=st[i][:, 2:4], in_=st[i][:, 2:4], func=AF.Sqrt), reads=[b_st[i]], writes=[b_st[i]])
                c.op("dve", lambda e: e.reciprocal(out=st[i][:, 2:4], in_=st[i][:, 2:4]), reads=[b_st[i]], writes=[b_st[i]])
                for u in range(2):
                    c.op("dve", lambda e: e.scalar_tensor_tensor(out=cn[i][:, u * 256:(u + 1) * 256], in0=pA[:, u * 256:(u + 1) * 256], scalar=st[i][:, 2 + u:3 + u], in1=gq[:, u * 256:(u + 1) * 256], op0=ALU.mult, op1=ALU.mult),
                         reads=[bpA, b_st[i], b_w], writes=[b_cn[i]])
                pT = self.ps[4 + i].ap().bitcast(BF16)
                c.tr_multi([(pT[:, k * 128:(k + 1) * 128], cn[i][:, k * 128:(k + 1) * 128], self.identb) for k in range(4)], reads=[b_cn[i], self.b_const], writes=[self.bps[4 + i]])
                c.op("act", lambda e: e.activation(out=cT_[:, :, s * 128:(s + 1) * 128], in_=pT[:, 0:512].rearrange("p (k n) -> p k n", k=4), func=AF.Copy), reads=[self.bps[4 + i]], writes=[bcT])

            def rope_proj(pairs1, pairs2, M, rtab, dst_ap, dst_buf, rd):
                nonlocal cnt
                i = cnt % 2
                cnt += 1
                P1, bP1 = self.ps[2 + i], self.bps[2 + i]
                P2, bP2 = self.ps[6 + i], self.bps[6 + i]
                c.mm(P1[0:M, :ncols], pairs1, reads=rd, writes=[bP1])
                if is_ctx or pairs2 is None:
                    c.op("act", lambda e: e.activation(out=dst_ap, in_=P1[0:M, :ncols], func=AF.Copy), reads=[bP1], writes=[dst_buf])
                else:
                    c.mm(P2[0:M, :ncols], pairs2, reads=rd + [b_wr], writes=[bP2])
                    c.op("dve", lambda e: e.tensor_tensor(out=t1s[i][0:M, :ncols], in0=P1[0:M, :ncols], in1=rtab[0:M, 0, :ncols], op=ALU.mult), reads=[bP1, brt], writes=[b_t1[i]])
                    c.op("dve", lambda e: e.tensor_tensor(out=t2s[i][0:M, :ncols], in0=P2[0:M, :ncols], in1=rtab[0:M, 1, :ncols], op=ALU.mult), reads=[bP2, brt], writes=[b_t2[i]])
                    c.op("pool", lambda e: e.tensor_tensor(out=dst_ap, in0=t1s[i][0:M, :ncols], in1=t2s[i][0:M, :ncols], op=ALU.add), reads=[b_t1[i], b_t2[i]], writes=[dst_buf])

            kr_, bkr = krT[gi % 2], b_krT[gi % 2]
            rope_proj([(Win[:, k, 512:544], hT[:, k, :ncols]) for k in range(8)], [(Wkr_rot[:, k, :], hT[:, k, :ncols]) for k in range(8)], 32, rk, kr_[:, :ncols], bkr, [bh, b_w])
            for h in range(16):
                qs, bq = qst[h % 2], b_qst[h % 2]
                rope_proj([(Wq[:, kc, h * 96:(h + 1) * 96], cT_[:, kc, :ncols]) for kc in range(2)],
                          [(Wqr[:, kc, h * 96:(h + 1) * 96], cT_[:, kc, :ncols]) for kc in range(2)], 96, rt, qs[:, :ncols], bq, [bcT, b_w])
                c.dma("sp", QT.ap()[h, :, col0:col0 + ncols], qs[:, :ncols], reads=[bq], writes=[b_QT])
                ks, bk = kst[h % 2], b_kst[h % 2]
                rope_proj([(Wkn[:, kc, h, :], cT_[:, 2 + kc, :ncols]) for kc in range(2)], None, 64, None, ks[:, :ncols], bk, [bcT, b_w])
                c.dma("sp", KTd.ap()[h, 0:64, col0:col0 + ncols], ks[:, :ncols], reads=[bk], writes=[b_KTd])
                c.dma("sp", KTd.ap()[h, 64:96, col0:col0 + ncols], kr_[:, :ncols], reads=[bkr], writes=[b_KTd])
            for s in range(n):
                vt, bvt = Vt[s % 2], b_Vt[s % 2]
                for hh in range(2):
                    i = cnt % 2
                    cnt += 1
                    Pv, bPv = self.ps[2 + i], self.bps[2 + i]
                    c.mm(Pv[:, :], [(cT_[:, 2 + kc, s * 128:(s + 1) * 128], Wv[:, kc, hh * 8:(hh + 1) * 8, :].rearrange("p h d -> p (h d)")) for kc in range(2)], reads=[bcT, b_w], writes=[bPv])
                    c.op("act", lambda e: e.activation(out=vt[:, hh * 8:(hh + 1) * 8, 0:64], in_=Pv[:, :].rearrange("p (h d) -> p h d", d=64), func=AF.Copy), reads=[bPv], writes=[bvt])
                c.dma("sp", Vd.ap()[:, t0 + s].rearrange("h p d -> p h d"), vt[:], reads=[bvt], writes=[b_Vd])
        c.barrier()
        c.sb_release(mark)
        QTs = [c.sb([96, T], BF16, "QTh") for _ in range(2)]
        KTs = [c.sb([96, T], BF16, "KTh") for _ in range(2)]
        Vhs = [c.sb([128, NT, 65], BF16, "Vh") for _ in range(2)]
        b_hd = [Buf(), Buf()]
        PTs = [c.sb([128, 512], BF16, "PT") for _ in range(4)]
        b_PT = [Buf() for _ in range(4)]
        osb = [c.sb([65, 512], F32, "osb") for _ in range(2)]
        b_osb = [Buf(), Buf()]
        ysb = [c.sb([64, 512], BF16, "ysb") for _ in range(2)]
        b_ysb = [Buf(), Buf()]
        rbs = [c.sb([64, 512], F32, "rbs") for _ in range(2)]
        b_rbs = [Buf(), Buf()]
        scale = 96 ** -0.5
        pi = 0; si = 0; oi = 0

        def load_head(h_):
            c.dma("sp", QTs[h_ % 2][:], QT.ap()[h_], reads=[b_QT], writes=[b_hd[h_ % 2]])
            c.dma("sp", KTs[h_ % 2][:], KTd.ap()[h_], reads=[b_KTd], writes=[b_hd[h_ % 2]])
            c.dma("sp", Vhs[h_ % 2][:], Vd.ap()[h_].rearrange("t p d -> p t d"), reads=[b_Vd], writes=[b_hd[h_ % 2]])
        load_head(0)
        for h in range(16):
            Qh, Kh, Vh, bhd = QTs[h % 2], KTs[h % 2], Vhs[h % 2], b_hd[h % 2]
            if h + 1 < 16:
                load_head(h + 1)
            units = []
            for (t0, n, is_ctx) in self.groups(4, with_ctx=need_ctx):
                keys = list(range(NCT)) if is_ctx else list(range(NT))
                gslot = oi % 2
                oi += 1
                for ki, kt in enumerate(keys):
                    units.append((t0 * 128, n * 128, kt, ki == 0, ki == len(keys) - 1, gslot))
            SB = (0, 1, 5, 6, 7)
            LA = 3

            def issue_S(ui):
                col0_, ncols_, kt_, _, _, _ = units[ui]
                bk_ = SB[(si + ui) % len(SB)]
                c.mm(self.ps[bk_][:, :ncols_], [(Kh[:, kt_ * 128:(kt_ + 1) * 128], Qh[:, col0_:col0_ + ncols_])], reads=[bhd], writes=[self.bps[bk_]])

            def epilogue(col0_, ncols_, gslot):
                oT, boT = self.ps[2 + gslot], self.bps[2 + gslot]
                o, bo = osb[gslot], b_osb[gslot]
                ys, bys = ysb[gslot], b_ysb[gslot]
                c.op("act", lambda e: e.activation(out=o[:, :ncols_], in_=oT[0:65, :ncols_], func=AF.Copy), reads=[boT], writes=[bo])
                c.mm(self.ps[4][0:64, :ncols_], [(self.ones32[64:65, 0:64], o[64:65, :ncols_])], reads=[self.b_const, bo], writes=[self.bps[4]])
                rb, brb = rbs[gslot], b_rbs[gslot]
                c.op("dve", lambda e: e.reciprocal(out=rb[:, :ncols_], in_=self.ps[4][0:64, :ncols_]), reads=[self.bps[4]], writes=[brb])
                c.op("dve", lambda e: e.tensor_tensor(out=ys[:, :ncols_], in0=o[0:64, :ncols_], in1=rb[:, :ncols_], op=ALU.mult), reads=[bo, brb], writes=[bys])
                c.dma("sp", YT.ap()[h, :, col0_:col0_ + ncols_], ys[:, :ncols_], reads=[bys], writes=[b_YT])

            pending = []
            for k0 in range(min(LA, len(units))):
                issue_S(k0)
            for ui, (col0, ncols, kt, first, last, gslot) in enumerate(units):
                bk = SB[(si + ui) % len(SB)]
                sT, bsT = self.ps[bk], self.bps[bk]
                if ui + LA < len(units):
                    issue_S(ui + LA)
                PT, bPT = PTs[pi % 4], b_PT[pi % 4]
                pi += 1
                oT, boT = self.ps[2 + gslot], self.bps[2 + gslot]
                c.op("act", lambda e: e.activation(out=PT[:, :ncols], in_=sT[:, :ncols], func=AF.Exp, scale=scale), reads=[bsT], writes=[bPT])
                c.mm(oT[0:65, :ncols], [(Vh[:, kt, :], PT[:, :ncols])], reads=[bhd, bPT], writes=[boT], start=first, stop=last)
                if pending and pending[0][0] <= ui:
                    _, args = pending.pop(0)
                    epilogue(*args)
                if last:
                    pending.append((ui + 4, (col0, ncols, gslot)))
            for _, args in pending:
                epilogue(*args)
            si += len(units)
        c.barrier()
        c.sb_release(mark)
        Wo = c.sb([64, 16, 1024], BF16, "Wo")
        b_wo = Buf()
        c.dma("pool", Wo[:], w_out.rearrange("(h d) n -> d h n", d=64), writes=[b_wo])
        self.attn_out(li, need_ctx, Wo, b_wo, YT, b_YT)
        self.phase_end()

    def attn_out(self, li, need_ctx, Wo, b_wo, YT, b_YT):
        c = self.c
        NCT, NT = self.NCT, self.NT
        G1x, bG1x = self.load_mod(li, 2, 0, "G1x")
        G1c, bG1c = self.load_mod(li, 2, 1, "G1c")
        yTs = [c.sb([64, 16, 128], BF16, "yT") for _ in range(2)]
        b_yT = [Buf(), Buf()]
        xts = [c.sb([128, D], F32, "xres") for _ in range(2)]
        b_xt = [Buf(), Buf()]
        tmps = [c.sb([128, D], F32, "rtmp") for _ in range(2)]
        b_tmp = [Buf(), Buf()]
        for bi, qb in enumerate(range(0 if need_ctx else NCT, NT)):
            is_ctx = qb < NCT
            xt, bxt = xts[bi % 2], b_xt[bi % 2]
            yT, byT = yTs[bi % 2], b_yT[bi % 2]
            tmp, btmp = tmps[bi % 2], b_tmp[bi % 2]
            c.dma("sp", xt[:], self.xs.ap()[qb * 128:(qb + 1) * 128, :], reads=[self.b_xs[qb]], writes=[bxt])
            c.dma("sp", yT[:], YT.ap()[:, :, qb * 128:(qb + 1) * 128].rearrange("h d t -> d h t"), reads=[b_YT], writes=[byT])
            G1, bG1 = (G1c, bG1c) if is_ctx else (G1x, bG1x)
            for nn in range(2):
                z, bz = self.ps[5 + nn], self.bps[5 + nn]
                c.mm(z[:, :], [(yT[:, hq, :], Wo[:, hq, nn * 512:(nn + 1) * 512]) for hq in range(16)], reads=[byT, b_wo], writes=[bz])
                c.op("dve", lambda e: e.tensor_tensor(out=tmp[:, nn * 512:(nn + 1) * 512], in0=z[:, :], in1=G1[:, nn * 512:(nn + 1) * 512], op=ALU.mult), reads=[bz, bG1], writes=[btmp])
            c.op("pool", lambda e: e.tensor_tensor(out=xt[:], in0=xt[:], in1=tmp[:], op=ALU.add), reads=[btmp, bxt], writes=[bxt])
            c.dma("sp", self.xs.ap()[qb * 128:(qb + 1) * 128, :], xt[:], reads=[bxt], writes=[self.b_xs[qb]])

    def layer_ssd(self, li, j, need_ctx):
        c, nc = self.c, self.nc
        T, NT, NCT, NL, NCX = self.T, self.NT, self.NCT, self.NL, self.NCX
        w_in = self.w["ssm_w_in"].ap()[j]
        XB = self.scratch(f"ssd_xb{li}", [24, 128, T], BF16)
        XC = self.scratch(f"ssd_xc{li}", [24, 128, T], BF16)
        Zs = self.scratch(f"ssd_z{li}", [NT, 128, 2048], BF16)
        DT = self.scratch(f"ssd_dt{li}", [NT, 128, 64], F32)
        Yd = [self.scratch(f"ssd_y{li}_{d}", [NT, 128, 2048], F32) for d in range(2)]
        b_XB = Buf(); b_XC = Buf(); b_Z = Buf(); b_DT = Buf(); b_Y = [Buf(), Buf()]
        b_w = Buf("ssd_w")
        Win = c.sb([128, 8, 5184], BF16, "ssdWin")
        for k in range(8):
            c.dma("pool", Win[:, k, :], w_in[k * 128:(k + 1) * 128, :], writes=[b_w])
        dtb = c.sb([128, 64], F32, "dtb")
        c.dma("sp", dtb[:], self.w["ssm_dt_bias"].ap()[j:j + 1].rearrange("o d h -> o (d h)").partition_broadcast(128), writes=[b_w])
        mark = c.sb_mark()
        A1x, bA1x = self.load_mod(li, 1, 0, "A1x")
        S1x, bS1x = self.load_mod(li, 0, 0, "S1x")
        A1c, bA1c = self.load_mod(li, 1, 1, "A1c")
        S1c, bS1c = self.load_mod(li, 0, 1, "S1c")
        self.alloc_norm_bufs(2)
        hTs = [c.sb([128, 8, 512], BF16, "hT") for _ in range(2)]
        b_hT = [Buf(), Buf()]
        stg = [c.sb([128, 512], BF16, "stg") for _ in range(3)]
        b_stg = [Buf() for _ in range(3)]
        zst = [c.sb([128, 2048], BF16, "zst") for _ in range(2)]
        b_zst = [Buf(), Buf()]
        dts = [c.sb([128, 64], F32, "dts") for _ in range(2)]
        b_dts = [Buf(), Buf()]
        cnt = 0
        for gi, (t0, n, is_ctx) in enumerate(self.groups(4)):
            ncols = n * 128
            col0 = t0 * 128
            hT, bh = hTs[gi % 2], b_hT[gi % 2]
            for s in range(n):
                if is_ctx:
                    self.norm_tile(t0 + s, A1c, bA1c, S1c, bS1c, hT, bh, s * 128, (t0 + s) % 2)
                else:
                    self.norm_tile(t0 + s, A1x, bA1x, S1x, bS1x, hT, bh, s * 128, (t0 + s) % 2)
            for fc in range(24):
                i = cnt % 3
                cnt += 1
                P, bP = self.ps[2 + i], self.bps[2 + i]
                c.mm(P[:, :ncols], [(Win[:, k, 2048 + fc * 128:2048 + (fc + 1) * 128], hT[:, k, :ncols]) for k in range(8)], reads=[b_w, bh], writes=[bP])
                c.op("act", lambda e: e.activation(out=stg[i][:, :ncols], in_=P[:, :ncols], func=AF.Copy), reads=[bP], writes=[b_stg[i]])
                c.dma("sp", XB.ap()[fc, :, col0:col0 + ncols], stg[i][:, :ncols], reads=[b_stg[i]], writes=[b_XB])
            for s in range(n):
                zs, bz = zst[s % 2], b_zst[s % 2]
                for zc in range(4):
                    i = cnt % 3
                    cnt += 1
                    P, bP = self.ps[2 + i], self.bps[2 + i]
                    c.mm(P[:, :], [(hT[:, k, s * 128:(s + 1) * 128], Win[:, k, zc * 512:(zc + 1) * 512]) for k in range(8)], reads=[b_w, bh], writes=[bP])
                    c.op("act", lambda e: e.activation(out=zs[:, zc * 512:(zc + 1) * 512], in_=P[:, :], func=AF.Silu), reads=[bP], writes=[bz])
                c.dma("sp", Zs.ap()[t0 + s], zs[:], reads=[bz], writes=[b_Z])
                i = cnt % 3
                cnt += 1
                P, bP = self.ps[2 + i], self.bps[2 + i]
                dt_, bdt = dts[s % 2], b_dts[s % 2]
                c.mm(P[:, 0:64], [(hT[:, k, s * 128:(s + 1) * 128], Win[:, k, 5120:5184]) for k in range(8)], reads=[b_w, bh], writes=[bP])
                c.op("dve", lambda e: e.tensor_tensor(out=dt_[:], in0=P[:, 0:64], in1=dtb[:], op=ALU.add), reads=[bP, b_w], writes=[bdt])
                c.op("act", lambda e: e.activation(out=dt_[:], in_=dt_[:], func=AF.Exp), reads=[bdt], writes=[bdt])
                c.op("act", lambda e: e.activation(out=dt_[:], in_=dt_[:], func=AF.Ln, bias=1.0), reads=[bdt], writes=[bdt])
                c.dma("sp", DT.ap()[t0 + s], dt_[:], reads=[bdt], writes=[b_DT])
        c.barrier()
        c.sb_release(self.mark0)
        cw = c.sb([128, 24, 5], F32, "convw"); cbias = c.sb([128, 24], F32, "convb")
        b_cw = Buf()
        c.dma("sp", cw[:], self.w["ssm_conv_wT"].ap()[j], writes=[b_cw])
        c.dma("sp", cbias[:], self.w["ssm_conv_bT"].ap()[j], writes=[b_cw])
        segs = [(0, NCX), (NCX, NL)]
        Lmax = max(NCX, NL)
        xp = [c.sb([128, Lmax + 4], BF16, "xp") for _ in range(2)]
        b_xp = [Buf(), Buf()]
        acc = [c.sb([128, Lmax], F32, "cacc") for _ in range(2)]
        b_acc = [Buf(), Buf()]
        cout = [c.sb([128, Lmax], BF16, "cout") for _ in range(2)]
        b_cout = [Buf(), Buf()]
        it = 0
        for fc in range(24):
            for (o0, L) in segs:
                i = it % 2
                it += 1
                c.op("pool", lambda e: e.memset(xp[i][:], 0.0), writes=[b_xp[i]])
                c.dma("sp", xp[i][:, 2:2 + L], XB.ap()[fc, :, o0:o0 + L], reads=[b_XB], writes=[b_xp[i]])
                c.op("dve", lambda e: e.tensor_scalar(out=acc[i][:, :L], in0=xp[i][:, 0:L], scalar1=cw[:, fc, 0:1], scalar2=None, op0=ALU.mult), reads=[b_xp[i], b_cw], writes=[b_acc[i]])
                for k in range(1, 5):
                    eng = "dve"
                    c.op(eng, lambda e: e.scalar_tensor_tensor(out=acc[i][:, :L], in0=xp[i][:, k:k + L], scalar=cw[:, fc, k:k + 1], in1=acc[i][:, :L], op0=ALU.mult, op1=ALU.add),
                         reads=[b_xp[i], b_cw, b_acc[i]], writes=[b_acc[i]])
                c.op("act", lambda e: e.activation(out=cout[i][:, :L], in_=acc[i][:, :L], func=AF.Silu, bias=cbias[:, fc:fc + 1]), reads=[b_acc[i], b_cw], writes=[b_cout[i]])
                c.dma("sp", XC.ap()[fc, :, o0:o0 + L], cout[i][:, :L], reads=[b_cout[i]], writes=[b_XC])
        c.barrier()
        c.sb_release(self.mark0)
        sel = c.sb([32, 32, 128], F32, "sel32"); b_sel = Buf()
        c.dma("sp", sel[:], self.w["sel32"].ap(), writes=[b_sel])
        aneg = c.sb([128, 64], F32, "aneg"); dsk = c.sb([128, 64], F32, "dsk"); b_an = Buf()
        c.dma("sp", aneg[:], self.w["ssm_a_log"].ap()[j:j + 1].rearrange("o d h -> o (d h)").partition_broadcast(128), writes=[b_an])
        c.op("act", lambda e: e.activation(out=aneg[:], in_=aneg[:], func=AF.Exp), reads=[b_an], writes=[b_an])
        c.op("act", lambda e: e.activation(out=aneg[:], in_=aneg[:], func=AF.Copy, scale=-1.0), reads=[b_an], writes=[b_an])
        c.dma("sp", dsk[:], self.w["ssm_d"].ap()[j:j + 1].rearrange("o d h -> o (d h)").partition_broadcast(128), writes=[b_an])
        c.op("dve", lambda e: e.tensor_tensor(out=dsk[:, 0:32], in0=dsk[:, 0:32], in1=dsk[:, 32:64], op=ALU.add), reads=[b_an], writes=[b_an])
        xcs = [c.sb([128, 24, 128], BF16, "xc") for _ in range(2)]; b_xc = [Buf(), Buf()]
        dtt = [c.sb([128, 64], F32, "dtt") for _ in range(2)]; b_dtt = [Buf(), Buf()]
        gt = [c.sb([128, 8, 32], F32, "gt") for _ in range(2)]; b_gt = [Buf(), Buf()]
        acT = [c.sb([32, 128], F32, "acT") for _ in range(2)]; b_acT = [Buf(), Buf()]
        nacT = [c.sb([32, 128], F32, "nacT") for _ in range(2)]
        xtok = [c.sb([128, 32, 64], F32, "xtok") for _ in range(2)]; b_xtok = [Buf(), Buf()]
        u = [c.sb([128, 32, 64], BF16, "u") for _ in range(2)]; b_u = [Buf(), Buf()]
        Vw = [c.sb([128, 32, 64], BF16, "Vw") for _ in range(2)]; b_Vw = [Buf(), Buf()]
        Btok = [c.sb([128, 4, 128], BF16, "Btok") for _ in range(2)]; b_Bt = [Buf(), Buf()]
        scm = [c.sb([128, 128], F32, "scm") for _ in range(2)]; b_scm = [Buf(), Buf()]
        aa = [c.sb([128, 512], F32, "aa") for _ in range(4)]; b_aa = [Buf() for _ in range(4)]
        EE = [c.sb([128, 512], F32, "EE") for _ in range(4)]; b_EE = [Buf() for _ in range(4)]
        MT = [c.sb([128, 512], BF16, "MT") for _ in range(4)]; b_MT = [Buf() for _ in range(4)]
        yi = [c.sb([128, 512], F32, "yi") for _ in range(2)]; b_yi = [Buf(), Buf()]
        Yt = [c.sb([128, 2048], F32, "Yt") for _ in range(2)]; b_Yt = [Buf(), Buf()]
        S32 = c.sb([128, 4, 512], F32, "S32"); Sb = c.sb([128, 4, 512], BF16, "Sb"); b_S = [Buf() for _ in range(4)]
        ci = 0; hi = 0
        for d in range(2):
            tri = self.trile32 if d == 0 else self.trige32
            order = list(range(NT)) if d == 0 else (list(range(NCT - 1, -1, -1)) + list(range(NT - 1, NCT - 1, -1)))
            c.op("dve", lambda e: e.memset(S32[:], 0.0), writes=b_S)
            c.op("pool", lambda e: e.memset(Sb[:], 0.0), writes=b_S)
            for ch in order:
                i = ci % 2
                ci += 1
                xc, bxc = xcs[i], b_xc[i]
                c.dma("sp", xc[:], XC.ap()[:, :, ch * 128:(ch + 1) * 128].rearrange("f p t -> p f t"), reads=[b_XC], writes=[bxc])
                c.dma("sp", dtt[i][:], DT.ap()[ch], reads=[b_DT], writes=[b_dtt[i]])
                g_, bg = gt[i], b_gt[i]
                dc = slice(d * 32, (d + 1) * 32)
                c.op("dve", lambda e: e.tensor_tensor(out=g_[:, 0, :], in0=dtt[i][:, dc], in1=aneg[:, dc], op=ALU.mult), reads=[b_dtt[i], b_an], writes=[bg])
                p0, bp0 = self.ps[0], self.bps[0]
                c.mm(p0[:, 0:32], [(tri, g_[:, 0, :])], reads=[self.b_const, bg], writes=[bp0])
                c.mm(p0[:, 32:64], [(self.ones32, g_[:, 0, :])], reads=[self.b_const, bg], writes=[bp0])
                c.mm(p0[0:32, 128:256], [(g_[:, 0, :], tri)], reads=[self.b_const, bg], writes=[bp0])
                c.op("act", lambda e: e.activation(out=g_[:, 1, :], in_=p0[:, 0:32], func=AF.Copy), reads=[bp0], writes=[bg])
                c.op("act", lambda e: e.activation(out=g_[:, 2, :], in_=p0[:, 0:32], func=AF.Copy, scale=-1.0), reads=[bp0], writes=[bg])
                c.op("act", lambda e: e.activation(out=g_[:, 3, :], in_=p0[:, 0:32], func=AF.Exp), reads=[bp0], writes=[bg])
                c.op("dve", lambda e: e.tensor_tensor(out=g_[:, 4, :], in0=p0[:, 32:64], in1=g_[:, 1, :], op=ALU.subtract), reads=[bp0, bg], writes=[bg])
                c.op("act", lambda e: e.activation(out=g_[:, 4, :], in_=g_[:, 4, :], func=AF.Exp), reads=[bg], writes=[bg])
                c.op("act", lambda e: e.activation(out=g_[:, 5, :], in_=p0[:, 32:64], func=AF.Exp), reads=[bp0], writes=[bg])
                c.op("act", lambda e: e.activation(out=acT[i][:], in_=p0[0:32, 128:256], func=AF.Copy), reads=[bp0], writes=[b_acT[i]])
                c.op("act", lambda e: e.activation(out=nacT[i][:], in_=p0[0:32, 128:256], func=AF.Copy, scale=-1.0), reads=[bp0], writes=[b_acT[i]])
                for hh in range(2):
                    pT = self.ps[1].ap().bitcast(BF16)
                    c.tr_multi([(pT[:, k * 128:(k + 1) * 128], xc[:, hh * 8 + k, :], self.identb) for k in range(8)], reads=[bxc, self.b_const], writes=[self.bps[1]])
                    c.op("act", lambda e: e.activation(out=xtok[i][:, hh * 16:(hh + 1) * 16, :].rearrange("p h d -> p (h d)"), in_=pT[:, :], func=AF.Copy), reads=[self.bps[1]], writes=[b_xtok[i]])
                pT = self.ps[1].ap().bitcast(BF16)
                c.tr_multi([(pT[:, k * 128:(k + 1) * 128], xc[:, 16 + k, :], self.identb) for k in range(4)], reads=[bxc, self.b_const], writes=[self.bps[1]])
                c.op("act", lambda e: e.activation(out=Btok[i][:].rearrange("p g n -> p (g n)"), in_=pT[:, 0:512], func=AF.Copy), reads=[self.bps[1]], writes=[b_Bt[i]])
                c.op("dve", lambda e: e.tensor_tensor(out=u[i][:], in0=xtok[i][:], in1=dtt[i][:, dc].unsqueeze(2).to_broadcast([128, 32, 64]), op=ALU.mult), reads=[b_xtok[i], b_dtt[i]], writes=[b_u[i]])
                c.op("pool", lambda e: e.tensor_tensor(out=Vw[i][:], in0=u[i][:], in1=g_[:, 4, :].unsqueeze(2).to_broadcast([128, 32, 64]), op=ALU.mult), reads=[b_u[i], bg], writes=[b_Vw[i]])
                Y, bY = Yt[i], b_Yt[i]
                for g in range(4):
                    pcb, bpcb = self.ps[2], self.bps[2]
                    c.mm(pcb[:, 0:128], [(xc[:, 16 + g, :], xc[:, 20 + g, :])], reads=[bxc], writes=[bpcb])
                    sm, bsm = scm[g % 2], b_scm[g % 2]
                    c.op("dve", lambda e: e.tensor_tensor(out=sm[:], in0=pcb[:, 0:128], in1=tri, op=ALU.mult), reads=[bpcb, self.b_const], writes=[bsm])
                    yps, byps = self.ps[4 + g % 2], self.bps[4 + g % 2]
                    for e8 in range(8):
                        h = g * 8 + e8
                        pbc, bpbc = self.ps[6 + e8 // 4], self.bps[6 + e8 // 4]
                        c.mm(pbc[:, (e8 % 4) * 128:(e8 % 4 + 1) * 128], [(sel[:, h, :], acT[i][:]), (nacT[i][:], sel[:, h, :])], reads=[b_sel, b_acT[i]], writes=[bpbc])
                    for hb in range(2):
                        k4 = (g % 2) * 2 + hb
                        pbc, bpbc = self.ps[6 + hb], self.bps[6 + hb]
                        c.op("dve", lambda e: e.tensor_scalar(out=aa[k4][:], in0=pbc[:, :], scalar1=0.0, scalar2=None, op0=ALU.min), reads=[bpbc], writes=[b_aa[k4]])
                        c.op("act", lambda e: e.activation(out=EE[k4][:], in_=aa[k4][:], func=AF.Exp), reads=[b_aa[k4]], writes=[b_EE[k4]])
                        c.op("pool", lambda e: e.tensor_tensor(out=MT[k4][:].rearrange("p (h t) -> p h t", h=4), in0=EE[k4][:].rearrange("p (h t) -> p h t", h=4),
                                                               in1=sm[:].unsqueeze(1).to_broadcast([128, 4, 128]), op=ALU.mult), reads=[bsm, b_EE[k4]], writes=[b_MT[k4]])
                    for e8 in range(8):
                        h = g * 8 + e8
                        k4 = (g % 2) * 2 + e8 // 4
                        c.mm(yps[:, e8 * 64:(e8 + 1) * 64], [(MT[k4][:, (e8 % 4) * 128:(e8 % 4 + 1) * 128], u[i][:, h, :])], reads=[b_MT[k4], b_u[i]], writes=[byps])
                    pin, bpin = self.ps[3], self.bps[3]
                    c.mm(pin[:, :], [(xc[:, 20 + g, :], Sb[:, g, :])], reads=[bxc, b_S[g]], writes=[bpin])
                    y_, byi = yi[g % 2], b_yi[g % 2]
                    c.op("dve", lambda e: e.tensor_tensor(out=y_[:].rearrange("p (h d) -> p h d", d=64), in0=pin[:, :].rearrange("p (h d) -> p h d", d=64),
                                                          in1=g_[:, 3, g * 8:(g + 1) * 8].unsqueeze(2).to_broadcast([128, 8, 64]), op=ALU.mult), reads=[bpin, bg], writes=[byi])
                    c.op("dve", lambda e: e.tensor_tensor(out=Y[:, g * 512:(g + 1) * 512], in0=yps[:, :], in1=y_[:], op=ALU.add), reads=[byps, byi], writes=[bY])
                    if d == 0:
                        c.op("pool", lambda e: e.tensor_tensor(out=y_[:].rearrange("p (h d) -> p h d", d=64), in0=xtok[i][:, g * 8:(g + 1) * 8, :],
                                                               in1=dsk[:, g * 8:(g + 1) * 8].unsqueeze(2).to_broadcast([128, 8, 64]), op=ALU.mult), reads=[b_xtok[i], b_an, bY], writes=[byi])
                        c.op("pool", lambda e: e.tensor_tensor(out=Y[:, g * 512:(g + 1) * 512], in0=Y[:, g * 512:(g + 1) * 512], in1=y_[:], op=ALU.add), reads=[byi], writes=[bY])
                    pst, bpst = self.ps[3], self.bps[3]
                    c.mm(pst[:, :], [(Btok[i][:, g, :], Vw[i][:, g * 8:(g + 1) * 8, :].rearrange("p h d -> p (h d)"))], reads=[b_Bt[i], b_Vw[i]], writes=[bpst])
                    c.op("dve", lambda e: e.tensor_tensor(out=S32[:, g, :].rearrange("p (h d) -> p h d", d=64), in0=S32[:, g, :].rearrange("p (h d) -> p h d", d=64),
                                                          in1=g_[:, 5, g * 8:(g + 1) * 8].unsqueeze(2).to_broadcast([128, 8, 64]), op=ALU.mult), reads=[bg], writes=[b_S[g]])
                    c.op("dve", lambda e: e.tensor_tensor(out=S32[:, g, :], in0=S32[:, g, :], in1=pst[:, :], op=ALU.add), reads=[bpst], writes=[b_S[g]])
                    c.op("act", lambda e: e.activation(out=Sb[:, g, :], in_=S32[:, g, :], func=AF.Copy), reads=[], writes=[b_S[g]])
                c.dma("sp", Yd[d].ap()[ch], Y[:], reads=[bY], writes=[b_Y[d]])
        c.barrier()
        c.sb_release(self.mark0)
        Wo = c.sb([128, 16, 1024], BF16, "ssdWo"); b_wo = Buf()
        c.dma("pool", Wo[:], self.w["ssm_w_out"].ap()[j].rearrange("(k p) n -> p k n", p=128), writes=[b_wo])
        ng = c.sb([128, 2048], F32, "ssdng")
        c.dma("sp", ng[:], self.w["ssm_norm_g"].ap()[j:j + 1, :].partition_broadcast(128), writes=[b_wo])
        self.gated_out(li, need_ctx, Yd, b_Y, Zs, b_Z, 2048, 4, ng, Wo, b_wo, False)
        self.phase_end()

    def gated_out(self, li, need_ctx, Yd, b_Y, Zs, b_Z, W, ngroups, ng, Wo, b_wo, gate_after):
        c = self.c
        NCT, NT = self.NCT, self.NT
        KC = W // 128
        gs = W // ngroups
        G1x, bG1x = self.load_mod(li, 2, 0, "G1x")
        G1c, bG1c = self.load_mod(li, 2, 1, "G1c")
        yf = [c.sb([128, W], F32, "yf") for _ in range(2)]; yb = [c.sb([128, W], F32, "yb") for _ in range(2)]
        zt = [c.sb([128, W], BF16, "zt") for _ in range(2)]
        b_in = [Buf(), Buf()]
        jk = c.sb([128, W], BF16, "gjunk"); b_jk = Buf()
        st = [c.sb([128, 2, 8], F32, "gst") for _ in range(2)]; b_st = [Buf(), Buf()]
        yn = [c.sb([128, W], BF16, "yn") for _ in range(2)]; b_yn = [Buf(), Buf()]
        yT = [c.sb([128, KC, 128], BF16, "gyT") for _ in range(2)]; b_yT = [Buf(), Buf()]
        xts = [c.sb([128, D], F32, "xres") for _ in range(2)]; b_xt = [Buf(), Buf()]
        tmps = [c.sb([128, D], F32, "rtmp") for _ in range(2)]; b_tmp = [Buf(), Buf()]
        for bi, qb in enumerate(range(0 if need_ctx else NCT, NT)):
            i = bi % 2
            is_ctx = qb < NCT
            c.dma("sp", yf[i][:], Yd[0].ap()[qb], reads=[b_Y[0]], writes=[b_in[i]])
            c.dma("sp", yb[i][:], Yd[1].ap()[qb], reads=[b_Y[1]], writes=[b_in[i]])
            c.dma("sp", zt[i][:], Zs.ap()[qb], reads=[b_Z], writes=[b_in[i]])
            c.dma("sp", xts[i][:], self.xs.ap()[qb * 128:(qb + 1) * 128, :], reads=[self.b_xs[qb]], writes=[b_xt[i]])
            c.op("pool", lambda e: e.tensor_tensor(out=yf[i][:], in0=yf[i][:], in1=yb[i][:], op=ALU.add), reads=[b_in[i]], writes=[b_in[i]])
            if not gate_after:
                c.op("dve", lambda e: e.tensor_tensor(out=yf[i][:], in0=yf[i][:], in1=zt[i][:], op=ALU.mult), reads=[b_in[i]], writes=[b_in[i]])
            c.op("dve", lambda e: e.memset(st[i][:], 0.0), writes=[b_st[i]])
            for g in range(ngroups):
                c.op("act", lambda e: e.activation(out=jk[:, g * gs:(g + 1) * gs], in_=yf[i][:, g * gs:(g + 1) * gs], func=AF.Square, accum_out=st[i][:, 0, g:g + 1]), reads=[b_in[i]], writes=[b_jk, b_st[i]])
            c.op("dve", lambda e: e.tensor_scalar(out=st[i][:, 1, :], in0=st[i][:, 0, :], scalar1=1.0 / gs, scalar2=EPS, op0=ALU.mult, op1=ALU.add), reads=[b_st[i]], writes=[b_st[i]])
            c.op("act", lambda e: e.activation(out=st[i][:, 1, :], in_=st[i][:, 1, :], func=AF.Sqrt), reads=[b_st[i]], writes=[b_st[i]])
            c.op("dve", lambda e: e.reciprocal(out=st[i][:, 1, :], in_=st[i][:, 1, :]), reads=[b_st[i]], writes=[b_st[i]])
            c.op("dve", lambda e: e.tensor_tensor(out=yf[i][:].rearrange("p (g d) -> p g d", g=ngroups), in0=yf[i][:].rearrange("p (g d) -> p g d", g=ngroups),
                                                  in1=st[i][:, 1, 0:ngroups].unsqueeze(2).to_broadcast([128, ngroups, gs]), op=ALU.mult), reads=[b_st[i], b_in[i]], writes=[b_in[i]])
            if gate_after:
                c.op("pool", lambda e: e.tensor_tensor(out=yf[i][:], in0=yf[i][:], in1=ng[:], op=ALU.mult), reads=[b_in[i], b_wo], writes=[b_in[i]])
                c.op("dve", lambda e: e.tensor_tensor(out=yn[i][:], in0=yf[i][:], in1=zt[i][:], op=ALU.mult), reads=[b_in[i]], writes=[b_yn[i]])
            else:
                c.op("pool", lambda e: e.tensor_tensor(out=yn[i][:], in0=yf[i][:], in1=ng[:], op=ALU.mult), reads=[b_in[i], b_wo], writes=[b_yn[i]])
            for hb in range(KC // 8):
                pT = self.ps[hb].ap().bitcast(BF16)
                c.tr_multi([(pT[:, k * 128:(k + 1) * 128], yn[i][:, (hb * 8 + k) * 128:(hb * 8 + k + 1) * 128], self.identb) for k in range(8)], reads=[b_yn[i], self.b_const], writes=[self.bps[hb]])
                c.op("act", lambda e: e.activation(out=yT[i][:, hb * 8:(hb + 1) * 8, :], in_=pT.rearrange("p (k n) -> p k n", k=8), func=AF.Copy), reads=[self.bps[hb]], writes=[b_yT[i]])
            G1, bG1 = (G1c, bG1c) if is_ctx else (G1x, bG1x)
            for nn in range(2):
                z, bz = self.ps[5 + nn], self.bps[5 + nn]
                c.mm(z[:, :], [(yT[i][:, k, :], Wo[:, k, nn * 512:(nn + 1) * 512]) for k in range(KC)], reads=[b_yT[i], b_wo], writes=[bz])
                c.op("dve", lambda e: e.tensor_tensor(out=tmps[i][:, nn * 512:(nn + 1) * 512], in0=z[:, :], in1=G1[:, nn * 512:(nn + 1) * 512], op=ALU.mult), reads=[bz, bG1], writes=[b_tmp[i]])
            c.op("pool", lambda e: e.tensor_tensor(out=xts[i][:], in0=xts[i][:], in1=tmps[i][:], op=ALU.add), reads=[b_tmp[i], b_xt[i]], writes=[b_xt[i]])
            c.dma("sp", self.xs.ap()[qb * 128:(qb + 1) * 128, :], xts[i][:], reads=[b_xt[i]], writes=[self.b_xs[qb]])

    def layer_mlstm(self, li, j, need_ctx):
        c, nc = self.c, self.nc
        T, NT, NCT = self.T, self.NT, self.NCT
        w_in = self.w["mlstm_w_in"].ap()[j]
        QK = self.scratch(f"ml_qk{li}", [2, 8, 64, T], BF16)
        Kt = self.scratch(f"ml_kt{li}", [NT, 128, 512], BF16)
        Va = self.scratch(f"ml_va{li}", [NT, 128, 8, 129], BF16)
        Os = self.scratch(f"ml_os{li}", [NT, 128, 1024], BF16)
        Gt = self.scratch(f"ml_gt{li}", [NT, 128, 32], F32)
        Yd = [self.scratch(f"ml_y{li}_{d}", [NT, 128, 1024], F32) for d in range(2)]
        b_QK = Buf(); b_Kt = Buf(); b_Va = Buf(); b_Os = Buf(); b_Gt = Buf(); b_Y = [Buf(), Buf()]
        b_w = Buf("ml_w")
        Win = c.sb([128, 8, 3104], BF16, "mlWin")
        for k in range(8):
            c.dma("pool", Win[:, k, :], w_in[k * 128:(k + 1) * 128, :], writes=[b_w])
        gb = c.sb([128, 32], F32, "mlgb")
        c.dma("sp", gb[:], self.w["mlstm_gate_b"].ap()[j:j + 1].rearrange("o a h -> o (a h)").partition_broadcast(128), writes=[b_w])
        A1x, bA1x = self.load_mod(li, 1, 0, "A1x")
        S1x, bS1x = self.load_mod(li, 0, 0, "S1x")
        A1c, bA1c = self.load_mod(li, 1, 1, "A1c")
        S1c, bS1c = self.load_mod(li, 0, 1, "S1c")
        self.alloc_norm_bufs(2)
        hTs = [c.sb([128, 8, 512], BF16, "hT") for _ in range(2)]
        b_hT = [Buf(), Buf()]
        stg = [c.sb([64, 512], BF16, "mlstg") for _ in range(3)]; b_stg = [Buf() for _ in range(3)]
        kst = [c.sb([128, 512], BF16, "mlkst") for _ in range(2)]; b_kst = [Buf(), Buf()]
        vst = [c.sb([128, 8, 129], BF16, "mlvst") for _ in range(2)]; b_vst = [Buf(), Buf()]
        ost = [c.sb([128, 1024], BF16, "mlost") for _ in range(2)]; b_ost = [Buf(), Buf()]
        gst = [c.sb([128, 32], F32, "mlgst") for _ in range(2)]; b_gst = [Buf(), Buf()]
        gtmp = [c.sb([128, 8], F32, "mlgtmp") for _ in range(2)]
        for i in range(2):
            c.op("pool", lambda e: e.memset(vst[i][:], 1.0), writes=[b_vst[i]])
        cnt = 0
        for gi, (t0, n, is_ctx) in enumerate(self.groups(4)):
            ncols = n * 128
            col0 = t0 * 128
            hT, bh = hTs[gi % 2], b_hT[gi % 2]
            for s in range(n):
                if is_ctx:
                    self.norm_tile(t0 + s, A1c, bA1c, S1c, bS1c, hT, bh, s * 128, (t0 + s) % 2)
                else:
                    self.norm_tile(t0 + s, A1x, bA1x, S1x, bS1x, hT, bh, s * 128, (t0 + s) % 2)
            for qk in range(2):
                for h in range(8):
                    i = cnt % 3
                    cnt += 1
                    P, bP = self.ps[2 + i], self.bps[2 + i]
                    col = qk * 512 + h * 64
                    c.mm(P[0:64, :ncols], [(Win[:, k, col:col + 64], hT[:, k, :ncols]) for k in range(8)], reads=[b_w, bh], writes=[bP])
                    c.op("act", lambda e: e.activation(out=stg[i][:, :ncols], in_=P[0:64, :ncols], func=AF.Copy, scale=(0.125 if qk == 1 else 1.0)), reads=[bP], writes=[b_stg[i]])
                    c.dma("sp", QK.ap()[qk, h, :, col0:col0 + ncols], stg[i][:, :ncols], reads=[b_stg[i]], writes=[b_QK])
            for s in range(n):
                sl = slice(s * 128, (s + 1) * 128)
                i2 = s % 2

                def tokproj(c0, w_):
                    nonlocal cnt
                    i = cnt % 3
                    cnt += 1
                    P, bP = self.ps[2 + i], self.bps[2 + i]
                    c.mm(P[:, :w_], [(hT[:, k, sl], Win[:, k, c0:c0 + w_]) for k in range(8)], reads=[b_w, bh], writes=[bP])
                    return P, bP
                P, bP = tokproj(512, 512)
                c.op("act", lambda e: e.activation(out=kst[i2][:], in_=P[:, :], func=AF.Copy, scale=0.125), reads=[bP], writes=[b_kst[i2]])
                c.dma("sp", Kt.ap()[t0 + s], kst[i2][:], reads=[b_kst[i2]], writes=[b_Kt])
                for vh in range(2):
                    P, bP = tokproj(1024 + vh * 512, 512)
                    c.op("act", lambda e: e.activation(out=vst[i2][:, vh * 4:(vh + 1) * 4, 0:128], in_=P[:, :].rearrange("p (h d) -> p h d", d=128), func=AF.Copy), reads=[bP], writes=[b_vst[i2]])
                c.dma("sp", Va.ap()[t0 + s], vst[i2][:], reads=[b_vst[i2]], writes=[b_Va])
                for oh in range(2):
                    P, bP = tokproj(2048 + oh * 512, 512)
                    c.op("act", lambda e: e.activation(out=ost[i2][:, oh * 512:(oh + 1) * 512], in_=P[:, :], func=AF.Sigmoid), reads=[bP], writes=[b_ost[i2]])
                c.dma("sp", Os.ap()[t0 + s], ost[i2][:], reads=[b_ost[i2]], writes=[b_Os])
                P, bP = tokproj(3072, 32)
                g_ = gst[i2]
                c.op("dve", lambda e: e.tensor_tensor(out=g_[:], in0=P[:, 0:32], in1=gb[:], op=ALU.add), reads=[bP, b_w], writes=[b_gst[i2]])
                for r in (1, 3):
                    cs_ = slice(r * 8, (r + 1) * 8)
                    c.op("act", lambda e: e.activation(out=g_[:, cs_], in_=g_[:, cs_], func=AF.Exp, scale=-1.0), reads=[b_gst[i2]], writes=[b_gst[i2]])
                    c.op("act", lambda e: e.activation(out=g_[:, cs_], in_=g_[:, cs_], func=AF.Ln, bias=1.0), reads=[b_gst[i2]], writes=[b_gst[i2]])
                    c.op("act", lambda e: e.activation(out=g_[:, cs_], in_=g_[:, cs_], func=AF.Copy, scale=-1.0), reads=[b_gst[i2]], writes=[b_gst[i2]])
                c.dma("sp", Gt.ap()[t0 + s], g_[:], reads=[b_gst[i2]], writes=[b_Gt])
        c.barrier()
        c.sb_release(self.mark0)
        sel = c.sb([8, 8, 128], F32, "sel8"); b_sel = Buf()
        c.dma("sp", sel[:], self.w["sel32"].ap()[0:8, 0:8, :], writes=[b_sel])
        qTs = [c.sb([64, 8, 128], BF16, "mqT") for _ in range(2)]; kTs = [c.sb([64, 8, 128], BF16, "mkT") for _ in range(2)]
        kts = [c.sb([128, 512], BF16, "mkt") for _ in range(2)]; vas = [c.sb([128, 8, 129], BF16, "mva") for _ in range(2)]
        gts = [c.sb([128, 32], F32, "mgt") for _ in range(2)]; b_ld = [Buf(), Buf()]
        GM = [c.sb([8, 12, 128], F32, "GM") for _ in range(2)]; b_GM = [Buf(), Buf()]
        sm8 = [c.sb([8, 8], F32, "sm8") for _ in range(2)]; b_sm8 = [Buf(), Buf()]
        ms = c.sb([8, 2], F32, "ms"); b_ms = Buf()
        dg = c.sb([8, 8], F32, "dg"); b_dg = Buf()
        tk = [c.sb([128, 40], F32, "tk") for _ in range(2)]; b_tk = [Buf(), Buf()]
        cwc = [c.sb([64, 8], F32, "cwc") for _ in range(2)]; b_cwc = [Buf(), Buf()]
        scm = [c.sb([128, 512], F32, "mscm") for _ in range(2)]; b_scm = [Buf() for _ in range(2)]
        aa = [c.sb([128, 512], F32, "maa") for _ in range(2)]; b_aa = [Buf() for _ in range(2)]
        EE = [c.sb([128, 512], F32, "mEE") for _ in range(2)]; b_EE = [Buf() for _ in range(2)]
        MT = [c.sb([128, 512], BF16, "mMT") for _ in range(2)]; b_MT = [Buf() for _ in range(2)]
        yi = [c.sb([128, 129], F32, "myi") for _ in range(4)]; b_yi = [Buf() for _ in range(4)]
        nd4 = [c.sb([128, 4, 132], F32, "mnd4") for _ in range(2)]; b_nd4 = [Buf() for _ in range(2)]
        Vw = [c.sb([128, 129], BF16, "mVw") for _ in range(4)]; b_Vw = [Buf() for _ in range(4)]
        Yt = [c.sb([128, 1024], F32, "mYt") for _ in range(2)]; b_Yt = [Buf(), Buf()]
        S32 = c.sb([64, 8, 129], F32, "mS32"); Sb = c.sb([64, 8, 129], BF16, "mSb"); b_S = [Buf() for _ in range(8)]
        ci = 0; hi = 0
        id8 = self.ident32[0:8, 0:8]
        for d in range(2):
            tri = self.trile32 if d == 0 else self.trige32
            order = list(range(NT)) if d == 0 else (list(range(NCT - 1, -1, -1)) + list(range(NT - 1, NCT - 1, -1)))
            c.op("dve", lambda e: e.memset(S32[:], 0.0), writes=b_S)
            c.op("pool", lambda e: e.memset(Sb[:], 0.0), writes=b_S)
            c.op("dve", lambda e: e.memset(ms[:], 0.0), writes=[b_ms])
            endc = 127 if d == 0 else 0
            def gate(ch, i):
                cols = slice(ch * 128, (ch + 1) * 128)
                bl = b_ld[i]
                c.dma("sp", qTs[i][:], QK.ap()[0, :, :, cols].rearrange("h d t -> d h t"), reads=[b_QK], writes=[bl])
                c.dma("sp", kTs[i][:], QK.ap()[1, :, :, cols].rearrange("h d t -> d h t"), reads=[b_QK], writes=[bl])
                c.dma("sp", kts[i][:], Kt.ap()[ch], reads=[b_Kt], writes=[bl])
                c.dma("sp", vas[i][:], Va.ap()[ch], reads=[b_Va], writes=[bl])
                c.dma("sp", gts[i][:], Gt.ap()[ch], reads=[b_Gt], writes=[bl])
                ig = gts[i][:, d * 16:d * 16 + 8]
                lf = gts[i][:, d * 16 + 8:d * 16 + 16]
                G, bG = GM[i], b_GM[i]
                s8, bs8 = sm8[i], b_sm8[i]
                t_, bt = tk[i], b_tk[i]
                p0, bp0 = self.ps[0], self.bps[0]
                c.mm(p0[0:8, 0:128], [(ig, self.ident32)], reads=[bl, self.b_const], writes=[bp0])
                c.mm(p0[0:8, 128:256], [(lf, tri)], reads=[bl, self.b_const], writes=[bp0])
                c.mm(p0[:, 256:264], [(tri, lf)], reads=[bl, self.b_const], writes=[bp0])
                c.op("act", lambda e: e.activation(out=G[:, 0:2, :], in_=p0[0:8, 0:256].rearrange("p (a t) -> p a t", a=2), func=AF.Copy), reads=[bp0], writes=[bG])
                c.op("act", lambda e: e.activation(out=t_[:, 32:40], in_=p0[:, 256:264], func=AF.Copy), reads=[bp0], writes=[bt])
                c.op("dve", lambda e: e.tensor_tensor(out=t_[:, 0:8], in0=ig, in1=t_[:, 32:40], op=ALU.subtract), reads=[bl, bt], writes=[bt])
                c.op("dve", lambda e: e.tensor_tensor(out=G[:, 2, :], in0=G[:, 0, :], in1=G[:, 1, :], op=ALU.subtract), reads=[bG], writes=[bG])
                src, dst = 2, 3
                for k in range(7):
                    sft = 1 << k
                    c.op("dve", lambda e: e.tensor_copy(out=G[:, dst, :], in_=G[:, src, :]), reads=[bG], writes=[bG])
                    if d == 0:
                        c.op("dve", lambda e: e.tensor_tensor(out=G[:, dst, sft:128], in0=G[:, src, sft:128], in1=G[:, src, 0:128 - sft], op=ALU.max), reads=[bG], writes=[bG])
                    else:
                        c.op("dve", lambda e: e.tensor_tensor(out=G[:, dst, 0:128 - sft], in0=G[:, src, 0:128 - sft], in1=G[:, src, sft:128], op=ALU.max), reads=[bG], writes=[bG])
                    src, dst = dst, (3 if dst == 4 else 4)
                cmr = src
                c.op("dve", lambda e: e.tensor_scalar(out=G[:, cmr, :], in0=G[:, cmr, :], scalar1=ms[:, 0:1], scalar2=None, op0=ALU.max), reads=[bG, b_ms], writes=[bG])
                c.op("act", lambda e: e.activation(out=G[:, 5, :], in_=G[:, cmr, :], func=AF.Copy, scale=-1.0), reads=[bG], writes=[bG])
                c.op("act", lambda e: e.activation(out=G[:, 6, :], in_=G[:, cmr, :], func=AF.Exp, scale=-1.0, bias=ms[:, 0:1]), reads=[bG, b_ms], writes=[bG])
                c.op("dve", lambda e: e.tensor_tensor(out=G[:, 7, :], in0=G[:, 1, :], in1=G[:, cmr, :], op=ALU.add), reads=[bG], writes=[bG])
                c.op("act", lambda e: e.activation(out=G[:, 7, :], in_=G[:, 7, :], func=AF.Exp, scale=-1.0), reads=[bG], writes=[bG])
                c.op("dve", lambda e: e.tensor_copy(out=s8[:, 0:1], in_=G[:, cmr, endc:endc + 1]), reads=[bG], writes=[bs8])
                c.op("dve", lambda e: e.tensor_scalar(out=s8[:, 1:2], in0=s8[:, 0:1], scalar1=-1.0, scalar2=None, op0=ALU.mult), reads=[bs8], writes=[bs8])
                c.op("dve", lambda e: e.tensor_copy(out=s8[:, 2:3], in_=G[:, 1, endc:endc + 1]), reads=[bG], writes=[bs8])
                c.op("act", lambda e: e.activation(out=G[:, 8, :], in_=G[:, 2, :], func=AF.Exp, bias=s8[:, 1:2]), reads=[bG, bs8], writes=[bG])
                c.op("act", lambda e: e.activation(out=s8[:, 3:4], in_=ms[:, 0:1], func=AF.Exp, bias=s8[:, 1:2]), reads=[b_ms, bs8], writes=[bs8])
                c.op("dve", lambda e: e.tensor_tensor(out=ms[:, 0:1], in0=s8[:, 2:3], in1=s8[:, 0:1], op=ALU.add), reads=[bs8, bG], writes=[b_ms])
                p1, bp1 = self.ps[1], self.bps[1]
                c.mm_multi([(p1[:, (r - 6) * 8:(r - 5) * 8], [(G[:, r, :], id8)]) for r in (6, 7, 8)], reads=[bG, self.b_const], writes=[bp1])
                c.op("act", lambda e: e.activation(out=t_[:, 8:32], in_=p1[:, 0:24], func=AF.Copy), reads=[bp1], writes=[bt])
                c.op("dve", lambda e: e.tensor_scalar(out=dg[:], in0=id8, scalar1=s8[:, 3:4], scalar2=None, op0=ALU.mult), reads=[self.b_const, bs8], writes=[b_dg])
                c.mm(p1[0:64, 32:40], [(self.ones32[0:8, 0:64], dg[:])], reads=[self.b_const, b_dg], writes=[bp1])
                c.op("act", lambda e: e.activation(out=cwc[i][:], in_=p1[0:64, 32:40], func=AF.Copy), reads=[bp1], writes=[b_cwc[i]])
            def heads(ch, i):
                bl = b_ld[i]; G, bG = GM[i], b_GM[i]; t_, bt = tk[i], b_tk[i]
                Y, bY = Yt[i], b_Yt[i]
                for hb0 in (0, 4):
                    psc, bpsc = self.ps[2], self.bps[2]
                    pbc, bpbc = self.ps[6], self.bps[6]
                    for q4 in range(4):
                        h = hb0 + q4
                        cs4 = slice(q4 * 128, (q4 + 1) * 128)
                        c.mm(psc[:, cs4], [(kTs[i][:, h, :], qTs[i][:, h, :])], reads=[bl], writes=[bpsc])
                        c.mm(pbc[:, cs4], [(sel[:, h, :], G[:, 5, :]), (G[:, 2, :], sel[:, h, :])], reads=[b_sel, bG], writes=[bpbc])
                    pins = []
                    for q4 in range(4):
                        h = hb0 + q4
                        bk = 3 if q4 < 2 else 7
                        pin_ap = self.ps[bk][:, (q4 % 2) * 256:(q4 % 2) * 256 + 129]
                        c.mm(pin_ap, [(qTs[i][:, h, :], Sb[:, h, :])], reads=[bl, b_S[h]], writes=[self.bps[bk]])
                        pins.append((pin_ap, self.bps[bk]))
                    k4 = (hb0 // 4)
                    c.op("dve", lambda e: e.tensor_tensor(out=scm[k4][:].rearrange("p (h t) -> p h t", h=4), in0=psc[:, :].rearrange("p (h t) -> p h t", h=4),
                                                          in1=tri.unsqueeze(1).to_broadcast([128, 4, 128]), op=ALU.mult), reads=[bpsc, self.b_const], writes=[b_scm[k4]])
                    c.op("dve", lambda e: e.tensor_scalar(out=aa[k4][:], in0=pbc[:, :], scalar1=0.0, scalar2=None, op0=ALU.min), reads=[bpbc], writes=[b_aa[k4]])
                    c.op("act", lambda e: e.activation(out=EE[k4][:], in_=aa[k4][:], func=AF.Exp), reads=[b_aa[k4]], writes=[b_EE[k4]])
                    c.op("pool", lambda e: e.tensor_tensor(out=MT[k4][:], in0=scm[k4][:], in1=EE[k4][:], op=ALU.mult), reads=[b_scm[k4], b_EE[k4]], writes=[b_MT[k4]])
                    for q4 in range(4):
                        h = hb0 + q4
                        pin_ap, bpin = pins[q4]
                        c.op("act", lambda e: e.activation(out=yi[q4][:], in_=pin_ap, func=AF.Copy, scale=t_[:, 8 + h:9 + h]), reads=[bpin, bt], writes=[b_yi[q4]])
                        c.op("pool", lambda e: e.tensor_tensor(out=Vw[q4][:], in0=vas[i][:, h, :], in1=t_[:, 24 + h:25 + h].to_broadcast([128, 129]), op=ALU.mult), reads=[bl, bt], writes=[b_Vw[q4]])
                    pnds = []; psts = []
                    for q4 in range(4):
                        h = hb0 + q4
                        bk = 4 + q4 // 2
                        pnd_ap = self.ps[bk][:, (q4 % 2) * 256:(q4 % 2) * 256 + 129]
                        c.mm(pnd_ap, [(MT[k4][:, q4 * 128:(q4 + 1) * 128], vas[i][:, h, :])], reads=[b_MT[k4], bl], writes=[self.bps[bk]])
                        pnds.append((pnd_ap, self.bps[bk]))
                    for q4 in range(4):
                        h = hb0 + q4
                        if q4 < 3:
                            pst_ap, bpst = self.ps[1][0:64, q4 * 129:(q4 + 1) * 129], self.bps[1]
                        else:
                            pst_ap, bpst = self.ps[0][0:64, 264:393], self.bps[0]
                        c.mm(pst_ap, [(kts[i][:, h * 64:(h + 1) * 64], Vw[q4][:])], reads=[bl, b_Vw[q4]], writes=[bpst])
                        psts.append((pst_ap, bpst))
                    n4 = nd4[k4]; bn = b_nd4[k4]
                    for q4 in range(4):
                        pnd_ap, bpnd = pnds[q4]
                        c.op("dve", lambda e: e.tensor_tensor(out=n4[:, q4, 0:129], in0=pnd_ap, in1=yi[q4][:], op=ALU.add), reads=[bpnd, b_yi[q4]], writes=[bn])
                    c.op("dve", lambda e: e.tensor_scalar(out=n4[:, :, 129:130], in0=n4[:, :, 128:129], scalar1=-1.0, scalar2=None, op0=ALU.mult), reads=[bn], writes=[bn])
                    c.op("dve", lambda e: e.tensor_tensor(out=n4[:, :, 130:131], in0=n4[:, :, 128:129], in1=n4[:, :, 129:130], op=ALU.max), reads=[bn], writes=[bn])
                    c.op("dve", lambda e: e.tensor_tensor(out=n4[:, :, 130:131], in0=n4[:, :, 130:131], in1=t_[:, 16 + hb0:20 + hb0].unsqueeze(2), op=ALU.max), reads=[bn, bt], writes=[bn])
                    c.op("dve", lambda e: e.reciprocal(out=n4[:, :, 131:132], in_=n4[:, :, 130:131]), reads=[bn], writes=[bn])
                    c.op("dve", lambda e: e.tensor_tensor(out=Y[:, hb0 * 128:(hb0 + 4) * 128].rearrange("p (h d) -> p h d", h=4), in0=n4[:, :, 0:128],
                                                          in1=n4[:, :, 131:132].to_broadcast([128, 4, 128]), op=ALU.mult), reads=[bn], writes=[bY])
                    for q4 in range(4):
                        h = hb0 + q4
                        pst_ap, bpst = psts[q4]
                        c.op("dve", lambda e: e.scalar_tensor_tensor(out=S32[:, h, :], in0=S32[:, h, :], scalar=cwc[i][:, h:h + 1], in1=pst_ap, op0=ALU.mult, op1=ALU.add), reads=[b_cwc[i], bpst], writes=[b_S[h]])
                        c.op("act", lambda e: e.activation(out=Sb[:, h, :], in_=S32[:, h, :], func=AF.Copy), reads=[], writes=[b_S[h]])
                c.dma("sp", Yd[d].ap()[ch], Y[:], reads=[bY], writes=[b_Y[d]])
            gate(order[0], 0)
            for idx, ch in enumerate(order):
                if idx + 1 < len(order):
                    gate(order[idx + 1], (idx + 1) % 2)
                heads(ch, idx % 2)
        c.barrier()
        c.sb_release(self.mark0)
        Wo = c.sb([128, 8, 1024], BF16, "mlWo"); b_wo = Buf()
        c.dma("pool", Wo[:], self.w["mlstm_w_out"].ap()[j].rearrange("(k p) n -> p k n", p=128), writes=[b_wo])
        ng = c.sb([128, 1024], F32, "mlng")
        c.dma("sp", ng[:], self.w["mlstm_norm_g"].ap()[j:j + 1, :].partition_broadcast(128), writes=[b_wo])
        self.gated_out(li, need_ctx, Yd, b_Y, Os, b_Os, 1024, 8, ng, Wo, b_wo, True)
        self.phase_end()

    def build(self, wshapes):
        self.inp("x", [self.NL, D])
        self.inp("ctx", [self.NCX, D])
        self.inp("cT", [128, 8, 2])
        self.inp("cmat", [128, 4, 128])
        self.inp("sel32", [32, 32, 128])
        self.inp("rope64", [2, 64, self.NL])
        self.inp("rope32", [2, 32, self.NL])
        self.inp("rope96", [2, 96, self.NL])
        self.inp("ssm_conv_wT", [wshapes["ssm_conv_w"][0], 128, 24, 5])
        self.inp("ssm_conv_bT", [wshapes["ssm_conv_w"][0], 128, 24])
        for k, s in wshapes.items():
            self.inp(k, s)
        self.out = self.nc.dram_tensor("out", [self.NL, D], F32, kind="ExternalOutput")
        self.setup_consts()
        self.prologue()
        cnt = {0: 0, 1: 0, 2: 0, 3: 0, 9: 0}
        for li, kind in enumerate(self.kinds):
            need_ctx = li < self.depth - 1
            j = cnt[kind]
            cnt[kind] += 1
            self.want_precast = li
            if kind == 0:
                self.layer_gqa(li, j, need_ctx)
            elif kind == 1:
                self.layer_ssd(li, j, need_ctx)
            elif kind == 2:
                self.layer_mlstm(li, j, need_ctx)
            elif kind == 3:
                self.layer_mla(li, j, need_ctx)
            self.layer_moe(li, need_ctx)
        self.final()
        return self.nc


WEIGHT_KEYS = ["norm1_g", "norm2_g", "w_mod", "b_mod", "moe_w_group", "moe_b_group", "moe_w_expert", "moe_b_expert",
               "moe_w_gate", "moe_w_up", "moe_w_down", "attn_w_in", "attn_sink", "attn_w_out",
               "ssm_w_in", "ssm_conv_w", "ssm_conv_b", "ssm_dt_bias", "ssm_a_log", "ssm_d", "ssm_norm_g", "ssm_w_out",
               "mlstm_w_in", "mlstm_gate_b", "mlstm_norm_g", "mlstm_w_out",
               "mla_w_in", "mla_q_norm_g", "mla_w_q_up", "mla_kv_norm_g", "mla_w_kv_up", "mla_w_out", "final_norm_g"]


def run_model(inputs, kinds, n_cores=None):
    x = np.asarray(inputs["x"], np.float32)
    ctx = np.asarray(inputs["ctx"], np.float32)
    c = np.asarray(inputs["c"], np.float32)
    c_ctx = np.asarray(inputs["c_ctx"], np.float32)
    B, n_lat, _ = x.shape
    n_ctx = ctx.shape[1]
    weights = {k: np.ascontiguousarray(np.asarray(inputs[k], np.float32)) for k in WEIGHT_KEYS}
    m = Model(n_lat, n_ctx, kinds)
    nc = m.build({k: v.shape for k, v in weights.items()})
    consts = host_consts(n_lat)
    in_maps = []
    for b in range(B):
        cT = np.stack([c[b].reshape(8, 128).T, c_ctx.reshape(8, 128).T], axis=-1)
        d = {"x": np.ascontiguousarray(x[b]), "ctx": np.ascontiguousarray(ctx[b]), "cT": np.ascontiguousarray(cT.astype(np.float32))}
        d.update(consts)
        d.update(weights)
        d["ssm_conv_wT"] = np.ascontiguousarray(weights["ssm_conv_w"].reshape(-1, 5, 24, 128).transpose(0, 3, 2, 1))
        d["ssm_conv_bT"] = np.ascontiguousarray(weights["ssm_conv_b"].reshape(-1, 24, 128).transpose(0, 2, 1))
        in_maps.append(d)
    res = run_bass_kernel_spmd(nc, in_maps, core_ids=list(range(B)))
    return np.stack([np.asarray(r["out"], np.float32) for r in res.results], axis=0)


def kernel(**inputs):
    return run_model(inputs, [0, 1, 2, 3])
```

```python
import numpy as np
import concourse.bass as bass
import concourse.mybir as mybir

F32 = mybir.dt.float32
BF16 = mybir.dt.bfloat16
AF = mybir.ActivationFunctionType
ALU = mybir.AluOpType
AX = mybir.AxisListType


class Buf:
    __slots__ = ("w", "r", "name")

    def __init__(self, name=""):
        self.w = None
        self.r = []
        self.name = name


class Ctx:
    EPOCH = 30000

    def __init__(self, nc, n_dma=None):
        self.nc = nc
        self.E = {"pe": nc.tensor, "act": nc.scalar, "dve": nc.vector, "pool": nc.gpsimd, "sp": nc.sync}
        self.csem = {}
        self.seen = {e: {} for e in self.E}
        self.semid = {}
        n_dma = n_dma or {"sp": 48, "act": 2, "pool": 30}
        self.dslots = {}
        self.drr = {}
        for q, n in n_dma.items():
            self.dslots[q] = [[self._new_sem(f"d{q}{i}"), 0] for i in range(n)]
            self.drr[q] = 0
        for e in ("pe", "act", "dve", "pool"):
            self.csem[e] = [self._new_sem(f"c{e}0"), 0, 0]
        self.sb_off = 0
        self.sb_base = 16512
        self.sb_cap = 229344 - 16512
        self.n_alloc = 0
        self.n_ins = 0
        self.n_wait = 0

    def _new_sem(self, name):
        s = self.nc.alloc_semaphore(name)
        self.semid[id(s)] = s
        return s

    def sb_mark(self):
        return self.sb_off

    def sb_release(self, mark):
        self.sb_off = mark

    def sb(self, shape, dtype, name=None):
        esz = 4 if dtype == F32 else 2
        if dtype in (mybir.dt.int32, mybir.dt.uint32):
            esz = 4
        n = 1
        for s in shape[1:]:
            n *= s
        nbytes = (n * esz + 63) // 64 * 64
        off = self.sb_off
        if off + nbytes > self.sb_cap:
            raise RuntimeError(f"SBUF overflow: want {nbytes} at {off} cap {self.sb_cap} ({name})")
        self.sb_off += nbytes
        self.n_alloc += 1
        t = self.nc.alloc_sbuf_tensor_at(f"{name or 't'}_{self.n_alloc}", list(shape), dtype, offset=self._abs(off))
        return t

    def _abs(self, off):
        return self.sb_base + off

    def _wait(self, eng, ev):
        if ev is None:
            return
        sem, val = ev
        k = id(sem)
        if self.seen[eng].get(k, 0) >= val:
            return
        self.E[eng].wait_ge(sem, val)
        self.n_wait += 1
        self.seen[eng][k] = val

    def _deps(self, eng, reads, writes):
        for b in reads:
            if b.w is not None and not (eng == "pe" and b.w[2] == "pe"):
                self._wait(eng, b.w[:2])
        for b in writes:
            if b.w is not None and not (eng == "pe" and b.w[2] == "pe"):
                self._wait(eng, b.w[:2])
            for r in b.r:
                if not (eng == "pe" and r[2] == "pe"):
                    self._wait(eng, r[:2])

    def _record(self, ev, reads, writes):
        for b in reads:
            b.r.append(ev)
            if len(b.r) > 64:
                b.r = b.r[-64:] if False else b.r
        for b in writes:
            b.w = ev
            b.r = []

    def _signal(self, eng, ins):
        st = self.csem[eng]
        if st[1] >= self.EPOCH:
            st = self.csem[eng] = [self._new_sem(f"c{eng}{st[2] + 1}"), 0, st[2] + 1]
        st[1] += 1
        ins.then_inc(st[0], 1)
        return (st[0], st[1], eng)

    def op(self, eng, fn, reads=(), writes=()):
        self._deps(eng, reads, writes)
        ins = fn(self.E[eng])
        self.n_ins += 1
        ev = self._signal(eng, ins)
        self._record(ev, reads, writes)
        return ev

    def mm(self, out, pairs, reads=(), writes=(), start=True, stop=True):
        self._deps("pe", reads, writes)
        n = len(pairs)
        ins = None
        for i, (l, r) in enumerate(pairs):
            ins = self.nc.tensor.matmul(out, l, r, start=(start and i == 0), stop=(stop and i == n - 1))
            self.n_ins += 1
        ev = self._signal("pe", ins)
        self._record(ev, reads, writes)
        return ev

    def mm_multi(self, groups, reads=(), writes=()):
        self._deps("pe", reads, writes)
        ins = None
        for out, pairs in groups:
            n = len(pairs)
            for i, (l, r) in enumerate(pairs):
                ins = self.nc.tensor.matmul(out, l, r, start=(i == 0), stop=(i == n - 1))
                self.n_ins += 1
        ev = self._signal("pe", ins)
        self._record(ev, reads, writes)
        return ev

    def tr(self, out, in_, ident, reads=(), writes=()):
        self._deps("pe", reads, writes)
        ins = self.nc.tensor.transpose(out, in_, ident)
        self.n_ins += 1
        ev = self._signal("pe", ins)
        self._record(ev, reads, writes)
        return ev

    def tr_multi(self, items, reads=(), writes=()):
        self._deps("pe", reads, writes)
        ins = None
        for out, in_, ident in items:
            ins = self.nc.tensor.transpose(out, in_, ident)
            self.n_ins += 1
        ev = self._signal("pe", ins)
        self._record(ev, reads, writes)
        return ev

    def dma(self, q, out, in_, reads=(), writes=()):
        self._deps(q, reads, writes)
        slots = self.dslots[q]
        i = self.drr[q]
        self.drr[q] = (i + 1) % len(slots)
        sl = slots[i]
        if sl[1] > 0:
            self._wait(q, (sl[0], sl[1]))
        ins = self.E[q].dma_start(out=out, in_=in_)
        self.n_ins += 1
        sl[1] += 16
        ins.then_inc(sl[0], 16)
        ev = (sl[0], sl[1], "dma")
        self._record(ev, reads, writes)
        return ev

    def barrier(self):
        evs = []
        for e, st in self.csem.items():
            if st[1] > 0:
                evs.append((st[0], st[1]))
        for q, slots in self.dslots.items():
            for sl in slots:
                if sl[1] > 0:
                    evs.append((sl[0], sl[1]))
        for eng in self.E:
            for ev in evs:
                self._wait(eng, ev)

    def finish(self, eng="sp"):
        evs = []
        for e, st in self.csem.items():
            if st[1] > 0:
                evs.append((st[0], st[1]))
        for q, slots in self.dslots.items():
            for sl in slots:
                if sl[1] > 0:
                    evs.append((sl[0], sl[1]))
        for ev in evs:
            self._wait(eng, ev)
from concourse.bass_utils import run_bass_kernel_spmd
D = 1024
EPS = 1e-6


def host_consts(n_lat):
    ident = np.eye(128, dtype=np.float32)
    ones = np.ones((128, 128), np.float32)
    j = np.arange(128)[:, None]
    i = np.arange(128)[None, :]
    tri_le = (j <= i).astype(np.float32)
    tri_ge = (j >= i).astype(np.float32)
    cm = np.stack([ident, ones, tri_le, tri_ge], axis=1)
    sel = np.zeros((32, 32, 128), np.float32)
    for h in range(32):
        sel[h, h, :] = 1.0
    rows = n_lat // 64
    row = np.repeat(np.arange(rows), 64).astype(np.float32)
    col = np.tile(np.arange(64), rows).astype(np.float32)

    def tab(rot_dim, nrep):
        q = rot_dim // 4
        inv = (10000.0 ** (-np.arange(q, dtype=np.float32) / q)).astype(np.float32)
        ang = np.concatenate([row[:, None] * inv, col[:, None] * inv], axis=-1).astype(np.float32)
        cs = np.cos(ang).astype(np.float32).T
        sn = np.sin(ang).astype(np.float32).T
        cs = np.concatenate([cs] * (2 * nrep), axis=0)
        sn = np.concatenate([sn] * (2 * nrep), axis=0)
        return np.ascontiguousarray(np.stack([cs, sn], axis=0))

    r32 = tab(32, 1)
    L = r32.shape[2]
    r96 = np.ascontiguousarray(np.concatenate([np.stack([np.ones((64, L), np.float32), np.zeros((64, L), np.float32)], axis=0), r32], axis=1))
    return {"cmat": np.ascontiguousarray(cm), "sel32": sel, "rope64": tab(64, 1), "rope32": r32, "rope96": r96}


class Model:
    def __init__(self, n_lat, n_ctx, kinds, debug=False):
        self.NL, self.NCX = n_lat, n_ctx
        self.T = n_lat + n_ctx
        self.NT = self.T // 128
        self.NCT = n_ctx // 128
        self.NLT = n_lat // 128
        self.kinds = kinds
        self.depth = len(kinds)
        self.nc = bass.Bass("TRN2", target_bir_lowering=False)
        self.c = Ctx(self.nc)
        self.w = {}

    def inp(self, name, shape, dtype=F32):
        t = self.nc.dram_tensor(name, list(shape), dtype, kind="ExternalInput")
        self.w[name] = t
        return t

    def scratch(self, name, shape, dtype):
        return self.nc.dram_tensor(name, list(shape), dtype)

    def declare(self, shapes):
        for k, s in shapes.items():
            self.inp(k, s)

    def groups(self, gsz, with_ctx=True):
        g = []
        if with_ctx:
            g.append((0, self.NCT, True))
        t = self.NCT
        while t < self.NT:
            n = min(gsz, self.NT - t)
            g.append((t, n, False))
            t += n
        return g

    def setup_consts(self):
        c, nc = self.c, self.nc
        self.cm32 = c.sb([128, 4, 128], F32, "cm32")
        self.cmb = c.sb([128, 4, 128], BF16, "cmb")
        self.b_const = Buf("const")
        c.dma("sp", self.cm32[:], self.w["cmat"].ap(), writes=[self.b_const])
        c.op("dve", lambda e: e.tensor_copy(out=self.cmb[:], in_=self.cm32[:]), reads=[self.b_const], writes=[self.b_const])
        self.ident32 = self.cm32[:, 0, :]
        self.ones32 = self.cm32[:, 1, :]
        self.trile32 = self.cm32[:, 2, :]
        self.trige32 = self.cm32[:, 3, :]
        self.identb = self.cmb[:, 0, :]
        self.onesb = self.cmb[:, 1, :]
        self.trileb = self.cmb[:, 2, :]
        self.trigeb = self.cmb[:, 3, :]
        self.ps = [nc.alloc_psum_tensor(f"psb{i}", [128, 512], F32) for i in range(8)]
        self.bps = [Buf(f"ps{i}") for i in range(8)]
        self.mark0 = c.sb_mark()

    def phase_end(self):
        self.c.barrier()
        self.c.sb_release(self.mark0)

    def prologue(self):
        c, nc = self.c, self.nc
        L = self.depth
        self.modv = self.scratch("modv", [L, 2, 6 * D], F32)
        self.xs = self.scratch("xs", [self.T, D], F32)
        cs = c.sb([128, 8, 2], F32, "cs")
        b_cs = Buf()
        c.dma("sp", cs[:], self.w["cT"].ap(), writes=[b_cs])
        c.op("act", lambda e: e.activation(out=cs[:], in_=cs[:], func=AF.Silu), reads=[b_cs], writes=[b_cs])
        b_xs = self.b_xs = [Buf(f"xs{t}") for t in range(self.NT)]
        c.dma("sp", self.xs.ap()[0:self.NCX, :], self.w["ctx"].ap(), writes=b_xs[0:self.NCT])
        c.dma("sp", self.xs.ap()[self.NCX:self.T, :], self.w["x"].ap(), writes=b_xs[self.NCT:])
        wm = [c.sb([128, 8, 512], F32, f"wm{i}") for i in range(2)]
        b_wm = [Buf(), Buf()]
        modsb = c.sb([2, 6 * D], F32, "modsb")
        b_mod = Buf()
        bmb = c.sb([2, 6 * D], F32, "bmb")
        gb = c.sb([2, 2, D], F32, "gb")
        b_misc = Buf()
        self.b_modv = [Buf(f"modv{i}") for i in range(L)]
        it = 0
        for li in range(L):
            c.dma("sp", bmb[:], self.w["b_mod"].ap()[li:li + 1, :].partition_broadcast(2), writes=[b_misc])
            c.dma("sp", gb[:, 0, :], self.w["norm1_g"].ap()[li:li + 1, :].partition_broadcast(2), writes=[b_misc])
            c.dma("sp", gb[:, 1, :], self.w["norm2_g"].ap()[li:li + 1, :].partition_broadcast(2), writes=[b_misc])
            for j in range(12):
                s = it % 2
                it += 1
                c.dma("sp", wm[s][:], self.w["w_mod"].ap()[li, :, j * 512:(j + 1) * 512].rearrange("(k p) n -> p k n", p=128), writes=[b_wm[s]])
                pb = it % 2
                c.mm(self.ps[pb][0:2, :], [(cs[:, k, :], wm[s][:, k, :]) for k in range(8)], reads=[b_cs, b_wm[s]], writes=[self.bps[pb]])
                c.op("dve", lambda e: e.tensor_tensor(out=modsb[:, j * 512:(j + 1) * 512], in0=self.ps[pb][0:2, :], in1=bmb[:, j * 512:(j + 1) * 512], op=ALU.add),
                     reads=[self.bps[pb], b_misc], writes=[b_mod])
            for (ch, gi) in ((1, 0), (4, 1)):
                c.op("dve", lambda e: e.scalar_tensor_tensor(out=modsb[:, ch * D:(ch + 1) * D], in0=modsb[:, ch * D:(ch + 1) * D], scalar=1.0, in1=gb[:, gi, :], op0=ALU.add, op1=ALU.mult),
                     reads=[b_mod, b_misc], writes=[b_mod])
            c.dma("sp", self.modv.ap()[li], modsb[:], reads=[b_mod], writes=[self.b_modv[li]])
        self.phase_end()

    def load_mod(self, li, chunk, row, name):
        c = self.c
        t = c.sb([128, D], F32, name)
        b = Buf(name)
        c.dma("sp", t[:], self.modv.ap()[li, row:row + 1, chunk * D:(chunk + 1) * D].partition_broadcast(128), reads=[self.b_modv[li]], writes=[b])
        return t, b

    def alloc_norm_bufs(self, nbuf=2):
        c = self.c
        if getattr(self, "want_precast", None) is not None:
            li_ = self.want_precast
            self.want_precast = None
            self.moe_precast(li_)
        self.nb = []
        for i in range(nbuf):
            d = dict(x=c.sb([128, D], F32, "nx"), bx=Buf(), junk=c.sb([128, D], BF16, "nj"), bj=Buf(),
                     st=c.sb([128, 2], F32, "nst"), bst=Buf(), tmp=c.sb([128, D], F32, "ntmp"), btmp=Buf(),
                     h=c.sb([128, D], BF16, "nh"), bh=Buf())
            self.nb.append(d)
        self.nbi = 0

    def norm_tile(self, tile, A, bA, B, bB, hT, b_hT, col0, psb, want_x=False):
        c = self.c
        d = self.nb[self.nbi % len(self.nb)]
        self.nbi += 1
        c.dma("sp", d["x"][:], self.xs.ap()[tile * 128:(tile + 1) * 128, :], reads=[self.b_xs[tile]], writes=[d["bx"]])
        c.op("dve", lambda e: e.memset(d["st"][:], 0.0), writes=[d["bst"]])
        c.op("act", lambda e: e.activation(out=d["junk"][:], in_=d["x"][:], func=AF.Square, accum_out=d["st"][:, 0:1]), reads=[d["bx"]], writes=[d["bj"], d["bst"]])
        c.op("dve", lambda e: e.tensor_scalar(out=d["st"][:, 1:2], in0=d["st"][:, 0:1], scalar1=1.0 / D, scalar2=EPS, op0=ALU.mult, op1=ALU.add), reads=[d["bst"]], writes=[d["bst"]])
        c.op("act", lambda e: e.activation(out=d["st"][:, 1:2], in_=d["st"][:, 1:2], func=AF.Sqrt), reads=[d["bst"]], writes=[d["bst"]])
        c.op("dve", lambda e: e.reciprocal(out=d["st"][:, 1:2], in_=d["st"][:, 1:2]), reads=[d["bst"]], writes=[d["bst"]])
        c.op("dve", lambda e: e.scalar_tensor_tensor(out=d["tmp"][:], in0=d["x"][:], scalar=d["st"][:, 1:2], in1=A[:], op0=ALU.mult, op1=ALU.mult),
             reads=[d["bx"], d["bst"], bA], writes=[d["btmp"]])
        c.op("pool", lambda e: e.tensor_tensor(out=d["h"][:], in0=d["tmp"][:], in1=B[:], op=ALU.add), reads=[d["btmp"], bB], writes=[d["bh"]])
        pT = self.ps[psb].ap().bitcast(BF16)
        c.tr_multi([(pT[:, k * 128:(k + 1) * 128], d["h"][:, k * 128:(k + 1) * 128], self.identb) for k in range(8)],
                   reads=[d["bh"], self.b_const], writes=[self.bps[psb]])
        c.op("act", lambda e: e.activation(out=hT[:, :, col0:col0 + 128], in_=pT.rearrange("p (k n) -> p k n", k=8), func=AF.Copy),
             reads=[self.bps[psb]], writes=[b_hT])
        return d

    def layer_gqa(self, li, j, need_ctx):
        c, nc = self.c, self.nc
        T, NT, NCT = self.T, self.NT, self.NCT
        w_in = self.w["attn_w_in"].ap()[j]
        w_out = self.w["attn_w_out"].ap()[j]
        QT = self.scratch(f"gqa_qt{li}", [4, NT, 64, 4, 128], BF16)
        b_QT = [Buf() for _ in range(NT)]
        b_w = Buf("gqa_w")
        Wq = c.sb([128, 8, 1024], BF16, "Wq")
        Wqr = c.sb([128, 8, 1024], BF16, "Wqr")
        Wk = c.sb([128, 8, 256], BF16, "Wk")
        Wkr = c.sb([128, 8, 256], BF16, "Wkr")
        Wv = c.sb([128, 8, 256], BF16, "Wv")
        c.dma("pool", Wq[:], w_in[:, 0:1024].rearrange("(k p) n -> p k n", p=128), writes=[b_w])
        c.dma("pool", Wk[:], w_in[:, 1024:1280].rearrange("(k p) n -> p k n", p=128), writes=[b_w])
        c.dma("pool", Wv[:], w_in[:, 1280:1536].rearrange("(k p) n -> p k n", p=128), writes=[b_w])
        b_wr = Buf("gqa_wr")
        for (W, Wr, nh) in ((Wq, Wqr, 16), (Wk, Wkr, 4)):
            for k in range(8):
                src = W[:, k, :].rearrange("p (h two i) -> p h two i", two=2, i=32)
                dst = Wr[:, k, :].rearrange("p (h two i) -> p h two i", two=2, i=32)
                c.op("act", lambda e: e.activation(out=dst[:, :, 0, :], in_=src[:, :, 1, :], func=AF.Copy, scale=-1.0), reads=[b_w], writes=[b_wr])
                c.op("dve", lambda e: e.tensor_copy(out=dst[:, :, 1, :], in_=src[:, :, 0, :]), reads=[b_w], writes=[b_wr])
        KT = c.sb([64, 4, T], BF16, "KT")
        b_KT = Buf("KT")
        Vs = c.sb([128, NT, 4, 65], BF16, "Vs")
        b_V = Buf("Vs")
        c.op("pool", lambda e: e.memset(Vs[:], 1.0), writes=[b_V])
        mark = c.sb_mark()
        A1x, bA1x = self.load_mod(li, 1, 0, "A1x")
        S1x, bS1x = self.load_mod(li, 0, 0, "S1x")
        A1c, bA1c = self.load_mod(li, 1, 1, "A1c")
        S1c, bS1c = self.load_mod(li, 0, 1, "S1c")
        self.alloc_norm_bufs(2)
        hTs = [c.sb([128, 8, 512], BF16, "hT") for _ in range(2)]
        b_hT = [Buf(), Buf()]
        rts = [c.sb([64, 2, 512], F32, "rt") for _ in range(2)]
        b_rt = [Buf(), Buf()]
        qst = [c.sb([64, 4, 512], BF16, "qst") for _ in range(2)]
        b_qst = [Buf(), Buf()]
        t1s = [c.sb([64, 512], F32, "t1") for _ in range(2)]
        t2s = [c.sb([64, 512], F32, "t2") for _ in range(2)]
        b_t1 = [Buf(), Buf()]
        b_t2 = [Buf(), Buf()]
        cnt = 0
        qcnt = 0
        for gi, (t0, n, is_ctx) in enumerate(self.groups(4)):
            ncols = n * 128
            hT, bh = hTs[gi % 2], b_hT[gi % 2]
            rt, brt = rts[gi % 2], b_rt[gi % 2]
            for s in range(n):
                if is_ctx:
                    self.norm_tile(t0 + s, A1c, bA1c, S1c, bS1c, hT, bh, s * 128, (t0 + s) % 2)
                else:
                    self.norm_tile(t0 + s, A1x, bA1x, S1x, bS1x, hT, bh, s * 128, (t0 + s) % 2)
            if not is_ctx:
                l0 = (t0 - NCT) * 128
                c.dma("sp", rt[:, :, :ncols], self.w["rope64"].ap()[:, :, l0:l0 + ncols].rearrange("two d l -> d two l"), writes=[brt])

            def proj_head(W, Wr, col, dst_ap, dst_buf):
                nonlocal cnt
                i = cnt % 2
                cnt += 1
                P1, bP1 = self.ps[2 + i], self.bps[2 + i]
                P2, bP2 = self.ps[4 + i], self.bps[4 + i]
                c.mm(P1[0:64, :ncols], [(W[:, k, col:col + 64], hT[:, k, :ncols]) for k in range(8)], reads=[b_w, bh], writes=[bP1])
                if is_ctx:
                    c.op("act", lambda e: e.activation(out=dst_ap, in_=P1[0:64, :ncols], func=AF.Copy), reads=[bP1], writes=[dst_buf])
                else:
                    c.mm(P2[0:64, :ncols], [(Wr[:, k, col:col + 64], hT[:, k, :ncols]) for k in range(8)], reads=[b_wr, bh], writes=[bP2])
                    c.op("dve", lambda e: e.tensor_tensor(out=t1s[i][:, :ncols], in0=P1[0:64, :ncols], in1=rt[:, 0, :ncols], op=ALU.mult), reads=[bP1, brt], writes=[b_t1[i]])
                    c.op("dve", lambda e: e.tensor_tensor(out=t2s[i][:, :ncols], in0=P2[0:64, :ncols], in1=rt[:, 1, :ncols], op=ALU.mult), reads=[bP2, brt], writes=[b_t2[i]])
                    c.op("pool", lambda e: e.tensor_tensor(out=dst_ap, in0=t1s[i][:, :ncols], in1=t2s[i][:, :ncols], op=ALU.add), reads=[b_t1[i], b_t2[i]], writes=[dst_buf])

            for kvh in range(4):
                qs, bq = qst[qcnt % 2], b_qst[qcnt % 2]
                qcnt += 1
                for g in range(4):
                    proj_head(Wq, Wqr, (kvh * 4 + g) * 64, qs[:, g, :ncols], bq)
                for qb_ in range(n):
                    c.dma("sp", QT.ap()[kvh, t0 + qb_], qs[:, :, qb_ * 128:(qb_ + 1) * 128], reads=[bq], writes=[b_QT[t0 + qb_]])
                proj_head(Wk, Wkr, kvh * 64, KT[:, kvh, t0 * 128:t0 * 128 + ncols], b_KT)
            for s in range(n):
                i = cnt % 2
                cnt += 1
                Pv, bPv = self.ps[6 + i], self.bps[6 + i]
                c.mm(Pv[:, 0:256], [(hT[:, k, s * 128:(s + 1) * 128], Wv[:, k, :]) for k in range(8)], reads=[b_w, bh], writes=[bPv])
                c.op("act", lambda e: e.activation(out=Vs[:, t0 + s, :, 0:64], in_=Pv[:, 0:256].rearrange("p (h d) -> p h d", h=4), func=AF.Copy), reads=[bPv], writes=[b_V])
        c.barrier()
        c.sb_release(mark)
        Wo = c.sb([64, 16, 1024], BF16, "Wo")
        b_wo = Buf()
        c.dma("pool", Wo[:], w_out.rearrange("(h d) n -> d h n", d=64), writes=[b_wo])
        sk = c.sb([65, 16], F32, "sk")
        b_sk = Buf()
        sinkrow = c.sb([65, 16, 128], F32, "sinkrow")
        c.dma("sp", sk[64:65, :], self.w["attn_sink"].ap()[j:j + 1, :], writes=[b_sk])
        c.op("act", lambda e: e.activation(out=sk[64:65, :], in_=sk[64:65, :], func=AF.Exp), reads=[b_sk], writes=[b_sk])
        c.op("dve", lambda e: e.tensor_copy(out=sinkrow[64:65, :, :], in_=sk[64:65, :].unsqueeze(2).to_broadcast([1, 16, 128])), reads=[b_sk], writes=[b_sk])
        G1x, bG1x = self.load_mod(li, 2, 0, "G1x")
        G1c, bG1c = self.load_mod(li, 2, 1, "G1c")
        Qts = [c.sb([64, 512], BF16, "Qt") for _ in range(4)]
        b_Qt = [Buf() for _ in range(4)]
        PTs = [c.sb([128, 512], BF16, "PT") for _ in range(4)]
        b_PT = [Buf() for _ in range(4)]
        osb = [c.sb([65, 512], F32, "osb") for _ in range(2)]
        b_osb = [Buf(), Buf()]
        rden = [c.sb([65, 512], F32, "rden") for _ in range(2)]
        b_rden = [Buf(), Buf()]
        yTs = [c.sb([64, 16, 128], BF16, "yT") for _ in range(2)]
        b_yT = [Buf(), Buf()]
        xts = [c.sb([128, D], F32, "xres") for _ in range(2)]
        b_xt = [Buf(), Buf()]
        tmps = [c.sb([128, D], F32, "rtmp") for _ in range(2)]
        b_tmp = [Buf(), Buf()]
        scale = 64 ** -0.5
        qi = 0
        pi = 0
        si = 0
        for bi, qb in enumerate(range(0 if need_ctx else NCT, NT)):
            is_ctx = qb < NCT
            if is_ctx:
                keys = [(t, None) for t in range(NCT)]
            else:
                keys = []
                if qb - 1 >= NCT:
                    keys.append((qb - 1, self.trigeb))
                keys.append((qb, None))
                if qb + 1 < NT:
                    keys.append((qb + 1, self.trileb))
                keys += [(t, None) for t in range(NCT)]
            xt, bxt = xts[bi % 2], b_xt[bi % 2]
            c.dma("sp", xt[:], self.xs.ap()[qb * 128:(qb + 1) * 128, :], reads=[self.b_xs[qb]], writes=[bxt])
            yT, byT = yTs[bi % 2], b_yT[bi % 2]
            for kvh in range(4):
                Qt, bQt = Qts[qi % 4], b_Qt[qi % 4]
                qi += 1
                c.dma("sp", Qt[:], QT.ap()[kvh, qb].rearrange("d g t -> d (g t)"), reads=[b_QT[qb]], writes=[bQt])
                oT, boT = self.ps[2 + kvh % 2], self.bps[2 + kvh % 2]
                SB = (0, 1, 7)
                LA = 2

                def issue_S(ki_):
                    kt_ = keys[ki_][0]
                    bk_ = SB[(si + ki_) % len(SB)]
                    c.mm(self.ps[bk_][:, :], [(KT[:, kvh, kt_ * 128:(kt_ + 1) * 128], Qt[:])], reads=[b_KT, bQt], writes=[self.bps[bk_]])
                for k0 in range(min(LA, len(keys))):
                    issue_S(k0)
                for ki, (kt, mask) in enumerate(keys):
                    bk = SB[(si + ki) % len(SB)]
                    sT, bsT = self.ps[bk], self.bps[bk]
                    if ki + LA < len(keys):
                        issue_S(ki + LA)
                    PT, bPT = PTs[pi % 4], b_PT[pi % 4]
                    pi += 1
                    c.op("act", lambda e: e.activation(out=PT[:], in_=sT[:, :], func=AF.Exp, scale=scale), reads=[bsT], writes=[bPT])
                    if mask is not None:
                        c.op("dve", lambda e: e.tensor_tensor(out=PT[:].rearrange("p (g t) -> p g t", g=4), in0=PT[:].rearrange("p (g t) -> p g t", g=4),
                                                              in1=mask.unsqueeze(1).to_broadcast([128, 4, 128]), op=ALU.mult), reads=[bPT, self.b_const], writes=[bPT])
                    c.mm(oT[0:65, :], [(Vs[:, kt, kvh, :], PT[:])], reads=[b_V, bPT], writes=[boT], start=(ki == 0), stop=(ki == len(keys) - 1))
                si += len(keys)
                o, bo = osb[kvh % 2], b_osb[kvh % 2]
                rd, brd = rden[kvh % 2], b_rden[kvh % 2]
                c.op("act", lambda e: e.activation(out=o[:], in_=oT[0:65, :], func=AF.Copy), reads=[boT], writes=[bo])
                c.op("dve", lambda e: e.tensor_tensor(out=rd[64:65, :], in0=o[64:65, :], in1=sinkrow[64:65, kvh * 4:(kvh + 1) * 4, :].rearrange("p g t -> p (g t)"), op=ALU.add),
                     reads=[bo, b_sk], writes=[brd])
                c.mm(self.ps[4][0:64, :], [(self.ones32[64:65, 0:64], rd[64:65, :])], reads=[self.b_const, brd], writes=[self.bps[4]])
                c.op("dve", lambda e: e.reciprocal(out=rd[0:64, :], in_=self.ps[4][0:64, :]), reads=[self.bps[4], brd], writes=[brd])
                c.op("dve", lambda e: e.tensor_tensor(out=yT[:, kvh * 4:(kvh + 1) * 4, :].rearrange("d g t -> d (g t)"), in0=o[0:64, :], in1=rd[0:64, :], op=ALU.mult),
                     reads=[bo, brd], writes=[byT])
            G1, bG1 = (G1c, bG1c) if is_ctx else (G1x, bG1x)
            tmp, btmp = tmps[bi % 2], b_tmp[bi % 2]
            for nn in range(2):
                z, bz = self.ps[5 + nn], self.bps[5 + nn]
                c.mm(z[:, :], [(yT[:, hq, :], Wo[:, hq, nn * 512:(nn + 1) * 512]) for hq in range(16)], reads=[byT, b_wo], writes=[bz])
                c.op("dve", lambda e: e.tensor_tensor(out=tmp[:, nn * 512:(nn + 1) * 512], in0=z[:, :], in1=G1[:, nn * 512:(nn + 1) * 512], op=ALU.mult), reads=[bz, bG1], writes=[btmp])
            c.op("pool", lambda e: e.tensor_tensor(out=xt[:], in0=xt[:], in1=tmp[:], op=ALU.add), reads=[btmp, bxt], writes=[bxt])
            c.dma("sp", self.xs.ap()[qb * 128:(qb + 1) * 128, :], xt[:], reads=[bxt], writes=[self.b_xs[qb]])
        self.phase_end()

    def moe_precast(self, li):
        c = self.c
        wg = self.w["moe_w_gate"].ap()[li]
        wu = self.w["moe_w_up"].ap()[li]
        wd = self.w["moe_w_down"].ap()[li]
        self.wgb = self.scratch(f"moe_wgb{li}", [16, 1024, 512], BF16)
        self.wdb = self.scratch(f"moe_wdb{li}", [16, 256, 1024], BF16)
        self.b_wgb = Buf(); self.b_wdb = Buf()
        for e2 in range(8):
            c.dma("pool", self.wgb.ap()[2 * e2:2 * e2 + 2, :, 0:256], wg[2 * e2:2 * e2 + 2], writes=[self.b_wgb])
            c.dma("pool", self.wgb.ap()[2 * e2:2 * e2 + 2, :, 256:512], wu[2 * e2:2 * e2 + 2], writes=[self.b_wgb])
            c.dma("pool", self.wdb.ap()[2 * e2:2 * e2 + 2], wd[2 * e2:2 * e2 + 2], writes=[self.b_wdb])
        self.precast_done = li

    def layer_moe(self, li, need_ctx):
        c, nc = self.c, self.nc
        NCT = self.NCT
        if getattr(self, "precast_done", -1) != li:
            self.moe_precast(li)
        wgb = self.wgb.ap()
        wdb = self.wdb.ap()
        b_w = Buf("moe_wr")
        Wr = c.sb([128, 8, 20], BF16, "Wr")
        c.dma("pool", Wr[:, :, 0:4], self.w["moe_w_group"].ap()[li].rearrange("(k p) n -> p k n", p=128), writes=[b_w])
        c.dma("pool", Wr[:, :, 4:20], self.w["moe_w_expert"].ap()[li].rearrange("(k p) n -> p k n", p=128), writes=[b_w])
        brow = c.sb([128, 20], F32, "brow")
        c.dma("sp", brow[:, 0:4], self.w["moe_b_group"].ap()[li:li + 1, :].partition_broadcast(128), writes=[b_w])
        c.dma("sp", brow[:, 4:20], self.w["moe_b_expert"].ap()[li:li + 1, :].partition_broadcast(128), writes=[b_w])
        sel16 = c.sb([16, 16, 128], BF16, "sel16")
        c.dma("pool", sel16[:], self.w["sel32"].ap()[0:16, 0:16, :], writes=[b_w])
        hT = c.sb([128, 8, 1024], BF16, "mhT")
        b_hT = Buf()
        act = c.sb([128, 16, 2, 1024], BF16, "mact")
        b_act = Buf()
        Wgu = [c.sb([128, 8, 512], BF16, "Wgu") for _ in range(2)]
        b_Wgu = [Buf(), Buf()]
        Wd = [c.sb([128, 16, 2, 256], BF16, "Wd") for _ in range(2)]
        b_Wd = [Buf(), Buf()]
        xq = [c.sb([128, 256], F32, "xq") for _ in range(4)]
        b_xq = [Buf() for _ in range(4)]
        xqi = 0
        self.alloc_norm_bufs(2)
        A2 = c.sb([128, D], F32, "A2"); S2 = c.sb([128, D], F32, "S2"); G2 = c.sb([128, D], F32, "G2")
        b_m = Buf()
        R = 8
        lg = c.sb([128, R, 20], F32, "lg"); le = c.sb([128, R, 16], F32, "le"); le2 = c.sb([128, R, 16], F32, "le2")
        gm = c.sb([128, R, 4], F32, "gm"); eg = c.sb([128, R, 4], F32, "eg"); pen = c.sb([128, R, 4], F32, "pen")
        mk1 = c.sb([128, R, 16], F32, "mk1"); mk2 = c.sb([128, R, 16], F32, "mk2"); cmb = c.sb([128, R, 16], F32, "cmb")
        sc = c.sb([128, 8, R], F32, "rsc")
        b_r = Buf("route")
        cmbT = c.sb([16, 1024], BF16, "cmbT")
        b_cT = Buf()
        s_sb = [c.sb([128, 512], F32, "msil") for _ in range(2)]
        b_s = [Buf(), Buf()]
        t_sb = [c.sb([128, 512], F32, "mt") for _ in range(2)]
        b_t = [Buf(), Buf()]
        tmpd = [c.sb([128, 256], F32, "mtd") for _ in range(2)]
        b_td = [Buf(), Buf()]
        wi = 0
        di = 0
        ui = 0
        for (t0, n, is_ctx) in self.groups(8, with_ctx=need_ctx):
            G = n * 128
            row = 1 if is_ctx else 0
            for (tl, ch) in ((S2, 3), (A2, 4), (G2, 5)):
                c.dma("sp", tl[:], self.modv.ap()[li, row:row + 1, ch * D:(ch + 1) * D].partition_broadcast(128), reads=[self.b_modv[li]], writes=[b_m])
            for s in range(n):
                self.norm_tile(t0 + s, A2, b_m, S2, b_m, hT, b_hT, s * 128, s % 2)
            lp, blp = self.ps[2], self.bps[2]
            c.mm_multi([(lp[:, s * 20:(s + 1) * 20], [(hT[:, k, s * 128:(s + 1) * 128], Wr[:, k, :]) for k in range(8)]) for s in range(n)],
                       reads=[b_hT, b_w], writes=[blp])
            V = lambda e: e
            lgn = lg[:, 0:n, :]
            c.op("dve", lambda e: e.tensor_tensor(out=lgn, in0=lp[:, 0:n * 20].rearrange("p (s j) -> p s j", j=20), in1=brow[:].unsqueeze(1).to_broadcast([128, n, 20]), op=ALU.add),
                 reads=[blp, b_w], writes=[b_r])
            R1 = [b_r]
            c.op("dve", lambda e: e.tensor_reduce(out=sc[:, 0, 0:n], in_=lgn[:, :, 0:4], axis=AX.X, op=ALU.max), reads=R1, writes=R1)
            c.op("dve", lambda e: e.tensor_tensor(out=gm[:, 0:n, :], in0=lgn[:, :, 0:4], in1=sc[:, 0, 0:n].unsqueeze(2).to_broadcast([128, n, 4]), op=ALU.is_equal), reads=R1, writes=R1)
            c.op("dve", lambda e: e.tensor_tensor(out=eg[:, 0:n, :], in0=lgn[:, :, 0:4], in1=sc[:, 0, 0:n].unsqueeze(2).to_broadcast([128, n, 4]), op=ALU.subtract), reads=R1, writes=R1)
            c.op("act", lambda e: e.activation(out=eg[:, 0:n, :], in_=eg[:, 0:n, :], func=AF.Exp), reads=R1, writes=R1)
            c.op("dve", lambda e: e.tensor_reduce(out=sc[:, 1, 0:n], in_=eg[:, 0:n, :], axis=AX.X, op=ALU.add), reads=R1, writes=R1)
            c.op("dve", lambda e: e.reciprocal(out=sc[:, 1, 0:n], in_=sc[:, 1, 0:n]), reads=R1, writes=R1)
            c.op("dve", lambda e: e.tensor_scalar(out=pen[:, 0:n, :], in0=gm[:, 0:n, :], scalar1=1.0, scalar2=1e30, op0=ALU.subtract, op1=ALU.mult), reads=R1, writes=R1)
            c.op("dve", lambda e: e.tensor_copy(out=le[:, 0:n, :], in_=lgn[:, :, 4:20]), reads=R1, writes=R1)
            lev = le[:, 0:n, :].rearrange("p s (g j) -> p (s g) j", g=4)
            c.op("dve", lambda e: e.tensor_tensor(out=lev, in0=lev, in1=pen[:, 0:n, :].rearrange("p s g -> p (s g)").unsqueeze(2).to_broadcast([128, n * 4, 4]), op=ALU.add), reads=R1, writes=R1)
            c.op("dve", lambda e: e.tensor_reduce(out=sc[:, 2, 0:n], in_=le[:, 0:n, :], axis=AX.X, op=ALU.max), reads=R1, writes=R1)
            c.op("dve", lambda e: e.tensor_tensor(out=mk1[:, 0:n, :], in0=le[:, 0:n, :], in1=sc[:, 2, 0:n].unsqueeze(2).to_broadcast([128, n, 16]), op=ALU.is_equal), reads=R1, writes=R1)
            c.op("dve", lambda e: e.scalar_tensor_tensor(out=le2[:, 0:n, :], in0=mk1[:, 0:n, :], scalar=-1e30, in1=le[:, 0:n, :], op0=ALU.mult, op1=ALU.add), reads=R1, writes=R1)
            c.op("dve", lambda e: e.tensor_reduce(out=sc[:, 3, 0:n], in_=le2[:, 0:n, :], axis=AX.X, op=ALU.max), reads=R1, writes=R1)
            c.op("dve", lambda e: e.tensor_tensor(out=mk2[:, 0:n, :], in0=le2[:, 0:n, :], in1=sc[:, 3, 0:n].unsqueeze(2).to_broadcast([128, n, 16]), op=ALU.is_equal), reads=R1, writes=R1)
            c.op("dve", lambda e: e.tensor_tensor(out=sc[:, 4, 0:n], in0=sc[:, 2, 0:n], in1=sc[:, 3, 0:n], op=ALU.subtract), reads=R1, writes=R1)
            c.op("act", lambda e: e.activation(out=sc[:, 4, 0:n], in_=sc[:, 4, 0:n], func=AF.Sigmoid), reads=R1, writes=R1)
            c.op("dve", lambda e: e.tensor_tensor(out=sc[:, 4, 0:n], in0=sc[:, 4, 0:n], in1=sc[:, 1, 0:n], op=ALU.mult), reads=R1, writes=R1)
            c.op("dve", lambda e: e.tensor_tensor(out=sc[:, 5, 0:n], in0=sc[:, 1, 0:n], in1=sc[:, 4, 0:n], op=ALU.subtract), reads=R1, writes=R1)
            c.op("dve", lambda e: e.tensor_tensor(out=mk1[:, 0:n, :], in0=mk1[:, 0:n, :], in1=sc[:, 4, 0:n].unsqueeze(2).to_broadcast([128, n, 16]), op=ALU.mult), reads=R1, writes=R1)
            c.op("dve", lambda e: e.tensor_tensor(out=mk2[:, 0:n, :], in0=mk2[:, 0:n, :], in1=sc[:, 5, 0:n].unsqueeze(2).to_broadcast([128, n, 16]), op=ALU.mult), reads=R1, writes=R1)
            c.op("dve", lambda e: e.tensor_tensor(out=cmb[:, 0:n, :], in0=mk1[:, 0:n, :], in1=mk2[:, 0:n, :], op=ALU.add), reads=R1, writes=R1)
            for hb in range((n + 3) // 4):
                s0, s1 = hb * 4, min(n, hb * 4 + 4)
                pc, bpc = self.ps[3 + hb], self.bps[3 + hb]
                c.tr_multi([(pc[0:16, (s - s0) * 128:(s - s0 + 1) * 128], cmb[:, s, :], self.ident32) for s in range(s0, s1)], reads=[b_r, self.b_const], writes=[bpc])
                c.op("act", lambda e: e.activation(out=cmbT[:, s0 * 128:s1 * 128], in_=pc[0:16, 0:(s1 - s0) * 128], func=AF.Copy), reads=[bpc], writes=[b_cT])
            ncb = (G + 511) // 512
            for ex in range(16):
                W, bW = Wgu[wi % 2], b_Wgu[wi % 2]
                wi += 1
                c.dma("sp", W[:], wgb[ex].rearrange("(k p) n -> p k n", p=128), reads=[self.b_wgb], writes=[bW])
                for cb in range(ncb):
                    c0 = cb * 512
                    cw = min(512, G - c0)
                    pbc, bpbc = self.ps[4 + (ui % 2)], self.bps[4 + (ui % 2)]
                    c.mm(pbc[:, :cw], [(sel16[:, ex, :], cmbT[:, c0:c0 + cw])], reads=[b_w, b_cT], writes=[bpbc])
                    for ffc in range(2):
                        i = ui % 2
                        ui += 1
                        pg_, bpg = self.ps[0 + i], self.bps[0 + i]
                        pu, bpu = self.ps[2 + i], self.bps[2 + i]
                        c.mm(pg_[:, :cw], [(W[:, k, ffc * 128:(ffc + 1) * 128], hT[:, k, c0:c0 + cw]) for k in range(8)], reads=[bW, b_hT], writes=[bpg])
                        c.mm(pu[:, :cw], [(W[:, k, 256 + ffc * 128:256 + (ffc + 1) * 128], hT[:, k, c0:c0 + cw]) for k in range(8)], reads=[bW, b_hT], writes=[bpu])
                        c.op("act", lambda e: e.activation(out=s_sb[i][:, :cw], in_=pg_[:, :cw], func=AF.Silu), reads=[bpg], writes=[b_s[i]])
                        c.op("dve", lambda e: e.tensor_tensor(out=t_sb[i][:, :cw], in0=s_sb[i][:, :cw], in1=pu[:, :cw], op=ALU.mult), reads=[b_s[i], bpu], writes=[b_t[i]])
                        c.op("dve", lambda e: e.tensor_tensor(out=act[:, ex, ffc, c0:c0 + cw], in0=t_sb[i][:, :cw], in1=pbc[:, :cw], op=ALU.mult), reads=[b_t[i], bpbc], writes=[b_act])
            for dq in range(4):
                Wdt, bWd = Wd[di % 2], b_Wd[di % 2]
                di += 1
                c.dma("sp", Wdt[:], wdb[:, :, dq * 256:(dq + 1) * 256].rearrange("e (f p) n -> p e f n", p=128), reads=[self.b_wdb], writes=[bWd])
                for s in range(n):
                    pa, bpa = self.ps[4 + s // 2], self.bps[4 + s // 2]
                    pav = pa[:, (s % 2) * 256:(s % 2 + 1) * 256]
                    c.mm(pav, [(act[:, ex, ffc, s * 128:(s + 1) * 128], Wdt[:, ex, ffc, :]) for ex in range(16) for ffc in range(2)], reads=[b_act, bWd], writes=[bpa])
                    i = s % 2
                    xq_, bxq = xq[xqi % 4], b_xq[xqi % 4]
                    xqi += 1
                    tl = t0 + s
                    c.dma("sp", xq_[:], self.xs.ap()[tl * 128:(tl + 1) * 128, dq * 256:(dq + 1) * 256], reads=[self.b_xs[tl]], writes=[bxq])
                    c.op("dve", lambda e: e.tensor_tensor(out=tmpd[i][:], in0=pav, in1=G2[:, dq * 256:(dq + 1) * 256], op=ALU.mult), reads=[bpa, b_m], writes=[b_td[i]])
                    c.op("pool", lambda e: e.tensor_tensor(out=xq_[:], in0=xq_[:], in1=tmpd[i][:], op=ALU.add), reads=[b_td[i], bxq], writes=[bxq])
                    c.dma("sp", self.xs.ap()[tl * 128:(tl + 1) * 128, dq * 256:(dq + 1) * 256], xq_[:], reads=[bxq], writes=[self.b_xs[tl]])
        self.phase_end()

    def final(self):
        c = self.c
        gf = c.sb([128, D], F32, "gfin")
        b_g = Buf()
        c.dma("sp", gf[:], self.w["final_norm_g"].ap().rearrange("(o n) -> o n", o=1).partition_broadcast(128), writes=[b_g])
        xs_ = [c.sb([128, D], F32, "fx") for _ in range(2)]
        js = [c.sb([128, D], BF16, "fj") for _ in range(2)]
        st = [c.sb([128, 2], F32, "fst") for _ in range(2)]
        ys = [c.sb([128, D], F32, "fy") for _ in range(2)]
        bx = [Buf(), Buf()]; bj = [Buf(), Buf()]; bs = [Buf(), Buf()]; by = [Buf(), Buf()]
        b_out = Buf()
        for t in range(self.NCT, self.NT):
            i = t % 2
            c.dma("sp", xs_[i][:], self.xs.ap()[t * 128:(t + 1) * 128, :], reads=[self.b_xs[t]], writes=[bx[i]])
            c.op("dve", lambda e: e.memset(st[i][:], 0.0), writes=[bs[i]])
            c.op("act", lambda e: e.activation(out=js[i][:], in_=xs_[i][:], func=AF.Square, accum_out=st[i][:, 0:1]), reads=[bx[i]], writes=[bj[i], bs[i]])
            c.op("dve", lambda e: e.tensor_scalar(out=st[i][:, 1:2], in0=st[i][:, 0:1], scalar1=1.0 / D, scalar2=EPS, op0=ALU.mult, op1=ALU.add), reads=[bs[i]], writes=[bs[i]])
            c.op("act", lambda e: e.activation(out=st[i][:, 1:2], in_=st[i][:, 1:2], func=AF.Sqrt), reads=[bs[i]], writes=[bs[i]])
            c.op("dve", lambda e: e.reciprocal(out=st[i][:, 1:2], in_=st[i][:, 1:2]), reads=[bs[i]], writes=[bs[i]])
            c.op("dve", lambda e: e.scalar_tensor_tensor(out=ys[i][:], in0=xs_[i][:], scalar=st[i][:, 1:2], in1=gf[:], op0=ALU.mult, op1=ALU.mult), reads=[bx[i], bs[i], b_g], writes=[by[i]])
            lt = t - self.NCT
            c.dma("sp", self.out.ap()[lt * 128:(lt + 1) * 128, :], ys[i][:], reads=[by[i]], writes=[b_out])
        self.c.finish("sp")

    def layer_mla(self, li, j, need_ctx):
        c, nc = self.c, self.nc
        T, NT, NCT = self.T, self.NT, self.NCT
        w_in = self.w["mla_w_in"].ap()[j]
        w_qup = self.w["mla_w_q_up"].ap()[j]
        w_kvup = self.w["mla_w_kv_up"].ap()[j]
        w_out = self.w["mla_w_out"].ap()[j]
        QT = self.scratch(f"mla_qt{li}", [16, 96, T], BF16)
        KTd = self.scratch(f"mla_kt{li}", [16, 96, T], BF16)
        Vd = self.scratch(f"mla_v{li}", [16, NT, 128, 65], BF16)
        YT = self.scratch(f"mla_yt{li}", [16, 64, T], BF16)
        b_QT = Buf(); b_KTd = Buf(); b_Vd = Buf(); b_YT = Buf()
        b_w = Buf("mla_w")
        Win = c.sb([128, 8, 544], BF16, "Win")
        c.dma("pool", Win[:], w_in.rearrange("(k p) n -> p k n", p=128), writes=[b_w])
        Wkr_rot = c.sb([128, 8, 32], BF16, "Wkrrot")
        Wq = c.sb([128, 2, 1536], BF16, "Wqup")
        c.dma("pool", Wq[:], w_qup.rearrange("(k p) n -> p k n", p=128), writes=[b_w])
        Wqr = c.sb([128, 2, 1536], BF16, "Wquprot")
        Wkn = c.sb([128, 2, 16, 64], BF16, "Wkn")
        Wv = c.sb([128, 2, 16, 64], BF16, "Wvv")
        kvv = w_kvup.rearrange("(k p) (h two d) -> p k h two d", p=128, two=2, d=64)
        for kc in range(2):
            c.dma("pool", Wkn[:, kc], kvv[:, kc, :, 0, :], writes=[b_w])
            c.dma("pool", Wv[:, kc], kvv[:, kc, :, 1, :], writes=[b_w])
        b_wr = Buf("mla_wr")
        c.op("pool", lambda e: e.memset(Wqr[:], 0.0), writes=[b_wr])
        for kc in range(2):
            src = Wq[:, kc, :].rearrange("p (h d) -> p h d", d=96)
            dst = Wqr[:, kc, :].rearrange("p (h d) -> p h d", d=96)
            c.op("act", lambda e: e.activation(out=dst[:, :, 64:80], in_=src[:, :, 80:96], func=AF.Copy, scale=-1.0), reads=[b_w], writes=[b_wr])
            c.op("dve", lambda e: e.tensor_copy(out=dst[:, :, 80:96], in_=src[:, :, 64:80]), reads=[b_w], writes=[b_wr])
        c.op("act", lambda e: e.activation(out=Wkr_rot[:, :, 0:16], in_=Win[:, :, 528:544], func=AF.Copy, scale=-1.0), reads=[b_w], writes=[b_wr])
        c.op("dve", lambda e: e.tensor_copy(out=Wkr_rot[:, :, 16:32], in_=Win[:, :, 512:528]), reads=[b_w], writes=[b_wr])
        gq = c.sb([128, 512], F32, "gqkv")
        c.dma("sp", gq[:, 0:256], self.w["mla_q_norm_g"].ap()[j:j + 1, :].partition_broadcast(128), writes=[b_w])
        c.dma("sp", gq[:, 256:512], self.w["mla_kv_norm_g"].ap()[j:j + 1, :].partition_broadcast(128), writes=[b_w])
        mark = c.sb_mark()
        A1x, bA1x = self.load_mod(li, 1, 0, "A1x")
        S1x, bS1x = self.load_mod(li, 0, 0, "S1x")
        A1c, bA1c = self.load_mod(li, 1, 1, "A1c")
        S1c, bS1c = self.load_mod(li, 0, 1, "S1c")
        self.alloc_norm_bufs(2)
        hTs = [c.sb([128, 8, 512], BF16, "hT") for _ in range(2)]
        b_hT = [Buf(), Buf()]
        cnT = [c.sb([128, 4, 512], BF16, "cnT") for _ in range(2)]
        b_cnT = [Buf(), Buf()]
        rts = [c.sb([96, 2, 512], F32, "rt96") for _ in range(2)]
        rks = [c.sb([32, 2, 512], F32, "rt32") for _ in range(2)]
        b_rt = [Buf(), Buf()]
        krT = [c.sb([32, 512], BF16, "krT") for _ in range(2)]
        b_krT = [Buf(), Buf()]
        st = [c.sb([128, 4], F32, "mst") for _ in range(2)]
        b_st = [Buf(), Buf()]
        jk = c.sb([128, 256], BF16, "mjunk"); b_jk = Buf()
        cn = [c.sb([128, 512], BF16, "cn") for _ in range(2)]
        b_cn = [Buf(), Buf()]
        qst = [c.sb([96, 512], BF16, "mqst") for _ in range(2)]
        b_qst = [Buf(), Buf()]
        kst = [c.sb([64, 512], BF16, "mkst") for _ in range(2)]
        b_kst = [Buf(), Buf()]
        t1s = [c.sb([96, 512], F32, "t1") for _ in range(2)]
        t2s = [c.sb([96, 512], F32, "t2") for _ in range(2)]
        b_t1 = [Buf(), Buf()]; b_t2 = [Buf(), Buf()]
        Vt = [c.sb([128, 16, 65], BF16, "Vt") for _ in range(2)]
        b_Vt = [Buf(), Buf()]
        for i in range(2):
            c.op("pool", lambda e: e.memset(Vt[i][:], 1.0), writes=[b_Vt[i]])
        cnt = 0
        ti = 0
        for gi, (t0, n, is_ctx) in enumerate(self.groups(4)):
            ncols = n * 128
            col0 = t0 * 128
            hT, bh = hTs[gi % 2], b_hT[gi % 2]
            cT_, bcT = cnT[gi % 2], b_cnT[gi % 2]
            rt, rk, brt = rts[gi % 2], rks[gi % 2], b_rt[gi % 2]
            for s in range(n):
                if is_ctx:
                    self.norm_tile(t0 + s, A1c, bA1c, S1c, bS1c, hT, bh, s * 128, (t0 + s) % 2)
                else:
                    self.norm_tile(t0 + s, A1x, bA1x, S1x, bS1x, hT, bh, s * 128, (t0 + s) % 2)
            if not is_ctx:
                l0 = (t0 - NCT) * 128
                c.dma("sp", rt[:, :, :ncols], self.w["rope96"].ap()[:, :, l0:l0 + ncols].rearrange("two d l -> d two l"), writes=[brt])
                c.dma("sp", rk[:, :, :ncols], self.w["rope32"].ap()[:, :, l0:l0 + ncols].rearrange("two d l -> d two l"), writes=[brt])
            for s in range(n):
                i = ti % 2
                ti += 1
                pA, bpA = self.ps[2 + i], self.bps[2 + i]
                c.mm(pA[:, :], [(hT[:, k, s * 128:(s + 1) * 128], Win[:, k, 0:512]) for k in range(8)], reads=[bh, b_w], writes=[bpA])
                c.op("dve", lambda e: e.memset(st[i][:], 0.0), writes=[b_st[i]])
                for u in range(2):
                    c.op("act", lambda e: e.activation(out=jk[:], in_=pA[:, u * 256:(u + 1) * 256], func=AF.Square, accum_out=st[i][:, u:u + 1]), reads=[bpA], writes=[b_jk, b_st[i]])
                c.op("dve", lambda e: e.tensor_scalar(out=st[i][:, 2:4], in0=st[i][:, 0:2], scalar1=1.0 / 256, scalar2=EPS, op0=ALU.mult, op1=ALU.add), reads=[b_st[i]], writes=[b_st[i]])
                c.op("act", lambda e: e.activation(out=st[i][:, 2:4], in_=st[i][:, 2:4], func=AF.Sqrt), reads=[b_st[i]], writes=[b_st[i]])
                c.op("dve", lambda e: e.reciprocal(out=st[i][:, 2:4], in_=st[i][:, 2:4]), reads=[b_st[i]], writes=[b_st[i]])
                for u in range(2):
                    c.op("dve", lambda e: e.scalar_tensor_tensor(out=cn[i][:, u * 256:(u + 1) * 256], in0=pA[:, u * 256:(u + 1) * 256], scalar=st[i][:, 2 + u:3 + u], in1=gq[:, u * 256:(u + 1) * 256], op0=ALU.mult, op1=ALU.mult),
                         reads=[bpA, b_st[i], b_w], writes=[b_cn[i]])
                pT = self.ps[4 + i].ap().bitcast(BF16)
                c.tr_multi([(pT[:, k * 128:(k + 1) * 128], cn[i][:, k * 128:(k + 1) * 128], self.identb) for k in range(4)], reads=[b_cn[i], self.b_const], writes=[self.bps[4 + i]])
                c.op("act", lambda e: e.activation(out=cT_[:, :, s * 128:(s + 1) * 128], in_=pT[:, 0:512].rearrange("p (k n) -> p k n", k=4), func=AF.Copy), reads=[self.bps[4 + i]], writes=[bcT])

            def rope_proj(pairs1, pairs2, M, rtab, dst_ap, dst_buf, rd):
                nonlocal cnt
                i = cnt % 2
                cnt += 1
                P1, bP1 = self.ps[2 + i], self.bps[2 + i]
                P2, bP2 = self.ps[6 + i], self.bps[6 + i]
                c.mm(P1[0:M, :ncols], pairs1, reads=rd, writes=[bP1])
                if is_ctx or pairs2 is None:
                    c.op("act", lambda e: e.activation(out=dst_ap, in_=P1[0:M, :ncols], func=AF.Copy), reads=[bP1], writes=[dst_buf])
                else:
                    c.mm(P2[0:M, :ncols], pairs2, reads=rd + [b_wr], writes=[bP2])
                    c.op("dve", lambda e: e.tensor_tensor(out=t1s[i][0:M, :ncols], in0=P1[0:M, :ncols], in1=rtab[0:M, 0, :ncols], op=ALU.mult), reads=[bP1, brt], writes=[b_t1[i]])
                    c.op("dve", lambda e: e.tensor_tensor(out=t2s[i][0:M, :ncols], in0=P2[0:M, :ncols], in1=rtab[0:M, 1, :ncols], op=ALU.mult), reads=[bP2, brt], writes=[b_t2[i]])
                    c.op("pool", lambda e: e.tensor_tensor(out=dst_ap, in0=t1s[i][0:M, :ncols], in1=t2s[i][0:M, :ncols], op=ALU.add), reads=[b_t1[i], b_t2[i]], writes=[dst_buf])

            kr_, bkr = krT[gi % 2], b_krT[gi % 2]
            rope_proj([(Win[:, k, 512:544], hT[:, k, :ncols]) for k in range(8)], [(Wkr_rot[:, k, :], hT[:, k, :ncols]) for k in range(8)], 32, rk, kr_[:, :ncols], bkr, [bh, b_w])
            for h in range(16):
                qs, bq = qst[h % 2], b_qst[h % 2]
                rope_proj([(Wq[:, kc, h * 96:(h + 1) * 96], cT_[:, kc, :ncols]) for kc in range(2)],
                          [(Wqr[:, kc, h * 96:(h + 1) * 96], cT_[:, kc, :ncols]) for kc in range(2)], 96, rt, qs[:, :ncols], bq, [bcT, b_w])
                c.dma("sp", QT.ap()[h, :, col0:col0 + ncols], qs[:, :ncols], reads=[bq], writes=[b_QT])
                ks, bk = kst[h % 2], b_kst[h % 2]
                rope_proj([(Wkn[:, kc, h, :], cT_[:, 2 + kc, :ncols]) for kc in range(2)], None, 64, None, ks[:, :ncols], bk, [bcT, b_w])
                c.dma("sp", KTd.ap()[h, 0:64, col0:col0 + ncols], ks[:, :ncols], reads=[bk], writes=[b_KTd])
                c.dma("sp", KTd.ap()[h, 64:96, col0:col0 + ncols], kr_[:, :ncols], reads=[bkr], writes=[b_KTd])
            for s in range(n):
                vt, bvt = Vt[s % 2], b_Vt[s % 2]
                for hh in range(2):
                    i = cnt % 2
                    cnt += 1
                    Pv, bPv = self.ps[2 + i], self.bps[2 + i]
                    c.mm(Pv[:, :], [(cT_[:, 2 + kc, s * 128:(s + 1) * 128], Wv[:, kc, hh * 8:(hh + 1) * 8, :].rearrange("p h d -> p (h d)")) for kc in range(2)], reads=[bcT, b_w], writes=[bPv])
                    c.op("act", lambda e: e.activation(out=vt[:, hh * 8:(hh + 1) * 8, 0:64], in_=Pv[:, :].rearrange("p (h d) -> p h d", d=64), func=AF.Copy), reads=[bPv], writes=[bvt])
                c.dma("sp", Vd.ap()[:, t0 + s].rearrange("h p d -> p h d"), vt[:], reads=[bvt], writes=[b_Vd])
        c.barrier()
        c.sb_release(mark)
        QTs = [c.sb([96, T], BF16, "QTh") for _ in range(2)]
        KTs = [c.sb([96, T], BF16, "KTh") for _ in range(2)]
        Vhs = [c.sb([128, NT, 65], BF16, "Vh") for _ in range(2)]
        b_hd = [Buf(), Buf()]
        PTs = [c.sb([128, 512], BF16, "PT") for _ in range(4)]
        b_PT = [Buf() for _ in range(4)]
        osb = [c.sb([65, 512], F32, "osb") for _ in range(2)]
        b_osb = [Buf(), Buf()]
        ysb = [c.sb([64, 512], BF16, "ysb") for _ in range(2)]
        b_ysb = [Buf(), Buf()]
        rbs = [c.sb([64, 512], F32, "rbs") for _ in range(2)]
        b_rbs = [Buf(), Buf()]
        scale = 96 ** -0.5
        pi = 0; si = 0; oi = 0

        def load_head(h_):
            c.dma("sp", QTs[h_ % 2][:], QT.ap()[h_], reads=[b_QT], writes=[b_hd[h_ % 2]])
            c.dma("sp", KTs[h_ % 2][:], KTd.ap()[h_], reads=[b_KTd], writes=[b_hd[h_ % 2]])
            c.dma("sp", Vhs[h_ % 2][:], Vd.ap()[h_].rearrange("t p d -> p t d"), reads=[b_Vd], writes=[b_hd[h_ % 2]])
        load_head(0)
        for h in range(16):
            Qh, Kh, Vh, bhd = QTs[h % 2], KTs[h % 2], Vhs[h % 2], b_hd[h % 2]
            if h + 1 < 16:
                load_head(h + 1)
            units = []
            for (t0, n, is_ctx) in self.groups(4, with_ctx=need_ctx):
                keys = list(range(NCT)) if is_ctx else list(range(NT))
                gslot = oi % 2
                oi += 1
                for ki, kt in enumerate(keys):
                    units.append((t0 * 128, n * 128, kt, ki == 0, ki == len(keys) - 1, gslot))
            SB = (0, 1, 5, 6, 7)
            LA = 3

            def issue_S(ui):
                col0_, ncols_, kt_, _, _, _ = units[ui]
                bk_ = SB[(si + ui) % len(SB)]
                c.mm(self.ps[bk_][:, :ncols_], [(Kh[:, kt_ * 128:(kt_ + 1) * 128], Qh[:, col0_:col0_ + ncols_])], reads=[bhd], writes=[self.bps[bk_]])

            def epilogue(col0_, ncols_, gslot):
                oT, boT = self.ps[2 + gslot], self.bps[2 + gslot]
                o, bo = osb[gslot], b_osb[gslot]
                ys, bys = ysb[gslot], b_ysb[gslot]
                c.op("act", lambda e: e.activation(out=o[:, :ncols_], in_=oT[0:65, :ncols_], func=AF.Copy), reads=[boT], writes=[bo])
                c.mm(self.ps[4][0:64, :ncols_], [(self.ones32[64:65, 0:64], o[64:65, :ncols_])], reads=[self.b_const, bo], writes=[self.bps[4]])
                rb, brb = rbs[gslot], b_rbs[gslot]
                c.op("dve", lambda e: e.reciprocal(out=rb[:, :ncols_], in_=self.ps[4][0:64, :ncols_]), reads=[self.bps[4]], writes=[brb])
                c.op("dve", lambda e: e.tensor_tensor(out=ys[:, :ncols_], in0=o[0:64, :ncols_], in1=rb[:, :ncols_], op=ALU.mult), reads=[bo, brb], writes=[bys])
                c.dma("sp", YT.ap()[h, :, col0_:col0_ + ncols_], ys[:, :ncols_], reads=[bys], writes=[b_YT])

            pending = []
            for k0 in range(min(LA, len(units))):
                issue_S(k0)
            for ui, (col0, ncols, kt, first, last, gslot) in enumerate(units):
                bk = SB[(si + ui) % len(SB)]
                sT, bsT = self.ps[bk], self.bps[bk]
                if ui + LA < len(units):
                    issue_S(ui + LA)
                PT, bPT = PTs[pi % 4], b_PT[pi % 4]
                pi += 1
                oT, boT = self.ps[2 + gslot], self.bps[2 + gslot]
                c.op("act", lambda e: e.activation(out=PT[:, :ncols], in_=sT[:, :ncols], func=AF.Exp, scale=scale), reads=[bsT], writes=[bPT])
                c.mm(oT[0:65, :ncols], [(Vh[:, kt, :], PT[:, :ncols])], reads=[bhd, bPT], writes=[boT], start=first, stop=last)
                if pending and pending[0][0] <= ui:
                    _, args = pending.pop(0)
                    epilogue(*args)
                if last:
                    pending.append((ui + 4, (col0, ncols, gslot)))
            for _, args in pending:
                epilogue(*args)
            si += len(units)
        c.barrier()
        c.sb_release(mark)
        Wo = c.sb([64, 16, 1024], BF16, "Wo")
        b_wo = Buf()
        c.dma("pool", Wo[:], w_out.rearrange("(h d) n -> d h n", d=64), writes=[b_wo])
        self.attn_out(li, need_ctx, Wo, b_wo, YT, b_YT)
        self.phase_end()

    def attn_out(self, li, need_ctx, Wo, b_wo, YT, b_YT):
        c = self.c
        NCT, NT = self.NCT, self.NT
        G1x, bG1x = self.load_mod(li, 2, 0, "G1x")
        G1c, bG1c = self.load_mod(li, 2, 1, "G1c")
        yTs = [c.sb([64, 16, 128], BF16, "yT") for _ in range(2)]
        b_yT = [Buf(), Buf()]
        xts = [c.sb([128, D], F32, "xres") for _ in range(2)]
        b_xt = [Buf(), Buf()]
        tmps = [c.sb([128, D], F32, "rtmp") for _ in range(2)]
        b_tmp = [Buf(), Buf()]
        for bi, qb in enumerate(range(0 if need_ctx else NCT, NT)):
            is_ctx = qb < NCT
            xt, bxt = xts[bi % 2], b_xt[bi % 2]
            yT, byT = yTs[bi % 2], b_yT[bi % 2]
            tmp, btmp = tmps[bi % 2], b_tmp[bi % 2]
            c.dma("sp", xt[:], self.xs.ap()[qb * 128:(qb + 1) * 128, :], reads=[self.b_xs[qb]], writes=[bxt])
            c.dma("sp", yT[:], YT.ap()[:, :, qb * 128:(qb + 1) * 128].rearrange("h d t -> d h t"), reads=[b_YT], writes=[byT])
            G1, bG1 = (G1c, bG1c) if is_ctx else (G1x, bG1x)
            for nn in range(2):
                z, bz = self.ps[5 + nn], self.bps[5 + nn]
                c.mm(z[:, :], [(yT[:, hq, :], Wo[:, hq, nn * 512:(nn + 1) * 512]) for hq in range(16)], reads=[byT, b_wo], writes=[bz])
                c.op("dve", lambda e: e.tensor_tensor(out=tmp[:, nn * 512:(nn + 1) * 512], in0=z[:, :], in1=G1[:, nn * 512:(nn + 1) * 512], op=ALU.mult), reads=[bz, bG1], writes=[btmp])
            c.op("pool", lambda e: e.tensor_tensor(out=xt[:], in0=xt[:], in1=tmp[:], op=ALU.add), reads=[btmp, bxt], writes=[bxt])
            c.dma("sp", self.xs.ap()[qb * 128:(qb + 1) * 128, :], xt[:], reads=[bxt], writes=[self.b_xs[qb]])

    def layer_ssd(self, li, j, need_ctx):
        c, nc = self.c, self.nc
        T, NT, NCT, NL, NCX = self.T, self.NT, self.NCT, self.NL, self.NCX
        w_in = self.w["ssm_w_in"].ap()[j]
        XB = self.scratch(f"ssd_xb{li}", [24, 128, T], BF16)
        XC = self.scratch(f"ssd_xc{li}", [24, 128, T], BF16)
        Zs = self.scratch(f"ssd_z{li}", [NT, 128, 2048], BF16)
        DT = self.scratch(f"ssd_dt{li}", [NT, 128, 64], F32)
        Yd = [self.scratch(f"ssd_y{li}_{d}", [NT, 128, 2048], F32) for d in range(2)]
        b_XB = Buf(); b_XC = Buf(); b_Z = Buf(); b_DT = Buf(); b_Y = [Buf(), Buf()]
        b_w = Buf("ssd_w")
        Win = c.sb([128, 8, 5184], BF16, "ssdWin")
        for k in range(8):
            c.dma("pool", Win[:, k, :], w_in[k * 128:(k + 1) * 128, :], writes=[b_w])
        dtb = c.sb([128, 64], F32, "dtb")
        c.dma("sp", dtb[:], self.w["ssm_dt_bias"].ap()[j:j + 1].rearrange("o d h -> o (d h)").partition_broadcast(128), writes=[b_w])
        mark = c.sb_mark()
        A1x, bA1x = self.load_mod(li, 1, 0, "A1x")
        S1x, bS1x = self.load_mod(li, 0, 0, "S1x")
        A1c, bA1c = self.load_mod(li, 1, 1, "A1c")
        S1c, bS1c = self.load_mod(li, 0, 1, "S1c")
        self.alloc_norm_bufs(2)
        hTs = [c.sb([128, 8, 512], BF16, "hT") for _ in range(2)]
        b_hT = [Buf(), Buf()]
        stg = [c.sb([128, 512], BF16, "stg") for _ in range(3)]
        b_stg = [Buf() for _ in range(3)]
        zst = [c.sb([128, 2048], BF16, "zst") for _ in range(2)]
        b_zst = [Buf(), Buf()]
        dts = [c.sb([128, 64], F32, "dts") for _ in range(2)]
        b_dts = [Buf(), Buf()]
        cnt = 0
        for gi, (t0, n, is_ctx) in enumerate(self.groups(4)):
            ncols = n * 128
            col0 = t0 * 128
            hT, bh = hTs[gi % 2], b_hT[gi % 2]
            for s in range(n):
                if is_ctx:
                    self.norm_tile(t0 + s, A1c, bA1c, S1c, bS1c, hT, bh, s * 128, (t0 + s) % 2)
                else:
                    self.norm_tile(t0 + s, A1x, bA1x, S1x, bS1x, hT, bh, s * 128, (t0 + s) % 2)
            for fc in range(24):
                i = cnt % 3
                cnt += 1
                P, bP = self.ps[2 + i], self.bps[2 + i]
                c.mm(P[:, :ncols], [(Win[:, k, 2048 + fc * 128:2048 + (fc + 1) * 128], hT[:, k, :ncols]) for k in range(8)], reads=[b_w, bh], writes=[bP])
                c.op("act", lambda e: e.activation(out=stg[i][:, :ncols], in_=P[:, :ncols], func=AF.Copy), reads=[bP], writes=[b_stg[i]])
                c.dma("sp", XB.ap()[fc, :, col0:col0 + ncols], stg[i][:, :ncols], reads=[b_stg[i]], writes=[b_XB])
            for s in range(n):
                zs, bz = zst[s % 2], b_zst[s % 2]
                for zc in range(4):
                    i = cnt % 3
                    cnt += 1
                    P, bP = self.ps[2 + i], self.bps[2 + i]
                    c.mm(P[:, :], [(hT[:, k, s * 128:(s + 1) * 128], Win[:, k, zc * 512:(zc + 1) * 512]) for k in range(8)], reads=[b_w, bh], writes=[bP])
                    c.op("act", lambda e: e.activation(out=zs[:, zc * 512:(zc + 1) * 512], in_=P[:, :], func=AF.Silu), reads=[bP], writes=[bz])
                c.dma("sp", Zs.ap()[t0 + s], zs[:], reads=[bz], writes=[b_Z])
                i = cnt % 3
                cnt += 1
                P, bP = self.ps[2 + i], self.bps[2 + i]
                dt_, bdt = dts[s % 2], b_dts[s % 2]
                c.mm(P[:, 0:64], [(hT[:, k, s * 128:(s + 1) * 128], Win[:, k, 5120:5184]) for k in range(8)], reads=[b_w, bh], writes=[bP])
                c.op("dve", lambda e: e.tensor_tensor(out=dt_[:], in0=P[:, 0:64], in1=dtb[:], op=ALU.add), reads=[bP, b_w], writes=[bdt])
                c.op("act", lambda e: e.activation(out=dt_[:], in_=dt_[:], func=AF.Exp), reads=[bdt], writes=[bdt])
                c.op("act", lambda e: e.activation(out=dt_[:], in_=dt_[:], func=AF.Ln, bias=1.0), reads=[bdt], writes=[bdt])
                c.dma("sp", DT.ap()[t0 + s], dt_[:], reads=[bdt], writes=[b_DT])
        c.barrier()
        c.sb_release(self.mark0)
        cw = c.sb([128, 24, 5], F32, "convw"); cbias = c.sb([128, 24], F32, "convb")
        b_cw = Buf()
        c.dma("sp", cw[:], self.w["ssm_conv_wT"].ap()[j], writes=[b_cw])
        c.dma("sp", cbias[:], self.w["ssm_conv_bT"].ap()[j], writes=[b_cw])
        segs = [(0, NCX), (NCX, NL)]
        Lmax = max(NCX, NL)
        xp = [c.sb([128, Lmax + 4], BF16, "xp") for _ in range(2)]
        b_xp = [Buf(), Buf()]
        acc = [c.sb([128, Lmax], F32, "cacc") for _ in range(2)]
        b_acc = [Buf(), Buf()]
        cout = [c.sb([128, Lmax], BF16, "cout") for _ in range(2)]
        b_cout = [Buf(), Buf()]
        it = 0
        for fc in range(24):
            for (o0, L) in segs:
                i = it % 2
                it += 1
                c.op("pool", lambda e: e.memset(xp[i][:], 0.0), writes=[b_xp[i]])
                c.dma("sp", xp[i][:, 2:2 + L], XB.ap()[fc, :, o0:o0 + L], reads=[b_XB], writes=[b_xp[i]])
                c.op("dve", lambda e: e.tensor_scalar(out=acc[i][:, :L], in0=xp[i][:, 0:L], scalar1=cw[:, fc, 0:1], scalar2=None, op0=ALU.mult), reads=[b_xp[i], b_cw], writes=[b_acc[i]])
                for k in range(1, 5):
                    eng = "dve"
                    c.op(eng, lambda e: e.scalar_tensor_tensor(out=acc[i][:, :L], in0=xp[i][:, k:k + L], scalar=cw[:, fc, k:k + 1], in1=acc[i][:, :L], op0=ALU.mult, op1=ALU.add),
                         reads=[b_xp[i], b_cw, b_acc[i]], writes=[b_acc[i]])
                c.op("act", lambda e: e.activation(out=cout[i][:, :L], in_=acc[i][:, :L], func=AF.Silu, bias=cbias[:, fc:fc + 1]), reads=[b_acc[i], b_cw], writes=[b_cout[i]])
                c.dma("sp", XC.ap()[fc, :, o0:o0 + L], cout[i][:, :L], reads=[b_cout[i]], writes=[b_XC])
        c.barrier()
        c.sb_release(self.mark0)
        sel = c.sb([32, 32, 128], F32, "sel32"); b_sel = Buf()
        c.dma("sp", sel[:], self.w["sel32"].ap(), writes=[b_sel])
        aneg = c.sb([128, 64], F32, "aneg"); dsk = c.sb([128, 64], F32, "dsk"); b_an = Buf()
        c.dma("sp", aneg[:], self.w["ssm_a_log"].ap()[j:j + 1].rearrange("o d h -> o (d h)").partition_broadcast(128), writes=[b_an])
        c.op("act", lambda e: e.activation(out=aneg[:], in_=aneg[:], func=AF.Exp), reads=[b_an], writes=[b_an])
        c.op("act", lambda e: e.activation(out=aneg[:], in_=aneg[:], func=AF.Copy, scale=-1.0), reads=[b_an], writes=[b_an])
        c.dma("sp", dsk[:], self.w["ssm_d"].ap()[j:j + 1].rearrange("o d h -> o (d h)").partition_broadcast(128), writes=[b_an])
        c.op("dve", lambda e: e.tensor_tensor(out=dsk[:, 0:32], in0=dsk[:, 0:32], in1=dsk[:, 32:64], op=ALU.add), reads=[b_an], writes=[b_an])
        xcs = [c.sb([128, 24, 128], BF16, "xc") for _ in range(2)]; b_xc = [Buf(), Buf()]
        dtt = [c.sb([128, 64], F32, "dtt") for _ in range(2)]; b_dtt = [Buf(), Buf()]
        gt = [c.sb([128, 8, 32], F32, "gt") for _ in range(2)]; b_gt = [Buf(), Buf()]
        acT = [c.sb([32, 128], F32, "acT") for _ in range(2)]; b_acT = [Buf(), Buf()]
        nacT = [c.sb([32, 128], F32, "nacT") for _ in range(2)]
        xtok = [c.sb([128, 32, 64], F32, "xtok") for _ in range(2)]; b_xtok = [Buf(), Buf()]
        u = [c.sb([128, 32, 64], BF16, "u") for _ in range(2)]; b_u = [Buf(), Buf()]
        Vw = [c.sb([128, 32, 64], BF16, "Vw") for _ in range(2)]; b_Vw = [Buf(), Buf()]
        Btok = [c.sb([128, 4, 128], BF16, "Btok") for _ in range(2)]; b_Bt = [Buf(), Buf()]
        scm = [c.sb([128, 128], F32, "scm") for _ in range(2)]; b_scm = [Buf(), Buf()]
        aa = [c.sb([128, 512], F32, "aa") for _ in range(4)]; b_aa = [Buf() for _ in range(4)]
        EE = [c.sb([128, 512], F32, "EE") for _ in range(4)]; b_EE = [Buf() for _ in range(4)]
        MT = [c.sb([128, 512], BF16, "MT") for _ in range(4)]; b_MT = [Buf() for _ in range(4)]
        yi = [c.sb([128, 512], F32, "yi") for _ in range(2)]; b_yi = [Buf(), Buf()]
        Yt = [c.sb([128, 2048], F32, "Yt") for _ in range(2)]; b_Yt = [Buf(), Buf()]
        S32 = c.sb([128, 4, 512], F32, "S32"); Sb = c.sb([128, 4, 512], BF16, "Sb"); b_S = [Buf() for _ in range(4)]
        ci = 0; hi = 0
        for d in range(2):
            tri = self.trile32 if d == 0 else self.trige32
            order = list(range(NT)) if d == 0 else (list(range(NCT - 1, -1, -1)) + list(range(NT - 1, NCT - 1, -1)))
            c.op("dve", lambda e: e.memset(S32[:], 0.0), writes=b_S)
            c.op("pool", lambda e: e.memset(Sb[:], 0.0), writes=b_S)
            def prep(ch, i):
                xc, bxc = xcs[i], b_xc[i]
                c.dma("sp", xc[:], XC.ap()[:, :, ch * 128:(ch + 1) * 128].rearrange("f p t -> p f t"), reads=[b_XC], writes=[bxc])
                c.dma("sp", dtt[i][:], DT.ap()[ch], reads=[b_DT], writes=[b_dtt[i]])
                g_, bg = gt[i], b_gt[i]
                dc = slice(d * 32, (d + 1) * 32)
                c.op("dve", lambda e: e.tensor_tensor(out=g_[:, 0, :], in0=dtt[i][:, dc], in1=aneg[:, dc], op=ALU.mult), reads=[b_dtt[i], b_an], writes=[bg])
                p0, bp0 = self.ps[0], self.bps[0]
                c.mm(p0[:, 0:32], [(tri, g_[:, 0, :])], reads=[self.b_const, bg], writes=[bp0])
                c.mm(p0[:, 32:64], [(self.ones32, g_[:, 0, :])], reads=[self.b_const, bg], writes=[bp0])
                c.mm(p0[0:32, 128:256], [(g_[:, 0, :], tri)], reads=[self.b_const, bg], writes=[bp0])
                c.op("act", lambda e: e.activation(out=g_[:, 1, :], in_=p0[:, 0:32], func=AF.Copy), reads=[bp0], writes=[bg])
                c.op("act", lambda e: e.activation(out=g_[:, 2, :], in_=p0[:, 0:32], func=AF.Copy, scale=-1.0), reads=[bp0], writes=[bg])
                c.op("act", lambda e: e.activation(out=g_[:, 3, :], in_=p0[:, 0:32], func=AF.Exp), reads=[bp0], writes=[bg])
                c.op("dve", lambda e: e.tensor_tensor(out=g_[:, 4, :], in0=p0[:, 32:64], in1=g_[:, 1, :], op=ALU.subtract), reads=[bp0, bg], writes=[bg])
                c.op("act", lambda e: e.activation(out=g_[:, 4, :], in_=g_[:, 4, :], func=AF.Exp), reads=[bg], writes=[bg])
                c.op("act", lambda e: e.activation(out=g_[:, 5, :], in_=p0[:, 32:64], func=AF.Exp), reads=[bp0], writes=[bg])
                c.op("act", lambda e: e.activation(out=acT[i][:], in_=p0[0:32, 128:256], func=AF.Copy), reads=[bp0], writes=[b_acT[i]])
                c.op("act", lambda e: e.activation(out=nacT[i][:], in_=p0[0:32, 128:256], func=AF.Copy, scale=-1.0), reads=[bp0], writes=[b_acT[i]])
                for hh in range(2):
                    pT = self.ps[1].ap().bitcast(BF16)
                    c.tr_multi([(pT[:, k * 128:(k + 1) * 128], xc[:, hh * 8 + k, :], self.identb) for k in range(8)], reads=[bxc, self.b_const], writes=[self.bps[1]])
                    c.op("act", lambda e: e.activation(out=xtok[i][:, hh * 16:(hh + 1) * 16, :].rearrange("p h d -> p (h d)"), in_=pT[:, :], func=AF.Copy), reads=[self.bps[1]], writes=[b_xtok[i]])
                pT = self.ps[1].ap().bitcast(BF16)
                c.tr_multi([(pT[:, k * 128:(k + 1) * 128], xc[:, 16 + k, :], self.identb) for k in range(4)], reads=[bxc, self.b_const], writes=[self.bps[1]])
                c.op("act", lambda e: e.activation(out=Btok[i][:].rearrange("p g n -> p (g n)"), in_=pT[:, 0:512], func=AF.Copy), reads=[self.bps[1]], writes=[b_Bt[i]])
                c.op("dve", lambda e: e.tensor_tensor(out=u[i][:], in0=xtok[i][:], in1=dtt[i][:, dc].unsqueeze(2).to_broadcast([128, 32, 64]), op=ALU.mult), reads=[b_xtok[i], b_dtt[i]], writes=[b_u[i]])
                c.op("pool", lambda e: e.tensor_tensor(out=Vw[i][:], in0=u[i][:], in1=g_[:, 4, :].unsqueeze(2).to_broadcast([128, 32, 64]), op=ALU.mult), reads=[b_u[i], bg], writes=[b_Vw[i]])
            def groups_(ch, i):
                xc, bxc = xcs[i], b_xc[i]
                g_, bg = gt[i], b_gt[i]
                dc = slice(d * 32, (d + 1) * 32)
                Y, bY = Yt[i], b_Yt[i]
                for g in range(4):
                    pcb, bpcb = self.ps[2], self.bps[2]
                    c.mm(pcb[:, 0:128], [(xc[:, 16 + g, :], xc[:, 20 + g, :])], reads=[bxc], writes=[bpcb])
                    sm, bsm = scm[g % 2], b_scm[g % 2]
                    c.op("dve", lambda e: e.tensor_tensor(out=sm[:], in0=pcb[:, 0:128], in1=tri, op=ALU.mult), reads=[bpcb, self.b_const], writes=[bsm])
                    yps, byps = self.ps[4 + g % 2], self.bps[4 + g % 2]
                    for e8 in range(8):
                        h = g * 8 + e8
                        pbc, bpbc = self.ps[6 + e8 // 4], self.bps[6 + e8 // 4]
                        c.mm(pbc[:, (e8 % 4) * 128:(e8 % 4 + 1) * 128], [(sel[:, h, :], acT[i][:]), (nacT[i][:], sel[:, h, :])], reads=[b_sel, b_acT[i]], writes=[bpbc])
                    for hb in range(2):
                        k4 = (g % 2) * 2 + hb
                        pbc, bpbc = self.ps[6 + hb], self.bps[6 + hb]
                        c.op("dve", lambda e: e.tensor_scalar(out=aa[k4][:], in0=pbc[:, :], scalar1=0.0, scalar2=None, op0=ALU.min), reads=[bpbc], writes=[b_aa[k4]])
                        c.op("act", lambda e: e.activation(out=EE[k4][:], in_=aa[k4][:], func=AF.Exp), reads=[b_aa[k4]], writes=[b_EE[k4]])
                        c.op("pool", lambda e: e.tensor_tensor(out=MT[k4][:].rearrange("p (h t) -> p h t", h=4), in0=EE[k4][:].rearrange("p (h t) -> p h t", h=4),
                                                               in1=sm[:].unsqueeze(1).to_broadcast([128, 4, 128]), op=ALU.mult), reads=[bsm, b_EE[k4]], writes=[b_MT[k4]])
                    for e8 in range(8):
                        h = g * 8 + e8
                        k4 = (g % 2) * 2 + e8 // 4
                        c.mm(yps[:, e8 * 64:(e8 + 1) * 64], [(MT[k4][:, (e8 % 4) * 128:(e8 % 4 + 1) * 128], u[i][:, h, :])], reads=[b_MT[k4], b_u[i]], writes=[byps])
                    pin, bpin = self.ps[3], self.bps[3]
                    c.mm(pin[:, :], [(xc[:, 20 + g, :], Sb[:, g, :])], reads=[bxc, b_S[g]], writes=[bpin])
                    y_, byi = yi[g % 2], b_yi[g % 2]
                    c.op("dve", lambda e: e.tensor_tensor(out=y_[:].rearrange("p (h d) -> p h d", d=64), in0=pin[:, :].rearrange("p (h d) -> p h d", d=64),
                                                          in1=g_[:, 3, g * 8:(g + 1) * 8].unsqueeze(2).to_broadcast([128, 8, 64]), op=ALU.mult), reads=[bpin, bg], writes=[byi])
                    c.op("dve", lambda e: e.tensor_tensor(out=Y[:, g * 512:(g + 1) * 512], in0=yps[:, :], in1=y_[:], op=ALU.add), reads=[byps, byi], writes=[bY])
                    if d == 0:
                        c.op("pool", lambda e: e.tensor_tensor(out=y_[:].rearrange("p (h d) -> p h d", d=64), in0=xtok[i][:, g * 8:(g + 1) * 8, :],
                                                               in1=dsk[:, g * 8:(g + 1) * 8].unsqueeze(2).to_broadcast([128, 8, 64]), op=ALU.mult), reads=[b_xtok[i], b_an, bY], writes=[byi])
                        c.op("pool", lambda e: e.tensor_tensor(out=Y[:, g * 512:(g + 1) * 512], in0=Y[:, g * 512:(g + 1) * 512], in1=y_[:], op=ALU.add), reads=[byi], writes=[bY])
                    pst, bpst = self.ps[3], self.bps[3]
                    c.mm(pst[:, :], [(Btok[i][:, g, :], Vw[i][:, g * 8:(g + 1) * 8, :].rearrange("p h d -> p (h d)"))], reads=[b_Bt[i], b_Vw[i]], writes=[bpst])
                    c.op("dve", lambda e: e.tensor_tensor(out=S32[:, g, :].rearrange("p (h d) -> p h d", d=64), in0=S32[:, g, :].rearrange("p (h d) -> p h d", d=64),
                                                          in1=g_[:, 5, g * 8:(g + 1) * 8].unsqueeze(2).to_broadcast([128, 8, 64]), op=ALU.mult), reads=[bg], writes=[b_S[g]])
                    c.op("dve", lambda e: e.tensor_tensor(out=S32[:, g, :], in0=S32[:, g, :], in1=pst[:, :], op=ALU.add), reads=[bpst], writes=[b_S[g]])
                    c.op("act", lambda e: e.activation(out=Sb[:, g, :], in_=S32[:, g, :], func=AF.Copy), reads=[], writes=[b_S[g]])
                c.dma("sp", Yd[d].ap()[ch], Y[:], reads=[bY], writes=[b_Y[d]])
            prep(order[0], 0)
            for idx, ch in enumerate(order):
                if idx + 1 < len(order):
                    prep(order[idx + 1], (idx + 1) % 2)
                groups_(ch, idx % 2)
        c.barrier()
        c.sb_release(self.mark0)
        Wo = c.sb([128, 16, 1024], BF16, "ssdWo"); b_wo = Buf()
        c.dma("pool", Wo[:], self.w["ssm_w_out"].ap()[j].rearrange("(k p) n -> p k n", p=128), writes=[b_wo])
        ng = c.sb([128, 2048], F32, "ssdng")
        c.dma("sp", ng[:], self.w["ssm_norm_g"].ap()[j:j + 1, :].partition_broadcast(128), writes=[b_wo])
        self.gated_out(li, need_ctx, Yd, b_Y, Zs, b_Z, 2048, 4, ng, Wo, b_wo, False)
        self.phase_end()

    def gated_out(self, li, need_ctx, Yd, b_Y, Zs, b_Z, W, ngroups, ng, Wo, b_wo, gate_after):
        c = self.c
        NCT, NT = self.NCT, self.NT
        KC = W // 128
        gs = W // ngroups
        G1x, bG1x = self.load_mod(li, 2, 0, "G1x")
        G1c, bG1c = self.load_mod(li, 2, 1, "G1c")
        yf = [c.sb([128, W], F32, "yf") for _ in range(2)]; yb = [c.sb([128, W], F32, "yb") for _ in range(2)]
        zt = [c.sb([128, W], BF16, "zt") for _ in range(2)]
        b_in = [Buf(), Buf()]
        jk = c.sb([128, W], BF16, "gjunk"); b_jk = Buf()
        st = [c.sb([128, 2, 8], F32, "gst") for _ in range(2)]; b_st = [Buf(), Buf()]
        yn = [c.sb([128, W], BF16, "yn") for _ in range(2)]; b_yn = [Buf(), Buf()]
        yT = [c.sb([128, KC, 128], BF16, "gyT") for _ in range(2)]; b_yT = [Buf(), Buf()]
        xts = [c.sb([128, D], F32, "xres") for _ in range(2)]; b_xt = [Buf(), Buf()]
        tmps = [c.sb([128, D], F32, "rtmp") for _ in range(2)]; b_tmp = [Buf(), Buf()]
        for bi, qb in enumerate(range(0 if need_ctx else NCT, NT)):
            i = bi % 2
            is_ctx = qb < NCT
            c.dma("sp", yf[i][:], Yd[0].ap()[qb], reads=[b_Y[0]], writes=[b_in[i]])
            c.dma("sp", yb[i][:], Yd[1].ap()[qb], reads=[b_Y[1]], writes=[b_in[i]])
            c.dma("sp", zt[i][:], Zs.ap()[qb], reads=[b_Z], writes=[b_in[i]])
            c.dma("sp", xts[i][:], self.xs.ap()[qb * 128:(qb + 1) * 128, :], reads=[self.b_xs[qb]], writes=[b_xt[i]])
            c.op("pool", lambda e: e.tensor_tensor(out=yf[i][:], in0=yf[i][:], in1=yb[i][:], op=ALU.add), reads=[b_in[i]], writes=[b_in[i]])
            if not gate_after:
                c.op("dve", lambda e: e.tensor_tensor(out=yf[i][:], in0=yf[i][:], in1=zt[i][:], op=ALU.mult), reads=[b_in[i]], writes=[b_in[i]])
            c.op("dve", lambda e: e.memset(st[i][:], 0.0), writes=[b_st[i]])
            for g in range(ngroups):
                c.op("act", lambda e: e.activation(out=jk[:, g * gs:(g + 1) * gs], in_=yf[i][:, g * gs:(g + 1) * gs], func=AF.Square, accum_out=st[i][:, 0, g:g + 1]), reads=[b_in[i]], writes=[b_jk, b_st[i]])
            c.op("dve", lambda e: e.tensor_scalar(out=st[i][:, 1, :], in0=st[i][:, 0, :], scalar1=1.0 / gs, scalar2=EPS, op0=ALU.mult, op1=ALU.add), reads=[b_st[i]], writes=[b_st[i]])
            c.op("act", lambda e: e.activation(out=st[i][:, 1, :], in_=st[i][:, 1, :], func=AF.Sqrt), reads=[b_st[i]], writes=[b_st[i]])
            c.op("dve", lambda e: e.reciprocal(out=st[i][:, 1, :], in_=st[i][:, 1, :]), reads=[b_st[i]], writes=[b_st[i]])
            c.op("dve", lambda e: e.tensor_tensor(out=yf[i][:].rearrange("p (g d) -> p g d", g=ngroups), in0=yf[i][:].rearrange("p (g d) -> p g d", g=ngroups),
                                                  in1=st[i][:, 1, 0:ngroups].unsqueeze(2).to_broadcast([128, ngroups, gs]), op=ALU.mult), reads=[b_st[i], b_in[i]], writes=[b_in[i]])
            if gate_after:
                c.op("pool", lambda e: e.tensor_tensor(out=yf[i][:], in0=yf[i][:], in1=ng[:], op=ALU.mult), reads=[b_in[i], b_wo], writes=[b_in[i]])
                c.op("dve", lambda e: e.tensor_tensor(out=yn[i][:], in0=yf[i][:], in1=zt[i][:], op=ALU.mult), reads=[b_in[i]], writes=[b_yn[i]])
            else:
                c.op("pool", lambda e: e.tensor_tensor(out=yn[i][:], in0=yf[i][:], in1=ng[:], op=ALU.mult), reads=[b_in[i], b_wo], writes=[b_yn[i]])
            for hb in range(KC // 8):
                pT = self.ps[hb].ap().bitcast(BF16)
                c.tr_multi([(pT[:, k * 128:(k + 1) * 128], yn[i][:, (hb * 8 + k) * 128:(hb * 8 + k + 1) * 128], self.identb) for k in range(8)], reads=[b_yn[i], self.b_const], writes=[self.bps[hb]])
                c.op("act", lambda e: e.activation(out=yT[i][:, hb * 8:(hb + 1) * 8, :], in_=pT.rearrange("p (k n) -> p k n", k=8), func=AF.Copy), reads=[self.bps[hb]], writes=[b_yT[i]])
            G1, bG1 = (G1c, bG1c) if is_ctx else (G1x, bG1x)
            for nn in range(2):
                z, bz = self.ps[5 + nn], self.bps[5 + nn]
                c.mm(z[:, :], [(yT[i][:, k, :], Wo[:, k, nn * 512:(nn + 1) * 512]) for k in range(KC)], reads=[b_yT[i], b_wo], writes=[bz])
                c.op("dve", lambda e: e.tensor_tensor(out=tmps[i][:, nn * 512:(nn + 1) * 512], in0=z[:, :], in1=G1[:, nn * 512:(nn + 1) * 512], op=ALU.mult), reads=[bz, bG1], writes=[b_tmp[i]])
            c.op("pool", lambda e: e.tensor_tensor(out=xts[i][:], in0=xts[i][:], in1=tmps[i][:], op=ALU.add), reads=[b_tmp[i], b_xt[i]], writes=[b_xt[i]])
            c.dma("sp", self.xs.ap()[qb * 128:(qb + 1) * 128, :], xts[i][:], reads=[b_xt[i]], writes=[self.b_xs[qb]])

    def layer_mlstm(self, li, j, need_ctx):
        c, nc = self.c, self.nc
        T, NT, NCT = self.T, self.NT, self.NCT
        w_in = self.w["mlstm_w_in"].ap()[j]
        QK = self.scratch(f"ml_qk{li}", [2, 8, 64, T], BF16)
        Kt = self.scratch(f"ml_kt{li}", [NT, 128, 512], BF16)
        Va = self.scratch(f"ml_va{li}", [NT, 128, 8, 129], BF16)
        Os = self.scratch(f"ml_os{li}", [NT, 128, 1024], BF16)
        Gt = self.scratch(f"ml_gt{li}", [NT, 128, 32], F32)
        Yd = [self.scratch(f"ml_y{li}_{d}", [NT, 128, 1024], F32) for d in range(2)]
        b_QK = Buf(); b_Kt = Buf(); b_Va = Buf(); b_Os = Buf(); b_Gt = Buf(); b_Y = [Buf(), Buf()]
        b_w = Buf("ml_w")
        Win = c.sb([128, 8, 3104], BF16, "mlWin")
        for k in range(8):
            c.dma("pool", Win[:, k, :], w_in[k * 128:(k + 1) * 128, :], writes=[b_w])
        gb = c.sb([128, 32], F32, "mlgb")
        c.dma("sp", gb[:], self.w["mlstm_gate_b"].ap()[j:j + 1].rearrange("o a h -> o (a h)").partition_broadcast(128), writes=[b_w])
        A1x, bA1x = self.load_mod(li, 1, 0, "A1x")
        S1x, bS1x = self.load_mod(li, 0, 0, "S1x")
        A1c, bA1c = self.load_mod(li, 1, 1, "A1c")
        S1c, bS1c = self.load_mod(li, 0, 1, "S1c")
        self.alloc_norm_bufs(2)
        hTs = [c.sb([128, 8, 512], BF16, "hT") for _ in range(2)]
        b_hT = [Buf(), Buf()]
        stg = [c.sb([64, 512], BF16, "mlstg") for _ in range(3)]; b_stg = [Buf() for _ in range(3)]
        kst = [c.sb([128, 512], BF16, "mlkst") for _ in range(2)]; b_kst = [Buf(), Buf()]
        vst = [c.sb([128, 8, 129], BF16, "mlvst") for _ in range(2)]; b_vst = [Buf(), Buf()]
        ost = [c.sb([128, 1024], BF16, "mlost") for _ in range(2)]; b_ost = [Buf(), Buf()]
        gst = [c.sb([128, 32], F32, "mlgst") for _ in range(2)]; b_gst = [Buf(), Buf()]
        gtmp = [c.sb([128, 8], F32, "mlgtmp") for _ in range(2)]
        for i in range(2):
            c.op("pool", lambda e: e.memset(vst[i][:], 1.0), writes=[b_vst[i]])
        cnt = 0
        for gi, (t0, n, is_ctx) in enumerate(self.groups(4)):
            ncols = n * 128
            col0 = t0 * 128
            hT, bh = hTs[gi % 2], b_hT[gi % 2]
            for s in range(n):
                if is_ctx:
                    self.norm_tile(t0 + s, A1c, bA1c, S1c, bS1c, hT, bh, s * 128, (t0 + s) % 2)
                else:
                    self.norm_tile(t0 + s, A1x, bA1x, S1x, bS1x, hT, bh, s * 128, (t0 + s) % 2)
            for qk in range(2):
                for h in range(8):
                    i = cnt % 3
                    cnt += 1
                    P, bP = self.ps[2 + i], self.bps[2 + i]
                    col = qk * 512 + h * 64
                    c.mm(P[0:64, :ncols], [(Win[:, k, col:col + 64], hT[:, k, :ncols]) for k in range(8)], reads=[b_w, bh], writes=[bP])
                    c.op("act", lambda e: e.activation(out=stg[i][:, :ncols], in_=P[0:64, :ncols], func=AF.Copy, scale=(0.125 if qk == 1 else 1.0)), reads=[bP], writes=[b_stg[i]])
                    c.dma("sp", QK.ap()[qk, h, :, col0:col0 + ncols], stg[i][:, :ncols], reads=[b_stg[i]], writes=[b_QK])
            for s in range(n):
                sl = slice(s * 128, (s + 1) * 128)
                i2 = s % 2

                def tokproj(c0, w_):
                    nonlocal cnt
                    i = cnt % 3
                    cnt += 1
                    P, bP = self.ps[2 + i], self.bps[2 + i]
                    c.mm(P[:, :w_], [(hT[:, k, sl], Win[:, k, c0:c0 + w_]) for k in range(8)], reads=[b_w, bh], writes=[bP])
                    return P, bP
                P, bP = tokproj(512, 512)
                c.op("act", lambda e: e.activation(out=kst[i2][:], in_=P[:, :], func=AF.Copy, scale=0.125), reads=[bP], writes=[b_kst[i2]])
                c.dma("sp", Kt.ap()[t0 + s], kst[i2][:], reads=[b_kst[i2]], writes=[b_Kt])
                for vh in range(2):
                    P, bP = tokproj(1024 + vh * 512, 512)
                    c.op("act", lambda e: e.activation(out=vst[i2][:, vh * 4:(vh + 1) * 4, 0:128], in_=P[:, :].rearrange("p (h d) -> p h d", d=128), func=AF.Copy), reads=[bP], writes=[b_vst[i2]])
                c.dma("sp", Va.ap()[t0 + s], vst[i2][:], reads=[b_vst[i2]], writes=[b_Va])
                for oh in range(2):
                    P, bP = tokproj(2048 + oh * 512, 512)
                    c.op("act", lambda e: e.activation(out=ost[i2][:, oh * 512:(oh + 1) * 512], in_=P[:, :], func=AF.Sigmoid), reads=[bP], writes=[b_ost[i2]])
                c.dma("sp", Os.ap()[t0 + s], ost[i2][:], reads=[b_ost[i2]], writes=[b_Os])
                P, bP = tokproj(3072, 32)
                g_ = gst[i2]
                c.op("dve", lambda e: e.tensor_tensor(out=g_[:], in0=P[:, 0:32], in1=gb[:], op=ALU.add), reads=[bP, b_w], writes=[b_gst[i2]])
                for r in (1, 3):
                    cs_ = slice(r * 8, (r + 1) * 8)
                    c.op("act", lambda e: e.activation(out=g_[:, cs_], in_=g_[:, cs_], func=AF.Exp, scale=-1.0), reads=[b_gst[i2]], writes=[b_gst[i2]])
                    c.op("act", lambda e: e.activation(out=g_[:, cs_], in_=g_[:, cs_], func=AF.Ln, bias=1.0), reads=[b_gst[i2]], writes=[b_gst[i2]])
                    c.op("act", lambda e: e.activation(out=g_[:, cs_], in_=g_[:, cs_], func=AF.Copy, scale=-1.0), reads=[b_gst[i2]], writes=[b_gst[i2]])
                c.dma("sp", Gt.ap()[t0 + s], g_[:], reads=[b_gst[i2]], writes=[b_Gt])
        c.barrier()
        c.sb_release(self.mark0)
        sel = c.sb([8, 8, 128], F32, "sel8"); b_sel = Buf()
        c.dma("sp", sel[:], self.w["sel32"].ap()[0:8, 0:8, :], writes=[b_sel])
        qTs = [c.sb([64, 8, 128], BF16, "mqT") for _ in range(2)]; kTs = [c.sb([64, 8, 128], BF16, "mkT") for _ in range(2)]
        kts = [c.sb([128, 512], BF16, "mkt") for _ in range(2)]; vas = [c.sb([128, 8, 129], BF16, "mva") for _ in range(2)]
        gts = [c.sb([128, 32], F32, "mgt") for _ in range(2)]; b_ld = [Buf(), Buf()]
        GM = [c.sb([8, 12, 128], F32, "GM") for _ in range(2)]; b_GM = [Buf(), Buf()]
        sm8 = [c.sb([8, 8], F32, "sm8") for _ in range(2)]; b_sm8 = [Buf(), Buf()]
        ms = c.sb([8, 2], F32, "ms"); b_ms = Buf()
        dg = c.sb([8, 8], F32, "dg"); b_dg = Buf()
        tk = [c.sb([128, 40], F32, "tk") for _ in range(2)]; b_tk = [Buf(), Buf()]
        cwc = [c.sb([64, 8], F32, "cwc") for _ in range(2)]; b_cwc = [Buf(), Buf()]
        scm = [c.sb([128, 512], F32, "mscm") for _ in range(2)]; b_scm = [Buf() for _ in range(2)]
        aa = [c.sb([128, 512], F32, "maa") for _ in range(2)]; b_aa = [Buf() for _ in range(2)]
        EE = [c.sb([128, 512], F32, "mEE") for _ in range(2)]; b_EE = [Buf() for _ in range(2)]
        MT = [c.sb([128, 512], BF16, "mMT") for _ in range(2)]; b_MT = [Buf() for _ in range(2)]
        yi = [c.sb([128, 129], F32, "myi") for _ in range(4)]; b_yi = [Buf() for _ in range(4)]
        nd4 = [c.sb([128, 4, 132], F32, "mnd4") for _ in range(2)]; b_nd4 = [Buf() for _ in range(2)]
        Vw = [c.sb([128, 129], BF16, "mVw") for _ in range(4)]; b_Vw = [Buf() for _ in range(4)]
        Yt = [c.sb([128, 1024], F32, "mYt") for _ in range(2)]; b_Yt = [Buf(), Buf()]
        S32 = c.sb([64, 8, 129], F32, "mS32"); Sb = c.sb([64, 8, 129], BF16, "mSb"); b_S = [Buf() for _ in range(8)]
        ci = 0; hi = 0
        id8 = self.ident32[0:8, 0:8]
        for d in range(2):
            tri = self.trile32 if d == 0 else self.trige32
            order = list(range(NT)) if d == 0 else (list(range(NCT - 1, -1, -1)) + list(range(NT - 1, NCT - 1, -1)))
            c.op("dve", lambda e: e.memset(S32[:], 0.0), writes=b_S)
            c.op("pool", lambda e: e.memset(Sb[:], 0.0), writes=b_S)
            c.op("dve", lambda e: e.memset(ms[:], 0.0), writes=[b_ms])
            endc = 127 if d == 0 else 0
            def gate(ch, i):
                cols = slice(ch * 128, (ch + 1) * 128)
                bl = b_ld[i]
                c.dma("sp", qTs[i][:], QK.ap()[0, :, :, cols].rearrange("h d t -> d h t"), reads=[b_QK], writes=[bl])
                c.dma("sp", kTs[i][:], QK.ap()[1, :, :, cols].rearrange("h d t -> d h t"), reads=[b_QK], writes=[bl])
                c.dma("sp", kts[i][:], Kt.ap()[ch], reads=[b_Kt], writes=[bl])
                c.dma("sp", vas[i][:], Va.ap()[ch], reads=[b_Va], writes=[bl])
                c.dma("sp", gts[i][:], Gt.ap()[ch], reads=[b_Gt], writes=[bl])
                ig = gts[i][:, d * 16:d * 16 + 8]
                lf = gts[i][:, d * 16 + 8:d * 16 + 16]
                G, bG = GM[i], b_GM[i]
                s8, bs8 = sm8[i], b_sm8[i]
                t_, bt = tk[i], b_tk[i]
                p0, bp0 = self.ps[0], self.bps[0]
                c.mm(p0[0:8, 0:128], [(ig, self.ident32)], reads=[bl, self.b_const], writes=[bp0])
                c.mm(p0[0:8, 128:256], [(lf, tri)], reads=[bl, self.b_const], writes=[bp0])
                c.mm(p0[:, 256:264], [(tri, lf)], reads=[bl, self.b_const], writes=[bp0])
                c.op("act", lambda e: e.activation(out=G[:, 0:2, :], in_=p0[0:8, 0:256].rearrange("p (a t) -> p a t", a=2), func=AF.Copy), reads=[bp0], writes=[bG])
                c.op("act", lambda e: e.activation(out=t_[:, 32:40], in_=p0[:, 256:264], func=AF.Copy), reads=[bp0], writes=[bt])
                c.op("dve", lambda e: e.tensor_tensor(out=t_[:, 0:8], in0=ig, in1=t_[:, 32:40], op=ALU.subtract), reads=[bl, bt], writes=[bt])
                c.op("dve", lambda e: e.tensor_tensor(out=G[:, 2, :], in0=G[:, 0, :], in1=G[:, 1, :], op=ALU.subtract), reads=[bG], writes=[bG])
                src, dst = 2, 3
                for k in range(7):
                    sft = 1 << k
                    c.op("dve", lambda e: e.tensor_copy(out=G[:, dst, :], in_=G[:, src, :]), reads=[bG], writes=[bG])
                    if d == 0:
                        c.op("dve", lambda e: e.tensor_tensor(out=G[:, dst, sft:128], in0=G[:, src, sft:128], in1=G[:, src, 0:128 - sft], op=ALU.max), reads=[bG], writes=[bG])
                    else:
                        c.op("dve", lambda e: e.tensor_tensor(out=G[:, dst, 0:128 - sft], in0=G[:, src, 0:128 - sft], in1=G[:, src, sft:128], op=ALU.max), reads=[bG], writes=[bG])
                    src, dst = dst, (3 if dst == 4 else 4)
                cmr = src
                c.op("dve", lambda e: e.tensor_scalar(out=G[:, cmr, :], in0=G[:, cmr, :], scalar1=ms[:, 0:1], scalar2=None, op0=ALU.max), reads=[bG, b_ms], writes=[bG])
                c.op("act", lambda e: e.activation(out=G[:, 5, :], in_=G[:, cmr, :], func=AF.Copy, scale=-1.0), reads=[bG], writes=[bG])
                c.op("act", lambda e: e.activation(out=G[:, 6, :], in_=G[:, cmr, :], func=AF.Exp, scale=-1.0, bias=ms[:, 0:1]), reads=[bG, b_ms], writes=[bG])
                c.op("dve", lambda e: e.tensor_tensor(out=G[:, 7, :], in0=G[:, 1, :], in1=G[:, cmr, :], op=ALU.add), reads=[bG], writes=[bG])
                c.op("act", lambda e: e.activation(out=G[:, 7, :], in_=G[:, 7, :], func=AF.Exp, scale=-1.0), reads=[bG], writes=[bG])
                c.op("dve", lambda e: e.tensor_copy(out=s8[:, 0:1], in_=G[:, cmr, endc:endc + 1]), reads=[bG], writes=[bs8])
                c.op("dve", lambda e: e.tensor_scalar(out=s8[:, 1:2], in0=s8[:, 0:1], scalar1=-1.0, scalar2=None, op0=ALU.mult), reads=[bs8], writes=[bs8])
                c.op("dve", lambda e: e.tensor_copy(out=s8[:, 2:3], in_=G[:, 1, endc:endc + 1]), reads=[bG], writes=[bs8])
                c.op("act", lambda e: e.activation(out=G[:, 8, :], in_=G[:, 2, :], func=AF.Exp, bias=s8[:, 1:2]), reads=[bG, bs8], writes=[bG])
                c.op("act", lambda e: e.activation(out=s8[:, 3:4], in_=ms[:, 0:1], func=AF.Exp, bias=s8[:, 1:2]), reads=[b_ms, bs8], writes=[bs8])
                c.op("dve", lambda e: e.tensor_tensor(out=ms[:, 0:1], in0=s8[:, 2:3], in1=s8[:, 0:1], op=ALU.add), reads=[bs8, bG], writes=[b_ms])
                p1, bp1 = self.ps[1], self.bps[1]
                c.mm_multi([(p1[:, (r - 6) * 8:(r - 5) * 8], [(G[:, r, :], id8)]) for r in (6, 7, 8)], reads=[bG, self.b_const], writes=[bp1])
                c.op("act", lambda e: e.activation(out=t_[:, 8:32], in_=p1[:, 0:24], func=AF.Copy), reads=[bp1], writes=[bt])
                c.op("dve", lambda e: e.tensor_scalar(out=dg[:], in0=id8, scalar1=s8[:, 3:4], scalar2=None, op0=ALU.mult), reads=[self.b_const, bs8], writes=[b_dg])
                c.mm(p1[0:64, 32:40], [(self.ones32[0:8, 0:64], dg[:])], reads=[self.b_const, b_dg], writes=[bp1])
                c.op("act", lambda e: e.activation(out=cwc[i][:], in_=p1[0:64, 32:40], func=AF.Copy), reads=[bp1], writes=[b_cwc[i]])
            def heads(ch, i):
                bl = b_ld[i]; G, bG = GM[i], b_GM[i]; t_, bt = tk[i], b_tk[i]
                Y, bY = Yt[i], b_Yt[i]
                for hb0 in (0, 4):
                    psc, bpsc = self.ps[2], self.bps[2]
                    pbc, bpbc = self.ps[6], self.bps[6]
                    for q4 in range(4):
                        h = hb0 + q4
                        cs4 = slice(q4 * 128, (q4 + 1) * 128)
                        c.mm(psc[:, cs4], [(kTs[i][:, h, :], qTs[i][:, h, :])], reads=[bl], writes=[bpsc])
                        c.mm(pbc[:, cs4], [(sel[:, h, :], G[:, 5, :]), (G[:, 2, :], sel[:, h, :])], reads=[b_sel, bG], writes=[bpbc])
                    pins = []
                    for q4 in range(4):
                        h = hb0 + q4
                        bk = 3 if q4 < 2 else 7
                        pin_ap = self.ps[bk][:, (q4 % 2) * 256:(q4 % 2) * 256 + 129]
                        c.mm(pin_ap, [(qTs[i][:, h, :], Sb[:, h, :])], reads=[bl, b_S[h]], writes=[self.bps[bk]])
                        pins.append((pin_ap, self.bps[bk]))
                    k4 = (hb0 // 4)
                    c.op("dve", lambda e: e.tensor_tensor(out=scm[k4][:].rearrange("p (h t) -> p h t", h=4), in0=psc[:, :].rearrange("p (h t) -> p h t", h=4),
                                                          in1=tri.unsqueeze(1).to_broadcast([128, 4, 128]), op=ALU.mult), reads=[bpsc, self.b_const], writes=[b_scm[k4]])
                    c.op("dve", lambda e: e.tensor_scalar(out=aa[k4][:], in0=pbc[:, :], scalar1=0.0, scalar2=None, op0=ALU.min), reads=[bpbc], writes=[b_aa[k4]])
                    c.op("act", lambda e: e.activation(out=EE[k4][:], in_=aa[k4][:], func=AF.Exp), reads=[b_aa[k4]], writes=[b_EE[k4]])
                    c.op("pool", lambda e: e.tensor_tensor(out=MT[k4][:], in0=scm[k4][:], in1=EE[k4][:], op=ALU.mult), reads=[b_scm[k4], b_EE[k4]], writes=[b_MT[k4]])
                    for q4 in range(4):
                        h = hb0 + q4
                        pin_ap, bpin = pins[q4]
                        c.op("act", lambda e: e.activation(out=yi[q4][:], in_=pin_ap, func=AF.Copy, scale=t_[:, 8 + h:9 + h]), reads=[bpin, bt], writes=[b_yi[q4]])
                        c.op("pool", lambda e: e.tensor_tensor(out=Vw[q4][:], in0=vas[i][:, h, :], in1=t_[:, 24 + h:25 + h].to_broadcast([128, 129]), op=ALU.mult), reads=[bl, bt], writes=[b_Vw[q4]])
                    pnds = []; psts = []
                    for q4 in range(4):
                        h = hb0 + q4
                        bk = 4 + q4 // 2
                        pnd_ap = self.ps[bk][:, (q4 % 2) * 256:(q4 % 2) * 256 + 129]
                        c.mm(pnd_ap, [(MT[k4][:, q4 * 128:(q4 + 1) * 128], vas[i][:, h, :])], reads=[b_MT[k4], bl], writes=[self.bps[bk]])
                        pnds.append((pnd_ap, self.bps[bk]))
                    for q4 in range(4):
                        h = hb0 + q4
                        if q4 < 3:
                            pst_ap, bpst = self.ps[1][0:64, q4 * 129:(q4 + 1) * 129], self.bps[1]
                        else:
                            pst_ap, bpst = self.ps[0][0:64, 264:393], self.bps[0]
                        c.mm(pst_ap, [(kts[i][:, h * 64:(h + 1) * 64], Vw[q4][:])], reads=[bl, b_Vw[q4]], writes=[bpst])
                        psts.append((pst_ap, bpst))
                    n4 = nd4[k4]; bn = b_nd4[k4]
                    for q4 in range(4):
                        pnd_ap, bpnd = pnds[q4]
                        c.op("dve", lambda e: e.tensor_tensor(out=n4[:, q4, 0:129], in0=pnd_ap, in1=yi[q4][:], op=ALU.add), reads=[bpnd, b_yi[q4]], writes=[bn])
                    c.op("dve", lambda e: e.tensor_scalar(out=n4[:, :, 129:130], in0=n4[:, :, 128:129], scalar1=-1.0, scalar2=None, op0=ALU.mult), reads=[bn], writes=[bn])
                    c.op("dve", lambda e: e.tensor_tensor(out=n4[:, :, 130:131], in0=n4[:, :, 128:129], in1=n4[:, :, 129:130], op=ALU.max), reads=[bn], writes=[bn])
                    c.op("dve", lambda e: e.tensor_tensor(out=n4[:, :, 130:131], in0=n4[:, :, 130:131], in1=t_[:, 16 + hb0:20 + hb0].unsqueeze(2), op=ALU.max), reads=[bn, bt], writes=[bn])
                    c.op("dve", lambda e: e.reciprocal(out=n4[:, :, 131:132], in_=n4[:, :, 130:131]), reads=[bn], writes=[bn])
                    c.op("dve", lambda e: e.tensor_tensor(out=Y[:, hb0 * 128:(hb0 + 4) * 128].rearrange("p (h d) -> p h d", h=4), in0=n4[:, :, 0:128],
                                                          in1=n4[:, :, 131:132].to_broadcast([128, 4, 128]), op=ALU.mult), reads=[bn], writes=[bY])
                    for q4 in range(4):
                        h = hb0 + q4
                        pst_ap, bpst = psts[q4]
                        c.op("dve", lambda e: e.scalar_tensor_tensor(out=S32[:, h, :], in0=S32[:, h, :], scalar=cwc[i][:, h:h + 1], in1=pst_ap, op0=ALU.mult, op1=ALU.add), reads=[b_cwc[i], bpst], writes=[b_S[h]])
                        c.op("act", lambda e: e.activation(out=Sb[:, h, :], in_=S32[:, h, :], func=AF.Copy), reads=[], writes=[b_S[h]])
                c.dma("sp", Yd[d].ap()[ch], Y[:], reads=[bY], writes=[b_Y[d]])
            gate(order[0], 0)
            for idx, ch in enumerate(order):
                if idx + 1 < len(order):
                    gate(order[idx + 1], (idx + 1) % 2)
                heads(ch, idx % 2)
        c.barrier()
        c.sb_release(self.mark0)
        Wo = c.sb([128, 8, 1024], BF16, "mlWo"); b_wo = Buf()
        c.dma("pool", Wo[:], self.w["mlstm_w_out"].ap()[j].rearrange("(k p) n -> p k n", p=128), writes=[b_wo])
        ng = c.sb([128, 1024], F32, "mlng")
        c.dma("sp", ng[:], self.w["mlstm_norm_g"].ap()[j:j + 1, :].partition_broadcast(128), writes=[b_wo])
        self.gated_out(li, need_ctx, Yd, b_Y, Os, b_Os, 1024, 8, ng, Wo, b_wo, True)
        self.phase_end()

    def build(self, wshapes):
        self.inp("x", [self.NL, D])
        self.inp("ctx", [self.NCX, D])
        self.inp("cT", [128, 8, 2])
        self.inp("cmat", [128, 4, 128])
        self.inp("sel32", [32, 32, 128])
        self.inp("rope64", [2, 64, self.NL])
        self.inp("rope32", [2, 32, self.NL])
        self.inp("rope96", [2, 96, self.NL])
        self.inp("ssm_conv_wT", [wshapes["ssm_conv_w"][0], 128, 24, 5])
        self.inp("ssm_conv_bT", [wshapes["ssm_conv_w"][0], 128, 24])
        for k, s in wshapes.items():
            self.inp(k, s)
        self.out = self.nc.dram_tensor("out", [self.NL, D], F32, kind="ExternalOutput")
        self.setup_consts()
        self.prologue()
        cnt = {0: 0, 1: 0, 2: 0, 3: 0, 9: 0}
        for li, kind in enumerate(self.kinds):
            need_ctx = li < self.depth - 1
            j = cnt[kind]
            cnt[kind] += 1
            self.want_precast = li
            if kind == 0:
                self.layer_gqa(li, j, need_ctx)
            elif kind == 1:
                self.layer_ssd(li, j, need_ctx)
            elif kind == 2:
                self.layer_mlstm(li, j, need_ctx)
            elif kind == 3:
                self.layer_mla(li, j, need_ctx)
            self.layer_moe(li, need_ctx)
        self.final()
        return self.nc


WEIGHT_KEYS = ["norm1_g", "norm2_g", "w_mod", "b_mod", "moe_w_group", "moe_b_group", "moe_w_expert", "moe_b_expert",
               "moe_w_gate", "moe_w_up", "moe_w_down", "attn_w_in", "attn_sink", "attn_w_out",
               "ssm_w_in", "ssm_conv_w", "ssm_conv_b", "ssm_dt_bias", "ssm_a_log", "ssm_d", "ssm_norm_g", "ssm_w_out",
               "mlstm_w_in", "mlstm_gate_b", "mlstm_norm_g", "mlstm_w_out",
               "mla_w_in", "mla_q_norm_g", "mla_w_q_up", "mla_kv_norm_g", "mla_w_kv_up", "mla_w_out", "final_norm_g"]


def run_model(inputs, kinds, n_cores=None):
    x = np.asarray(inputs["x"], np.float32)
    ctx = np.asarray(inputs["ctx"], np.float32)
    c = np.asarray(inputs["c"], np.float32)
    c_ctx = np.asarray(inputs["c_ctx"], np.float32)
    B, n_lat, _ = x.shape
    n_ctx = ctx.shape[1]
    weights = {k: np.ascontiguousarray(np.asarray(inputs[k], np.float32)) for k in WEIGHT_KEYS}
    m = Model(n_lat, n_ctx, kinds)
    nc = m.build({k: v.shape for k, v in weights.items()})
    consts = host_consts(n_lat)
    in_maps = []
    for b in range(B):
        cT = np.stack([c[b].reshape(8, 128).T, c_ctx.reshape(8, 128).T], axis=-1)
        d = {"x": np.ascontiguousarray(x[b]), "ctx": np.ascontiguousarray(ctx[b]), "cT": np.ascontiguousarray(cT.astype(np.float32))}
        d.update(consts)
        d.update(weights)
        d["ssm_conv_wT"] = np.ascontiguousarray(weights["ssm_conv_w"].reshape(-1, 5, 24, 128).transpose(0, 3, 2, 1))
        d["ssm_conv_bT"] = np.ascontiguousarray(weights["ssm_conv_b"].reshape(-1, 24, 128).transpose(0, 2, 1))
        in_maps.append(d)
    res = run_bass_kernel_spmd(nc, in_maps, core_ids=list(range(B)))
    return np.stack([np.asarray(r["out"], np.float32) for r in res.results], axis=0)


def kernel(**inputs):
    return run_model(inputs, [0, 1, 2, 3])
```

```python
import numpy as np
import concourse.bass as bass
import concourse.mybir as mybir

F32 = mybir.dt.float32
BF16 = mybir.dt.bfloat16
AF = mybir.ActivationFunctionType
ALU = mybir.AluOpType
AX = mybir.AxisListType


class Buf:
    __slots__ = ("w", "r", "name")

    def __init__(self, name=""):
        self.w = None
        self.r = []
        self.name = name


class Ctx:
    EPOCH = 30000

    def __init__(self, nc, n_dma=None):
        self.nc = nc
        self.E = {"pe": nc.tensor, "act": nc.scalar, "dve": nc.vector, "pool": nc.gpsimd, "sp": nc.sync}
        self.csem = {}
        self.seen = {e: {} for e in self.E}
        self.semid = {}
        n_dma = n_dma or {"sp": 48, "act": 2, "pool": 30}
        self.dslots = {}
        self.drr = {}
        for q, n in n_dma.items():
            self.dslots[q] = [[self._new_sem(f"d{q}{i}"), 0] for i in range(n)]
            self.drr[q] = 0
        for e in ("pe", "act", "dve", "pool"):
            self.csem[e] = [self._new_sem(f"c{e}0"), 0, 0]
        self.sb_off = 0
        self.sb_base = 16512
        self.sb_cap = 229344 - 16512
        self.n_alloc = 0
        self.n_ins = 0
        self.n_wait = 0

    def _new_sem(self, name):
        s = self.nc.alloc_semaphore(name)
        self.semid[id(s)] = s
        return s

    def sb_mark(self):
        return self.sb_off

    def sb_release(self, mark):
        self.sb_off = mark

    def sb(self, shape, dtype, name=None):
        esz = 4 if dtype == F32 else 2
        if dtype in (mybir.dt.int32, mybir.dt.uint32):
            esz = 4
        n = 1
        for s in shape[1:]:
            n *= s
        nbytes = (n * esz + 63) // 64 * 64
        off = self.sb_off
        if off + nbytes > self.sb_cap:
            raise RuntimeError(f"SBUF overflow: want {nbytes} at {off} cap {self.sb_cap} ({name})")
        self.sb_off += nbytes
        self.n_alloc += 1
        t = self.nc.alloc_sbuf_tensor_at(f"{name or 't'}_{self.n_alloc}", list(shape), dtype, offset=self._abs(off))
        return t

    def _abs(self, off):
        return self.sb_base + off

    def _wait(self, eng, ev):
        if ev is None:
            return
        sem, val = ev
        k = id(sem)
        if self.seen[eng].get(k, 0) >= val:
            return
        self.E[eng].wait_ge(sem, val)
        self.n_wait += 1
        self.seen[eng][k] = val

    def _deps(self, eng, reads, writes):
        for b in reads:
            if b.w is not None and not (eng == "pe" and b.w[2] == "pe"):
                self._wait(eng, b.w[:2])
        for b in writes:
            if b.w is not None and not (eng == "pe" and b.w[2] == "pe"):
                self._wait(eng, b.w[:2])
            for r in b.r:
                if not (eng == "pe" and r[2] == "pe"):
                    self._wait(eng, r[:2])

    def _record(self, ev, reads, writes):
        for b in reads:
            b.r.append(ev)
            if len(b.r) > 64:
                b.r = b.r[-64:] if False else b.r
        for b in writes:
            b.w = ev
            b.r = []

    def _signal(self, eng, ins):
        st = self.csem[eng]
        if st[1] >= self.EPOCH:
            st = self.csem[eng] = [self._new_sem(f"c{eng}{st[2] + 1}"), 0, st[2] + 1]
        st[1] += 1
        ins.then_inc(st[0], 1)
        return (st[0], st[1], eng)

    def op(self, eng, fn, reads=(), writes=()):
        self._deps(eng, reads, writes)
        ins = fn(self.E[eng])
        self.n_ins += 1
        ev = self._signal(eng, ins)
        self._record(ev, reads, writes)
        return ev

    def mm(self, out, pairs, reads=(), writes=(), start=True, stop=True):
        self._deps("pe", reads, writes)
        n = len(pairs)
        ins = None
        for i, (l, r) in enumerate(pairs):
            ins = self.nc.tensor.matmul(out, l, r, start=(start and i == 0), stop=(stop and i == n - 1))
            self.n_ins += 1
        ev = self._signal("pe", ins)
        self._record(ev, reads, writes)
        return ev

    def mm_multi(self, groups, reads=(), writes=()):
        self._deps("pe", reads, writes)
        ins = None
        for out, pairs in groups:
            n = len(pairs)
            for i, (l, r) in enumerate(pairs):
                ins = self.nc.tensor.matmul(out, l, r, start=(i == 0), stop=(i == n - 1))
                self.n_ins += 1
        ev = self._signal("pe", ins)
        self._record(ev, reads, writes)
        return ev

    def tr(self, out, in_, ident, reads=(), writes=()):
        self._deps("pe", reads, writes)
        ins = self.nc.tensor.transpose(out, in_, ident)
        self.n_ins += 1
        ev = self._signal("pe", ins)
        self._record(ev, reads, writes)
        return ev

    def tr_multi(self, items, reads=(), writes=()):
        self._deps("pe", reads, writes)
        ins = None
        for out, in_, ident in items:
            ins = self.nc.tensor.transpose(out, in_, ident)
            self.n_ins += 1
        ev = self._signal("pe", ins)
        self._record(ev, reads, writes)
        return ev

    def dma(self, q, out, in_, reads=(), writes=()):
        self._deps(q, reads, writes)
        slots = self.dslots[q]
        i = self.drr[q]
        self.drr[q] = (i + 1) % len(slots)
        sl = slots[i]
        if sl[1] > 0:
            self._wait(q, (sl[0], sl[1]))
        ins = self.E[q].dma_start(out=out, in_=in_)
        self.n_ins += 1
        sl[1] += 16
        ins.then_inc(sl[0], 16)
        ev = (sl[0], sl[1], "dma")
        self._record(ev, reads, writes)
        return ev

    def barrier(self):
        evs = []
        for e, st in self.csem.items():
            if st[1] > 0:
                evs.append((st[0], st[1]))
        for q, slots in self.dslots.items():
            for sl in slots:
                if sl[1] > 0:
                    evs.append((sl[0], sl[1]))
        for eng in self.E:
            for ev in evs:
                self._wait(eng, ev)

    def finish(self, eng="sp"):
        evs = []
        for e, st in self.csem.items():
            if st[1] > 0:
                evs.append((st[0], st[1]))
        for q, slots in self.dslots.items():
            for sl in slots:
                if sl[1] > 0:
                    evs.append((sl[0], sl[1]))
        for ev in evs:
            self._wait(eng, ev)
from concourse.bass_utils import run_bass_kernel_spmd
D = 1024
EPS = 1e-6


def host_consts(n_lat):
    ident = np.eye(128, dtype=np.float32)
    ones = np.ones((128, 128), np.float32)
    j = np.arange(128)[:, None]
    i = np.arange(128)[None, :]
    tri_le = (j <= i).astype(np.float32)
    tri_ge = (j >= i).astype(np.float32)
    cm = np.stack([ident, ones, tri_le, tri_ge], axis=1)
    sel = np.zeros((32, 32, 128), np.float32)
    for h in range(32):
        sel[h, h, :] = 1.0
    rows = n_lat // 64
    row = np.repeat(np.arange(rows), 64).astype(np.float32)
    col = np.tile(np.arange(64), rows).astype(np.float32)

    def tab(rot_dim, nrep):
        q = rot_dim // 4
        inv = (10000.0 ** (-np.arange(q, dtype=np.float32) / q)).astype(np.float32)
        ang = np.concatenate([row[:, None] * inv, col[:, None] * inv], axis=-1).astype(np.float32)
        cs = np.cos(ang).astype(np.float32).T
        sn = np.sin(ang).astype(np.float32).T
        cs = np.concatenate([cs] * (2 * nrep), axis=0)
        sn = np.concatenate([sn] * (2 * nrep), axis=0)
        return np.ascontiguousarray(np.stack([cs, sn], axis=0))

    r32 = tab(32, 1)
    L = r32.shape[2]
    r96 = np.ascontiguousarray(np.concatenate([np.stack([np.ones((64, L), np.float32), np.zeros((64, L), np.float32)], axis=0), r32], axis=1))
    return {"cmat": np.ascontiguousarray(cm), "sel32": sel, "rope64": tab(64, 1), "rope32": r32, "rope96": r96}


class Model:
    def __init__(self, n_lat, n_ctx, kinds, debug=False):
        self.NL, self.NCX = n_lat, n_ctx
        self.T = n_lat + n_ctx
        self.NT = self.T // 128
        self.NCT = n_ctx // 128
        self.NLT = n_lat // 128
        self.kinds = kinds
        self.depth = len(kinds)
        self.nc = bass.Bass("TRN2", target_bir_lowering=False)
        self.c = Ctx(self.nc)
        self.w = {}

    def inp(self, name, shape, dtype=F32):
        t = self.nc.dram_tensor(name, list(shape), dtype, kind="ExternalInput")
        self.w[name] = t
        return t

    def scratch(self, name, shape, dtype):
        return self.nc.dram_tensor(name, list(shape), dtype)

    def declare(self, shapes):
        for k, s in shapes.items():
            self.inp(k, s)

    def groups(self, gsz, with_ctx=True):
        g = []
        if with_ctx:
            g.append((0, self.NCT, True))
        t = self.NCT
        while t < self.NT:
            n = min(gsz, self.NT - t)
            g.append((t, n, False))
            t += n
        return g

    def setup_consts(self):
        c, nc = self.c, self.nc
        self.cm32 = c.sb([128, 4, 128], F32, "cm32")
        self.cmb = c.sb([128, 4, 128], BF16, "cmb")
        self.b_const = Buf("const")
        c.dma("sp", self.cm32[:], self.w["cmat"].ap(), writes=[self.b_const])
        c.op("dve", lambda e: e.tensor_copy(out=self.cmb[:], in_=self.cm32[:]), reads=[self.b_const], writes=[self.b_const])
        self.ident32 = self.cm32[:, 0, :]
        self.ones32 = self.cm32[:, 1, :]
        self.trile32 = self.cm32[:, 2, :]
        self.trige32 = self.cm32[:, 3, :]
        self.identb = self.cmb[:, 0, :]
        self.onesb = self.cmb[:, 1, :]
        self.trileb = self.cmb[:, 2, :]
        self.trigeb = self.cmb[:, 3, :]
        self.ps = [nc.alloc_psum_tensor(f"psb{i}", [128, 512], F32) for i in range(8)]
        self.bps = [Buf(f"ps{i}") for i in range(8)]
        self.mark0 = c.sb_mark()

    def phase_end(self):
        self.c.barrier()
        self.c.sb_release(self.mark0)

    def prologue(self):
        c, nc = self.c, self.nc
        L = self.depth
        self.modv = self.scratch("modv", [L, 2, 6 * D], F32)
        self.xs = self.scratch("xs", [self.T, D], F32)
        cs = c.sb([128, 8, 2], F32, "cs")
        b_cs = Buf()
        c.dma("sp", cs[:], self.w["cT"].ap(), writes=[b_cs])
        c.op("act", lambda e: e.activation(out=cs[:], in_=cs[:], func=AF.Silu), reads=[b_cs], writes=[b_cs])
        b_xs = self.b_xs = [Buf(f"xs{t}") for t in range(self.NT)]
        c.dma("sp", self.xs.ap()[0:self.NCX, :], self.w["ctx"].ap(), writes=b_xs[0:self.NCT])
        c.dma("sp", self.xs.ap()[self.NCX:self.T, :], self.w["x"].ap(), writes=b_xs[self.NCT:])
        wm = [c.sb([128, 8, 512], F32, f"wm{i}") for i in range(2)]
        b_wm = [Buf(), Buf()]
        modsb = c.sb([2, 6 * D], F32, "modsb")
        b_mod = Buf()
        bmb = c.sb([2, 6 * D], F32, "bmb")
        gb = c.sb([2, 2, D], F32, "gb")
        b_misc = Buf()
        self.b_modv = [Buf(f"modv{i}") for i in range(L)]
        it = 0
        for li in range(L):
            c.dma("sp", bmb[:], self.w["b_mod"].ap()[li:li + 1, :].partition_broadcast(2), writes=[b_misc])
            c.dma("sp", gb[:, 0, :], self.w["norm1_g"].ap()[li:li + 1, :].partition_broadcast(2), writes=[b_misc])
            c.dma("sp", gb[:, 1, :], self.w["norm2_g"].ap()[li:li + 1, :].partition_broadcast(2), writes=[b_misc])
            for j in range(12):
                s = it % 2
                it += 1
                c.dma("sp", wm[s][:], self.w["w_mod"].ap()[li, :, j * 512:(j + 1) * 512].rearrange("(k p) n -> p k n", p=128), writes=[b_wm[s]])
                pb = it % 2
                c.mm(self.ps[pb][0:2, :], [(cs[:, k, :], wm[s][:, k, :]) for k in range(8)], reads=[b_cs, b_wm[s]], writes=[self.bps[pb]])
                c.op("dve", lambda e: e.tensor_tensor(out=modsb[:, j * 512:(j + 1) * 512], in0=self.ps[pb][0:2, :], in1=bmb[:, j * 512:(j + 1) * 512], op=ALU.add),
                     reads=[self.bps[pb], b_misc], writes=[b_mod])
            for (ch, gi) in ((1, 0), (4, 1)):
                c.op("dve", lambda e: e.scalar_tensor_tensor(out=modsb[:, ch * D:(ch + 1) * D], in0=modsb[:, ch * D:(ch + 1) * D], scalar=1.0, in1=gb[:, gi, :], op0=ALU.add, op1=ALU.mult),
                     reads=[b_mod, b_misc], writes=[b_mod])
            c.dma("sp", self.modv.ap()[li], modsb[:], reads=[b_mod], writes=[self.b_modv[li]])
        self.phase_end()

    def load_mod(self, li, chunk, row, name):
        c = self.c
        t = c.sb([128, D], F32, name)
        b = Buf(name)
        c.dma("sp", t[:], self.modv.ap()[li, row:row + 1, chunk * D:(chunk + 1) * D].partition_broadcast(128), reads=[self.b_modv[li]], writes=[b])
        return t, b

    def alloc_norm_bufs(self, nbuf=2):
        c = self.c
        if getattr(self, "want_precast", None) is not None:
            li_ = self.want_precast
            self.want_precast = None
            self.moe_precast(li_)
        self.nb = []
        for i in range(nbuf):
            d = dict(x=c.sb([128, D], F32, "nx"), bx=Buf(), junk=c.sb([128, D], BF16, "nj"), bj=Buf(),
                     st=c.sb([128, 2], F32, "nst"), bst=Buf(), tmp=c.sb([128, D], F32, "ntmp"), btmp=Buf(),
                     h=c.sb([128, D], BF16, "nh"), bh=Buf())
            self.nb.append(d)
        self.nbi = 0

    def norm_tile(self, tile, A, bA, B, bB, hT, b_hT, col0, psb, want_x=False):
        c = self.c
        d = self.nb[self.nbi % len(self.nb)]
        self.nbi += 1
        c.dma("sp", d["x"][:], self.xs.ap()[tile * 128:(tile + 1) * 128, :], reads=[self.b_xs[tile]], writes=[d["bx"]])
        c.op("dve", lambda e: e.memset(d["st"][:], 0.0), writes=[d["bst"]])
        c.op("act", lambda e: e.activation(out=d["junk"][:], in_=d["x"][:], func=AF.Square, accum_out=d["st"][:, 0:1]), reads=[d["bx"]], writes=[d["bj"], d["bst"]])
        c.op("dve", lambda e: e.tensor_scalar(out=d["st"][:, 1:2], in0=d["st"][:, 0:1], scalar1=1.0 / D, scalar2=EPS, op0=ALU.mult, op1=ALU.add), reads=[d["bst"]], writes=[d["bst"]])
        c.op("act", lambda e: e.activation(out=d["st"][:, 1:2], in_=d["st"][:, 1:2], func=AF.Sqrt), reads=[d["bst"]], writes=[d["bst"]])
        c.op("dve", lambda e: e.reciprocal(out=d["st"][:, 1:2], in_=d["st"][:, 1:2]), reads=[d["bst"]], writes=[d["bst"]])
        c.op("dve", lambda e: e.scalar_tensor_tensor(out=d["tmp"][:], in0=d["x"][:], scalar=d["st"][:, 1:2], in1=A[:], op0=ALU.mult, op1=ALU.mult),
             reads=[d["bx"], d["bst"], bA], writes=[d["btmp"]])
        c.op("pool", lambda e: e.tensor_tensor(out=d["h"][:], in0=d["tmp"][:], in1=B[:], op=ALU.add), reads=[d["btmp"], bB], writes=[d["bh"]])
        pT = self.ps[psb].ap().bitcast(BF16)
        c.tr_multi([(pT[:, k * 128:(k + 1) * 128], d["h"][:, k * 128:(k + 1) * 128], self.identb) for k in range(8)],
                   reads=[d["bh"], self.b_const], writes=[self.bps[psb]])
        c.op("act", lambda e: e.activation(out=hT[:, :, col0:col0 + 128], in_=pT.rearrange("p (k n) -> p k n", k=8), func=AF.Copy),
             reads=[self.bps[psb]], writes=[b_hT])
        return d

    def layer_gqa(self, li, j, need_ctx):
        c, nc = self.c, self.nc
        T, NT, NCT = self.T, self.NT, self.NCT
        w_in = self.w["attn_w_in"].ap()[j]
        w_out = self.w["attn_w_out"].ap()[j]
        QT = self.scratch(f"gqa_qt{li}", [4, NT, 64, 4, 128], BF16)
        b_QT = [Buf() for _ in range(NT)]
        b_w = Buf("gqa_w")
        Wq = c.sb([128, 8, 1024], BF16, "Wq")
        Wqr = c.sb([128, 8, 1024], BF16, "Wqr")
        Wk = c.sb([128, 8, 256], BF16, "Wk")
        Wkr = c.sb([128, 8, 256], BF16, "Wkr")
        Wv = c.sb([128, 8, 256], BF16, "Wv")
        c.dma("pool", Wq[:], w_in[:, 0:1024].rearrange("(k p) n -> p k n", p=128), writes=[b_w])
        c.dma("pool", Wk[:], w_in[:, 1024:1280].rearrange("(k p) n -> p k n", p=128), writes=[b_w])
        c.dma("pool", Wv[:], w_in[:, 1280:1536].rearrange("(k p) n -> p k n", p=128), writes=[b_w])
        b_wr = Buf("gqa_wr")
        for (W, Wr, nh) in ((Wq, Wqr, 16), (Wk, Wkr, 4)):
            for k in range(8):
                src = W[:, k, :].rearrange("p (h two i) -> p h two i", two=2, i=32)
                dst = Wr[:, k, :].rearrange("p (h two i) -> p h two i", two=2, i=32)
                c.op("act", lambda e: e.activation(out=dst[:, :, 0, :], in_=src[:, :, 1, :], func=AF.Copy, scale=-1.0), reads=[b_w], writes=[b_wr])
                c.op("dve", lambda e: e.tensor_copy(out=dst[:, :, 1, :], in_=src[:, :, 0, :]), reads=[b_w], writes=[b_wr])
        KT = c.sb([64, 4, T], BF16, "KT")
        b_KT = Buf("KT")
        Vs = c.sb([128, NT, 4, 65], BF16, "Vs")
        b_V = Buf("Vs")
        c.op("pool", lambda e: e.memset(Vs[:], 1.0), writes=[b_V])
        mark = c.sb_mark()
        A1x, bA1x = self.load_mod(li, 1, 0, "A1x")
        S1x, bS1x = self.load_mod(li, 0, 0, "S1x")
        A1c, bA1c = self.load_mod(li, 1, 1, "A1c")
        S1c, bS1c = self.load_mod(li, 0, 1, "S1c")
        self.alloc_norm_bufs(2)
        hTs = [c.sb([128, 8, 512], BF16, "hT") for _ in range(2)]
        b_hT = [Buf(), Buf()]
        rts = [c.sb([64, 2, 512], F32, "rt") for _ in range(2)]
        b_rt = [Buf(), Buf()]
        qst = [c.sb([64, 4, 512], BF16, "qst") for _ in range(2)]
        b_qst = [Buf(), Buf()]
        t1s = [c.sb([64, 512], F32, "t1") for _ in range(2)]
        t2s = [c.sb([64, 512], F32, "t2") for _ in range(2)]
        b_t1 = [Buf(), Buf()]
        b_t2 = [Buf(), Buf()]
        cnt = 0
        qcnt = 0
        for gi, (t0, n, is_ctx) in enumerate(self.groups(4)):
            ncols = n * 128
            hT, bh = hTs[gi % 2], b_hT[gi % 2]
            rt, brt = rts[gi % 2], b_rt[gi % 2]
            for s in range(n):
                if is_ctx:
                    self.norm_tile(t0 + s, A1c, bA1c, S1c, bS1c, hT, bh, s * 128, (t0 + s) % 2)
                else:
                    self.norm_tile(t0 + s, A1x, bA1x, S1x, bS1x, hT, bh, s * 128, (t0 + s) % 2)
            if not is_ctx:
                l0 = (t0 - NCT) * 128
                c.dma("sp", rt[:, :, :ncols], self.w["rope64"].ap()[:, :, l0:l0 + ncols].rearrange("two d l -> d two l"), writes=[brt])

            def proj_head(W, Wr, col, dst_ap, dst_buf):
                nonlocal cnt
                i = cnt % 2
                cnt += 1
                P1, bP1 = self.ps[2 + i], self.bps[2 + i]
                P2, bP2 = self.ps[4 + i], self.bps[4 + i]
                c.mm(P1[0:64, :ncols], [(W[:, k, col:col + 64], hT[:, k, :ncols]) for k in range(8)], reads=[b_w, bh], writes=[bP1])
                if is_ctx:
                    c.op("act", lambda e: e.activation(out=dst_ap, in_=P1[0:64, :ncols], func=AF.Copy), reads=[bP1], writes=[dst_buf])
                else:
                    c.mm(P2[0:64, :ncols], [(Wr[:, k, col:col + 64], hT[:, k, :ncols]) for k in range(8)], reads=[b_wr, bh], writes=[bP2])
                    c.op("dve", lambda e: e.tensor_tensor(out=t1s[i][:, :ncols], in0=P1[0:64, :ncols], in1=rt[:, 0, :ncols], op=ALU.mult), reads=[bP1, brt], writes=[b_t1[i]])
                    c.op("dve", lambda e: e.tensor_tensor(out=t2s[i][:, :ncols], in0=P2[0:64, :ncols], in1=rt[:, 1, :ncols], op=ALU.mult), reads=[bP2, brt], writes=[b_t2[i]])
                    c.op("pool", lambda e: e.tensor_tensor(out=dst_ap, in0=t1s[i][:, :ncols], in1=t2s[i][:, :ncols], op=ALU.add), reads=[b_t1[i], b_t2[i]], writes=[dst_buf])

            for kvh in range(4):
                qs, bq = qst[qcnt % 2], b_qst[qcnt % 2]
                qcnt += 1
                for g in range(4):
                    proj_head(Wq, Wqr, (kvh * 4 + g) * 64, qs[:, g, :ncols], bq)
                for qb_ in range(n):
                    c.dma("sp", QT.ap()[kvh, t0 + qb_], qs[:, :, qb_ * 128:(qb_ + 1) * 128], reads=[bq], writes=[b_QT[t0 + qb_]])
                proj_head(Wk, Wkr, kvh * 64, KT[:, kvh, t0 * 128:t0 * 128 + ncols], b_KT)
            for s in range(n):
                i = cnt % 2
                cnt += 1
                Pv, bPv = self.ps[6 + i], self.bps[6 + i]
                c.mm(Pv[:, 0:256], [(hT[:, k, s * 128:(s + 1) * 128], Wv[:, k, :]) for k in range(8)], reads=[b_w, bh], writes=[bPv])
                c.op("act", lambda e: e.activation(out=Vs[:, t0 + s, :, 0:64], in_=Pv[:, 0:256].rearrange("p (h d) -> p h d", h=4), func=AF.Copy), reads=[bPv], writes=[b_V])
        c.barrier()
        c.sb_release(mark)
        Wo = c.sb([64, 16, 1024], BF16, "Wo")
        b_wo = Buf()
        c.dma("pool", Wo[:], w_out.rearrange("(h d) n -> d h n", d=64), writes=[b_wo])
        sk = c.sb([65, 16], F32, "sk")
        b_sk = Buf()
        sinkrow = c.sb([65, 16, 128], F32, "sinkrow")
        c.dma("sp", sk[64:65, :], self.w["attn_sink"].ap()[j:j + 1, :], writes=[b_sk])
        c.op("act", lambda e: e.activation(out=sk[64:65, :], in_=sk[64:65, :], func=AF.Exp), reads=[b_sk], writes=[b_sk])
        c.op("dve", lambda e: e.tensor_copy(out=sinkrow[64:65, :, :], in_=sk[64:65, :].unsqueeze(2).to_broadcast([1, 16, 128])), reads=[b_sk], writes=[b_sk])
        G1x, bG1x = self.load_mod(li, 2, 0, "G1x")
        G1c, bG1c = self.load_mod(li, 2, 1, "G1c")
        Qts = [c.sb([64, 512], BF16, "Qt") for _ in range(4)]
        b_Qt = [Buf() for _ in range(4)]
        PTs = [c.sb([128, 512], BF16, "PT") for _ in range(4)]
        b_PT = [Buf() for _ in range(4)]
        osb = [c.sb([65, 512], F32, "osb") for _ in range(2)]
        b_osb = [Buf(), Buf()]
        rden = [c.sb([65, 512], F32, "rden") for _ in range(2)]
        b_rden = [Buf(), Buf()]
        yTs = [c.sb([64, 16, 128], BF16, "yT") for _ in range(2)]
        b_yT = [Buf(), Buf()]
        xts = [c.sb([128, D], F32, "xres") for _ in range(2)]
        b_xt = [Buf(), Buf()]
        tmps = [c.sb([128, D], F32, "rtmp") for _ in range(2)]
        b_tmp = [Buf(), Buf()]
        scale = 64 ** -0.5
        qi = 0
        pi = 0
        si = 0
        for bi, qb in enumerate(range(0 if need_ctx else NCT, NT)):
            is_ctx = qb < NCT
            if is_ctx:
                keys = [(t, None) for t in range(NCT)]
            else:
                keys = []
                if qb - 1 >= NCT:
                    keys.append((qb - 1, self.trigeb))
                keys.append((qb, None))
                if qb + 1 < NT:
                    keys.append((qb + 1, self.trileb))
                keys += [(t, None) for t in range(NCT)]
            xt, bxt = xts[bi % 2], b_xt[bi % 2]
            c.dma("sp", xt[:], self.xs.ap()[qb * 128:(qb + 1) * 128, :], reads=[self.b_xs[qb]], writes=[bxt])
            yT, byT = yTs[bi % 2], b_yT[bi % 2]
            for kvh in range(4):
                Qt, bQt = Qts[qi % 4], b_Qt[qi % 4]
                qi += 1
                c.dma("sp", Qt[:], QT.ap()[kvh, qb].rearrange("d g t -> d (g t)"), reads=[b_QT[qb]], writes=[bQt])
                oT, boT = self.ps[2 + kvh % 2], self.bps[2 + kvh % 2]
                SB = (0, 1, 7)
                LA = 2

                def issue_S(ki_):
                    kt_ = keys[ki_][0]
                    bk_ = SB[(si + ki_) % len(SB)]
                    c.mm(self.ps[bk_][:, :], [(KT[:, kvh, kt_ * 128:(kt_ + 1) * 128], Qt[:])], reads=[b_KT, bQt], writes=[self.bps[bk_]])
                for k0 in range(min(LA, len(keys))):
                    issue_S(k0)
                for ki, (kt, mask) in enumerate(keys):
                    bk = SB[(si + ki) % len(SB)]
                    sT, bsT = self.ps[bk], self.bps[bk]
                    if ki + LA < len(keys):
                        issue_S(ki + LA)
                    PT, bPT = PTs[pi % 4], b_PT[pi % 4]
                    pi += 1
                    c.op("act", lambda e: e.activation(out=PT[:], in_=sT[:, :], func=AF.Exp, scale=scale), reads=[bsT], writes=[bPT])
                    if mask is not None:
                        c.op("dve", lambda e: e.tensor_tensor(out=PT[:].rearrange("p (g t) -> p g t", g=4), in0=PT[:].rearrange("p (g t) -> p g t", g=4),
                                                              in1=mask.unsqueeze(1).to_broadcast([128, 4, 128]), op=ALU.mult), reads=[bPT, self.b_const], writes=[bPT])
                    c.mm(oT[0:65, :], [(Vs[:, kt, kvh, :], PT[:])], reads=[b_V, bPT], writes=[boT], start=(ki == 0), stop=(ki == len(keys) - 1))
                si += len(keys)
                o, bo = osb[kvh % 2], b_osb[kvh % 2]
                rd, brd = rden[kvh % 2], b_rden[kvh % 2]
                c.op("act", lambda e: e.activation(out=o[:], in_=oT[0:65, :], func=AF.Copy), reads=[boT], writes=[bo])
                c.op("dve", lambda e: e.tensor_tensor(out=rd[64:65, :], in0=o[64:65, :], in1=sinkrow[64:65, kvh * 4:(kvh + 1) * 4, :].rearrange("p g t -> p (g t)"), op=ALU.add),
                     reads=[bo, b_sk], writes=[brd])
                c.mm(self.ps[4][0:64, :], [(self.ones32[64:65, 0:64], rd[64:65, :])], reads=[self.b_const, brd], writes=[self.bps[4]])
                c.op("dve", lambda e: e.reciprocal(out=rd[0:64, :], in_=self.ps[4][0:64, :]), reads=[self.bps[4], brd], writes=[brd])
                c.op("dve", lambda e: e.tensor_tensor(out=yT[:, kvh * 4:(kvh + 1) * 4, :].rearrange("d g t -> d (g t)"), in0=o[0:64, :], in1=rd[0:64, :], op=ALU.mult),
                     reads=[bo, brd], writes=[byT])
            G1, bG1 = (G1c, bG1c) if is_ctx else (G1x, bG1x)
            tmp, btmp = tmps[bi % 2], b_tmp[bi % 2]
            for nn in range(2):
                z, bz = self.ps[5 + nn], self.bps[5 + nn]
                c.mm(z[:, :], [(yT[:, hq, :], Wo[:, hq, nn * 512:(nn + 1) * 512]) for hq in range(16)], reads=[byT, b_wo], writes=[bz])
                c.op("dve", lambda e: e.tensor_tensor(out=tmp[:, nn * 512:(nn + 1) * 512], in0=z[:, :], in1=G1[:, nn * 512:(nn + 1) * 512], op=ALU.mult), reads=[bz, bG1], writes=[btmp])
            c.op("pool", lambda e: e.tensor_tensor(out=xt[:], in0=xt[:], in1=tmp[:], op=ALU.add), reads=[btmp, bxt], writes=[bxt])
            c.dma("sp", self.xs.ap()[qb * 128:(qb + 1) * 128, :], xt[:], reads=[bxt], writes=[self.b_xs[qb]])
        self.phase_end()

    def moe_precast(self, li):
        c = self.c
        wg = self.w["moe_w_gate"].ap()[li]
        wu = self.w["moe_w_up"].ap()[li]
        wd = self.w["moe_w_down"].ap()[li]
        self.wgb = self.scratch(f"moe_wgb{li}", [16, 1024, 512], BF16)
        self.wdb = self.scratch(f"moe_wdb{li}", [16, 256, 1024], BF16)
        self.b_wgb = Buf(); self.b_wdb = Buf()
        for e2 in range(8):
            c.dma("pool", self.wgb.ap()[2 * e2:2 * e2 + 2, :, 0:256], wg[2 * e2:2 * e2 + 2], writes=[self.b_wgb])
            c.dma("pool", self.wgb.ap()[2 * e2:2 * e2 + 2, :, 256:512], wu[2 * e2:2 * e2 + 2], writes=[self.b_wgb])
            c.dma("pool", self.wdb.ap()[2 * e2:2 * e2 + 2], wd[2 * e2:2 * e2 + 2], writes=[self.b_wdb])
        self.precast_done = li

    def layer_moe(self, li, need_ctx):
        c, nc = self.c, self.nc
        NCT = self.NCT
        if getattr(self, "precast_done", -1) != li:
            self.moe_precast(li)
        wgb = self.wgb.ap()
        wdb = self.wdb.ap()
        b_w = Buf("moe_wr")
        Wr = c.sb([128, 8, 20], BF16, "Wr")
        c.dma("pool", Wr[:, :, 0:4], self.w["moe_w_group"].ap()[li].rearrange("(k p) n -> p k n", p=128), writes=[b_w])
        c.dma("pool", Wr[:, :, 4:20], self.w["moe_w_expert"].ap()[li].rearrange("(k p) n -> p k n", p=128), writes=[b_w])
        brow = c.sb([128, 20], F32, "brow")
        c.dma("sp", brow[:, 0:4], self.w["moe_b_group"].ap()[li:li + 1, :].partition_broadcast(128), writes=[b_w])
        c.dma("sp", brow[:, 4:20], self.w["moe_b_expert"].ap()[li:li + 1, :].partition_broadcast(128), writes=[b_w])
        sel16 = c.sb([16, 16, 128], BF16, "sel16")
        c.dma("pool", sel16[:], self.w["sel32"].ap()[0:16, 0:16, :], writes=[b_w])
        hT = c.sb([128, 8, 1024], BF16, "mhT")
        b_hT = Buf()
        act = c.sb([128, 16, 2, 1024], BF16, "mact")
        b_act = Buf()
        Wgu = [c.sb([128, 8, 512], BF16, "Wgu") for _ in range(2)]
        b_Wgu = [Buf(), Buf()]
        Wd = [c.sb([128, 16, 2, 256], BF16, "Wd") for _ in range(2)]
        b_Wd = [Buf(), Buf()]
        xq = [c.sb([128, 256], F32, "xq") for _ in range(4)]
        b_xq = [Buf() for _ in range(4)]
        xqi = 0
        self.alloc_norm_bufs(2)
        A2 = c.sb([128, D], F32, "A2"); S2 = c.sb([128, D], F32, "S2"); G2 = c.sb([128, D], F32, "G2")
        b_m = Buf()
        R = 8
        lg = c.sb([128, R, 20], F32, "lg"); le = c.sb([128, R, 16], F32, "le"); le2 = c.sb([128, R, 16], F32, "le2")
        gm = c.sb([128, R, 4], F32, "gm"); eg = c.sb([128, R, 4], F32, "eg"); pen = c.sb([128, R, 4], F32, "pen")
        mk1 = c.sb([128, R, 16], F32, "mk1"); mk2 = c.sb([128, R, 16], F32, "mk2"); cmb = c.sb([128, R, 16], F32, "cmb")
        sc = c.sb([128, 8, R], F32, "rsc")
        b_r = Buf("route")
        cmbT = c.sb([16, 1024], BF16, "cmbT")
        b_cT = Buf()
        s_sb = [c.sb([128, 512], F32, "msil") for _ in range(2)]
        b_s = [Buf(), Buf()]
        t_sb = [c.sb([128, 512], F32, "mt") for _ in range(2)]
        b_t = [Buf(), Buf()]
        tmpd = [c.sb([128, 256], F32, "mtd") for _ in range(2)]
        b_td = [Buf(), Buf()]
        wi = 0
        di = 0
        ui = 0
        for (t0, n, is_ctx) in self.groups(8, with_ctx=need_ctx):
            G = n * 128
            row = 1 if is_ctx else 0
            for (tl, ch) in ((S2, 3), (A2, 4), (G2, 5)):
                c.dma("sp", tl[:], self.modv.ap()[li, row:row + 1, ch * D:(ch + 1) * D].partition_broadcast(128), reads=[self.b_modv[li]], writes=[b_m])
            for s in range(n):
                self.norm_tile(t0 + s, A2, b_m, S2, b_m, hT, b_hT, s * 128, s % 2)
            lp, blp = self.ps[2], self.bps[2]
            c.mm_multi([(lp[:, s * 20:(s + 1) * 20], [(hT[:, k, s * 128:(s + 1) * 128], Wr[:, k, :]) for k in range(8)]) for s in range(n)],
                       reads=[b_hT, b_w], writes=[blp])
            V = lambda e: e
            lgn = lg[:, 0:n, :]
            c.op("dve", lambda e: e.tensor_tensor(out=lgn, in0=lp[:, 0:n * 20].rearrange("p (s j) -> p s j", j=20), in1=brow[:].unsqueeze(1).to_broadcast([128, n, 20]), op=ALU.add),
                 reads=[blp, b_w], writes=[b_r])
            R1 = [b_r]
            c.op("dve", lambda e: e.tensor_reduce(out=sc[:, 0, 0:n], in_=lgn[:, :, 0:4], axis=AX.X, op=ALU.max), reads=R1, writes=R1)
            c.op("dve", lambda e: e.tensor_tensor(out=gm[:, 0:n, :], in0=lgn[:, :, 0:4], in1=sc[:, 0, 0:n].unsqueeze(2).to_broadcast([128, n, 4]), op=ALU.is_equal), reads=R1, writes=R1)
            c.op("dve", lambda e: e.tensor_tensor(out=eg[:, 0:n, :], in0=lgn[:, :, 0:4], in1=sc[:, 0, 0:n].unsqueeze(2).to_broadcast([128, n, 4]), op=ALU.subtract), reads=R1, writes=R1)
            c.op("act", lambda e: e.activation(out=eg[:, 0:n, :], in_=eg[:, 0:n, :], func=AF.Exp), reads=R1, writes=R1)
            c.op("dve", lambda e: e.tensor_reduce(out=sc[:, 1, 0:n], in_=eg[:, 0:n, :], axis=AX.X, op=ALU.add), reads=R1, writes=R1)
            c.op("dve", lambda e: e.reciprocal(out=sc[:, 1, 0:n], in_=sc[:, 1, 0:n]), reads=R1, writes=R1)
            c.op("dve", lambda e: e.tensor_scalar(out=pen[:, 0:n, :], in0=gm[:, 0:n, :], scalar1=1.0, scalar2=1e30, op0=ALU.subtract, op1=ALU.mult), reads=R1, writes=R1)
            c.op("dve", lambda e: e.tensor_copy(out=le[:, 0:n, :], in_=lgn[:, :, 4:20]), reads=R1, writes=R1)
            lev = le[:, 0:n, :].rearrange("p s (g j) -> p (s g) j", g=4)
            c.op("dve", lambda e: e.tensor_tensor(out=lev, in0=lev, in1=pen[:, 0:n, :].rearrange("p s g -> p (s g)").unsqueeze(2).to_broadcast([128, n * 4, 4]), op=ALU.add), reads=R1, writes=R1)
            c.op("dve", lambda e: e.tensor_reduce(out=sc[:, 2, 0:n], in_=le[:, 0:n, :], axis=AX.X, op=ALU.max), reads=R1, writes=R1)
            c.op("dve", lambda e: e.tensor_tensor(out=mk1[:, 0:n, :], in0=le[:, 0:n, :], in1=sc[:, 2, 0:n].unsqueeze(2).to_broadcast([128, n, 16]), op=ALU.is_equal), reads=R1, writes=R1)
            c.op("dve", lambda e: e.scalar_tensor_tensor(out=le2[:, 0:n, :], in0=mk1[:, 0:n, :], scalar=-1e30, in1=le[:, 0:n, :], op0=ALU.mult, op1=ALU.add), reads=R1, writes=R1)
            c.op("dve", lambda e: e.tensor_reduce(out=sc[:, 3, 0:n], in_=le2[:, 0:n, :], axis=AX.X, op=ALU.max), reads=R1, writes=R1)
            c.op("dve", lambda e: e.tensor_tensor(out=mk2[:, 0:n, :], in0=le2[:, 0:n, :], in1=sc[:, 3, 0:n].unsqueeze(2).to_broadcast([128, n, 16]), op=ALU.is_equal), reads=R1, writes=R1)
            c.op("dve", lambda e: e.tensor_tensor(out=sc[:, 4, 0:n], in0=sc[:, 2, 0:n], in1=sc[:, 3, 0:n], op=ALU.subtract), reads=R1, writes=R1)
            c.op("act", lambda e: e.activation(out=sc[:, 4, 0:n], in_=sc[:, 4, 0:n], func=AF.Sigmoid), reads=R1, writes=R1)
            c.op("dve", lambda e: e.tensor_tensor(out=sc[:, 4, 0:n], in0=sc[:, 4, 0:n], in1=sc[:, 1, 0:n], op=ALU.mult), reads=R1, writes=R1)
            c.op("dve", lambda e: e.tensor_tensor(out=sc[:, 5, 0:n], in0=sc[:, 1, 0:n], in1=sc[:, 4, 0:n], op=ALU.subtract), reads=R1, writes=R1)
            c.op("dve", lambda e: e.tensor_tensor(out=mk1[:, 0:n, :], in0=mk1[:, 0:n, :], in1=sc[:, 4, 0:n].unsqueeze(2).to_broadcast([128, n, 16]), op=ALU.mult), reads=R1, writes=R1)
            c.op("dve", lambda e: e.tensor_tensor(out=mk2[:, 0:n, :], in0=mk2[:, 0:n, :], in1=sc[:, 5, 0:n].unsqueeze(2).to_broadcast([128, n, 16]), op=ALU.mult), reads=R1, writes=R1)
            c.op("dve", lambda e: e.tensor_tensor(out=cmb[:, 0:n, :], in0=mk1[:, 0:n, :], in1=mk2[:, 0:n, :], op=ALU.add), reads=R1, writes=R1)
            for hb in range((n + 3) // 4):
                s0, s1 = hb * 4, min(n, hb * 4 + 4)
                pc, bpc = self.ps[3 + hb], self.bps[3 + hb]
                c.tr_multi([(pc[0:16, (s - s0) * 128:(s - s0 + 1) * 128], cmb[:, s, :], self.ident32) for s in range(s0, s1)], reads=[b_r, self.b_const], writes=[bpc])
                c.op("act", lambda e: e.activation(out=cmbT[:, s0 * 128:s1 * 128], in_=pc[0:16, 0:(s1 - s0) * 128], func=AF.Copy), reads=[bpc], writes=[b_cT])
            ncb = (G + 511) // 512
            for ex in range(16):
                W, bW = Wgu[wi % 2], b_Wgu[wi % 2]
                wi += 1
                c.dma("sp", W[:], wgb[ex].rearrange("(k p) n -> p k n", p=128), reads=[self.b_wgb], writes=[bW])
                for cb in range(ncb):
                    c0 = cb * 512
                    cw = min(512, G - c0)
                    pbc, bpbc = self.ps[4 + (ui % 2)], self.bps[4 + (ui % 2)]
                    c.mm(pbc[:, :cw], [(sel16[:, ex, :], cmbT[:, c0:c0 + cw])], reads=[b_w, b_cT], writes=[bpbc])
                    for ffc in range(2):
                        i = ui % 2
                        ui += 1
                        pg_, bpg = self.ps[0 + i], self.bps[0 + i]
                        pu, bpu = self.ps[2 + i], self.bps[2 + i]
                        c.mm(pg_[:, :cw], [(W[:, k, ffc * 128:(ffc + 1) * 128], hT[:, k, c0:c0 + cw]) for k in range(8)], reads=[bW, b_hT], writes=[bpg])
                        c.mm(pu[:, :cw], [(W[:, k, 256 + ffc * 128:256 + (ffc + 1) * 128], hT[:, k, c0:c0 + cw]) for k in range(8)], reads=[bW, b_hT], writes=[bpu])
                        c.op("act", lambda e: e.activation(out=s_sb[i][:, :cw], in_=pg_[:, :cw], func=AF.Silu), reads=[bpg], writes=[b_s[i]])
                        c.op("dve", lambda e: e.tensor_tensor(out=t_sb[i][:, :cw], in0=s_sb[i][:, :cw], in1=pu[:, :cw], op=ALU.mult), reads=[b_s[i], bpu], writes=[b_t[i]])
                        c.op("dve", lambda e: e.tensor_tensor(out=act[:, ex, ffc, c0:c0 + cw], in0=t_sb[i][:, :cw], in1=pbc[:, :cw], op=ALU.mult), reads=[b_t[i], bpbc], writes=[b_act])
            for dq in range(4):
                Wdt, bWd = Wd[di % 2], b_Wd[di % 2]
                di += 1
                c.dma("sp", Wdt[:], wdb[:, :, dq * 256:(dq + 1) * 256].rearrange("e (f p) n -> p e f n", p=128), reads=[self.b_wdb], writes=[bWd])
                for s in range(n):
                    pa, bpa = self.ps[4 + s // 2], self.bps[4 + s // 2]
                    pav = pa[:, (s % 2) * 256:(s % 2 + 1) * 256]
                    c.mm(pav, [(act[:, ex, ffc, s * 128:(s + 1) * 128], Wdt[:, ex, ffc, :]) for ex in range(16) for ffc in range(2)], reads=[b_act, bWd], writes=[bpa])
                    i = s % 2
                    xq_, bxq = xq[xqi % 4], b_xq[xqi % 4]
                    xqi += 1
                    tl = t0 + s
                    c.dma("sp", xq_[:], self.xs.ap()[tl * 128:(tl + 1) * 128, dq * 256:(dq + 1) * 256], reads=[self.b_xs[tl]], writes=[bxq])
                    c.op("dve", lambda e: e.tensor_tensor(out=tmpd[i][:], in0=pav, in1=G2[:, dq * 256:(dq + 1) * 256], op=ALU.mult), reads=[bpa, b_m], writes=[b_td[i]])
                    c.op("pool", lambda e: e.tensor_tensor(out=xq_[:], in0=xq_[:], in1=tmpd[i][:], op=ALU.add), reads=[b_td[i], bxq], writes=[bxq])
                    c.dma("sp", self.xs.ap()[tl * 128:(tl + 1) * 128, dq * 256:(dq + 1) * 256], xq_[:], reads=[bxq], writes=[self.b_xs[tl]])
        self.phase_end()

    def final(self):
        c = self.c
        gf = c.sb([128, D], F32, "gfin")
        b_g = Buf()
        c.dma("sp", gf[:], self.w["final_norm_g"].ap().rearrange("(o n) -> o n", o=1).partition_broadcast(128), writes=[b_g])
        xs_ = [c.sb([128, D], F32, "fx") for _ in range(2)]
        js = [c.sb([128, D], BF16, "fj") for _ in range(2)]
        st = [c.sb([128, 2], F32, "fst") for _ in range(2)]
        ys = [c.sb([128, D], F32, "fy") for _ in range(2)]
        bx = [Buf(), Buf()]; bj = [Buf(), Buf()]; bs = [Buf(), Buf()]; by = [Buf(), Buf()]
        b_out = Buf()
        for t in range(self.NCT, self.NT):
            i = t % 2
            c.dma("sp", xs_[i][:], self.xs.ap()[t * 128:(t + 1) * 128, :], reads=[self.b_xs[t]], writes=[bx[i]])
            c.op("dve", lambda e: e.memset(st[i][:], 0.0), writes=[bs[i]])
            c.op("act", lambda e: e.activation(out=js[i][:], in_=xs_[i][:], func=AF.Square, accum_out=st[i][:, 0:1]), reads=[bx[i]], writes=[bj[i], bs[i]])
            c.op("dve", lambda e: e.tensor_scalar(out=st[i][:, 1:2], in0=st[i][:, 0:1], scalar1=1.0 / D, scalar2=EPS, op0=ALU.mult, op1=ALU.add), reads=[bs[i]], writes=[bs[i]])
            c.op("act", lambda e: e.activation(out=st[i][:, 1:2], in_=st[i][:, 1:2], func=AF.Sqrt), reads=[bs[i]], writes=[bs[i]])
            c.op("dve", lambda e: e.reciprocal(out=st[i][:, 1:2], in_=st[i][:, 1:2]), reads=[bs[i]], writes=[bs[i]])
            c.op("dve", lambda e: e.scalar_tensor_tensor(out=ys[i][:], in0=xs_[i][:], scalar=st[i][:, 1:2], in1=gf[:], op0=ALU.mult, op1=ALU.mult), reads=[bx[i], bs[i], b_g], writes=[by[i]])
            lt = t - self.NCT
            c.dma("sp", self.out.ap()[lt * 128:(lt + 1) * 128, :], ys[i][:], reads=[by[i]], writes=[b_out])
        self.c.finish("sp")

    def layer_mla(self, li, j, need_ctx):
        c, nc = self.c, self.nc
        T, NT, NCT = self.T, self.NT, self.NCT
        w_in = self.w["mla_w_in"].ap()[j]
        w_qup = self.w["mla_w_q_up"].ap()[j]
        w_kvup = self.w["mla_w_kv_up"].ap()[j]
        w_out = self.w["mla_w_out"].ap()[j]
        QT = self.scratch(f"mla_qt{li}", [16, 96, T], BF16)
        KTd = self.scratch(f"mla_kt{li}", [16, 96, T], BF16)
        Vd = self.scratch(f"mla_v{li}", [16, NT, 128, 65], BF16)
        YT = self.scratch(f"mla_yt{li}", [16, 64, T], BF16)
        b_QT = Buf(); b_KTd = Buf(); b_Vd = Buf(); b_YT = Buf()
        b_w = Buf("mla_w")
        Win = c.sb([128, 8, 544], BF16, "Win")
        c.dma("pool", Win[:], w_in.rearrange("(k p) n -> p k n", p=128), writes=[b_w])
        Wkr_rot = c.sb([128, 8, 32], BF16, "Wkrrot")
        Wq = c.sb([128, 2, 1536], BF16, "Wqup")
        c.dma("pool", Wq[:], w_qup.rearrange("(k p) n -> p k n", p=128), writes=[b_w])
        Wqr = c.sb([128, 2, 1536], BF16, "Wquprot")
        Wkn = c.sb([128, 2, 16, 64], BF16, "Wkn")
        Wv = c.sb([128, 2, 16, 64], BF16, "Wvv")
        kvv = w_kvup.rearrange("(k p) (h two d) -> p k h two d", p=128, two=2, d=64)
        for kc in range(2):
            c.dma("pool", Wkn[:, kc], kvv[:, kc, :, 0, :], writes=[b_w])
            c.dma("pool", Wv[:, kc], kvv[:, kc, :, 1, :], writes=[b_w])
        b_wr = Buf("mla_wr")
        c.op("pool", lambda e: e.memset(Wqr[:], 0.0), writes=[b_wr])
        for kc in range(2):
            src = Wq[:, kc, :].rearrange("p (h d) -> p h d", d=96)
            dst = Wqr[:, kc, :].rearrange("p (h d) -> p h d", d=96)
            c.op("act", lambda e: e.activation(out=dst[:, :, 64:80], in_=src[:, :, 80:96], func=AF.Copy, scale=-1.0), reads=[b_w], writes=[b_wr])
            c.op("dve", lambda e: e.tensor_copy(out=dst[:, :, 80:96], in_=src[:, :, 64:80]), reads=[b_w], writes=[b_wr])
        c.op("act", lambda e: e.activation(out=Wkr_rot[:, :, 0:16], in_=Win[:, :, 528:544], func=AF.Copy, scale=-1.0), reads=[b_w], writes=[b_wr])
        c.op("dve", lambda e: e.tensor_copy(out=Wkr_rot[:, :, 16:32], in_=Win[:, :, 512:528]), reads=[b_w], writes=[b_wr])
        gq = c.sb([128, 512], F32, "gqkv")
        c.dma("sp", gq[:, 0:256], self.w["mla_q_norm_g"].ap()[j:j + 1, :].partition_broadcast(128), writes=[b_w])
        c.dma("sp", gq[:, 256:512], self.w["mla_kv_norm_g"].ap()[j:j + 1, :].partition_broadcast(128), writes=[b_w])
        mark = c.sb_mark()
        A1x, bA1x = self.load_mod(li, 1, 0, "A1x")
        S1x, bS1x = self.load_mod(li, 0, 0, "S1x")
        A1c, bA1c = self.load_mod(li, 1, 1, "A1c")
        S1c, bS1c = self.load_mod(li, 0, 1, "S1c")
        self.alloc_norm_bufs(2)
        hTs = [c.sb([128, 8, 512], BF16, "hT") for _ in range(2)]
        b_hT = [Buf(), Buf()]
        cnT = [c.sb([128, 4, 512], BF16, "cnT") for _ in range(2)]
        b_cnT = [Buf(), Buf()]
        rts = [c.sb([96, 2, 512], F32, "rt96") for _ in range(2)]
        rks = [c.sb([32, 2, 512], F32, "rt32") for _ in range(2)]
        b_rt = [Buf(), Buf()]
        krT = [c.sb([32, 512], BF16, "krT") for _ in range(2)]
        b_krT = [Buf(), Buf()]
        st = [c.sb([128, 4], F32, "mst") for _ in range(2)]
        b_st = [Buf(), Buf()]
        jk = c.sb([128, 256], BF16, "mjunk"); b_jk = Buf()
        cn = [c.sb([128, 512], BF16, "cn") for _ in range(2)]
        b_cn = [Buf(), Buf()]
        qst = [c.sb([96, 512], BF16, "mqst") for _ in range(2)]
        b_qst = [Buf(), Buf()]
        kst = [c.sb([64, 512], BF16, "mkst") for _ in range(2)]
        b_kst = [Buf(), Buf()]
        t1s = [c.sb([96, 512], F32, "t1") for _ in range(2)]
        t2s = [c.sb([96, 512], F32, "t2") for _ in range(2)]
        b_t1 = [Buf(), Buf()]; b_t2 = [Buf(), Buf()]
        Vt = [c.sb([128, 16, 65], BF16, "Vt") for _ in range(2)]
        b_Vt = [Buf(), Buf()]
        for i in range(2):
            c.op("pool", lambda e: e.memset(Vt[i][:], 1.0), writes=[b_Vt[i]])
        cnt = 0
        ti = 0
        for gi, (t0, n, is_ctx) in enumerate(self.groups(4)):
            ncols = n * 128
            col0 = t0 * 128
            hT, bh = hTs[gi % 2], b_hT[gi % 2]
            cT_, bcT = cnT[gi % 2], b_cnT[gi % 2]
            rt, rk, brt = rts[gi % 2], rks[gi % 2], b_rt[gi % 2]
            for s in range(n):
                if is_ctx:
                    self.norm_tile(t0 + s, A1c, bA1c, S1c, bS1c, hT, bh, s * 128, (t0 + s) % 2)
                else:
                    self.norm_tile(t0 + s, A1x, bA1x, S1x, bS1x, hT, bh, s * 128, (t0 + s) % 2)
            if not is_ctx:
                l0 = (t0 - NCT) * 128
                c.dma("sp", rt[:, :, :ncols], self.w["rope96"].ap()[:, :, l0:l0 + ncols].rearrange("two d l -> d two l"), writes=[brt])
                c.dma("sp", rk[:, :, :ncols], self.w["rope32"].ap()[:, :, l0:l0 + ncols].rearrange("two d l -> d two l"), writes=[brt])
            for s in range(n):
                i = ti % 2
                ti += 1
                pA, bpA = self.ps[2 + i], self.bps[2 + i]
                c.mm(pA[:, :], [(hT[:, k, s * 128:(s + 1) * 128], Win[:, k, 0:512]) for k in range(8)], reads=[bh, b_w], writes=[bpA])
                c.op("dve", lambda e: e.memset(st[i][:], 0.0), writes=[b_st[i]])
                for u in range(2):
                    c.op("act", lambda e: e.activation(out=jk[:], in_=pA[:, u * 256:(u + 1) * 256], func=AF.Square, accum_out=st[i][:, u:u + 1]), reads=[bpA], writes=[b_jk, b_st[i]])
                c.op("dve", lambda e: e.tensor_scalar(out=st[i][:, 2:4], in0=st[i][:, 0:2], scalar1=1.0 / 256, scalar2=EPS, op0=ALU.mult, op1=ALU.add), reads=[b_st[i]], writes=[b_st[i]])
                c.op("act", lambda e: e.activation(out=st[i][:, 2:4], in_=st[i][:, 2:4], func=AF.Sqrt), reads=[b_st[i]], writes=[b_st[i]])
                c.op("dve", lambda e: e.reciprocal(out=st[i][:, 2:4], in_=st[i][:, 2:4]), reads=[b_st[i]], writes=[b_st[i]])
                for u in range(2):
                    c.op("dve", lambda e: e.scalar_tensor_tensor(out=cn[i][:, u * 256:(u + 1) * 256], in0=pA[:, u * 256:(u + 1) * 256], scalar=st[i][:, 2 + u:3 + u], in1=gq[:, u * 256:(u + 1) * 256], op0=ALU.mult, op1=ALU.mult),
                         reads=[bpA, b_st[i], b_w], writes=[b_cn[i]])
                pT = self.ps[4 + i].ap().bitcast(BF16)
                c.tr_multi([(pT[:, k * 128:(k + 1) * 128], cn[i][:, k * 128:(k + 1) * 128], self.identb) for k in range(4)], reads=[b_cn[i], self.b_const], writes=[self.bps[4 + i]])
                c.op("act", lambda e: e.activation(out=cT_[:, :, s * 128:(s + 1) * 128], in_=pT[:, 0:512].rearrange("p (k n) -> p k n", k=4), func=AF.Copy), reads=[self.bps[4 + i]], writes=[bcT])

            def rope_proj(pairs1, pairs2, M, rtab, dst_ap, dst_buf, rd):
                nonlocal cnt
                i = cnt % 2
                cnt += 1
                P1, bP1 = self.ps[2 + i], self.bps[2 + i]
                P2, bP2 = self.ps[6 + i], self.bps[6 + i]
                c.mm(P1[0:M, :ncols], pairs1, reads=rd, writes=[bP1])
                if is_ctx or pairs2 is None:
                    c.op("act", lambda e: e.activation(out=dst_ap, in_=P1[0:M, :ncols], func=AF.Copy), reads=[bP1], writes=[dst_buf])
                else:
                    c.mm(P2[0:M, :ncols], pairs2, reads=rd + [b_wr], writes=[bP2])
                    c.op("dve", lambda e: e.tensor_tensor(out=t1s[i][0:M, :ncols], in0=P1[0:M, :ncols], in1=rtab[0:M, 0, :ncols], op=ALU.mult), reads=[bP1, brt], writes=[b_t1[i]])
                    c.op("dve", lambda e: e.tensor_tensor(out=t2s[i][0:M, :ncols], in0=P2[0:M, :ncols], in1=rtab[0:M, 1, :ncols], op=ALU.mult), reads=[bP2, brt], writes=[b_t2[i]])
                    c.op("pool", lambda e: e.tensor_tensor(out=dst_ap, in0=t1s[i][0:M, :ncols], in1=t2s[i][0:M, :ncols], op=ALU.add), reads=[b_t1[i], b_t2[i]], writes=[dst_buf])

            kr_, bkr = krT[gi % 2], b_krT[gi % 2]
            rope_proj([(Win[:, k, 512:544], hT[:, k, :ncols]) for k in range(8)], [(Wkr_rot[:, k, :], hT[:, k, :ncols]) for k in range(8)], 32, rk, kr_[:, :ncols], bkr, [bh, b_w])
            for h in range(16):
                qs, bq = qst[h % 2], b_qst[h % 2]
                rope_proj([(Wq[:, kc, h * 96:(h + 1) * 96], cT_[:, kc, :ncols]) for kc in range(2)],
                          [(Wqr[:, kc, h * 96:(h + 1) * 96], cT_[:, kc, :ncols]) for kc in range(2)], 96, rt, qs[:, :ncols], bq, [bcT, b_w])
                c.dma("sp", QT.ap()[h, :, col0:col0 + ncols], qs[:, :ncols], reads=[bq], writes=[b_QT])
                ks, bk = kst[h % 2], b_kst[h % 2]
                rope_proj([(Wkn[:, kc, h, :], cT_[:, 2 + kc, :ncols]) for kc in range(2)], None, 64, None, ks[:, :ncols], bk, [bcT, b_w])
                c.dma("sp", KTd.ap()[h, 0:64, col0:col0 + ncols], ks[:, :ncols], reads=[bk], writes=[b_KTd])
                c.dma("sp", KTd.ap()[h, 64:96, col0:col0 + ncols], kr_[:, :ncols], reads=[bkr], writes=[b_KTd])
            for s in range(n):
                vt, bvt = Vt[s % 2], b_Vt[s % 2]
                for hh in range(2):
                    i = cnt % 2
                    cnt += 1
                    Pv, bPv = self.ps[2 + i], self.bps[2 + i]
                    c.mm(Pv[:, :], [(cT_[:, 2 + kc, s * 128:(s + 1) * 128], Wv[:, kc, hh * 8:(hh + 1) * 8, :].rearrange("p h d -> p (h d)")) for kc in range(2)], reads=[bcT, b_w], writes=[bPv])
                    c.op("act", lambda e: e.activation(out=vt[:, hh * 8:(hh + 1) * 8, 0:64], in_=Pv[:, :].rearrange("p (h d) -> p h d", d=64), func=AF.Copy), reads=[bPv], writes=[bvt])
                c.dma("sp", Vd.ap()[:, t0 + s].rearrange("h p d -> p h d"), vt[:], reads=[bvt], writes=[b_Vd])
        c.barrier()
        c.sb_release(mark)
        QTs = [c.sb([96, T], BF16, "QTh") for _ in range(2)]
        KTs = [c.sb([96, T], BF16, "KTh") for _ in range(2)]
        Vhs = [c.sb([128, NT, 65], BF16, "Vh") for _ in range(2)]
        b_hd = [Buf(), Buf()]
        PTs = [c.sb([128, 512], BF16, "PT") for _ in range(4)]
        b_PT = [Buf() for _ in range(4)]
        osb = [c.sb([65, 512], F32, "osb") for _ in range(2)]
        b_osb = [Buf(), Buf()]
        ysb = [c.sb([64, 512], BF16, "ysb") for _ in range(2)]
        b_ysb = [Buf(), Buf()]
        rbs = [c.sb([64, 512], F32, "rbs") for _ in range(2)]
        b_rbs = [Buf(), Buf()]
        scale = 96 ** -0.5
        pi = 0; si = 0; oi = 0

        def load_head(h_):
            c.dma("sp", QTs[h_ % 2][:], QT.ap()[h_], reads=[b_QT], writes=[b_hd[h_ % 2]])
            c.dma("sp", KTs[h_ % 2][:], KTd.ap()[h_], reads=[b_KTd], writes=[b_hd[h_ % 2]])
            c.dma("sp", Vhs[h_ % 2][:], Vd.ap()[h_].rearrange("t p d -> p t d"), reads=[b_Vd], writes=[b_hd[h_ % 2]])
        load_head(0)
        for h in range(16):
            Qh, Kh, Vh, bhd = QTs[h % 2], KTs[h % 2], Vhs[h % 2], b_hd[h % 2]
            if h + 1 < 16:
                load_head(h + 1)
            units = []
            for (t0, n, is_ctx) in self.groups(4, with_ctx=need_ctx):
                keys = list(range(NCT)) if is_ctx else list(range(NT))
                gslot = oi % 2
                oi += 1
                for ki, kt in enumerate(keys):
                    units.append((t0 * 128, n * 128, kt, ki == 0, ki == len(keys) - 1, gslot))
            SB = (0, 1, 5, 6, 7)
            LA = 3

            def issue_S(ui):
                col0_, ncols_, kt_, _, _, _ = units[ui]
                bk_ = SB[(si + ui) % len(SB)]
                c.mm(self.ps[bk_][:, :ncols_], [(Kh[:, kt_ * 128:(kt_ + 1) * 128], Qh[:, col0_:col0_ + ncols_])], reads=[bhd], writes=[self.bps[bk_]])

            def epilogue(col0_, ncols_, gslot):
                oT, boT = self.ps[2 + gslot], self.bps[2 + gslot]
                o, bo = osb[gslot], b_osb[gslot]
                ys, bys = ysb[gslot], b_ysb[gslot]
                c.op("act", lambda e: e.activation(out=o[:, :ncols_], in_=oT[0:65, :ncols_], func=AF.Copy), reads=[boT], writes=[bo])
                c.mm(self.ps[4][0:64, :ncols_], [(self.ones32[64:65, 0:64], o[64:65, :ncols_])], reads=[self.b_const, bo], writes=[self.bps[4]])
                rb, brb = rbs[gslot], b_rbs[gslot]
                c.op("dve", lambda e: e.reciprocal(out=rb[:, :ncols_], in_=self.ps[4][0:64, :ncols_]), reads=[self.bps[4]], writes=[brb])
                c.op("dve", lambda e: e.tensor_tensor(out=ys[:, :ncols_], in0=o[0:64, :ncols_], in1=rb[:, :ncols_], op=ALU.mult), reads=[bo, brb], writes=[bys])
                c.dma("sp", YT.ap()[h, :, col0_:col0_ + ncols_], ys[:, :ncols_], reads=[bys], writes=[b_YT])

            pending = []
            for k0 in range(min(LA, len(units))):
                issue_S(k0)
            for ui, (col0, ncols, kt, first, last, gslot) in enumerate(units):
                bk = SB[(si + ui) % len(SB)]
                sT, bsT = self.ps[bk], self.bps[bk]
                if ui + LA < len(units):
                    issue_S(ui + LA)
                PT, bPT = PTs[pi % 4], b_PT[pi % 4]
                pi += 1
                oT, boT = self.ps[2 + gslot], self.bps[2 + gslot]
                c.op("act", lambda e: e.activation(out=PT[:, :ncols], in_=sT[:, :ncols], func=AF.Exp, scale=scale), reads=[bsT], writes=[bPT])
                c.mm(oT[0:65, :ncols], [(Vh[:, kt, :], PT[:, :ncols])], reads=[bhd, bPT], writes=[boT], start=first, stop=last)
                if pending and pending[0][0] <= ui:
                    _, args = pending.pop(0)
                    epilogue(*args)
                if last:
                    pending.append((ui + 4, (col0, ncols, gslot)))
            for _, args in pending:
                epilogue(*args)
            si += len(units)
        c.barrier()
        c.sb_release(mark)
        Wo = c.sb([64, 16, 1024], BF16, "Wo")
        b_wo = Buf()
        c.dma("pool", Wo[:], w_out.rearrange("(h d) n -> d h n", d=64), writes=[b_wo])
        self.attn_out(li, need_ctx, Wo, b_wo, YT, b_YT)
        self.phase_end()

    def attn_out(self, li, need_ctx, Wo, b_wo, YT, b_YT):
        c = self.c
        NCT, NT = self.NCT, self.NT
        G1x, bG1x = self.load_mod(li, 2, 0, "G1x")
        G1c, bG1c = self.load_mod(li, 2, 1, "G1c")
        yTs = [c.sb([64, 16, 128], BF16, "yT") for _ in range(2)]
        b_yT = [Buf(), Buf()]
        xts = [c.sb([128, D], F32, "xres") for _ in range(2)]
        b_xt = [Buf(), Buf()]
        tmps = [c.sb([128, D], F32, "rtmp") for _ in range(2)]
        b_tmp = [Buf(), Buf()]
        for bi, qb in enumerate(range(0 if need_ctx else NCT, NT)):
            is_ctx = qb < NCT
            xt, bxt = xts[bi % 2], b_xt[bi % 2]
            yT, byT = yTs[bi % 2], b_yT[bi % 2]
            tmp, btmp = tmps[bi % 2], b_tmp[bi % 2]
            c.dma("sp", xt[:], self.xs.ap()[qb * 128:(qb + 1) * 128, :], reads=[self.b_xs[qb]], writes=[bxt])
            c.dma("sp", yT[:], YT.ap()[:, :, qb * 128:(qb + 1) * 128].rearrange("h d t -> d h t"), reads=[b_YT], writes=[byT])
            G1, bG1 = (G1c, bG1c) if is_ctx else (G1x, bG1x)
            for nn in range(2):
                z, bz = self.ps[5 + nn], self.bps[5 + nn]
                c.mm(z[:, :], [(yT[:, hq, :], Wo[:, hq, nn * 512:(nn + 1) * 512]) for hq in range(16)], reads=[byT, b_wo], writes=[bz])
                c.op("dve", lambda e: e.tensor_tensor(out=tmp[:, nn * 512:(nn + 1) * 512], in0=z[:, :], in1=G1[:, nn * 512:(nn + 1) * 512], op=ALU.mult), reads=[bz, bG1], writes=[btmp])
            c.op("pool", lambda e: e.tensor_tensor(out=xt[:], in0=xt[:], in1=tmp[:], op=ALU.add), reads=[btmp, bxt], writes=[bxt])
            c.dma("sp", self.xs.ap()[qb * 128:(qb + 1) * 128, :], xt[:], reads=[bxt], writes=[self.b_xs[qb]])

    def layer_ssd(self, li, j, need_ctx):
        c, nc = self.c, self.nc
        T, NT, NCT, NL, NCX = self.T, self.NT, self.NCT, self.NL, self.NCX
        w_in = self.w["ssm_w_in"].ap()[j]
        XB = self.scratch(f"ssd_xb{li}", [24, 128, T], BF16)
        XC = self.scratch(f"ssd_xc{li}", [24, 128, T], BF16)
        Zs = self.scratch(f"ssd_z{li}", [NT, 128, 2048], BF16)
        DT = self.scratch(f"ssd_dt{li}", [NT, 128, 64], F32)
        Yd = [self.scratch(f"ssd_y{li}_{d}", [NT, 128, 2048], F32) for d in range(2)]
        b_XB = Buf(); b_XC = Buf(); b_Z = Buf(); b_DT = Buf(); b_Y = [Buf(), Buf()]
        b_w = Buf("ssd_w")
        Win = c.sb([128, 8, 5184], BF16, "ssdWin")
        for k in range(8):
            c.dma("pool", Win[:, k, :], w_in[k * 128:(k + 1) * 128, :], writes=[b_w])
        dtb = c.sb([128, 64], F32, "dtb")
        c.dma("sp", dtb[:], self.w["ssm_dt_bias"].ap()[j:j + 1].rearrange("o d h -> o (d h)").partition_broadcast(128), writes=[b_w])
        mark = c.sb_mark()
        A1x, bA1x = self.load_mod(li, 1, 0, "A1x")
        S1x, bS1x = self.load_mod(li, 0, 0, "S1x")
        A1c, bA1c = self.load_mod(li, 1, 1, "A1c")
        S1c, bS1c = self.load_mod(li, 0, 1, "S1c")
        self.alloc_norm_bufs(2)
        hTs = [c.sb([128, 8, 512], BF16, "hT") for _ in range(2)]
        b_hT = [Buf(), Buf()]
        stg = [c.sb([128, 512], BF16, "stg") for _ in range(3)]
        b_stg = [Buf() for _ in range(3)]
        zst = [c.sb([128, 2048], BF16, "zst") for _ in range(2)]
        b_zst = [Buf(), Buf()]
        dts = [c.sb([128, 64], F32, "dts") for _ in range(2)]
        b_dts = [Buf(), Buf()]
        cnt = 0
        for gi, (t0, n, is_ctx) in enumerate(self.groups(4)):
            ncols = n * 128
            col0 = t0 * 128
            hT, bh = hTs[gi % 2], b_hT[gi % 2]
            for s in range(n):
                if is_ctx:
                    self.norm_tile(t0 + s, A1c, bA1c, S1c, bS1c, hT, bh, s * 128, (t0 + s) % 2)
                else:
                    self.norm_tile(t0 + s, A1x, bA1x, S1x, bS1x, hT, bh, s * 128, (t0 + s) % 2)
            for fc in range(24):
                i = cnt % 3
                cnt += 1
                P, bP = self.ps[2 + i], self.bps[2 + i]
                c.mm(P[:, :ncols], [(Win[:, k, 2048 + fc * 128:2048 + (fc + 1) * 128], hT[:, k, :ncols]) for k in range(8)], reads=[b_w, bh], writes=[bP])
                c.op("act", lambda e: e.activation(out=stg[i][:, :ncols], in_=P[:, :ncols], func=AF.Copy), reads=[bP], writes=[b_stg[i]])
                c.dma("sp", XB.ap()[fc, :, col0:col0 + ncols], stg[i][:, :ncols], reads=[b_stg[i]], writes=[b_XB])
            for s in range(n):
                zs, bz = zst[s % 2], b_zst[s % 2]
                for zc in range(4):
                    i = cnt % 3
                    cnt += 1
                    P, bP = self.ps[2 + i], self.bps[2 + i]
                    c.mm(P[:, :], [(hT[:, k, s * 128:(s + 1) * 128], Win[:, k, zc * 512:(zc + 1) * 512]) for k in range(8)], reads=[b_w, bh], writes=[bP])
                    c.op("act", lambda e: e.activation(out=zs[:, zc * 512:(zc + 1) * 512], in_=P[:, :], func=AF.Silu), reads=[bP], writes=[bz])
                c.dma("sp", Zs.ap()[t0 + s], zs[:], reads=[bz], writes=[b_Z])
                i = cnt % 3
                cnt += 1
                P, bP = self.ps[2 + i], self.bps[2 + i]
                dt_, bdt = dts[s % 2], b_dts[s % 2]
                c.mm(P[:, 0:64], [(hT[:, k, s * 128:(s + 1) * 128], Win[:, k, 5120:5184]) for k in range(8)], reads=[b_w, bh], writes=[bP])
                c.op("dve", lambda e: e.tensor_tensor(out=dt_[:], in0=P[:, 0:64], in1=dtb[:], op=ALU.add), reads=[bP, b_w], writes=[bdt])
                c.op("act", lambda e: e.activation(out=dt_[:], in_=dt_[:], func=AF.Exp), reads=[bdt], writes=[bdt])
                c.op("act", lambda e: e.activation(out=dt_[:], in_=dt_[:], func=AF.Ln, bias=1.0), reads=[bdt], writes=[bdt])
                c.dma("sp", DT.ap()[t0 + s], dt_[:], reads=[bdt], writes=[b_DT])
        c.barrier()
        c.sb_release(self.mark0)
        cw = c.sb([128, 24, 5], F32, "convw"); cbias = c.sb([128, 24], F32, "convb")
        b_cw = Buf()
        c.dma("sp", cw[:], self.w["ssm_conv_wT"].ap()[j], writes=[b_cw])
        c.dma("sp", cbias[:], self.w["ssm_conv_bT"].ap()[j], writes=[b_cw])
        segs = [(0, NCX), (NCX, NL)]
        Lmax = max(NCX, NL)
        xp = [c.sb([128, Lmax + 4], BF16, "xp") for _ in range(2)]
        b_xp = [Buf(), Buf()]
        acc = [c.sb([128, Lmax], F32, "cacc") for _ in range(2)]
        b_acc = [Buf(), Buf()]
        cout = [c.sb([128, Lmax], BF16, "cout") for _ in range(2)]
        b_cout = [Buf(), Buf()]
        it = 0
        for fc in range(24):
            for (o0, L) in segs:
                i = it % 2
                it += 1
                c.op("pool", lambda e: e.memset(xp[i][:], 0.0), writes=[b_xp[i]])
                c.dma("sp", xp[i][:, 2:2 + L], XB.ap()[fc, :, o0:o0 + L], reads=[b_XB], writes=[b_xp[i]])
                c.op("dve", lambda e: e.tensor_scalar(out=acc[i][:, :L], in0=xp[i][:, 0:L], scalar1=cw[:, fc, 0:1], scalar2=None, op0=ALU.mult), reads=[b_xp[i], b_cw], writes=[b_acc[i]])
                for k in range(1, 5):
                    eng = "dve"
                    c.op(eng, lambda e: e.scalar_tensor_tensor(out=acc[i][:, :L], in0=xp[i][:, k:k + L], scalar=cw[:, fc, k:k + 1], in1=acc[i][:, :L], op0=ALU.mult, op1=ALU.add),
                         reads=[b_xp[i], b_cw, b_acc[i]], writes=[b_acc[i]])
                c.op("act", lambda e: e.activation(out=cout[i][:, :L], in_=acc[i][:, :L], func=AF.Silu, bias=cbias[:, fc:fc + 1]), reads=[b_acc[i], b_cw], writes=[b_cout[i]])
                c.dma("sp", XC.ap()[fc, :, o0:o0 + L], cout[i][:, :L], reads=[b_cout[i]], writes=[b_XC])
        c.barrier()
        c.sb_release(self.mark0)
        sel = c.sb([32, 32, 128], F32, "sel32"); b_sel = Buf()
        c.dma("sp", sel[:], self.w["sel32"].ap(), writes=[b_sel])
        aneg = c.sb([128, 64], F32, "aneg"); dsk = c.sb([128, 64], F32, "dsk"); b_an = Buf()
        c.dma("sp", aneg[:], self.w["ssm_a_log"].ap()[j:j + 1].rearrange("o d h -> o (d h)").partition_broadcast(128), writes=[b_an])
        c.op("act", lambda e: e.activation(out=aneg[:], in_=aneg[:], func=AF.Exp), reads=[b_an], writes=[b_an])
        c.op("act", lambda e: e.activation(out=aneg[:], in_=aneg[:], func=AF.Copy, scale=-1.0), reads=[b_an], writes=[b_an])
        c.dma("sp", dsk[:], self.w["ssm_d"].ap()[j:j + 1].rearrange("o d h -> o (d h)").partition_broadcast(128), writes=[b_an])
        c.op("dve", lambda e: e.tensor_tensor(out=dsk[:, 0:32], in0=dsk[:, 0:32], in1=dsk[:, 32:64], op=ALU.add), reads=[b_an], writes=[b_an])
        xcs = [c.sb([128, 24, 128], BF16, "xc") for _ in range(2)]; b_xc = [Buf(), Buf()]
        dtt = [c.sb([128, 64], F32, "dtt") for _ in range(2)]; b_dtt = [Buf(), Buf()]
        gt = [c.sb([128, 8, 32], F32, "gt") for _ in range(2)]; b_gt = [Buf(), Buf()]
        acT = [c.sb([32, 128], F32, "acT") for _ in range(2)]; b_acT = [Buf(), Buf()]
        nacT = [c.sb([32, 128], F32, "nacT") for _ in range(2)]
        xtok = [c.sb([128, 32, 64], F32, "xtok") for _ in range(2)]; b_xtok = [Buf(), Buf()]
        u = [c.sb([128, 32, 64], BF16, "u") for _ in range(2)]; b_u = [Buf(), Buf()]
        Vw = [c.sb([128, 32, 64], BF16, "Vw") for _ in range(2)]; b_Vw = [Buf(), Buf()]
        Btok = [c.sb([128, 4, 128], BF16, "Btok") for _ in range(2)]; b_Bt = [Buf(), Buf()]
        scm = [c.sb([128, 128], F32, "scm") for _ in range(2)]; b_scm = [Buf(), Buf()]
        aa = [c.sb([128, 512], F32, "aa") for _ in range(4)]; b_aa = [Buf() for _ in range(4)]
        EE = [c.sb([128, 512], F32, "EE") for _ in range(4)]; b_EE = [Buf() for _ in range(4)]
        MT = [c.sb([128, 512], BF16, "MT") for _ in range(4)]; b_MT = [Buf() for _ in range(4)]
        yi = [c.sb([128, 512], F32, "yi") for _ in range(2)]; b_yi = [Buf(), Buf()]
        Yt = [c.sb([128, 2048], F32, "Yt") for _ in range(2)]; b_Yt = [Buf(), Buf()]
        S32 = c.sb([128, 4, 512], F32, "S32"); Sb = c.sb([128, 4, 512], BF16, "Sb"); b_S = [Buf() for _ in range(4)]
        ci = 0; hi = 0
        for d in range(2):
            tri = self.trile32 if d == 0 else self.trige32
            order = list(range(NT)) if d == 0 else (list(range(NCT - 1, -1, -1)) + list(range(NT - 1, NCT - 1, -1)))
            c.op("dve", lambda e: e.memset(S32[:], 0.0), writes=b_S)
            c.op("pool", lambda e: e.memset(Sb[:], 0.0), writes=b_S)
            def prep_load(ch, i):
                xc, bxc = xcs[i], b_xc[i]
                c.dma("sp", xc[:], XC.ap()[:, :, ch * 128:(ch + 1) * 128].rearrange("f p t -> p f t"), reads=[b_XC], writes=[bxc])
                c.dma("sp", dtt[i][:], DT.ap()[ch], reads=[b_DT], writes=[b_dtt[i]])

            def prep(ch, i):
                xc, bxc = xcs[i], b_xc[i]
                g_, bg = gt[i], b_gt[i]
                dc = slice(d * 32, (d + 1) * 32)
                c.op("dve", lambda e: e.tensor_tensor(out=g_[:, 0, :], in0=dtt[i][:, dc], in1=aneg[:, dc], op=ALU.mult), reads=[b_dtt[i], b_an], writes=[bg])
                p0, bp0 = self.ps[0], self.bps[0]
                c.mm(p0[:, 0:32], [(tri, g_[:, 0, :])], reads=[self.b_const, bg], writes=[bp0])
                c.mm(p0[:, 32:64], [(self.ones32, g_[:, 0, :])], reads=[self.b_const, bg], writes=[bp0])
                c.mm(p0[0:32, 128:256], [(g_[:, 0, :], tri)], reads=[self.b_const, bg], writes=[bp0])
                c.op("act", lambda e: e.activation(out=g_[:, 1, :], in_=p0[:, 0:32], func=AF.Copy), reads=[bp0], writes=[bg])
                c.op("act", lambda e: e.activation(out=g_[:, 2, :], in_=p0[:, 0:32], func=AF.Copy, scale=-1.0), reads=[bp0], writes=[bg])
                c.op("act", lambda e: e.activation(out=g_[:, 3, :], in_=p0[:, 0:32], func=AF.Exp), reads=[bp0], writes=[bg])
                c.op("dve", lambda e: e.tensor_tensor(out=g_[:, 4, :], in0=p0[:, 32:64], in1=g_[:, 1, :], op=ALU.subtract), reads=[bp0, bg], writes=[bg])
                c.op("act", lambda e: e.activation(out=g_[:, 4, :], in_=g_[:, 4, :], func=AF.Exp), reads=[bg], writes=[bg])
                c.op("act", lambda e: e.activation(out=g_[:, 5, :], in_=p0[:, 32:64], func=AF.Exp), reads=[bp0], writes=[bg])
                c.op("act", lambda e: e.activation(out=acT[i][:], in_=p0[0:32, 128:256], func=AF.Copy), reads=[bp0], writes=[b_acT[i]])
                c.op("act", lambda e: e.activation(out=nacT[i][:], in_=p0[0:32, 128:256], func=AF.Copy, scale=-1.0), reads=[bp0], writes=[b_acT[i]])
                for hh in range(2):
                    pT = self.ps[1].ap().bitcast(BF16)
                    c.tr_multi([(pT[:, k * 128:(k + 1) * 128], xc[:, hh * 8 + k, :], self.identb) for k in range(8)], reads=[bxc, self.b_const], writes=[self.bps[1]])
                    c.op("act", lambda e: e.activation(out=xtok[i][:, hh * 16:(hh + 1) * 16, :].rearrange("p h d -> p (h d)"), in_=pT[:, :], func=AF.Copy), reads=[self.bps[1]], writes=[b_xtok[i]])
                pT = self.ps[1].ap().bitcast(BF16)
                c.tr_multi([(pT[:, k * 128:(k + 1) * 128], xc[:, 16 + k, :], self.identb) for k in range(4)], reads=[bxc, self.b_const], writes=[self.bps[1]])
                c.op("act", lambda e: e.activation(out=Btok[i][:].rearrange("p g n -> p (g n)"), in_=pT[:, 0:512], func=AF.Copy), reads=[self.bps[1]], writes=[b_Bt[i]])
                c.op("dve", lambda e: e.tensor_tensor(out=u[i][:], in0=xtok[i][:], in1=dtt[i][:, dc].unsqueeze(2).to_broadcast([128, 32, 64]), op=ALU.mult), reads=[b_xtok[i], b_dtt[i]], writes=[b_u[i]])
                c.op("pool", lambda e: e.tensor_tensor(out=Vw[i][:], in0=u[i][:], in1=g_[:, 4, :].unsqueeze(2).to_broadcast([128, 32, 64]), op=ALU.mult), reads=[b_u[i], bg], writes=[b_Vw[i]])
            def groups_(ch, i, nxt):
                if nxt is not None:
                    prep_load(*nxt)
                xc, bxc = xcs[i], b_xc[i]
                g_, bg = gt[i], b_gt[i]
                dc = slice(d * 32, (d + 1) * 32)
                Y, bY = Yt[i], b_Yt[i]
                for g in range(4):
                    if g == 2 and nxt is not None:
                        prep(*nxt)
                    pcb, bpcb = self.ps[2], self.bps[2]
                    c.mm(pcb[:, 0:128], [(xc[:, 16 + g, :], xc[:, 20 + g, :])], reads=[bxc], writes=[bpcb])
                    sm, bsm = scm[g % 2], b_scm[g % 2]
                    c.op("dve", lambda e: e.tensor_tensor(out=sm[:], in0=pcb[:, 0:128], in1=tri, op=ALU.mult), reads=[bpcb, self.b_const], writes=[bsm])
                    yps, byps = self.ps[4 + g % 2], self.bps[4 + g % 2]
                    for e8 in range(8):
                        h = g * 8 + e8
                        pbc, bpbc = self.ps[6 + e8 // 4], self.bps[6 + e8 // 4]
                        c.mm(pbc[:, (e8 % 4) * 128:(e8 % 4 + 1) * 128], [(sel[:, h, :], acT[i][:]), (nacT[i][:], sel[:, h, :])], reads=[b_sel, b_acT[i]], writes=[bpbc])
                    for hb in range(2):
                        k4 = (g % 2) * 2 + hb
                        pbc, bpbc = self.ps[6 + hb], self.bps[6 + hb]
                        c.op("dve", lambda e: e.tensor_scalar(out=aa[k4][:], in0=pbc[:, :], scalar1=0.0, scalar2=None, op0=ALU.min), reads=[bpbc], writes=[b_aa[k4]])
                        c.op("act", lambda e: e.activation(out=EE[k4][:], in_=aa[k4][:], func=AF.Exp), reads=[b_aa[k4]], writes=[b_EE[k4]])
                        c.op("pool", lambda e: e.tensor_tensor(out=MT[k4][:].rearrange("p (h t) -> p h t", h=4), in0=EE[k4][:].rearrange("p (h t) -> p h t", h=4),
                                                               in1=sm[:].unsqueeze(1).to_broadcast([128, 4, 128]), op=ALU.mult), reads=[bsm, b_EE[k4]], writes=[b_MT[k4]])
                    for e8 in range(8):
                        h = g * 8 + e8
                        k4 = (g % 2) * 2 + e8 // 4
                        c.mm(yps[:, e8 * 64:(e8 + 1) * 64], [(MT[k4][:, (e8 % 4) * 128:(e8 % 4 + 1) * 128], u[i][:, h, :])], reads=[b_MT[k4], b_u[i]], writes=[byps])
                    pin, bpin = self.ps[3], self.bps[3]
                    c.mm(pin[:, :], [(xc[:, 20 + g, :], Sb[:, g, :])], reads=[bxc, b_S[g]], writes=[bpin])
                    y_, byi = yi[g % 2], b_yi[g % 2]
                    c.op("dve", lambda e: e.tensor_tensor(out=y_[:].rearrange("p (h d) -> p h d", d=64), in0=pin[:, :].rearrange("p (h d) -> p h d", d=64),
                                                          in1=g_[:, 3, g * 8:(g + 1) * 8].unsqueeze(2).to_broadcast([128, 8, 64]), op=ALU.mult), reads=[bpin, bg], writes=[byi])
                    c.op("dve", lambda e: e.tensor_tensor(out=Y[:, g * 512:(g + 1) * 512], in0=yps[:, :], in1=y_[:], op=ALU.add), reads=[byps, byi], writes=[bY])
                    if d == 0:
                        c.op("pool", lambda e: e.tensor_tensor(out=y_[:].rearrange("p (h d) -> p h d", d=64), in0=xtok[i][:, g * 8:(g + 1) * 8, :],
                                                               in1=dsk[:, g * 8:(g + 1) * 8].unsqueeze(2).to_broadcast([128, 8, 64]), op=ALU.mult), reads=[b_xtok[i], b_an, bY], writes=[byi])
                        c.op("pool", lambda e: e.tensor_tensor(out=Y[:, g * 512:(g + 1) * 512], in0=Y[:, g * 512:(g + 1) * 512], in1=y_[:], op=ALU.add), reads=[byi], writes=[bY])
                    pst, bpst = self.ps[3], self.bps[3]
                    c.mm(pst[:, :], [(Btok[i][:, g, :], Vw[i][:, g * 8:(g + 1) * 8, :].rearrange("p h d -> p (h d)"))], reads=[b_Bt[i], b_Vw[i]], writes=[bpst])
                    c.op("dve", lambda e: e.tensor_tensor(out=S32[:, g, :].rearrange("p (h d) -> p h d", d=64), in0=S32[:, g, :].rearrange("p (h d) -> p h d", d=64),
                                                          in1=g_[:, 5, g * 8:(g + 1) * 8].unsqueeze(2).to_broadcast([128, 8, 64]), op=ALU.mult), reads=[bg], writes=[b_S[g]])
                    c.op("dve", lambda e: e.tensor_tensor(out=S32[:, g, :], in0=S32[:, g, :], in1=pst[:, :], op=ALU.add), reads=[bpst], writes=[b_S[g]])
                    c.op("act", lambda e: e.activation(out=Sb[:, g, :], in_=S32[:, g, :], func=AF.Copy), reads=[], writes=[b_S[g]])
                c.dma("sp", Yd[d].ap()[ch], Y[:], reads=[bY], writes=[b_Y[d]])
            prep_load(order[0], 0)
            prep(order[0], 0)
            for idx, ch in enumerate(order):
                groups_(ch, idx % 2, (order[idx + 1], (idx + 1) % 2) if idx + 1 < len(order) else None)
        c.barrier()
        c.sb_release(self.mark0)
        Wo = c.sb([128, 16, 1024], BF16, "ssdWo"); b_wo = Buf()
        c.dma("pool", Wo[:], self.w["ssm_w_out"].ap()[j].rearrange("(k p) n -> p k n", p=128), writes=[b_wo])
        ng = c.sb([128, 2048], F32, "ssdng")
        c.dma("sp", ng[:], self.w["ssm_norm_g"].ap()[j:j + 1, :].partition_broadcast(128), writes=[b_wo])
        self.gated_out(li, need_ctx, Yd, b_Y, Zs, b_Z, 2048, 4, ng, Wo, b_wo, False)
        self.phase_end()

    def gated_out(self, li, need_ctx, Yd, b_Y, Zs, b_Z, W, ngroups, ng, Wo, b_wo, gate_after):
        c = self.c
        NCT, NT = self.NCT, self.NT
        KC = W // 128
        gs = W // ngroups
        G1x, bG1x = self.load_mod(li, 2, 0, "G1x")
        G1c, bG1c = self.load_mod(li, 2, 1, "G1c")
        yf = [c.sb([128, W], F32, "yf") for _ in range(2)]; yb = [c.sb([128, W], F32, "yb") for _ in range(2)]
        zt = [c.sb([128, W], BF16, "zt") for _ in range(2)]
        b_in = [Buf(), Buf()]
        jk = c.sb([128, W], BF16, "gjunk"); b_jk = Buf()
        st = [c.sb([128, 2, 8], F32, "gst") for _ in range(2)]; b_st = [Buf(), Buf()]
        yn = [c.sb([128, W], BF16, "yn") for _ in range(2)]; b_yn = [Buf(), Buf()]
        yT = [c.sb([128, KC, 128], BF16, "gyT") for _ in range(2)]; b_yT = [Buf(), Buf()]
        xts = [c.sb([128, D], F32, "xres") for _ in range(2)]; b_xt = [Buf(), Buf()]
        tmps = [c.sb([128, D], F32, "rtmp") for _ in range(2)]; b_tmp = [Buf(), Buf()]
        for bi, qb in enumerate(range(0 if need_ctx else NCT, NT)):
            i = bi % 2
            is_ctx = qb < NCT
            c.dma("sp", yf[i][:], Yd[0].ap()[qb], reads=[b_Y[0]], writes=[b_in[i]])
            c.dma("sp", yb[i][:], Yd[1].ap()[qb], reads=[b_Y[1]], writes=[b_in[i]])
            c.dma("sp", zt[i][:], Zs.ap()[qb], reads=[b_Z], writes=[b_in[i]])
            c.dma("sp", xts[i][:], self.xs.ap()[qb * 128:(qb + 1) * 128, :], reads=[self.b_xs[qb]], writes=[b_xt[i]])
            c.op("pool", lambda e: e.tensor_tensor(out=yf[i][:], in0=yf[i][:], in1=yb[i][:], op=ALU.add), reads=[b_in[i]], writes=[b_in[i]])
            if not gate_after:
                c.op("dve", lambda e: e.tensor_tensor(out=yf[i][:], in0=yf[i][:], in1=zt[i][:], op=ALU.mult), reads=[b_in[i]], writes=[b_in[i]])
            c.op("dve", lambda e: e.memset(st[i][:], 0.0), writes=[b_st[i]])
            for g in range(ngroups):
                c.op("act", lambda e: e.activation(out=jk[:, g * gs:(g + 1) * gs], in_=yf[i][:, g * gs:(g + 1) * gs], func=AF.Square, accum_out=st[i][:, 0, g:g + 1]), reads=[b_in[i]], writes=[b_jk, b_st[i]])
            c.op("dve", lambda e: e.tensor_scalar(out=st[i][:, 1, :], in0=st[i][:, 0, :], scalar1=1.0 / gs, scalar2=EPS, op0=ALU.mult, op1=ALU.add), reads=[b_st[i]], writes=[b_st[i]])
            c.op("act", lambda e: e.activation(out=st[i][:, 1, :], in_=st[i][:, 1, :], func=AF.Sqrt), reads=[b_st[i]], writes=[b_st[i]])
            c.op("dve", lambda e: e.reciprocal(out=st[i][:, 1, :], in_=st[i][:, 1, :]), reads=[b_st[i]], writes=[b_st[i]])
            c.op("dve", lambda e: e.tensor_tensor(out=yf[i][:].rearrange("p (g d) -> p g d", g=ngroups), in0=yf[i][:].rearrange("p (g d) -> p g d", g=ngroups),
                                                  in1=st[i][:, 1, 0:ngroups].unsqueeze(2).to_broadcast([128, ngroups, gs]), op=ALU.mult), reads=[b_st[i], b_in[i]], writes=[b_in[i]])
            if gate_after:
                c.op("pool", lambda e: e.tensor_tensor(out=yf[i][:], in0=yf[i][:], in1=ng[:], op=ALU.mult), reads=[b_in[i], b_wo], writes=[b_in[i]])
                c.op("dve", lambda e: e.tensor_tensor(out=yn[i][:], in0=yf[i][:], in1=zt[i][:], op=ALU.mult), reads=[b_in[i]], writes=[b_yn[i]])
            else:
                c.op("pool", lambda e: e.tensor_tensor(out=yn[i][:], in0=yf[i][:], in1=ng[:], op=ALU.mult), reads=[b_in[i], b_wo], writes=[b_yn[i]])
            for hb in range(KC // 8):
                pT = self.ps[hb].ap().bitcast(BF16)
                c.tr_multi([(pT[:, k * 128:(k + 1) * 128], yn[i][:, (hb * 8 + k) * 128:(hb * 8 + k + 1) * 128], self.identb) for k in range(8)], reads=[b_yn[i], self.b_const], writes=[self.bps[hb]])
                c.op("act", lambda e: e.activation(out=yT[i][:, hb * 8:(hb + 1) * 8, :], in_=pT.rearrange("p (k n) -> p k n", k=8), func=AF.Copy), reads=[self.bps[hb]], writes=[b_yT[i]])
            G1, bG1 = (G1c, bG1c) if is_ctx else (G1x, bG1x)
            for nn in range(2):
                z, bz = self.ps[5 + nn], self.bps[5 + nn]
                c.mm(z[:, :], [(yT[i][:, k, :], Wo[:, k, nn * 512:(nn + 1) * 512]) for k in range(KC)], reads=[b_yT[i], b_wo], writes=[bz])
                c.op("dve", lambda e: e.tensor_tensor(out=tmps[i][:, nn * 512:(nn + 1) * 512], in0=z[:, :], in1=G1[:, nn * 512:(nn + 1) * 512], op=ALU.mult), reads=[bz, bG1], writes=[b_tmp[i]])
            c.op("pool", lambda e: e.tensor_tensor(out=xts[i][:], in0=xts[i][:], in1=tmps[i][:], op=ALU.add), reads=[b_tmp[i], b_xt[i]], writes=[b_xt[i]])
            c.dma("sp", self.xs.ap()[qb * 128:(qb + 1) * 128, :], xts[i][:], reads=[b_xt[i]], writes=[self.b_xs[qb]])

    def layer_mlstm(self, li, j, need_ctx):
        c, nc = self.c, self.nc
        T, NT, NCT = self.T, self.NT, self.NCT
        w_in = self.w["mlstm_w_in"].ap()[j]
        QK = self.scratch(f"ml_qk{li}", [2, 8, 64, T], BF16)
        Kt = self.scratch(f"ml_kt{li}", [NT, 128, 512], BF16)
        Va = self.scratch(f"ml_va{li}", [NT, 128, 8, 129], BF16)
        Os = self.scratch(f"ml_os{li}", [NT, 128, 1024], BF16)
        Gt = self.scratch(f"ml_gt{li}", [NT, 128, 32], F32)
        Yd = [self.scratch(f"ml_y{li}_{d}", [NT, 128, 1024], F32) for d in range(2)]
        b_QK = Buf(); b_Kt = Buf(); b_Va = Buf(); b_Os = Buf(); b_Gt = Buf(); b_Y = [Buf(), Buf()]
        b_w = Buf("ml_w")
        Win = c.sb([128, 8, 3104], BF16, "mlWin")
        for k in range(8):
            c.dma("pool", Win[:, k, :], w_in[k * 128:(k + 1) * 128, :], writes=[b_w])
        gb = c.sb([128, 32], F32, "mlgb")
        c.dma("sp", gb[:], self.w["mlstm_gate_b"].ap()[j:j + 1].rearrange("o a h -> o (a h)").partition_broadcast(128), writes=[b_w])
        A1x, bA1x = self.load_mod(li, 1, 0, "A1x")
        S1x, bS1x = self.load_mod(li, 0, 0, "S1x")
        A1c, bA1c = self.load_mod(li, 1, 1, "A1c")
        S1c, bS1c = self.load_mod(li, 0, 1, "S1c")
        self.alloc_norm_bufs(2)
        hTs = [c.sb([128, 8, 512], BF16, "hT") for _ in range(2)]
        b_hT = [Buf(), Buf()]
        stg = [c.sb([64, 512], BF16, "mlstg") for _ in range(3)]; b_stg = [Buf() for _ in range(3)]
        kst = [c.sb([128, 512], BF16, "mlkst") for _ in range(2)]; b_kst = [Buf(), Buf()]
        vst = [c.sb([128, 8, 129], BF16, "mlvst") for _ in range(2)]; b_vst = [Buf(), Buf()]
        ost = [c.sb([128, 1024], BF16, "mlost") for _ in range(2)]; b_ost = [Buf(), Buf()]
        gst = [c.sb([128, 32], F32, "mlgst") for _ in range(2)]; b_gst = [Buf(), Buf()]
        gtmp = [c.sb([128, 8], F32, "mlgtmp") for _ in range(2)]
        for i in range(2):
            c.op("pool", lambda e: e.memset(vst[i][:], 1.0), writes=[b_vst[i]])
        cnt = 0
        for gi, (t0, n, is_ctx) in enumerate(self.groups(4)):
            ncols = n * 128
            col0 = t0 * 128
            hT, bh = hTs[gi % 2], b_hT[gi % 2]
            for s in range(n):
                if is_ctx:
                    self.norm_tile(t0 + s, A1c, bA1c, S1c, bS1c, hT, bh, s * 128, (t0 + s) % 2)
                else:
                    self.norm_tile(t0 + s, A1x, bA1x, S1x, bS1x, hT, bh, s * 128, (t0 + s) % 2)
            for qk in range(2):
                for h in range(8):
                    i = cnt % 3
                    cnt += 1
                    P, bP = self.ps[2 + i], self.bps[2 + i]
                    col = qk * 512 + h * 64
                    c.mm(P[0:64, :ncols], [(Win[:, k, col:col + 64], hT[:, k, :ncols]) for k in range(8)], reads=[b_w, bh], writes=[bP])
                    c.op("act", lambda e: e.activation(out=stg[i][:, :ncols], in_=P[0:64, :ncols], func=AF.Copy, scale=(0.125 if qk == 1 else 1.0)), reads=[bP], writes=[b_stg[i]])
                    c.dma("sp", QK.ap()[qk, h, :, col0:col0 + ncols], stg[i][:, :ncols], reads=[b_stg[i]], writes=[b_QK])
            for s in range(n):
                sl = slice(s * 128, (s + 1) * 128)
                i2 = s % 2

                def tokproj(c0, w_):
                    nonlocal cnt
                    i = cnt % 3
                    cnt += 1
                    P, bP = self.ps[2 + i], self.bps[2 + i]
                    c.mm(P[:, :w_], [(hT[:, k, sl], Win[:, k, c0:c0 + w_]) for k in range(8)], reads=[b_w, bh], writes=[bP])
                    return P, bP
                P, bP = tokproj(512, 512)
                c.op("act", lambda e: e.activation(out=kst[i2][:], in_=P[:, :], func=AF.Copy, scale=0.125), reads=[bP], writes=[b_kst[i2]])
                c.dma("sp", Kt.ap()[t0 + s], kst[i2][:], reads=[b_kst[i2]], writes=[b_Kt])
                for vh in range(2):
                    P, bP = tokproj(1024 + vh * 512, 512)
                    c.op("act", lambda e: e.activation(out=vst[i2][:, vh * 4:(vh + 1) * 4, 0:128], in_=P[:, :].rearrange("p (h d) -> p h d", d=128), func=AF.Copy), reads=[bP], writes=[b_vst[i2]])
                c.dma("sp", Va.ap()[t0 + s], vst[i2][:], reads=[b_vst[i2]], writes=[b_Va])
                for oh in range(2):
                    P, bP = tokproj(2048 + oh * 512, 512)
                    c.op("act", lambda e: e.activation(out=ost[i2][:, oh * 512:(oh + 1) * 512], in_=P[:, :], func=AF.Sigmoid), reads=[bP], writes=[b_ost[i2]])
                c.dma("sp", Os.ap()[t0 + s], ost[i2][:], reads=[b_ost[i2]], writes=[b_Os])
                P, bP = tokproj(3072, 32)
                g_ = gst[i2]
                c.op("dve", lambda e: e.tensor_tensor(out=g_[:], in0=P[:, 0:32], in1=gb[:], op=ALU.add), reads=[bP, b_w], writes=[b_gst[i2]])
                for r in (1, 3):
                    cs_ = slice(r * 8, (r + 1) * 8)
                    c.op("act", lambda e: e.activation(out=g_[:, cs_], in_=g_[:, cs_], func=AF.Exp, scale=-1.0), reads=[b_gst[i2]], writes=[b_gst[i2]])
                    c.op("act", lambda e: e.activation(out=g_[:, cs_], in_=g_[:, cs_], func=AF.Ln, bias=1.0), reads=[b_gst[i2]], writes=[b_gst[i2]])
                    c.op("act", lambda e: e.activation(out=g_[:, cs_], in_=g_[:, cs_], func=AF.Copy, scale=-1.0), reads=[b_gst[i2]], writes=[b_gst[i2]])
                c.dma("sp", Gt.ap()[t0 + s], g_[:], reads=[b_gst[i2]], writes=[b_Gt])
        c.barrier()
        c.sb_release(self.mark0)
        sel = c.sb([8, 8, 128], F32, "sel8"); b_sel = Buf()
        c.dma("sp", sel[:], self.w["sel32"].ap()[0:8, 0:8, :], writes=[b_sel])
        qTs = [c.sb([64, 8, 128], BF16, "mqT") for _ in range(2)]; kTs = [c.sb([64, 8, 128], BF16, "mkT") for _ in range(2)]
        kts = [c.sb([128, 512], BF16, "mkt") for _ in range(2)]; vas = [c.sb([128, 8, 129], BF16, "mva") for _ in range(2)]
        gts = [c.sb([128, 32], F32, "mgt") for _ in range(2)]; b_ld = [Buf(), Buf()]
        GM = [c.sb([8, 12, 128], F32, "GM") for _ in range(2)]; b_GM = [Buf(), Buf()]
        sm8 = [c.sb([8, 8], F32, "sm8") for _ in range(2)]; b_sm8 = [Buf(), Buf()]
        ms = c.sb([8, 2], F32, "ms"); b_ms = Buf()
        dg = c.sb([8, 8], F32, "dg"); b_dg = Buf()
        tk = [c.sb([128, 40], F32, "tk") for _ in range(2)]; b_tk = [Buf(), Buf()]
        cwc = [c.sb([64, 8], F32, "cwc") for _ in range(2)]; b_cwc = [Buf(), Buf()]
        scm = [c.sb([128, 512], F32, "mscm") for _ in range(2)]; b_scm = [Buf() for _ in range(2)]
        aa = [c.sb([128, 512], F32, "maa") for _ in range(2)]; b_aa = [Buf() for _ in range(2)]
        EE = [c.sb([128, 512], F32, "mEE") for _ in range(2)]; b_EE = [Buf() for _ in range(2)]
        MT = [c.sb([128, 512], BF16, "mMT") for _ in range(2)]; b_MT = [Buf() for _ in range(2)]
        yi = [c.sb([128, 129], F32, "myi") for _ in range(4)]; b_yi = [Buf() for _ in range(4)]
        nd4 = [c.sb([128, 4, 132], F32, "mnd4") for _ in range(2)]; b_nd4 = [Buf() for _ in range(2)]
        Vw = [c.sb([128, 129], BF16, "mVw") for _ in range(4)]; b_Vw = [Buf() for _ in range(4)]
        Yt = [c.sb([128, 1024], F32, "mYt") for _ in range(2)]; b_Yt = [Buf(), Buf()]
        S32 = c.sb([64, 8, 129], F32, "mS32"); Sb = c.sb([64, 8, 129], BF16, "mSb"); b_S = [Buf() for _ in range(8)]
        ci = 0; hi = 0
        id8 = self.ident32[0:8, 0:8]
        for d in range(2):
            tri = self.trile32 if d == 0 else self.trige32
            order = list(range(NT)) if d == 0 else (list(range(NCT - 1, -1, -1)) + list(range(NT - 1, NCT - 1, -1)))
            c.op("dve", lambda e: e.memset(S32[:], 0.0), writes=b_S)
            c.op("pool", lambda e: e.memset(Sb[:], 0.0), writes=b_S)
            c.op("dve", lambda e: e.memset(ms[:], 0.0), writes=[b_ms])
            endc = 127 if d == 0 else 0
            def gate_load(ch, i):
                cols = slice(ch * 128, (ch + 1) * 128)
                bl = b_ld[i]
                c.dma("sp", qTs[i][:], QK.ap()[0, :, :, cols].rearrange("h d t -> d h t"), reads=[b_QK], writes=[bl])
                c.dma("sp", kTs[i][:], QK.ap()[1, :, :, cols].rearrange("h d t -> d h t"), reads=[b_QK], writes=[bl])
                c.dma("sp", kts[i][:], Kt.ap()[ch], reads=[b_Kt], writes=[bl])
                c.dma("sp", vas[i][:], Va.ap()[ch], reads=[b_Va], writes=[bl])
                c.dma("sp", gts[i][:], Gt.ap()[ch], reads=[b_Gt], writes=[bl])

            def gate(ch, i):
                bl = b_ld[i]
                ig = gts[i][:, d * 16:d * 16 + 8]
                lf = gts[i][:, d * 16 + 8:d * 16 + 16]
                G, bG = GM[i], b_GM[i]
                s8, bs8 = sm8[i], b_sm8[i]
                t_, bt = tk[i], b_tk[i]
                p0, bp0 = self.ps[0], self.bps[0]
                c.mm(p0[0:8, 0:128], [(ig, self.ident32)], reads=[bl, self.b_const], writes=[bp0])
                c.mm(p0[0:8, 128:256], [(lf, tri)], reads=[bl, self.b_const], writes=[bp0])
                c.mm(p0[:, 256:264], [(tri, lf)], reads=[bl, self.b_const], writes=[bp0])
                c.op("act", lambda e: e.activation(out=G[:, 0:2, :], in_=p0[0:8, 0:256].rearrange("p (a t) -> p a t", a=2), func=AF.Copy), reads=[bp0], writes=[bG])
                c.op("act", lambda e: e.activation(out=t_[:, 32:40], in_=p0[:, 256:264], func=AF.Copy), reads=[bp0], writes=[bt])
                c.op("dve", lambda e: e.tensor_tensor(out=t_[:, 0:8], in0=ig, in1=t_[:, 32:40], op=ALU.subtract), reads=[bl, bt], writes=[bt])
                c.op("dve", lambda e: e.tensor_tensor(out=G[:, 2, :], in0=G[:, 0, :], in1=G[:, 1, :], op=ALU.subtract), reads=[bG], writes=[bG])
                src, dst = 2, 3
                for k in range(7):
                    sft = 1 << k
                    c.op("dve", lambda e: e.tensor_copy(out=G[:, dst, :], in_=G[:, src, :]), reads=[bG], writes=[bG])
                    if d == 0:
                        c.op("dve", lambda e: e.tensor_tensor(out=G[:, dst, sft:128], in0=G[:, src, sft:128], in1=G[:, src, 0:128 - sft], op=ALU.max), reads=[bG], writes=[bG])
                    else:
                        c.op("dve", lambda e: e.tensor_tensor(out=G[:, dst, 0:128 - sft], in0=G[:, src, 0:128 - sft], in1=G[:, src, sft:128], op=ALU.max), reads=[bG], writes=[bG])
                    src, dst = dst, (3 if dst == 4 else 4)
                cmr = src
                c.op("dve", lambda e: e.tensor_scalar(out=G[:, cmr, :], in0=G[:, cmr, :], scalar1=ms[:, 0:1], scalar2=None, op0=ALU.max), reads=[bG, b_ms], writes=[bG])
                c.op("act", lambda e: e.activation(out=G[:, 5, :], in_=G[:, cmr, :], func=AF.Copy, scale=-1.0), reads=[bG], writes=[bG])
                c.op("act", lambda e: e.activation(out=G[:, 6, :], in_=G[:, cmr, :], func=AF.Exp, scale=-1.0, bias=ms[:, 0:1]), reads=[bG, b_ms], writes=[bG])
                c.op("dve", lambda e: e.tensor_tensor(out=G[:, 7, :], in0=G[:, 1, :], in1=G[:, cmr, :], op=ALU.add), reads=[bG], writes=[bG])
                c.op("act", lambda e: e.activation(out=G[:, 7, :], in_=G[:, 7, :], func=AF.Exp, scale=-1.0), reads=[bG], writes=[bG])
                c.op("dve", lambda e: e.tensor_copy(out=s8[:, 0:1], in_=G[:, cmr, endc:endc + 1]), reads=[bG], writes=[bs8])
                c.op("dve", lambda e: e.tensor_scalar(out=s8[:, 1:2], in0=s8[:, 0:1], scalar1=-1.0, scalar2=None, op0=ALU.mult), reads=[bs8], writes=[bs8])
                c.op("dve", lambda e: e.tensor_copy(out=s8[:, 2:3], in_=G[:, 1, endc:endc + 1]), reads=[bG], writes=[bs8])
                c.op("act", lambda e: e.activation(out=G[:, 8, :], in_=G[:, 2, :], func=AF.Exp, bias=s8[:, 1:2]), reads=[bG, bs8], writes=[bG])
                c.op("act", lambda e: e.activation(out=s8[:, 3:4], in_=ms[:, 0:1], func=AF.Exp, bias=s8[:, 1:2]), reads=[b_ms, bs8], writes=[bs8])
                c.op("dve", lambda e: e.tensor_tensor(out=ms[:, 0:1], in0=s8[:, 2:3], in1=s8[:, 0:1], op=ALU.add), reads=[bs8, bG], writes=[b_ms])
                p1, bp1 = self.ps[1], self.bps[1]
                c.mm_multi([(p1[:, (r - 6) * 8:(r - 5) * 8], [(G[:, r, :], id8)]) for r in (6, 7, 8)], reads=[bG, self.b_const], writes=[bp1])
                c.op("act", lambda e: e.activation(out=t_[:, 8:32], in_=p1[:, 0:24], func=AF.Copy), reads=[bp1], writes=[bt])
                c.op("dve", lambda e: e.tensor_scalar(out=dg[:], in0=id8, scalar1=s8[:, 3:4], scalar2=None, op0=ALU.mult), reads=[self.b_const, bs8], writes=[b_dg])
                c.mm(p1[0:64, 32:40], [(self.ones32[0:8, 0:64], dg[:])], reads=[self.b_const, b_dg], writes=[bp1])
                c.op("act", lambda e: e.activation(out=cwc[i][:], in_=p1[0:64, 32:40], func=AF.Copy), reads=[bp1], writes=[b_cwc[i]])
            def heads(ch, i, nxt):
                if nxt is not None:
                    gate_load(*nxt)
                bl = b_ld[i]; G, bG = GM[i], b_GM[i]; t_, bt = tk[i], b_tk[i]
                Y, bY = Yt[i], b_Yt[i]
                for hb0 in (0, 4):
                    if hb0 == 4 and nxt is not None:
                        gate(*nxt)
                    psc, bpsc = self.ps[2], self.bps[2]
                    pbc, bpbc = self.ps[6], self.bps[6]
                    for q4 in range(4):
                        h = hb0 + q4
                        cs4 = slice(q4 * 128, (q4 + 1) * 128)
                        c.mm(psc[:, cs4], [(kTs[i][:, h, :], qTs[i][:, h, :])], reads=[bl], writes=[bpsc])
                        c.mm(pbc[:, cs4], [(sel[:, h, :], G[:, 5, :]), (G[:, 2, :], sel[:, h, :])], reads=[b_sel, bG], writes=[bpbc])
                    pins = []
                    for q4 in range(4):
                        h = hb0 + q4
                        bk = 3 if q4 < 2 else 7
                        pin_ap = self.ps[bk][:, (q4 % 2) * 256:(q4 % 2) * 256 + 129]
                        c.mm(pin_ap, [(qTs[i][:, h, :], Sb[:, h, :])], reads=[bl, b_S[h]], writes=[self.bps[bk]])
                        pins.append((pin_ap, self.bps[bk]))
                    k4 = (hb0 // 4)
                    c.op("dve", lambda e: e.tensor_tensor(out=scm[k4][:].rearrange("p (h t) -> p h t", h=4), in0=psc[:, :].rearrange("p (h t) -> p h t", h=4),
                                                          in1=tri.unsqueeze(1).to_broadcast([128, 4, 128]), op=ALU.mult), reads=[bpsc, self.b_const], writes=[b_scm[k4]])
                    c.op("dve", lambda e: e.tensor_scalar(out=aa[k4][:], in0=pbc[:, :], scalar1=0.0, scalar2=None, op0=ALU.min), reads=[bpbc], writes=[b_aa[k4]])
                    c.op("act", lambda e: e.activation(out=EE[k4][:], in_=aa[k4][:], func=AF.Exp), reads=[b_aa[k4]], writes=[b_EE[k4]])
                    c.op("pool", lambda e: e.tensor_tensor(out=MT[k4][:], in0=scm[k4][:], in1=EE[k4][:], op=ALU.mult), reads=[b_scm[k4], b_EE[k4]], writes=[b_MT[k4]])
                    for q4 in range(4):
                        h = hb0 + q4
                        pin_ap, bpin = pins[q4]
                        c.op("act", lambda e: e.activation(out=yi[q4][:], in_=pin_ap, func=AF.Copy, scale=t_[:, 8 + h:9 + h]), reads=[bpin, bt], writes=[b_yi[q4]])
                        c.op("pool", lambda e: e.tensor_tensor(out=Vw[q4][:], in0=vas[i][:, h, :], in1=t_[:, 24 + h:25 + h].to_broadcast([128, 129]), op=ALU.mult), reads=[bl, bt], writes=[b_Vw[q4]])
                    pnds = []; psts = []
                    for q4 in range(4):
                        h = hb0 + q4
                        bk = 4 + q4 // 2
                        pnd_ap = self.ps[bk][:, (q4 % 2) * 256:(q4 % 2) * 256 + 129]
                        c.mm(pnd_ap, [(MT[k4][:, q4 * 128:(q4 + 1) * 128], vas[i][:, h, :])], reads=[b_MT[k4], bl], writes=[self.bps[bk]])
                        pnds.append((pnd_ap, self.bps[bk]))
                    for q4 in range(4):
                        h = hb0 + q4
                        if q4 < 3:
                            pst_ap, bpst = self.ps[1][0:64, q4 * 129:(q4 + 1) * 129], self.bps[1]
                        else:
                            pst_ap, bpst = self.ps[0][0:64, 264:393], self.bps[0]
                        c.mm(pst_ap, [(kts[i][:, h * 64:(h + 1) * 64], Vw[q4][:])], reads=[bl, b_Vw[q4]], writes=[bpst])
                        psts.append((pst_ap, bpst))
                    n4 = nd4[k4]; bn = b_nd4[k4]
                    for q4 in range(4):
                        pnd_ap, bpnd = pnds[q4]
                        c.op("dve", lambda e: e.tensor_tensor(out=n4[:, q4, 0:129], in0=pnd_ap, in1=yi[q4][:], op=ALU.add), reads=[bpnd, b_yi[q4]], writes=[bn])
                    c.op("dve", lambda e: e.tensor_scalar(out=n4[:, :, 129:130], in0=n4[:, :, 128:129], scalar1=-1.0, scalar2=None, op0=ALU.mult), reads=[bn], writes=[bn])
                    c.op("dve", lambda e: e.tensor_tensor(out=n4[:, :, 130:131], in0=n4[:, :, 128:129], in1=n4[:, :, 129:130], op=ALU.max), reads=[bn], writes=[bn])
                    c.op("dve", lambda e: e.tensor_tensor(out=n4[:, :, 130:131], in0=n4[:, :, 130:131], in1=t_[:, 16 + hb0:20 + hb0].unsqueeze(2), op=ALU.max), reads=[bn, bt], writes=[bn])
                    c.op("dve", lambda e: e.reciprocal(out=n4[:, :, 131:132], in_=n4[:, :, 130:131]), reads=[bn], writes=[bn])
                    c.op("dve", lambda e: e.tensor_tensor(out=Y[:, hb0 * 128:(hb0 + 4) * 128].rearrange("p (h d) -> p h d", h=4), in0=n4[:, :, 0:128],
                                                          in1=n4[:, :, 131:132].to_broadcast([128, 4, 128]), op=ALU.mult), reads=[bn], writes=[bY])
                    for q4 in range(4):
                        h = hb0 + q4
                        pst_ap, bpst = psts[q4]
                        c.op("dve", lambda e: e.scalar_tensor_tensor(out=S32[:, h, :], in0=S32[:, h, :], scalar=cwc[i][:, h:h + 1], in1=pst_ap, op0=ALU.mult, op1=ALU.add), reads=[b_cwc[i], bpst], writes=[b_S[h]])
                        c.op("act", lambda e: e.activation(out=Sb[:, h, :], in_=S32[:, h, :], func=AF.Copy), reads=[], writes=[b_S[h]])
                c.dma("sp", Yd[d].ap()[ch], Y[:], reads=[bY], writes=[b_Y[d]])
            gate_load(order[0], 0)
            gate(order[0], 0)
            for idx, ch in enumerate(order):
                heads(ch, idx % 2, (order[idx + 1], (idx + 1) % 2) if idx + 1 < len(order) else None)
        c.barrier()
        c.sb_release(self.mark0)
        Wo = c.sb([128, 8, 1024], BF16, "mlWo"); b_wo = Buf()
        c.dma("pool", Wo[:], self.w["mlstm_w_out"].ap()[j].rearrange("(k p) n -> p k n", p=128), writes=[b_wo])
        ng = c.sb([128, 1024], F32, "mlng")
        c.dma("sp", ng[:], self.w["mlstm_norm_g"].ap()[j:j + 1, :].partition_broadcast(128), writes=[b_wo])
        self.gated_out(li, need_ctx, Yd, b_Y, Os, b_Os, 1024, 8, ng, Wo, b_wo, True)
        self.phase_end()

    def build(self, wshapes):
        self.inp("x", [self.NL, D])
        self.inp("ctx", [self.NCX, D])
        self.inp("cT", [128, 8, 2])
        self.inp("cmat", [128, 4, 128])
        self.inp("sel32", [32, 32, 128])
        self.inp("rope64", [2, 64, self.NL])
        self.inp("rope32", [2, 32, self.NL])
        self.inp("rope96", [2, 96, self.NL])
        self.inp("ssm_conv_wT", [wshapes["ssm_conv_w"][0], 128, 24, 5])
        self.inp("ssm_conv_bT", [wshapes["ssm_conv_w"][0], 128, 24])
        for k, s in wshapes.items():
            self.inp(k, s)
        self.out = self.nc.dram_tensor("out", [self.NL, D], F32, kind="ExternalOutput")
        self.setup_consts()
        self.prologue()
        cnt = {0: 0, 1: 0, 2: 0, 3: 0, 9: 0}
        for li, kind in enumerate(self.kinds):
            need_ctx = li < self.depth - 1
            j = cnt[kind]
            cnt[kind] += 1
            self.want_precast = li
            if kind == 0:
                self.layer_gqa(li, j, need_ctx)
            elif kind == 1:
                self.layer_ssd(li, j, need_ctx)
            elif kind == 2:
                self.layer_mlstm(li, j, need_ctx)
            elif kind == 3:
                self.layer_mla(li, j, need_ctx)
            self.layer_moe(li, need_ctx)
        self.final()
        return self.nc


WEIGHT_KEYS = ["norm1_g", "norm2_g", "w_mod", "b_mod", "moe_w_group", "moe_b_group", "moe_w_expert", "moe_b_expert",
               "moe_w_gate", "moe_w_up", "moe_w_down", "attn_w_in", "attn_sink", "attn_w_out",
               "ssm_w_in", "ssm_conv_w", "ssm_conv_b", "ssm_dt_bias", "ssm_a_log", "ssm_d", "ssm_norm_g", "ssm_w_out",
               "mlstm_w_in", "mlstm_gate_b", "mlstm_norm_g", "mlstm_w_out",
               "mla_w_in", "mla_q_norm_g", "mla_w_q_up", "mla_kv_norm_g", "mla_w_kv_up", "mla_w_out", "final_norm_g"]


def run_model(inputs, kinds, n_cores=None):
    x = np.asarray(inputs["x"], np.float32)
    ctx = np.asarray(inputs["ctx"], np.float32)
    c = np.asarray(inputs["c"], np.float32)
    c_ctx = np.asarray(inputs["c_ctx"], np.float32)
    B, n_lat, _ = x.shape
    n_ctx = ctx.shape[1]
    weights = {k: np.ascontiguousarray(np.asarray(inputs[k], np.float32)) for k in WEIGHT_KEYS}
    m = Model(n_lat, n_ctx, kinds)
    nc = m.build({k: v.shape for k, v in weights.items()})
    consts = host_consts(n_lat)
    in_maps = []
    for b in range(B):
        cT = np.stack([c[b].reshape(8, 128).T, c_ctx.reshape(8, 128).T], axis=-1)
        d = {"x": np.ascontiguousarray(x[b]), "ctx": np.ascontiguousarray(ctx[b]), "cT": np.ascontiguousarray(cT.astype(np.float32))}
        d.update(consts)
        d.update(weights)
        d["ssm_conv_wT"] = np.ascontiguousarray(weights["ssm_conv_w"].reshape(-1, 5, 24, 128).transpose(0, 3, 2, 1))
        d["ssm_conv_bT"] = np.ascontiguousarray(weights["ssm_conv_b"].reshape(-1, 24, 128).transpose(0, 2, 1))
        in_maps.append(d)
    res = run_bass_kernel_spmd(nc, in_maps, core_ids=list(range(B)))
    return np.stack([np.asarray(r["out"], np.float32) for r in res.results], axis=0)


def kernel(**inputs):
    return run_model(inputs, [0, 1, 2, 3])
```

```python
import numpy as np
import concourse.bass as bass
import concourse.mybir as mybir

F32 = mybir.dt.float32
BF16 = mybir.dt.bfloat16
AF = mybir.ActivationFunctionType
ALU = mybir.AluOpType
AX = mybir.AxisListType


class Buf:
    __slots__ = ("w", "r", "name")

    def __init__(self, name=""):
        self.w = None
        self.r = []
        self.name = name


class Ctx:
    EPOCH = 30000

    def __init__(self, nc, n_dma=None):
        self.nc = nc
        self.E = {"pe": nc.tensor, "act": nc.scalar, "dve": nc.vector, "pool": nc.gpsimd, "sp": nc.sync}
        self.csem = {}
        self.seen = {e: {} for e in self.E}
        self.semid = {}
        n_dma = n_dma or {"sp": 48, "act": 2, "pool": 30}
        self.dslots = {}
        self.drr = {}
        for q, n in n_dma.items():
            self.dslots[q] = [[self._new_sem(f"d{q}{i}"), 0] for i in range(n)]
            self.drr[q] = 0
        for e in ("pe", "act", "dve", "pool"):
            self.csem[e] = [self._new_sem(f"c{e}0"), 0, 0]
        self.sb_off = 0
        self.sb_base = 16512
        self.sb_cap = 229344 - 16512
        self.n_alloc = 0
        self.n_ins = 0
        self.n_wait = 0

    def _new_sem(self, name):
        s = self.nc.alloc_semaphore(name)
        self.semid[id(s)] = s
        return s

    def sb_mark(self):
        return self.sb_off

    def sb_release(self, mark):
        self.sb_off = mark

    def sb(self, shape, dtype, name=None):
        esz = 4 if dtype == F32 else 2
        if dtype in (mybir.dt.int32, mybir.dt.uint32):
            esz = 4
        n = 1
        for s in shape[1:]:
            n *= s
        nbytes = (n * esz + 63) // 64 * 64
        off = self.sb_off
        if off + nbytes > self.sb_cap:
            raise RuntimeError(f"SBUF overflow: want {nbytes} at {off} cap {self.sb_cap} ({name})")
        self.sb_off += nbytes
        self.n_alloc += 1
        t = self.nc.alloc_sbuf_tensor_at(f"{name or 't'}_{self.n_alloc}", list(shape), dtype, offset=self._abs(off))
        return t

    def _abs(self, off):
        return self.sb_base + off

    def _wait(self, eng, ev):
        if ev is None:
            return
        sem, val = ev
        k = id(sem)
        if self.seen[eng].get(k, 0) >= val:
            return
        self.E[eng].wait_ge(sem, val)
        self.n_wait += 1
        self.seen[eng][k] = val

    def _deps(self, eng, reads, writes):
        for b in reads:
            if b.w is not None and not (eng == "pe" and b.w[2] == "pe"):
                self._wait(eng, b.w[:2])
        for b in writes:
            if b.w is not None and not (eng == "pe" and b.w[2] == "pe"):
                self._wait(eng, b.w[:2])
            for r in b.r:
                if not (eng == "pe" and r[2] == "pe"):
                    self._wait(eng, r[:2])

    def _record(self, ev, reads, writes):
        for b in reads:
            b.r.append(ev)
            if len(b.r) > 64:
                b.r = b.r[-64:] if False else b.r
        for b in writes:
            b.w = ev
            b.r = []

    def _signal(self, eng, ins):
        st = self.csem[eng]
        if st[1] >= self.EPOCH:
            st = self.csem[eng] = [self._new_sem(f"c{eng}{st[2] + 1}"), 0, st[2] + 1]
        st[1] += 1
        ins.then_inc(st[0], 1)
        return (st[0], st[1], eng)

    def op(self, eng, fn, reads=(), writes=()):
        self._deps(eng, reads, writes)
        ins = fn(self.E[eng])
        self.n_ins += 1
        ev = self._signal(eng, ins)
        self._record(ev, reads, writes)
        return ev

    def mm(self, out, pairs, reads=(), writes=(), start=True, stop=True):
        self._deps("pe", reads, writes)
        n = len(pairs)
        ins = None
        for i, (l, r) in enumerate(pairs):
            ins = self.nc.tensor.matmul(out, l, r, start=(start and i == 0), stop=(stop and i == n - 1))
            self.n_ins += 1
        ev = self._signal("pe", ins)
        self._record(ev, reads, writes)
        return ev

    def mm_multi(self, groups, reads=(), writes=()):
        self._deps("pe", reads, writes)
        ins = None
        for out, pairs in groups:
            n = len(pairs)
            for i, (l, r) in enumerate(pairs):
                ins = self.nc.tensor.matmul(out, l, r, start=(i == 0), stop=(i == n - 1))
                self.n_ins += 1
        ev = self._signal("pe", ins)
        self._record(ev, reads, writes)
        return ev

    def tr(self, out, in_, ident, reads=(), writes=()):
        self._deps("pe", reads, writes)
        ins = self.nc.tensor.transpose(out, in_, ident)
        self.n_ins += 1
        ev = self._signal("pe", ins)
        self._record(ev, reads, writes)
        return ev

    def tr_multi(self, items, reads=(), writes=()):
        self._deps("pe", reads, writes)
        ins = None
        for out, in_, ident in items:
            ins = self.nc.tensor.transpose(out, in_, ident)
            self.n_ins += 1
        ev = self._signal("pe", ins)
        self._record(ev, reads, writes)
        return ev

    def dma(self, q, out, in_, reads=(), writes=()):
        self._deps(q, reads, writes)
        slots = self.dslots[q]
        i = self.drr[q]
        self.drr[q] = (i + 1) % len(slots)
        sl = slots[i]
        if sl[1] > 0:
            self._wait(q, (sl[0], sl[1]))
        ins = self.E[q].dma_start(out=out, in_=in_)
        self.n_ins += 1
        sl[1] += 16
        ins.then_inc(sl[0], 16)
        ev = (sl[0], sl[1], "dma")
        self._record(ev, reads, writes)
        return ev

    def barrier(self):
        evs = []
        for e, st in self.csem.items():
            if st[1] > 0:
                evs.append((st[0], st[1]))
        for q, slots in self.dslots.items():
            for sl in slots:
                if sl[1] > 0:
                    evs.append((sl[0], sl[1]))
        for eng in self.E:
            for ev in evs:
                self._wait(eng, ev)

    def finish(self, eng="sp"):
        evs = []
        for e, st in self.csem.items():
            if st[1] > 0:
                evs.append((st[0], st[1]))
        for q, slots in self.dslots.items():
            for sl in slots:
                if sl[1] > 0:
                    evs.append((sl[0], sl[1]))
        for ev in evs:
            self._wait(eng, ev)
from concourse.bass_utils import run_bass_kernel_spmd
D = 1024
EPS = 1e-6


def host_consts(n_lat):
    ident = np.eye(128, dtype=np.float32)
    ones = np.ones((128, 128), np.float32)
    j = np.arange(128)[:, None]
    i = np.arange(128)[None, :]
    tri_le = (j <= i).astype(np.float32)
    tri_ge = (j >= i).astype(np.float32)
    cm = np.stack([ident, ones, tri_le, tri_ge], axis=1)
    sel = np.zeros((32, 32, 128), np.float32)
    for h in range(32):
        sel[h, h, :] = 1.0
    rows = n_lat // 64
    row = np.repeat(np.arange(rows), 64).astype(np.float32)
    col = np.tile(np.arange(64), rows).astype(np.float32)

    def tab(rot_dim, nrep):
        q = rot_dim // 4
        inv = (10000.0 ** (-np.arange(q, dtype=np.float32) / q)).astype(np.float32)
        ang = np.concatenate([row[:, None] * inv, col[:, None] * inv], axis=-1).astype(np.float32)
        cs = np.cos(ang).astype(np.float32).T
        sn = np.sin(ang).astype(np.float32).T
        cs = np.concatenate([cs] * (2 * nrep), axis=0)
        sn = np.concatenate([sn] * (2 * nrep), axis=0)
        return np.ascontiguousarray(np.stack([cs, sn], axis=0))

    r32 = tab(32, 1)
    L = r32.shape[2]
    r96 = np.ascontiguousarray(np.concatenate([np.stack([np.ones((64, L), np.float32), np.zeros((64, L), np.float32)], axis=0), r32], axis=1))
    return {"cmat": np.ascontiguousarray(cm), "sel32": sel, "rope64": tab(64, 1), "rope32": r32, "rope96": r96}


class Model:
    def __init__(self, n_lat, n_ctx, kinds, debug=False):
        self.NL, self.NCX = n_lat, n_ctx
        self.T = n_lat + n_ctx
        self.NT = self.T // 128
        self.NCT = n_ctx // 128
        self.NLT = n_lat // 128
        self.kinds = kinds
        self.depth = len(kinds)
        self.nc = bass.Bass("TRN2", target_bir_lowering=False)
        self.c = Ctx(self.nc)
        self.w = {}

    def inp(self, name, shape, dtype=F32):
        t = self.nc.dram_tensor(name, list(shape), dtype, kind="ExternalInput")
        self.w[name] = t
        return t

    def scratch(self, name, shape, dtype):
        return self.nc.dram_tensor(name, list(shape), dtype)

    def declare(self, shapes):
        for k, s in shapes.items():
            self.inp(k, s)

    def groups(self, gsz, with_ctx=True):
        g = []
        if with_ctx:
            g.append((0, self.NCT, True))
        t = self.NCT
        while t < self.NT:
            n = min(gsz, self.NT - t)
            g.append((t, n, False))
            t += n
        return g

    def setup_consts(self):
        c, nc = self.c, self.nc
        self.cm32 = c.sb([128, 4, 128], F32, "cm32")
        self.cmb = c.sb([128, 4, 128], BF16, "cmb")
        self.b_const = Buf("const")
        c.dma("sp", self.cm32[:], self.w["cmat"].ap(), writes=[self.b_const])
        c.op("dve", lambda e: e.tensor_copy(out=self.cmb[:], in_=self.cm32[:]), reads=[self.b_const], writes=[self.b_const])
        self.ident32 = self.cm32[:, 0, :]
        self.ones32 = self.cm32[:, 1, :]
        self.trile32 = self.cm32[:, 2, :]
        self.trige32 = self.cm32[:, 3, :]
        self.identb = self.cmb[:, 0, :]
        self.onesb = self.cmb[:, 1, :]
        self.trileb = self.cmb[:, 2, :]
        self.trigeb = self.cmb[:, 3, :]
        self.ps = [nc.alloc_psum_tensor(f"psb{i}", [128, 512], F32) for i in range(8)]
        self.bps = [Buf(f"ps{i}") for i in range(8)]
        self.mark0 = c.sb_mark()

    def phase_end(self):
        self.c.barrier()
        self.c.sb_release(self.mark0)

    def prologue(self):
        c, nc = self.c, self.nc
        L = self.depth
        self.modv = self.scratch("modv", [L, 2, 6 * D], F32)
        self.xs = self.scratch("xs", [self.T, D], F32)
        cs = c.sb([128, 8, 2], F32, "cs")
        b_cs = Buf()
        c.dma("sp", cs[:], self.w["cT"].ap(), writes=[b_cs])
        c.op("act", lambda e: e.activation(out=cs[:], in_=cs[:], func=AF.Silu), reads=[b_cs], writes=[b_cs])
        b_xs = self.b_xs = [Buf(f"xs{t}") for t in range(self.NT)]
        c.dma("sp", self.xs.ap()[0:self.NCX, :], self.w["ctx"].ap(), writes=b_xs[0:self.NCT])
        c.dma("sp", self.xs.ap()[self.NCX:self.T, :], self.w["x"].ap(), writes=b_xs[self.NCT:])
        wm = [c.sb([128, 8, 512], F32, f"wm{i}") for i in range(2)]
        b_wm = [Buf(), Buf()]
        modsb = c.sb([2, 6 * D], F32, "modsb")
        b_mod = Buf()
        bmb = c.sb([2, 6 * D], F32, "bmb")
        gb = c.sb([2, 2, D], F32, "gb")
        b_misc = Buf()
        self.b_modv = [Buf(f"modv{i}") for i in range(L)]
        it = 0
        for li in range(L):
            c.dma("sp", bmb[:], self.w["b_mod"].ap()[li:li + 1, :].partition_broadcast(2), writes=[b_misc])
            c.dma("sp", gb[:, 0, :], self.w["norm1_g"].ap()[li:li + 1, :].partition_broadcast(2), writes=[b_misc])
            c.dma("sp", gb[:, 1, :], self.w["norm2_g"].ap()[li:li + 1, :].partition_broadcast(2), writes=[b_misc])
            for j in range(12):
                s = it % 2
                it += 1
                c.dma("sp", wm[s][:], self.w["w_mod"].ap()[li, :, j * 512:(j + 1) * 512].rearrange("(k p) n -> p k n", p=128), writes=[b_wm[s]])
                pb = it % 2
                c.mm(self.ps[pb][0:2, :], [(cs[:, k, :], wm[s][:, k, :]) for k in range(8)], reads=[b_cs, b_wm[s]], writes=[self.bps[pb]])
                c.op("dve", lambda e: e.tensor_tensor(out=modsb[:, j * 512:(j + 1) * 512], in0=self.ps[pb][0:2, :], in1=bmb[:, j * 512:(j + 1) * 512], op=ALU.add),
                     reads=[self.bps[pb], b_misc], writes=[b_mod])
            for (ch, gi) in ((1, 0), (4, 1)):
                c.op("dve", lambda e: e.scalar_tensor_tensor(out=modsb[:, ch * D:(ch + 1) * D], in0=modsb[:, ch * D:(ch + 1) * D], scalar=1.0, in1=gb[:, gi, :], op0=ALU.add, op1=ALU.mult),
                     reads=[b_mod, b_misc], writes=[b_mod])
            c.dma("sp", self.modv.ap()[li], modsb[:], reads=[b_mod], writes=[self.b_modv[li]])
        self.phase_end()

    def load_mod(self, li, chunk, row, name):
        c = self.c
        t = c.sb([128, D], F32, name)
        b = Buf(name)
        c.dma("sp", t[:], self.modv.ap()[li, row:row + 1, chunk * D:(chunk + 1) * D].partition_broadcast(128), reads=[self.b_modv[li]], writes=[b])
        return t, b

    def alloc_norm_bufs(self, nbuf=2):
        c = self.c
        if getattr(self, "want_precast", None) is not None:
            li_ = self.want_precast
            self.want_precast = None
            self.moe_precast(li_)
        self.nb = []
        for i in range(nbuf):
            d = dict(x=c.sb([128, D], F32, "nx"), bx=Buf(), junk=c.sb([128, D], BF16, "nj"), bj=Buf(),
                     st=c.sb([128, 2], F32, "nst"), bst=Buf(), tmp=c.sb([128, D], F32, "ntmp"), btmp=Buf(),
                     h=c.sb([128, D], BF16, "nh"), bh=Buf())
            self.nb.append(d)
        self.nbi = 0

    def norm_tile(self, tile, A, bA, B, bB, hT, b_hT, col0, psb, want_x=False):
        c = self.c
        d = self.nb[self.nbi % len(self.nb)]
        self.nbi += 1
        c.dma("sp", d["x"][:], self.xs.ap()[tile * 128:(tile + 1) * 128, :], reads=[self.b_xs[tile]], writes=[d["bx"]])
        c.op("dve", lambda e: e.memset(d["st"][:], 0.0), writes=[d["bst"]])
        c.op("act", lambda e: e.activation(out=d["junk"][:], in_=d["x"][:], func=AF.Square, accum_out=d["st"][:, 0:1]), reads=[d["bx"]], writes=[d["bj"], d["bst"]])
        c.op("dve", lambda e: e.tensor_scalar(out=d["st"][:, 1:2], in0=d["st"][:, 0:1], scalar1=1.0 / D, scalar2=EPS, op0=ALU.mult, op1=ALU.add), reads=[d["bst"]], writes=[d["bst"]])
        c.op("act", lambda e: e.activation(out=d["st"][:, 1:2], in_=d["st"][:, 1:2], func=AF.Sqrt), reads=[d["bst"]], writes=[d["bst"]])
        c.op("dve", lambda e: e.reciprocal(out=d["st"][:, 1:2], in_=d["st"][:, 1:2]), reads=[d["bst"]], writes=[d["bst"]])
        c.op("dve", lambda e: e.scalar_tensor_tensor(out=d["tmp"][:], in0=d["x"][:], scalar=d["st"][:, 1:2], in1=A[:], op0=ALU.mult, op1=ALU.mult),
             reads=[d["bx"], d["bst"], bA], writes=[d["btmp"]])
        c.op("pool", lambda e: e.tensor_tensor(out=d["h"][:], in0=d["tmp"][:], in1=B[:], op=ALU.add), reads=[d["btmp"], bB], writes=[d["bh"]])
        pT = self.ps[psb].ap().bitcast(BF16)
        c.tr_multi([(pT[:, k * 128:(k + 1) * 128], d["h"][:, k * 128:(k + 1) * 128], self.identb) for k in range(8)],
                   reads=[d["bh"], self.b_const], writes=[self.bps[psb]])
        c.op("act", lambda e: e.activation(out=hT[:, :, col0:col0 + 128], in_=pT.rearrange("p (k n) -> p k n", k=8), func=AF.Copy),
             reads=[self.bps[psb]], writes=[b_hT])
        return d

    def layer_gqa(self, li, j, need_ctx):
        c, nc = self.c, self.nc
        T, NT, NCT = self.T, self.NT, self.NCT
        w_in = self.w["attn_w_in"].ap()[j]
        w_out = self.w["attn_w_out"].ap()[j]
        QT = self.scratch(f"gqa_qt{li}", [4, NT, 64, 4, 128], BF16)
        b_QT = [Buf() for _ in range(NT)]
        b_w = Buf("gqa_w")
        Wq = c.sb([128, 8, 1024], BF16, "Wq")
        Wqr = c.sb([128, 8, 1024], BF16, "Wqr")
        Wk = c.sb([128, 8, 256], BF16, "Wk")
        Wkr = c.sb([128, 8, 256], BF16, "Wkr")
        Wv = c.sb([128, 8, 256], BF16, "Wv")
        c.dma("pool", Wq[:], w_in[:, 0:1024].rearrange("(k p) n -> p k n", p=128), writes=[b_w])
        c.dma("pool", Wk[:], w_in[:, 1024:1280].rearrange("(k p) n -> p k n", p=128), writes=[b_w])
        c.dma("pool", Wv[:], w_in[:, 1280:1536].rearrange("(k p) n -> p k n", p=128), writes=[b_w])
        b_wr = Buf("gqa_wr")
        for (W, Wr, nh) in ((Wq, Wqr, 16), (Wk, Wkr, 4)):
            for k in range(8):
                src = W[:, k, :].rearrange("p (h two i) -> p h two i", two=2, i=32)
                dst = Wr[:, k, :].rearrange("p (h two i) -> p h two i", two=2, i=32)
                c.op("act", lambda e: e.activation(out=dst[:, :, 0, :], in_=src[:, :, 1, :], func=AF.Copy, scale=-1.0), reads=[b_w], writes=[b_wr])
                c.op("dve", lambda e: e.tensor_copy(out=dst[:, :, 1, :], in_=src[:, :, 0, :]), reads=[b_w], writes=[b_wr])
        KT = c.sb([64, 4, T], BF16, "KT")
        b_KT = Buf("KT")
        Vs = c.sb([128, NT, 4, 65], BF16, "Vs")
        b_V = Buf("Vs")
        c.op("pool", lambda e: e.memset(Vs[:], 1.0), writes=[b_V])
        mark = c.sb_mark()
        A1x, bA1x = self.load_mod(li, 1, 0, "A1x")
        S1x, bS1x = self.load_mod(li, 0, 0, "S1x")
        A1c, bA1c = self.load_mod(li, 1, 1, "A1c")
        S1c, bS1c = self.load_mod(li, 0, 1, "S1c")
        self.alloc_norm_bufs(2)
        hTs = [c.sb([128, 8, 512], BF16, "hT") for _ in range(2)]
        b_hT = [Buf(), Buf()]
        rts = [c.sb([64, 2, 512], F32, "rt") for _ in range(2)]
        b_rt = [Buf(), Buf()]
        qst = [c.sb([64, 4, 512], BF16, "qst") for _ in range(2)]
        b_qst = [Buf(), Buf()]
        t1s = [c.sb([64, 512], F32, "t1") for _ in range(2)]
        t2s = [c.sb([64, 512], F32, "t2") for _ in range(2)]
        b_t1 = [Buf(), Buf()]
        b_t2 = [Buf(), Buf()]
        cnt = 0
        qcnt = 0
        for gi, (t0, n, is_ctx) in enumerate(self.groups(4)):
            ncols = n * 128
            hT, bh = hTs[gi % 2], b_hT[gi % 2]
            rt, brt = rts[gi % 2], b_rt[gi % 2]
            for s in range(n):
                if is_ctx:
                    self.norm_tile(t0 + s, A1c, bA1c, S1c, bS1c, hT, bh, s * 128, (t0 + s) % 2)
                else:
                    self.norm_tile(t0 + s, A1x, bA1x, S1x, bS1x, hT, bh, s * 128, (t0 + s) % 2)
            if not is_ctx:
                l0 = (t0 - NCT) * 128
                c.dma("sp", rt[:, :, :ncols], self.w["rope64"].ap()[:, :, l0:l0 + ncols].rearrange("two d l -> d two l"), writes=[brt])

            def proj_head(W, Wr, col, dst_ap, dst_buf):
                nonlocal cnt
                i = cnt % 2
                cnt += 1
                P1, bP1 = self.ps[2 + i], self.bps[2 + i]
                P2, bP2 = self.ps[4 + i], self.bps[4 + i]
                c.mm(P1[0:64, :ncols], [(W[:, k, col:col + 64], hT[:, k, :ncols]) for k in range(8)], reads=[b_w, bh], writes=[bP1])
                if is_ctx:
                    c.op("act", lambda e: e.activation(out=dst_ap, in_=P1[0:64, :ncols], func=AF.Copy), reads=[bP1], writes=[dst_buf])
                else:
                    c.mm(P2[0:64, :ncols], [(Wr[:, k, col:col + 64], hT[:, k, :ncols]) for k in range(8)], reads=[b_wr, bh], writes=[bP2])
                    c.op("dve", lambda e: e.tensor_tensor(out=t1s[i][:, :ncols], in0=P1[0:64, :ncols], in1=rt[:, 0, :ncols], op=ALU.mult), reads=[bP1, brt], writes=[b_t1[i]])
                    c.op("dve", lambda e: e.tensor_tensor(out=t2s[i][:, :ncols], in0=P2[0:64, :ncols], in1=rt[:, 1, :ncols], op=ALU.mult), reads=[bP2, brt], writes=[b_t2[i]])
                    c.op("pool", lambda e: e.tensor_tensor(out=dst_ap, in0=t1s[i][:, :ncols], in1=t2s[i][:, :ncols], op=ALU.add), reads=[b_t1[i], b_t2[i]], writes=[dst_buf])

            for kvh in range(4):
                qs, bq = qst[qcnt % 2], b_qst[qcnt % 2]
                qcnt += 1
                for g in range(4):
                    proj_head(Wq, Wqr, (kvh * 4 + g) * 64, qs[:, g, :ncols], bq)
                for qb_ in range(n):
                    c.dma("sp", QT.ap()[kvh, t0 + qb_], qs[:, :, qb_ * 128:(qb_ + 1) * 128], reads=[bq], writes=[b_QT[t0 + qb_]])
                proj_head(Wk, Wkr, kvh * 64, KT[:, kvh, t0 * 128:t0 * 128 + ncols], b_KT)
            for s in range(n):
                i = cnt % 2
                cnt += 1
                Pv, bPv = self.ps[6 + i], self.bps[6 + i]
                c.mm(Pv[:, 0:256], [(hT[:, k, s * 128:(s + 1) * 128], Wv[:, k, :]) for k in range(8)], reads=[b_w, bh], writes=[bPv])
                c.op("act", lambda e: e.activation(out=Vs[:, t0 + s, :, 0:64], in_=Pv[:, 0:256].rearrange("p (h d) -> p h d", h=4), func=AF.Copy), reads=[bPv], writes=[b_V])
        c.barrier()
        c.sb_release(mark)
        Wo = c.sb([64, 16, 1024], BF16, "Wo")
        b_wo = Buf()
        c.dma("pool", Wo[:], w_out.rearrange("(h d) n -> d h n", d=64), writes=[b_wo])
        sk = c.sb([65, 16], F32, "sk")
        b_sk = Buf()
        sinkrow = c.sb([65, 16, 128], F32, "sinkrow")
        c.dma("sp", sk[64:65, :], self.w["attn_sink"].ap()[j:j + 1, :], writes=[b_sk])
        c.op("act", lambda e: e.activation(out=sk[64:65, :], in_=sk[64:65, :], func=AF.Exp), reads=[b_sk], writes=[b_sk])
        c.op("dve", lambda e: e.tensor_copy(out=sinkrow[64:65, :, :], in_=sk[64:65, :].unsqueeze(2).to_broadcast([1, 16, 128])), reads=[b_sk], writes=[b_sk])
        G1x, bG1x = self.load_mod(li, 2, 0, "G1x")
        G1c, bG1c = self.load_mod(li, 2, 1, "G1c")
        Qts = [c.sb([64, 512], BF16, "Qt") for _ in range(8)]
        b_Qt = [Buf() for _ in range(8)]
        PTs = [c.sb([128, 512], BF16, "PT") for _ in range(4)]
        b_PT = [Buf() for _ in range(4)]
        osb = [c.sb([65, 512], F32, "osb") for _ in range(2)]
        b_osb = [Buf(), Buf()]
        rden = [c.sb([65, 512], F32, "rden") for _ in range(2)]
        b_rden = [Buf(), Buf()]
        yTs = [c.sb([64, 16, 128], BF16, "yT") for _ in range(2)]
        b_yT = [Buf(), Buf()]
        xts = [c.sb([128, D], F32, "xres") for _ in range(2)]
        b_xt = [Buf(), Buf()]
        tmps = [c.sb([128, D], F32, "rtmp") for _ in range(2)]
        b_tmp = [Buf(), Buf()]
        scale = 64 ** -0.5
        qi = 0
        pi = 0
        si = 0
        qbs = list(range(0 if need_ctx else NCT, NT))

        def ga_load(bi_):
            qb_ = qbs[bi_]
            c.dma("sp", xts[bi_ % 2][:], self.xs.ap()[qb_ * 128:(qb_ + 1) * 128, :], reads=[self.b_xs[qb_]], writes=[b_xt[bi_ % 2]])
            for kvh_ in range(4):
                q8 = (bi_ % 2) * 4 + kvh_
                c.dma("sp", Qts[q8][:], QT.ap()[kvh_, qb_].rearrange("d g t -> d (g t)"), reads=[b_QT[qb_]], writes=[b_Qt[q8]])
        ga_load(0)
        for bi, qb in enumerate(qbs):
            is_ctx = qb < NCT
            if bi + 1 < len(qbs):
                ga_load(bi + 1)
            if is_ctx:
                keys = [(t, None) for t in range(NCT)]
            else:
                keys = []
                if qb - 1 >= NCT:
                    keys.append((qb - 1, self.trigeb))
                keys.append((qb, None))
                if qb + 1 < NT:
                    keys.append((qb + 1, self.trileb))
                keys += [(t, None) for t in range(NCT)]
            xt, bxt = xts[bi % 2], b_xt[bi % 2]
            yT, byT = yTs[bi % 2], b_yT[bi % 2]
            for kvh in range(4):
                Qt, bQt = Qts[(bi % 2) * 4 + kvh], b_Qt[(bi % 2) * 4 + kvh]
                oT, boT = self.ps[2 + kvh % 2], self.bps[2 + kvh % 2]
                SB = (0, 1, 7)
                LA = 2

                def issue_S(ki_):
                    kt_ = keys[ki_][0]
                    bk_ = SB[(si + ki_) % len(SB)]
                    c.mm(self.ps[bk_][:, :], [(KT[:, kvh, kt_ * 128:(kt_ + 1) * 128], Qt[:])], reads=[b_KT, bQt], writes=[self.bps[bk_]])
                for k0 in range(min(LA, len(keys))):
                    issue_S(k0)
                for ki, (kt, mask) in enumerate(keys):
                    bk = SB[(si + ki) % len(SB)]
                    sT, bsT = self.ps[bk], self.bps[bk]
                    if ki + LA < len(keys):
                        issue_S(ki + LA)
                    PT, bPT = PTs[pi % 4], b_PT[pi % 4]
                    pi += 1
                    c.op("act", lambda e: e.activation(out=PT[:], in_=sT[:, :], func=AF.Exp, scale=scale), reads=[bsT], writes=[bPT])
                    if mask is not None:
                        c.op("dve", lambda e: e.tensor_tensor(out=PT[:].rearrange("p (g t) -> p g t", g=4), in0=PT[:].rearrange("p (g t) -> p g t", g=4),
                                                              in1=mask.unsqueeze(1).to_broadcast([128, 4, 128]), op=ALU.mult), reads=[bPT, self.b_const], writes=[bPT])
                    c.mm(oT[0:65, :], [(Vs[:, kt, kvh, :], PT[:])], reads=[b_V, bPT], writes=[boT], start=(ki == 0), stop=(ki == len(keys) - 1))
                si += len(keys)
                o, bo = osb[kvh % 2], b_osb[kvh % 2]
                rd, brd = rden[kvh % 2], b_rden[kvh % 2]
                c.op("act", lambda e: e.activation(out=o[:], in_=oT[0:65, :], func=AF.Copy), reads=[boT], writes=[bo])
                c.op("dve", lambda e: e.tensor_tensor(out=rd[64:65, :], in0=o[64:65, :], in1=sinkrow[64:65, kvh * 4:(kvh + 1) * 4, :].rearrange("p g t -> p (g t)"), op=ALU.add),
                     reads=[bo, b_sk], writes=[brd])
                c.mm(self.ps[4][0:64, :], [(self.ones32[64:65, 0:64], rd[64:65, :])], reads=[self.b_const, brd], writes=[self.bps[4]])
                c.op("dve", lambda e: e.reciprocal(out=rd[0:64, :], in_=self.ps[4][0:64, :]), reads=[self.bps[4], brd], writes=[brd])
                c.op("dve", lambda e: e.tensor_tensor(out=yT[:, kvh * 4:(kvh + 1) * 4, :].rearrange("d g t -> d (g t)"), in0=o[0:64, :], in1=rd[0:64, :], op=ALU.mult),
                     reads=[bo, brd], writes=[byT])
            G1, bG1 = (G1c, bG1c) if is_ctx else (G1x, bG1x)
            tmp, btmp = tmps[bi % 2], b_tmp[bi % 2]
            for nn in range(2):
                z, bz = self.ps[5 + nn], self.bps[5 + nn]
                c.mm(z[:, :], [(yT[:, hq, :], Wo[:, hq, nn * 512:(nn + 1) * 512]) for hq in range(16)], reads=[byT, b_wo], writes=[bz])
                c.op("dve", lambda e: e.tensor_tensor(out=tmp[:, nn * 512:(nn + 1) * 512], in0=z[:, :], in1=G1[:, nn * 512:(nn + 1) * 512], op=ALU.mult), reads=[bz, bG1], writes=[btmp])
            c.op("pool", lambda e: e.tensor_tensor(out=xt[:], in0=xt[:], in1=tmp[:], op=ALU.add), reads=[btmp, bxt], writes=[bxt])
            c.dma("sp", self.xs.ap()[qb * 128:(qb + 1) * 128, :], xt[:], reads=[bxt], writes=[self.b_xs[qb]])
        self.phase_end()

    def moe_precast(self, li):
        c = self.c
        wg = self.w["moe_w_gate"].ap()[li]
        wu = self.w["moe_w_up"].ap()[li]
        wd = self.w["moe_w_down"].ap()[li]
        self.wgb = self.scratch(f"moe_wgb{li}", [16, 1024, 512], BF16)
        self.wdb = self.scratch(f"moe_wdb{li}", [16, 256, 1024], BF16)
        self.b_wgb = Buf(); self.b_wdb = Buf()
        for e2 in range(8):
            c.dma("pool", self.wgb.ap()[2 * e2:2 * e2 + 2, :, 0:256], wg[2 * e2:2 * e2 + 2], writes=[self.b_wgb])
            c.dma("pool", self.wgb.ap()[2 * e2:2 * e2 + 2, :, 256:512], wu[2 * e2:2 * e2 + 2], writes=[self.b_wgb])
            c.dma("pool", self.wdb.ap()[2 * e2:2 * e2 + 2], wd[2 * e2:2 * e2 + 2], writes=[self.b_wdb])
        self.precast_done = li

    def layer_moe(self, li, need_ctx):
        c, nc = self.c, self.nc
        NCT = self.NCT
        if getattr(self, "precast_done", -1) != li:
            self.moe_precast(li)
        wgb = self.wgb.ap()
        wdb = self.wdb.ap()
        b_w = Buf("moe_wr")
        Wr = c.sb([128, 8, 20], BF16, "Wr")
        c.dma("pool", Wr[:, :, 0:4], self.w["moe_w_group"].ap()[li].rearrange("(k p) n -> p k n", p=128), writes=[b_w])
        c.dma("pool", Wr[:, :, 4:20], self.w["moe_w_expert"].ap()[li].rearrange("(k p) n -> p k n", p=128), writes=[b_w])
        brow = c.sb([128, 20], F32, "brow")
        c.dma("sp", brow[:, 0:4], self.w["moe_b_group"].ap()[li:li + 1, :].partition_broadcast(128), writes=[b_w])
        c.dma("sp", brow[:, 4:20], self.w["moe_b_expert"].ap()[li:li + 1, :].partition_broadcast(128), writes=[b_w])
        sel16 = c.sb([16, 16, 128], BF16, "sel16")
        c.dma("pool", sel16[:], self.w["sel32"].ap()[0:16, 0:16, :], writes=[b_w])
        hT = c.sb([128, 8, 1024], BF16, "mhT")
        b_hT = Buf()
        act = c.sb([128, 16, 2, 1024], BF16, "mact")
        b_act = Buf()
        Wgu = [c.sb([128, 8, 512], BF16, "Wgu") for _ in range(2)]
        b_Wgu = [Buf(), Buf()]
        Wd = [c.sb([128, 16, 2, 256], BF16, "Wd") for _ in range(2)]
        b_Wd = [Buf(), Buf()]
        xq = [c.sb([128, 256], F32, "xq") for _ in range(4)]
        b_xq = [Buf() for _ in range(4)]
        xqi = 0
        self.alloc_norm_bufs(2)
        A2 = c.sb([128, D], F32, "A2"); S2 = c.sb([128, D], F32, "S2"); G2 = c.sb([128, D], F32, "G2")
        b_m = Buf()
        R = 8
        lg = c.sb([128, R, 20], F32, "lg"); le = c.sb([128, R, 16], F32, "le"); le2 = c.sb([128, R, 16], F32, "le2")
        gm = c.sb([128, R, 4], F32, "gm"); eg = c.sb([128, R, 4], F32, "eg"); pen = c.sb([128, R, 4], F32, "pen")
        mk1 = c.sb([128, R, 16], F32, "mk1"); mk2 = c.sb([128, R, 16], F32, "mk2"); cmb = c.sb([128, R, 16], F32, "cmb")
        sc = c.sb([128, 8, R], F32, "rsc")
        b_r = Buf("route")
        cmbT = c.sb([16, 1024], BF16, "cmbT")
        b_cT = Buf()
        s_sb = [c.sb([128, 512], F32, "msil") for _ in range(2)]
        b_s = [Buf(), Buf()]
        t_sb = [c.sb([128, 512], F32, "mt") for _ in range(2)]
        b_t = [Buf(), Buf()]
        tmpd = [c.sb([128, 256], F32, "mtd") for _ in range(2)]
        b_td = [Buf(), Buf()]
        wi = 0
        di = 0
        ui = 0
        for (t0, n, is_ctx) in self.groups(8, with_ctx=need_ctx):
            G = n * 128
            row = 1 if is_ctx else 0
            for (tl, ch) in ((S2, 3), (A2, 4), (G2, 5)):
                c.dma("sp", tl[:], self.modv.ap()[li, row:row + 1, ch * D:(ch + 1) * D].partition_broadcast(128), reads=[self.b_modv[li]], writes=[b_m])
            for s in range(n):
                self.norm_tile(t0 + s, A2, b_m, S2, b_m, hT, b_hT, s * 128, s % 2)
            lp, blp = self.ps[2], self.bps[2]
            c.mm_multi([(lp[:, s * 20:(s + 1) * 20], [(hT[:, k, s * 128:(s + 1) * 128], Wr[:, k, :]) for k in range(8)]) for s in range(n)],
                       reads=[b_hT, b_w], writes=[blp])
            V = lambda e: e
            lgn = lg[:, 0:n, :]
            c.op("dve", lambda e: e.tensor_tensor(out=lgn, in0=lp[:, 0:n * 20].rearrange("p (s j) -> p s j", j=20), in1=brow[:].unsqueeze(1).to_broadcast([128, n, 20]), op=ALU.add),
                 reads=[blp, b_w], writes=[b_r])
            R1 = [b_r]
            c.op("dve", lambda e: e.tensor_reduce(out=sc[:, 0, 0:n], in_=lgn[:, :, 0:4], axis=AX.X, op=ALU.max), reads=R1, writes=R1)
            c.op("dve", lambda e: e.tensor_tensor(out=gm[:, 0:n, :], in0=lgn[:, :, 0:4], in1=sc[:, 0, 0:n].unsqueeze(2).to_broadcast([128, n, 4]), op=ALU.is_equal), reads=R1, writes=R1)
            c.op("dve", lambda e: e.tensor_tensor(out=eg[:, 0:n, :], in0=lgn[:, :, 0:4], in1=sc[:, 0, 0:n].unsqueeze(2).to_broadcast([128, n, 4]), op=ALU.subtract), reads=R1, writes=R1)
            c.op("act", lambda e: e.activation(out=eg[:, 0:n, :], in_=eg[:, 0:n, :], func=AF.Exp), reads=R1, writes=R1)
            c.op("dve", lambda e: e.tensor_reduce(out=sc[:, 1, 0:n], in_=eg[:, 0:n, :], axis=AX.X, op=ALU.add), reads=R1, writes=R1)
            c.op("dve", lambda e: e.reciprocal(out=sc[:, 1, 0:n], in_=sc[:, 1, 0:n]), reads=R1, writes=R1)
            c.op("dve", lambda e: e.tensor_scalar(out=pen[:, 0:n, :], in0=gm[:, 0:n, :], scalar1=1.0, scalar2=1e30, op0=ALU.subtract, op1=ALU.mult), reads=R1, writes=R1)
            c.op("dve", lambda e: e.tensor_copy(out=le[:, 0:n, :], in_=lgn[:, :, 4:20]), reads=R1, writes=R1)
            lev = le[:, 0:n, :].rearrange("p s (g j) -> p (s g) j", g=4)
            c.op("dve", lambda e: e.tensor_tensor(out=lev, in0=lev, in1=pen[:, 0:n, :].rearrange("p s g -> p (s g)").unsqueeze(2).to_broadcast([128, n * 4, 4]), op=ALU.add), reads=R1, writes=R1)
            c.op("dve", lambda e: e.tensor_reduce(out=sc[:, 2, 0:n], in_=le[:, 0:n, :], axis=AX.X, op=ALU.max), reads=R1, writes=R1)
            c.op("dve", lambda e: e.tensor_tensor(out=mk1[:, 0:n, :], in0=le[:, 0:n, :], in1=sc[:, 2, 0:n].unsqueeze(2).to_broadcast([128, n, 16]), op=ALU.is_equal), reads=R1, writes=R1)
            c.op("dve", lambda e: e.scalar_tensor_tensor(out=le2[:, 0:n, :], in0=mk1[:, 0:n, :], scalar=-1e30, in1=le[:, 0:n, :], op0=ALU.mult, op1=ALU.add), reads=R1, writes=R1)
            c.op("dve", lambda e: e.tensor_reduce(out=sc[:, 3, 0:n], in_=le2[:, 0:n, :], axis=AX.X, op=ALU.max), reads=R1, writes=R1)
            c.op("dve", lambda e: e.tensor_tensor(out=mk2[:, 0:n, :], in0=le2[:, 0:n, :], in1=sc[:, 3, 0:n].unsqueeze(2).to_broadcast([128, n, 16]), op=ALU.is_equal), reads=R1, writes=R1)
            c.op("dve", lambda e: e.tensor_tensor(out=sc[:, 4, 0:n], in0=sc[:, 2, 0:n], in1=sc[:, 3, 0:n], op=ALU.subtract), reads=R1, writes=R1)
            c.op("act", lambda e: e.activation(out=sc[:, 4, 0:n], in_=sc[:, 4, 0:n], func=AF.Sigmoid), reads=R1, writes=R1)
            c.op("dve", lambda e: e.tensor_tensor(out=sc[:, 4, 0:n], in0=sc[:, 4, 0:n], in1=sc[:, 1, 0:n], op=ALU.mult), reads=R1, writes=R1)
            c.op("dve", lambda e: e.tensor_tensor(out=sc[:, 5, 0:n], in0=sc[:, 1, 0:n], in1=sc[:, 4, 0:n], op=ALU.subtract), reads=R1, writes=R1)
            c.op("dve", lambda e: e.tensor_tensor(out=mk1[:, 0:n, :], in0=mk1[:, 0:n, :], in1=sc[:, 4, 0:n].unsqueeze(2).to_broadcast([128, n, 16]), op=ALU.mult), reads=R1, writes=R1)
            c.op("dve", lambda e: e.tensor_tensor(out=mk2[:, 0:n, :], in0=mk2[:, 0:n, :], in1=sc[:, 5, 0:n].unsqueeze(2).to_broadcast([128, n, 16]), op=ALU.mult), reads=R1, writes=R1)
            c.op("dve", lambda e: e.tensor_tensor(out=cmb[:, 0:n, :], in0=mk1[:, 0:n, :], in1=mk2[:, 0:n, :], op=ALU.add), reads=R1, writes=R1)
            for hb in range((n + 3) // 4):
                s0, s1 = hb * 4, min(n, hb * 4 + 4)
                pc, bpc = self.ps[3 + hb], self.bps[3 + hb]
                c.tr_multi([(pc[0:16, (s - s0) * 128:(s - s0 + 1) * 128], cmb[:, s, :], self.ident32) for s in range(s0, s1)], reads=[b_r, self.b_const], writes=[bpc])
                c.op("act", lambda e: e.activation(out=cmbT[:, s0 * 128:s1 * 128], in_=pc[0:16, 0:(s1 - s0) * 128], func=AF.Copy), reads=[bpc], writes=[b_cT])
            ncb = (G + 511) // 512
            for ex in range(16):
                W, bW = Wgu[wi % 2], b_Wgu[wi % 2]
                wi += 1
                c.dma("sp", W[:], wgb[ex].rearrange("(k p) n -> p k n", p=128), reads=[self.b_wgb], writes=[bW])
                for cb in range(ncb):
                    c0 = cb * 512
                    cw = min(512, G - c0)
                    pbc, bpbc = self.ps[4 + (ui % 2)], self.bps[4 + (ui % 2)]
                    c.mm(pbc[:, :cw], [(sel16[:, ex, :], cmbT[:, c0:c0 + cw])], reads=[b_w, b_cT], writes=[bpbc])
                    for ffc in range(2):
                        i = ui % 2
                        ui += 1
                        pg_, bpg = self.ps[0 + i], self.bps[0 + i]
                        pu, bpu = self.ps[2 + i], self.bps[2 + i]
                        c.mm(pg_[:, :cw], [(W[:, k, ffc * 128:(ffc + 1) * 128], hT[:, k, c0:c0 + cw]) for k in range(8)], reads=[bW, b_hT], writes=[bpg])
                        c.mm(pu[:, :cw], [(W[:, k, 256 + ffc * 128:256 + (ffc + 1) * 128], hT[:, k, c0:c0 + cw]) for k in range(8)], reads=[bW, b_hT], writes=[bpu])
                        c.op("act", lambda e: e.activation(out=s_sb[i][:, :cw], in_=pg_[:, :cw], func=AF.Silu), reads=[bpg], writes=[b_s[i]])
                        c.op("dve", lambda e: e.tensor_tensor(out=t_sb[i][:, :cw], in0=s_sb[i][:, :cw], in1=pu[:, :cw], op=ALU.mult), reads=[b_s[i], bpu], writes=[b_t[i]])
                        c.op("dve", lambda e: e.tensor_tensor(out=act[:, ex, ffc, c0:c0 + cw], in0=t_sb[i][:, :cw], in1=pbc[:, :cw], op=ALU.mult), reads=[b_t[i], bpbc], writes=[b_act])
            for dq in range(4):
                Wdt, bWd = Wd[di % 2], b_Wd[di % 2]
                di += 1
                c.dma("sp", Wdt[:], wdb[:, :, dq * 256:(dq + 1) * 256].rearrange("e (f p) n -> p e f n", p=128), reads=[self.b_wdb], writes=[bWd])
                for s in range(n):
                    pa, bpa = self.ps[4 + s // 2], self.bps[4 + s // 2]
                    pav = pa[:, (s % 2) * 256:(s % 2 + 1) * 256]
                    c.mm(pav, [(act[:, ex, ffc, s * 128:(s + 1) * 128], Wdt[:, ex, ffc, :]) for ex in range(16) for ffc in range(2)], reads=[b_act, bWd], writes=[bpa])
                    i = s % 2
                    xq_, bxq = xq[xqi % 4], b_xq[xqi % 4]
                    xqi += 1
                    tl = t0 + s
                    c.dma("sp", xq_[:], self.xs.ap()[tl * 128:(tl + 1) * 128, dq * 256:(dq + 1) * 256], reads=[self.b_xs[tl]], writes=[bxq])
                    c.op("dve", lambda e: e.tensor_tensor(out=tmpd[i][:], in0=pav, in1=G2[:, dq * 256:(dq + 1) * 256], op=ALU.mult), reads=[bpa, b_m], writes=[b_td[i]])
                    c.op("pool", lambda e: e.tensor_tensor(out=xq_[:], in0=xq_[:], in1=tmpd[i][:], op=ALU.add), reads=[b_td[i], bxq], writes=[bxq])
                    c.dma("sp", self.xs.ap()[tl * 128:(tl + 1) * 128, dq * 256:(dq + 1) * 256], xq_[:], reads=[bxq], writes=[self.b_xs[tl]])
        self.phase_end()

    def final(self):
        c = self.c
        gf = c.sb([128, D], F32, "gfin")
        b_g = Buf()
        c.dma("sp", gf[:], self.w["final_norm_g"].ap().rearrange("(o n) -> o n", o=1).partition_broadcast(128), writes=[b_g])
        xs_ = [c.sb([128, D], F32, "fx") for _ in range(2)]
        js = [c.sb([128, D], BF16, "fj") for _ in range(2)]
        st = [c.sb([128, 2], F32, "fst") for _ in range(2)]
        ys = [c.sb([128, D], F32, "fy") for _ in range(2)]
        bx = [Buf(), Buf()]; bj = [Buf(), Buf()]; bs = [Buf(), Buf()]; by = [Buf(), Buf()]
        b_out = Buf()
        for t in range(self.NCT, self.NT):
            i = t % 2
            c.dma("sp", xs_[i][:], self.xs.ap()[t * 128:(t + 1) * 128, :], reads=[self.b_xs[t]], writes=[bx[i]])
            c.op("dve", lambda e: e.memset(st[i][:], 0.0), writes=[bs[i]])
            c.op("act", lambda e: e.activation(out=js[i][:], in_=xs_[i][:], func=AF.Square, accum_out=st[i][:, 0:1]), reads=[bx[i]], writes=[bj[i], bs[i]])
            c.op("dve", lambda e: e.tensor_scalar(out=st[i][:, 1:2], in0=st[i][:, 0:1], scalar1=1.0 / D, scalar2=EPS, op0=ALU.mult, op1=ALU.add), reads=[bs[i]], writes=[bs[i]])
            c.op("act", lambda e: e.activation(out=st[i][:, 1:2], in_=st[i][:, 1:2], func=AF.Sqrt), reads=[bs[i]], writes=[bs[i]])
            c.op("dve", lambda e: e.reciprocal(out=st[i][:, 1:2], in_=st[i][:, 1:2]), reads=[bs[i]], writes=[bs[i]])
            c.op("dve", lambda e: e.scalar_tensor_tensor(out=ys[i][:], in0=xs_[i][:], scalar=st[i][:, 1:2], in1=gf[:], op0=ALU.mult, op1=ALU.mult), reads=[bx[i], bs[i], b_g], writes=[by[i]])
            lt = t - self.NCT
            c.dma("sp", self.out.ap()[lt * 128:(lt + 1) * 128, :], ys[i][:], reads=[by[i]], writes=[b_out])
        self.c.finish("sp")

    def layer_mla(self, li, j, need_ctx):
        c, nc = self.c, self.nc
        T, NT, NCT = self.T, self.NT, self.NCT
        w_in = self.w["mla_w_in"].ap()[j]
        w_qup = self.w["mla_w_q_up"].ap()[j]
        w_kvup = self.w["mla_w_kv_up"].ap()[j]
        w_out = self.w["mla_w_out"].ap()[j]
        QT = self.scratch(f"mla_qt{li}", [16, 96, T], BF16)
        KTd = self.scratch(f"mla_kt{li}", [16, 96, T], BF16)
        Vd = self.scratch(f"mla_v{li}", [16, NT, 128, 65], BF16)
        YT = self.scratch(f"mla_yt{li}", [16, 64, T], BF16)
        b_QT = Buf(); b_KTd = Buf(); b_Vd = Buf(); b_YT = Buf()
        b_w = Buf("mla_w")
        Win = c.sb([128, 8, 544], BF16, "Win")
        c.dma("pool", Win[:], w_in.rearrange("(k p) n -> p k n", p=128), writes=[b_w])
        Wkr_rot = c.sb([128, 8, 32], BF16, "Wkrrot")
        Wq = c.sb([128, 2, 1536], BF16, "Wqup")
        c.dma("pool", Wq[:], w_qup.rearrange("(k p) n -> p k n", p=128), writes=[b_w])
        Wqr = c.sb([128, 2, 1536], BF16, "Wquprot")
        Wkn = c.sb([128, 2, 16, 64], BF16, "Wkn")
        Wv = c.sb([128, 2, 16, 64], BF16, "Wvv")
        kvv = w_kvup.rearrange("(k p) (h two d) -> p k h two d", p=128, two=2, d=64)
        for kc in range(2):
            c.dma("pool", Wkn[:, kc], kvv[:, kc, :, 0, :], writes=[b_w])
            c.dma("pool", Wv[:, kc], kvv[:, kc, :, 1, :], writes=[b_w])
        b_wr = Buf("mla_wr")
        c.op("pool", lambda e: e.memset(Wqr[:], 0.0), writes=[b_wr])
        for kc in range(2):
            src = Wq[:, kc, :].rearrange("p (h d) -> p h d", d=96)
            dst = Wqr[:, kc, :].rearrange("p (h d) -> p h d", d=96)
            c.op("act", lambda e: e.activation(out=dst[:, :, 64:80], in_=src[:, :, 80:96], func=AF.Copy, scale=-1.0), reads=[b_w], writes=[b_wr])
            c.op("dve", lambda e: e.tensor_copy(out=dst[:, :, 80:96], in_=src[:, :, 64:80]), reads=[b_w], writes=[b_wr])
        c.op("act", lambda e: e.activation(out=Wkr_rot[:, :, 0:16], in_=Win[:, :, 528:544], func=AF.Copy, scale=-1.0), reads=[b_w], writes=[b_wr])
        c.op("dve", lambda e: e.tensor_copy(out=Wkr_rot[:, :, 16:32], in_=Win[:, :, 512:528]), reads=[b_w], writes=[b_wr])
        gq = c.sb([128, 512], F32, "gqkv")
        c.dma("sp", gq[:, 0:256], self.w["mla_q_norm_g"].ap()[j:j + 1, :].partition_broadcast(128), writes=[b_w])
        c.dma("sp", gq[:, 256:512], self.w["mla_kv_norm_g"].ap()[j:j + 1, :].partition_broadcast(128), writes=[b_w])
        mark = c.sb_mark()
        A1x, bA1x = self.load_mod(li, 1, 0, "A1x")
        S1x, bS1x = self.load_mod(li, 0, 0, "S1x")
        A1c, bA1c = self.load_mod(li, 1, 1, "A1c")
        S1c, bS1c = self.load_mod(li, 0, 1, "S1c")
        self.alloc_norm_bufs(2)
        hTs = [c.sb([128, 8, 512], BF16, "hT") for _ in range(2)]
        b_hT = [Buf(), Buf()]
        cnT = [c.sb([128, 4, 512], BF16, "cnT") for _ in range(2)]
        b_cnT = [Buf(), Buf()]
        rts = [c.sb([96, 2, 512], F32, "rt96") for _ in range(2)]
        rks = [c.sb([32, 2, 512], F32, "rt32") for _ in range(2)]
        b_rt = [Buf(), Buf()]
        krT = [c.sb([32, 512], BF16, "krT") for _ in range(2)]
        b_krT = [Buf(), Buf()]
        st = [c.sb([128, 4], F32, "mst") for _ in range(2)]
        b_st = [Buf(), Buf()]
        jk = c.sb([128, 256], BF16, "mjunk"); b_jk = Buf()
        cn = [c.sb([128, 512], BF16, "cn") for _ in range(2)]
        b_cn = [Buf(), Buf()]
        qst = [c.sb([96, 512], BF16, "mqst") for _ in range(2)]
        b_qst = [Buf(), Buf()]
        kst = [c.sb([64, 512], BF16, "mkst") for _ in range(2)]
        b_kst = [Buf(), Buf()]
        t1s = [c.sb([96, 512], F32, "t1") for _ in range(2)]
        t2s = [c.sb([96, 512], F32, "t2") for _ in range(2)]
        b_t1 = [Buf(), Buf()]; b_t2 = [Buf(), Buf()]
        Vt = [c.sb([128, 16, 65], BF16, "Vt") for _ in range(2)]
        b_Vt = [Buf(), Buf()]
        for i in range(2):
            c.op("pool", lambda e: e.memset(Vt[i][:], 1.0), writes=[b_Vt[i]])
        cnt = 0
        ti = 0
        for gi, (t0, n, is_ctx) in enumerate(self.groups(4)):
            ncols = n * 128
            col0 = t0 * 128
            hT, bh = hTs[gi % 2], b_hT[gi % 2]
            cT_, bcT = cnT[gi % 2], b_cnT[gi % 2]
            rt, rk, brt = rts[gi % 2], rks[gi % 2], b_rt[gi % 2]
            for s in range(n):
                if is_ctx:
                    self.norm_tile(t0 + s, A1c, bA1c, S1c, bS1c, hT, bh, s * 128, (t0 + s) % 2)
                else:
                    self.norm_tile(t0 + s, A1x, bA1x, S1x, bS1x, hT, bh, s * 128, (t0 + s) % 2)
            if not is_ctx:
                l0 = (t0 - NCT) * 128
                c.dma("sp", rt[:, :, :ncols], self.w["rope96"].ap()[:, :, l0:l0 + ncols].rearrange("two d l -> d two l"), writes=[brt])
                c.dma("sp", rk[:, :, :ncols], self.w["rope32"].ap()[:, :, l0:l0 + ncols].rearrange("two d l -> d two l"), writes=[brt])
            for s in range(n):
                i = ti % 2
                ti += 1
                pA, bpA = self.ps[2 + i], self.bps[2 + i]
                c.mm(pA[:, :], [(hT[:, k, s * 128:(s + 1) * 128], Win[:, k, 0:512]) for k in range(8)], reads=[bh, b_w], writes=[bpA])
                c.op("dve", lambda e: e.memset(st[i][:], 0.0), writes=[b_st[i]])
                for u in range(2):
                    c.op("act", lambda e: e.activation(out=jk[:], in_=pA[:, u * 256:(u + 1) * 256], func=AF.Square, accum_out=st[i][:, u:u + 1]), reads=[bpA], writes=[b_jk, b_st[i]])
                c.op("dve", lambda e: e.tensor_scalar(out=st[i][:, 2:4], in0=st[i][:, 0:2], scalar1=1.0 / 256, scalar2=EPS, op0=ALU.mult, op1=ALU.add), reads=[b_st[i]], writes=[b_st[i]])
                c.op("act", lambda e: e.activation(out=st[i][:, 2:4], in_=st[i][:, 2:4], func=AF.Sqrt), reads=[b_st[i]], writes=[b_st[i]])
                c.op("dve", lambda e: e.reciprocal(out=st[i][:, 2:4], in_=st[i][:, 2:4]), reads=[b_st[i]], writes=[b_st[i]])
                for u in range(2):
                    c.op("dve", lambda e: e.scalar_tensor_tensor(out=cn[i][:, u * 256:(u + 1) * 256], in0=pA[:, u * 256:(u + 1) * 256], scalar=st[i][:, 2 + u:3 + u], in1=gq[:, u * 256:(u + 1) * 256], op0=ALU.mult, op1=ALU.mult),
                         reads=[bpA, b_st[i], b_w], writes=[b_cn[i]])
                pT = self.ps[4 + i].ap().bitcast(BF16)
                c.tr_multi([(pT[:, k * 128:(k + 1) * 128], cn[i][:, k * 128:(k + 1) * 128], self.identb) for k in range(4)], reads=[b_cn[i], self.b_const], writes=[self.bps[4 + i]])
                c.op("act", lambda e: e.activation(out=cT_[:, :, s * 128:(s + 1) * 128], in_=pT[:, 0:512].rearrange("p (k n) -> p k n", k=4), func=AF.Copy), reads=[self.bps[4 + i]], writes=[bcT])

            def rope_proj(pairs1, pairs2, M, rtab, dst_ap, dst_buf, rd):
                nonlocal cnt
                i = cnt % 2
                cnt += 1
                P1, bP1 = self.ps[2 + i], self.bps[2 + i]
                P2, bP2 = self.ps[6 + i], self.bps[6 + i]
                c.mm(P1[0:M, :ncols], pairs1, reads=rd, writes=[bP1])
                if is_ctx or pairs2 is None:
                    c.op("act", lambda e: e.activation(out=dst_ap, in_=P1[0:M, :ncols], func=AF.Copy), reads=[bP1], writes=[dst_buf])
                else:
                    c.mm(P2[0:M, :ncols], pairs2, reads=rd + [b_wr], writes=[bP2])
                    c.op("dve", lambda e: e.tensor_tensor(out=t1s[i][0:M, :ncols], in0=P1[0:M, :ncols], in1=rtab[0:M, 0, :ncols], op=ALU.mult), reads=[bP1, brt], writes=[b_t1[i]])
                    c.op("dve", lambda e: e.tensor_tensor(out=t2s[i][0:M, :ncols], in0=P2[0:M, :ncols], in1=rtab[0:M, 1, :ncols], op=ALU.mult), reads=[bP2, brt], writes=[b_t2[i]])
                    c.op("pool", lambda e: e.tensor_tensor(out=dst_ap, in0=t1s[i][0:M, :ncols], in1=t2s[i][0:M, :ncols], op=ALU.add), reads=[b_t1[i], b_t2[i]], writes=[dst_buf])

            kr_, bkr = krT[gi % 2], b_krT[gi % 2]
            rope_proj([(Win[:, k, 512:544], hT[:, k, :ncols]) for k in range(8)], [(Wkr_rot[:, k, :], hT[:, k, :ncols]) for k in range(8)], 32, rk, kr_[:, :ncols], bkr, [bh, b_w])
            for h in range(16):
                qs, bq = qst[h % 2], b_qst[h % 2]
                rope_proj([(Wq[:, kc, h * 96:(h + 1) * 96], cT_[:, kc, :ncols]) for kc in range(2)],
                          [(Wqr[:, kc, h * 96:(h + 1) * 96], cT_[:, kc, :ncols]) for kc in range(2)], 96, rt, qs[:, :ncols], bq, [bcT, b_w])
                c.dma("sp", QT.ap()[h, :, col0:col0 + ncols], qs[:, :ncols], reads=[bq], writes=[b_QT])
                ks, bk = kst[h % 2], b_kst[h % 2]
                rope_proj([(Wkn[:, kc, h, :], cT_[:, 2 + kc, :ncols]) for kc in range(2)], None, 64, None, ks[:, :ncols], bk, [bcT, b_w])
                c.dma("sp", KTd.ap()[h, 0:64, col0:col0 + ncols], ks[:, :ncols], reads=[bk], writes=[b_KTd])
                c.dma("sp", KTd.ap()[h, 64:96, col0:col0 + ncols], kr_[:, :ncols], reads=[bkr], writes=[b_KTd])
            for s in range(n):
                vt, bvt = Vt[s % 2], b_Vt[s % 2]
                for hh in range(2):
                    i = cnt % 2
                    cnt += 1
                    Pv, bPv = self.ps[2 + i], self.bps[2 + i]
                    c.mm(Pv[:, :], [(cT_[:, 2 + kc, s * 128:(s + 1) * 128], Wv[:, kc, hh * 8:(hh + 1) * 8, :].rearrange("p h d -> p (h d)")) for kc in range(2)], reads=[bcT, b_w], writes=[bPv])
                    c.op("act", lambda e: e.activation(out=vt[:, hh * 8:(hh + 1) * 8, 0:64], in_=Pv[:, :].rearrange("p (h d) -> p h d", d=64), func=AF.Copy), reads=[bPv], writes=[bvt])
                c.dma("sp", Vd.ap()[:, t0 + s].rearrange("h p d -> p h d"), vt[:], reads=[bvt], writes=[b_Vd])
        c.barrier()
        c.sb_release(mark)
        QTs = [c.sb([96, T], BF16, "QTh") for _ in range(2)]
        KTs = [c.sb([96, T], BF16, "KTh") for _ in range(2)]
        Vhs = [c.sb([128, NT, 65], BF16, "Vh") for _ in range(2)]
        b_hd = [Buf(), Buf()]
        PTs = [c.sb([128, 512], BF16, "PT") for _ in range(4)]
        b_PT = [Buf() for _ in range(4)]
        osb = [c.sb([65, 512], F32, "osb") for _ in range(2)]
        b_osb = [Buf(), Buf()]
        ysb = [c.sb([64, 512], BF16, "ysb") for _ in range(2)]
        b_ysb = [Buf(), Buf()]
        rbs = [c.sb([64, 512], F32, "rbs") for _ in range(2)]
        b_rbs = [Buf(), Buf()]
        scale = 96 ** -0.5
        pi = 0; si = 0; oi = 0

        def load_head(h_):
            c.dma("sp", QTs[h_ % 2][:], QT.ap()[h_], reads=[b_QT], writes=[b_hd[h_ % 2]])
            c.dma("sp", KTs[h_ % 2][:], KTd.ap()[h_], reads=[b_KTd], writes=[b_hd[h_ % 2]])
            c.dma("sp", Vhs[h_ % 2][:], Vd.ap()[h_].rearrange("t p d -> p t d"), reads=[b_Vd], writes=[b_hd[h_ % 2]])
        load_head(0)
        for h in range(16):
            Qh, Kh, Vh, bhd = QTs[h % 2], KTs[h % 2], Vhs[h % 2], b_hd[h % 2]
            if h + 1 < 16:
                load_head(h + 1)
            units = []
            for (t0, n, is_ctx) in self.groups(4, with_ctx=need_ctx):
                keys = list(range(NCT)) if is_ctx else list(range(NT))
                gslot = oi % 2
                oi += 1
                for ki, kt in enumerate(keys):
                    units.append((t0 * 128, n * 128, kt, ki == 0, ki == len(keys) - 1, gslot))
            SB = (0, 1, 5, 6, 7)
            LA = 3

            def issue_S(ui):
                col0_, ncols_, kt_, _, _, _ = units[ui]
                bk_ = SB[(si + ui) % len(SB)]
                c.mm(self.ps[bk_][:, :ncols_], [(Kh[:, kt_ * 128:(kt_ + 1) * 128], Qh[:, col0_:col0_ + ncols_])], reads=[bhd], writes=[self.bps[bk_]])

            def epilogue(col0_, ncols_, gslot):
                oT, boT = self.ps[2 + gslot], self.bps[2 + gslot]
                o, bo = osb[gslot], b_osb[gslot]
                ys, bys = ysb[gslot], b_ysb[gslot]
                c.op("act", lambda e: e.activation(out=o[:, :ncols_], in_=oT[0:65, :ncols_], func=AF.Copy), reads=[boT], writes=[bo])
                c.mm(self.ps[4][0:64, :ncols_], [(self.ones32[64:65, 0:64], o[64:65, :ncols_])], reads=[self.b_const, bo], writes=[self.bps[4]])
                rb, brb = rbs[gslot], b_rbs[gslot]
                c.op("dve", lambda e: e.reciprocal(out=rb[:, :ncols_], in_=self.ps[4][0:64, :ncols_]), reads=[self.bps[4]], writes=[brb])
                c.op("dve", lambda e: e.tensor_tensor(out=ys[:, :ncols_], in0=o[0:64, :ncols_], in1=rb[:, :ncols_], op=ALU.mult), reads=[bo, brb], writes=[bys])
                c.dma("sp", YT.ap()[h, :, col0_:col0_ + ncols_], ys[:, :ncols_], reads=[bys], writes=[b_YT])

            pending = []
            for k0 in range(min(LA, len(units))):
                issue_S(k0)
            for ui, (col0, ncols, kt, first, last, gslot) in enumerate(units):
                bk = SB[(si + ui) % len(SB)]
                sT, bsT = self.ps[bk], self.bps[bk]
                if ui + LA < len(units):
                    issue_S(ui + LA)
                PT, bPT = PTs[pi % 4], b_PT[pi % 4]
                pi += 1
                oT, boT = self.ps[2 + gslot], self.bps[2 + gslot]
                c.op("act", lambda e: e.activation(out=PT[:, :ncols], in_=sT[:, :ncols], func=AF.Exp, scale=scale), reads=[bsT], writes=[bPT])
                c.mm(oT[0:65, :ncols], [(Vh[:, kt, :], PT[:, :ncols])], reads=[bhd, bPT], writes=[boT], start=first, stop=last)
                if pending and pending[0][0] <= ui:
                    _, args = pending.pop(0)
                    epilogue(*args)
                if last:
                    pending.append((ui + 4, (col0, ncols, gslot)))
            for _, args in pending:
                epilogue(*args)
            si += len(units)
        c.barrier()
        c.sb_release(mark)
        Wo = c.sb([64, 16, 1024], BF16, "Wo")
        b_wo = Buf()
        c.dma("pool", Wo[:], w_out.rearrange("(h d) n -> d h n", d=64), writes=[b_wo])
        self.attn_out(li, need_ctx, Wo, b_wo, YT, b_YT)
        self.phase_end()

    def attn_out(self, li, need_ctx, Wo, b_wo, YT, b_YT):
        c = self.c
        NCT, NT = self.NCT, self.NT
        G1x, bG1x = self.load_mod(li, 2, 0, "G1x")
        G1c, bG1c = self.load_mod(li, 2, 1, "G1c")
        yTs = [c.sb([64, 16, 128], BF16, "yT") for _ in range(2)]
        b_yT = [Buf(), Buf()]
        xts = [c.sb([128, D], F32, "xres") for _ in range(2)]
        b_xt = [Buf(), Buf()]
        tmps = [c.sb([128, D], F32, "rtmp") for _ in range(2)]
        b_tmp = [Buf(), Buf()]
        qbs = list(range(0 if need_ctx else NCT, NT))

        def ao_load(bi_):
            qb_ = qbs[bi_]
            c.dma("sp", xts[bi_ % 2][:], self.xs.ap()[qb_ * 128:(qb_ + 1) * 128, :], reads=[self.b_xs[qb_]], writes=[b_xt[bi_ % 2]])
            c.dma("sp", yTs[bi_ % 2][:], YT.ap()[:, :, qb_ * 128:(qb_ + 1) * 128].rearrange("h d t -> d h t"), reads=[b_YT], writes=[b_yT[bi_ % 2]])
        ao_load(0)
        for bi, qb in enumerate(qbs):
            is_ctx = qb < NCT
            xt, bxt = xts[bi % 2], b_xt[bi % 2]
            yT, byT = yTs[bi % 2], b_yT[bi % 2]
            tmp, btmp = tmps[bi % 2], b_tmp[bi % 2]
            if bi + 1 < len(qbs):
                ao_load(bi + 1)
            G1, bG1 = (G1c, bG1c) if is_ctx else (G1x, bG1x)
            for nn in range(2):
                z, bz = self.ps[5 + nn], self.bps[5 + nn]
                c.mm(z[:, :], [(yT[:, hq, :], Wo[:, hq, nn * 512:(nn + 1) * 512]) for hq in range(16)], reads=[byT, b_wo], writes=[bz])
                c.op("dve", lambda e: e.tensor_tensor(out=tmp[:, nn * 512:(nn + 1) * 512], in0=z[:, :], in1=G1[:, nn * 512:(nn + 1) * 512], op=ALU.mult), reads=[bz, bG1], writes=[btmp])
            c.op("pool", lambda e: e.tensor_tensor(out=xt[:], in0=xt[:], in1=tmp[:], op=ALU.add), reads=[btmp, bxt], writes=[bxt])
            c.dma("sp", self.xs.ap()[qb * 128:(qb + 1) * 128, :], xt[:], reads=[bxt], writes=[self.b_xs[qb]])

    def layer_ssd(self, li, j, need_ctx):
        c, nc = self.c, self.nc
        T, NT, NCT, NL, NCX = self.T, self.NT, self.NCT, self.NL, self.NCX
        w_in = self.w["ssm_w_in"].ap()[j]
        XB = self.scratch(f"ssd_xb{li}", [24, 128, T], BF16)
        XC = self.scratch(f"ssd_xc{li}", [24, 128, T], BF16)
        Zs = self.scratch(f"ssd_z{li}", [NT, 128, 2048], BF16)
        DT = self.scratch(f"ssd_dt{li}", [NT, 128, 64], F32)
        Yd = [self.scratch(f"ssd_y{li}_{d}", [NT, 128, 2048], F32) for d in range(2)]
        b_XB = Buf(); b_XC = Buf(); b_Z = Buf(); b_DT = Buf(); b_Y = [Buf(), Buf()]
        b_w = Buf("ssd_w")
        Win = c.sb([128, 8, 5184], BF16, "ssdWin")
        for k in range(8):
            c.dma("pool", Win[:, k, :], w_in[k * 128:(k + 1) * 128, :], writes=[b_w])
        dtb = c.sb([128, 64], F32, "dtb")
        c.dma("sp", dtb[:], self.w["ssm_dt_bias"].ap()[j:j + 1].rearrange("o d h -> o (d h)").partition_broadcast(128), writes=[b_w])
        mark = c.sb_mark()
        A1x, bA1x = self.load_mod(li, 1, 0, "A1x")
        S1x, bS1x = self.load_mod(li, 0, 0, "S1x")
        A1c, bA1c = self.load_mod(li, 1, 1, "A1c")
        S1c, bS1c = self.load_mod(li, 0, 1, "S1c")
        self.alloc_norm_bufs(2)
        hTs = [c.sb([128, 8, 512], BF16, "hT") for _ in range(2)]
        b_hT = [Buf(), Buf()]
        stg = [c.sb([128, 512], BF16, "stg") for _ in range(3)]
        b_stg = [Buf() for _ in range(3)]
        zst = [c.sb([128, 2048], BF16, "zst") for _ in range(2)]
        b_zst = [Buf(), Buf()]
        dts = [c.sb([128, 64], F32, "dts") for _ in range(2)]
        b_dts = [Buf(), Buf()]
        cnt = 0
        for gi, (t0, n, is_ctx) in enumerate(self.groups(4)):
            ncols = n * 128
            col0 = t0 * 128
            hT, bh = hTs[gi % 2], b_hT[gi % 2]
            for s in range(n):
                if is_ctx:
                    self.norm_tile(t0 + s, A1c, bA1c, S1c, bS1c, hT, bh, s * 128, (t0 + s) % 2)
                else:
                    self.norm_tile(t0 + s, A1x, bA1x, S1x, bS1x, hT, bh, s * 128, (t0 + s) % 2)
            for fc in range(24):
                i = cnt % 3
                cnt += 1
                P, bP = self.ps[2 + i], self.bps[2 + i]
                c.mm(P[:, :ncols], [(Win[:, k, 2048 + fc * 128:2048 + (fc + 1) * 128], hT[:, k, :ncols]) for k in range(8)], reads=[b_w, bh], writes=[bP])
                c.op("act", lambda e: e.activation(out=stg[i][:, :ncols], in_=P[:, :ncols], func=AF.Copy), reads=[bP], writes=[b_stg[i]])
                c.dma("sp", XB.ap()[fc, :, col0:col0 + ncols], stg[i][:, :ncols], reads=[b_stg[i]], writes=[b_XB])
            for s in range(n):
                zs, bz = zst[s % 2], b_zst[s % 2]
                for zc in range(4):
                    i = cnt % 3
                    cnt += 1
                    P, bP = self.ps[2 + i], self.bps[2 + i]
                    c.mm(P[:, :], [(hT[:, k, s * 128:(s + 1) * 128], Win[:, k, zc * 512:(zc + 1) * 512]) for k in range(8)], reads=[b_w, bh], writes=[bP])
                    c.op("act", lambda e: e.activation(out=zs[:, zc * 512:(zc + 1) * 512], in_=P[:, :], func=AF.Silu), reads=[bP], writes=[bz])
                c.dma("sp", Zs.ap()[t0 + s], zs[:], reads=[bz], writes=[b_Z])
                i = cnt % 3
                cnt += 1
                P, bP = self.ps[2 + i], self.bps[2 + i]
                dt_, bdt = dts[s % 2], b_dts[s % 2]
                c.mm(P[:, 0:64], [(hT[:, k, s * 128:(s + 1) * 128], Win[:, k, 5120:5184]) for k in range(8)], reads=[b_w, bh], writes=[bP])
                c.op("dve", lambda e: e.tensor_tensor(out=dt_[:], in0=P[:, 0:64], in1=dtb[:], op=ALU.add), reads=[bP, b_w], writes=[bdt])
                c.op("act", lambda e: e.activation(out=dt_[:], in_=dt_[:], func=AF.Exp), reads=[bdt], writes=[bdt])
                c.op("act", lambda e: e.activation(out=dt_[:], in_=dt_[:], func=AF.Ln, bias=1.0), reads=[bdt], writes=[bdt])
                c.dma("sp", DT.ap()[t0 + s], dt_[:], reads=[bdt], writes=[b_DT])
        c.barrier()
        c.sb_release(self.mark0)
        cw = c.sb([128, 24, 5], F32, "convw"); cbias = c.sb([128, 24], F32, "convb")
        b_cw = Buf()
        c.dma("sp", cw[:], self.w["ssm_conv_wT"].ap()[j], writes=[b_cw])
        c.dma("sp", cbias[:], self.w["ssm_conv_bT"].ap()[j], writes=[b_cw])
        segs = [(0, NCX), (NCX, NL)]
        Lmax = max(NCX, NL)
        xp = [c.sb([128, Lmax + 4], BF16, "xp") for _ in range(2)]
        b_xp = [Buf(), Buf()]
        acc = [c.sb([128, Lmax], F32, "cacc") for _ in range(2)]
        b_acc = [Buf(), Buf()]
        cout = [c.sb([128, Lmax], BF16, "cout") for _ in range(2)]
        b_cout = [Buf(), Buf()]
        it = 0
        for fc in range(24):
            for (o0, L) in segs:
                i = it % 2
                it += 1
                c.op("pool", lambda e: e.memset(xp[i][:], 0.0), writes=[b_xp[i]])
                c.dma("sp", xp[i][:, 2:2 + L], XB.ap()[fc, :, o0:o0 + L], reads=[b_XB], writes=[b_xp[i]])
                c.op("dve", lambda e: e.tensor_scalar(out=acc[i][:, :L], in0=xp[i][:, 0:L], scalar1=cw[:, fc, 0:1], scalar2=None, op0=ALU.mult), reads=[b_xp[i], b_cw], writes=[b_acc[i]])
                for k in range(1, 5):
                    eng = "dve"
                    c.op(eng, lambda e: e.scalar_tensor_tensor(out=acc[i][:, :L], in0=xp[i][:, k:k + L], scalar=cw[:, fc, k:k + 1], in1=acc[i][:, :L], op0=ALU.mult, op1=ALU.add),
                         reads=[b_xp[i], b_cw, b_acc[i]], writes=[b_acc[i]])
                c.op("act", lambda e: e.activation(out=cout[i][:, :L], in_=acc[i][:, :L], func=AF.Silu, bias=cbias[:, fc:fc + 1]), reads=[b_acc[i], b_cw], writes=[b_cout[i]])
                c.dma("sp", XC.ap()[fc, :, o0:o0 + L], cout[i][:, :L], reads=[b_cout[i]], writes=[b_XC])
        c.barrier()
        c.sb_release(self.mark0)
        sel = c.sb([32, 32, 128], F32, "sel32"); b_sel = Buf()
        c.dma("sp", sel[:], self.w["sel32"].ap(), writes=[b_sel])
        aneg = c.sb([128, 64], F32, "aneg"); dsk = c.sb([128, 64], F32, "dsk"); b_an = Buf()
        c.dma("sp", aneg[:], self.w["ssm_a_log"].ap()[j:j + 1].rearrange("o d h -> o (d h)").partition_broadcast(128), writes=[b_an])
        c.op("act", lambda e: e.activation(out=aneg[:], in_=aneg[:], func=AF.Exp), reads=[b_an], writes=[b_an])
        c.op("act", lambda e: e.activation(out=aneg[:], in_=aneg[:], func=AF.Copy, scale=-1.0), reads=[b_an], writes=[b_an])
        c.dma("sp", dsk[:], self.w["ssm_d"].ap()[j:j + 1].rearrange("o d h -> o (d h)").partition_broadcast(128), writes=[b_an])
        c.op("dve", lambda e: e.tensor_tensor(out=dsk[:, 0:32], in0=dsk[:, 0:32], in1=dsk[:, 32:64], op=ALU.add), reads=[b_an], writes=[b_an])
        xcs = [c.sb([128, 24, 128], BF16, "xc") for _ in range(2)]; b_xc = [Buf(), Buf()]
        dtt = [c.sb([128, 64], F32, "dtt") for _ in range(2)]; b_dtt = [Buf(), Buf()]
        gt = [c.sb([128, 8, 32], F32, "gt") for _ in range(2)]; b_gt = [Buf(), Buf()]
        acT = [c.sb([32, 128], F32, "acT") for _ in range(2)]; b_acT = [Buf(), Buf()]
        nacT = [c.sb([32, 128], F32, "nacT") for _ in range(2)]
        xtok = [c.sb([128, 32, 64], F32, "xtok") for _ in range(2)]; b_xtok = [Buf(), Buf()]
        u = [c.sb([128, 32, 64], BF16, "u") for _ in range(2)]; b_u = [Buf(), Buf()]
        Vw = [c.sb([128, 32, 64], BF16, "Vw") for _ in range(2)]; b_Vw = [Buf(), Buf()]
        Btok = [c.sb([128, 4, 128], BF16, "Btok") for _ in range(2)]; b_Bt = [Buf(), Buf()]
        scm = [c.sb([128, 128], F32, "scm") for _ in range(2)]; b_scm = [Buf(), Buf()]
        aa = [c.sb([128, 512], F32, "aa") for _ in range(4)]; b_aa = [Buf() for _ in range(4)]
        EE = [c.sb([128, 512], F32, "EE") for _ in range(4)]; b_EE = [Buf() for _ in range(4)]
        MT = [c.sb([128, 512], BF16, "MT") for _ in range(4)]; b_MT = [Buf() for _ in range(4)]
        yi = [c.sb([128, 512], F32, "yi") for _ in range(2)]; b_yi = [Buf(), Buf()]
        Yt = [c.sb([128, 2048], F32, "Yt") for _ in range(2)]; b_Yt = [Buf(), Buf()]
        S32 = c.sb([128, 4, 512], F32, "S32"); Sb = c.sb([128, 4, 512], BF16, "Sb"); b_S = [Buf() for _ in range(4)]
        ci = 0; hi = 0
        for d in range(2):
            tri = self.trile32 if d == 0 else self.trige32
            order = list(range(NT)) if d == 0 else (list(range(NCT - 1, -1, -1)) + list(range(NT - 1, NCT - 1, -1)))
            c.op("dve", lambda e: e.memset(S32[:], 0.0), writes=b_S)
            c.op("pool", lambda e: e.memset(Sb[:], 0.0), writes=b_S)
            def prep_load(ch, i):
                xc, bxc = xcs[i], b_xc[i]
                c.dma("sp", xc[:], XC.ap()[:, :, ch * 128:(ch + 1) * 128].rearrange("f p t -> p f t"), reads=[b_XC], writes=[bxc])
                c.dma("sp", dtt[i][:], DT.ap()[ch], reads=[b_DT], writes=[b_dtt[i]])

            def prep(ch, i):
                xc, bxc = xcs[i], b_xc[i]
                g_, bg = gt[i], b_gt[i]
                dc = slice(d * 32, (d + 1) * 32)
                c.op("dve", lambda e: e.tensor_tensor(out=g_[:, 0, :], in0=dtt[i][:, dc], in1=aneg[:, dc], op=ALU.mult), reads=[b_dtt[i], b_an], writes=[bg])
                p0, bp0 = self.ps[0], self.bps[0]
                c.mm(p0[:, 0:32], [(tri, g_[:, 0, :])], reads=[self.b_const, bg], writes=[bp0])
                c.mm(p0[:, 32:64], [(self.ones32, g_[:, 0, :])], reads=[self.b_const, bg], writes=[bp0])
                c.mm(p0[0:32, 128:256], [(g_[:, 0, :], tri)], reads=[self.b_const, bg], writes=[bp0])
                c.op("act", lambda e: e.activation(out=g_[:, 1, :], in_=p0[:, 0:32], func=AF.Copy), reads=[bp0], writes=[bg])
                c.op("act", lambda e: e.activation(out=g_[:, 2, :], in_=p0[:, 0:32], func=AF.Copy, scale=-1.0), reads=[bp0], writes=[bg])
                c.op("act", lambda e: e.activation(out=g_[:, 3, :], in_=p0[:, 0:32], func=AF.Exp), reads=[bp0], writes=[bg])
                c.op("dve", lambda e: e.tensor_tensor(out=g_[:, 4, :], in0=p0[:, 32:64], in1=g_[:, 1, :], op=ALU.subtract), reads=[bp0, bg], writes=[bg])
                c.op("act", lambda e: e.activation(out=g_[:, 4, :], in_=g_[:, 4, :], func=AF.Exp), reads=[bg], writes=[bg])
                c.op("act", lambda e: e.activation(out=g_[:, 5, :], in_=p0[:, 32:64], func=AF.Exp), reads=[bp0], writes=[bg])
                c.op("act", lambda e: e.activation(out=acT[i][:], in_=p0[0:32, 128:256], func=AF.Copy), reads=[bp0], writes=[b_acT[i]])
                c.op("act", lambda e: e.activation(out=nacT[i][:], in_=p0[0:32, 128:256], func=AF.Copy, scale=-1.0), reads=[bp0], writes=[b_acT[i]])
                for hh in range(2):
                    pT = self.ps[1].ap().bitcast(BF16)
                    c.tr_multi([(pT[:, k * 128:(k + 1) * 128], xc[:, hh * 8 + k, :], self.identb) for k in range(8)], reads=[bxc, self.b_const], writes=[self.bps[1]])
                    c.op("act", lambda e: e.activation(out=xtok[i][:, hh * 16:(hh + 1) * 16, :].rearrange("p h d -> p (h d)"), in_=pT[:, :], func=AF.Copy), reads=[self.bps[1]], writes=[b_xtok[i]])
                pT = self.ps[1].ap().bitcast(BF16)
                c.tr_multi([(pT[:, k * 128:(k + 1) * 128], xc[:, 16 + k, :], self.identb) for k in range(4)], reads=[bxc, self.b_const], writes=[self.bps[1]])
                c.op("act", lambda e: e.activation(out=Btok[i][:].rearrange("p g n -> p (g n)"), in_=pT[:, 0:512], func=AF.Copy), reads=[self.bps[1]], writes=[b_Bt[i]])
                c.op("dve", lambda e: e.tensor_tensor(out=u[i][:], in0=xtok[i][:], in1=dtt[i][:, dc].unsqueeze(2).to_broadcast([128, 32, 64]), op=ALU.mult), reads=[b_xtok[i], b_dtt[i]], writes=[b_u[i]])
                c.op("pool", lambda e: e.tensor_tensor(out=Vw[i][:], in0=u[i][:], in1=g_[:, 4, :].unsqueeze(2).to_broadcast([128, 32, 64]), op=ALU.mult), reads=[b_u[i], bg], writes=[b_Vw[i]])
            def groups_(ch, i, nxt):
                if nxt is not None:
                    prep_load(*nxt)
                xc, bxc = xcs[i], b_xc[i]
                g_, bg = gt[i], b_gt[i]
                dc = slice(d * 32, (d + 1) * 32)
                Y, bY = Yt[i], b_Yt[i]
                for g in range(4):
                    if g == 2 and nxt is not None:
                        prep(*nxt)
                    pcb, bpcb = self.ps[2], self.bps[2]
                    c.mm(pcb[:, 0:128], [(xc[:, 16 + g, :], xc[:, 20 + g, :])], reads=[bxc], writes=[bpcb])
                    sm, bsm = scm[g % 2], b_scm[g % 2]
                    c.op("dve", lambda e: e.tensor_tensor(out=sm[:], in0=pcb[:, 0:128], in1=tri, op=ALU.mult), reads=[bpcb, self.b_const], writes=[bsm])
                    yps, byps = self.ps[4 + g % 2], self.bps[4 + g % 2]
                    for e8 in range(8):
                        h = g * 8 + e8
                        pbc, bpbc = self.ps[6 + e8 // 4], self.bps[6 + e8 // 4]
                        c.mm(pbc[:, (e8 % 4) * 128:(e8 % 4 + 1) * 128], [(sel[:, h, :], acT[i][:]), (nacT[i][:], sel[:, h, :])], reads=[b_sel, b_acT[i]], writes=[bpbc])
                    for hb in range(2):
                        k4 = (g % 2) * 2 + hb
                        pbc, bpbc = self.ps[6 + hb], self.bps[6 + hb]
                        c.op("dve", lambda e: e.tensor_scalar(out=aa[k4][:], in0=pbc[:, :], scalar1=0.0, scalar2=None, op0=ALU.min), reads=[bpbc], writes=[b_aa[k4]])
                        c.op("act", lambda e: e.activation(out=EE[k4][:], in_=aa[k4][:], func=AF.Exp), reads=[b_aa[k4]], writes=[b_EE[k4]])
                        c.op("pool", lambda e: e.tensor_tensor(out=MT[k4][:].rearrange("p (h t) -> p h t", h=4), in0=EE[k4][:].rearrange("p (h t) -> p h t", h=4),
                                                               in1=sm[:].unsqueeze(1).to_broadcast([128, 4, 128]), op=ALU.mult), reads=[bsm, b_EE[k4]], writes=[b_MT[k4]])
                    for e8 in range(8):
                        h = g * 8 + e8
                        k4 = (g % 2) * 2 + e8 // 4
                        c.mm(yps[:, e8 * 64:(e8 + 1) * 64], [(MT[k4][:, (e8 % 4) * 128:(e8 % 4 + 1) * 128], u[i][:, h, :])], reads=[b_MT[k4], b_u[i]], writes=[byps])
                    pin, bpin = self.ps[3], self.bps[3]
                    c.mm(pin[:, :], [(xc[:, 20 + g, :], Sb[:, g, :])], reads=[bxc, b_S[g]], writes=[bpin])
                    y_, byi = yi[g % 2], b_yi[g % 2]
                    c.op("dve", lambda e: e.tensor_tensor(out=y_[:].rearrange("p (h d) -> p h d", d=64), in0=pin[:, :].rearrange("p (h d) -> p h d", d=64),
                                                          in1=g_[:, 3, g * 8:(g + 1) * 8].unsqueeze(2).to_broadcast([128, 8, 64]), op=ALU.mult), reads=[bpin, bg], writes=[byi])
                    c.op("dve", lambda e: e.tensor_tensor(out=Y[:, g * 512:(g + 1) * 512], in0=yps[:, :], in1=y_[:], op=ALU.add), reads=[byps, byi], writes=[bY])
                    if d == 0:
                        c.op("pool", lambda e: e.tensor_tensor(out=y_[:].rearrange("p (h d) -> p h d", d=64), in0=xtok[i][:, g * 8:(g + 1) * 8, :],
                                                               in1=dsk[:, g * 8:(g + 1) * 8].unsqueeze(2).to_broadcast([128, 8, 64]), op=ALU.mult), reads=[b_xtok[i], b_an, bY], writes=[byi])
                        c.op("pool", lambda e: e.tensor_tensor(out=Y[:, g * 512:(g + 1) * 512], in0=Y[:, g * 512:(g + 1) * 512], in1=y_[:], op=ALU.add), reads=[byi], writes=[bY])
                    pst, bpst = self.ps[3], self.bps[3]
                    c.mm(pst[:, :], [(Btok[i][:, g, :], Vw[i][:, g * 8:(g + 1) * 8, :].rearrange("p h d -> p (h d)"))], reads=[b_Bt[i], b_Vw[i]], writes=[bpst])
                    c.op("dve", lambda e: e.tensor_tensor(out=S32[:, g, :].rearrange("p (h d) -> p h d", d=64), in0=S32[:, g, :].rearrange("p (h d) -> p h d", d=64),
                                                          in1=g_[:, 5, g * 8:(g + 1) * 8].unsqueeze(2).to_broadcast([128, 8, 64]), op=ALU.mult), reads=[bg], writes=[b_S[g]])
                    c.op("dve", lambda e: e.tensor_tensor(out=S32[:, g, :], in0=S32[:, g, :], in1=pst[:, :], op=ALU.add), reads=[bpst], writes=[b_S[g]])
                    c.op("act", lambda e: e.activation(out=Sb[:, g, :], in_=S32[:, g, :], func=AF.Copy), reads=[], writes=[b_S[g]])
                c.dma("sp", Yd[d].ap()[ch], Y[:], reads=[bY], writes=[b_Y[d]])
            prep_load(order[0], 0)
            prep(order[0], 0)
            for idx, ch in enumerate(order):
                groups_(ch, idx % 2, (order[idx + 1], (idx + 1) % 2) if idx + 1 < len(order) else None)
        c.barrier()
        c.sb_release(self.mark0)
        Wo = c.sb([128, 16, 1024], BF16, "ssdWo"); b_wo = Buf()
        c.dma("pool", Wo[:], self.w["ssm_w_out"].ap()[j].rearrange("(k p) n -> p k n", p=128), writes=[b_wo])
        ng = c.sb([128, 2048], F32, "ssdng")
        c.dma("sp", ng[:], self.w["ssm_norm_g"].ap()[j:j + 1, :].partition_broadcast(128), writes=[b_wo])
        self.gated_out(li, need_ctx, Yd, b_Y, Zs, b_Z, 2048, 4, ng, Wo, b_wo, False)
        self.phase_end()

    def gated_out(self, li, need_ctx, Yd, b_Y, Zs, b_Z, W, ngroups, ng, Wo, b_wo, gate_after):
        c = self.c
        NCT, NT = self.NCT, self.NT
        KC = W // 128
        gs = W // ngroups
        G1x, bG1x = self.load_mod(li, 2, 0, "G1x")
        G1c, bG1c = self.load_mod(li, 2, 1, "G1c")
        yf = [c.sb([128, W], F32, "yf") for _ in range(2)]; yb = [c.sb([128, W], F32, "yb") for _ in range(2)]
        zt = [c.sb([128, W], BF16, "zt") for _ in range(2)]
        b_in = [Buf(), Buf()]
        jk = c.sb([128, W], BF16, "gjunk"); b_jk = Buf()
        st = [c.sb([128, 2, 8], F32, "gst") for _ in range(2)]; b_st = [Buf(), Buf()]
        yn = [c.sb([128, W], BF16, "yn") for _ in range(2)]; b_yn = [Buf(), Buf()]
        yT = [c.sb([128, KC, 128], BF16, "gyT") for _ in range(2)]; b_yT = [Buf(), Buf()]
        xts = [c.sb([128, D], F32, "xres") for _ in range(2)]; b_xt = [Buf(), Buf()]
        tmps = [c.sb([128, D], F32, "rtmp") for _ in range(2)]; b_tmp = [Buf(), Buf()]
        qbs = list(range(0 if need_ctx else NCT, NT))

        def go_load(bi_):
            i_ = bi_ % 2
            qb_ = qbs[bi_]
            c.dma("sp", yf[i_][:], Yd[0].ap()[qb_], reads=[b_Y[0]], writes=[b_in[i_]])
            c.dma("sp", yb[i_][:], Yd[1].ap()[qb_], reads=[b_Y[1]], writes=[b_in[i_]])
            c.dma("sp", zt[i_][:], Zs.ap()[qb_], reads=[b_Z], writes=[b_in[i_]])
            c.dma("sp", xts[i_][:], self.xs.ap()[qb_ * 128:(qb_ + 1) * 128, :], reads=[self.b_xs[qb_]], writes=[b_xt[i_]])
        go_load(0)
        for bi, qb in enumerate(qbs):
            i = bi % 2
            is_ctx = qb < NCT
            if bi + 1 < len(qbs):
                go_load(bi + 1)
            c.op("pool", lambda e: e.tensor_tensor(out=yf[i][:], in0=yf[i][:], in1=yb[i][:], op=ALU.add), reads=[b_in[i]], writes=[b_in[i]])
            if not gate_after:
                c.op("dve", lambda e: e.tensor_tensor(out=yf[i][:], in0=yf[i][:], in1=zt[i][:], op=ALU.mult), reads=[b_in[i]], writes=[b_in[i]])
            c.op("dve", lambda e: e.memset(st[i][:], 0.0), writes=[b_st[i]])
            for g in range(ngroups):
                c.op("act", lambda e: e.activation(out=jk[:, g * gs:(g + 1) * gs], in_=yf[i][:, g * gs:(g + 1) * gs], func=AF.Square, accum_out=st[i][:, 0, g:g + 1]), reads=[b_in[i]], writes=[b_jk, b_st[i]])
            c.op("dve", lambda e: e.tensor_scalar(out=st[i][:, 1, :], in0=st[i][:, 0, :], scalar1=1.0 / gs, scalar2=EPS, op0=ALU.mult, op1=ALU.add), reads=[b_st[i]], writes=[b_st[i]])
            c.op("act", lambda e: e.activation(out=st[i][:, 1, :], in_=st[i][:, 1, :], func=AF.Sqrt), reads=[b_st[i]], writes=[b_st[i]])
            c.op("dve", lambda e: e.reciprocal(out=st[i][:, 1, :], in_=st[i][:, 1, :]), reads=[b_st[i]], writes=[b_st[i]])
            c.op("dve", lambda e: e.tensor_tensor(out=yf[i][:].rearrange("p (g d) -> p g d", g=ngroups), in0=yf[i][:].rearrange("p (g d) -> p g d", g=ngroups),
                                                  in1=st[i][:, 1, 0:ngroups].unsqueeze(2).to_broadcast([128, ngroups, gs]), op=ALU.mult), reads=[b_st[i], b_in[i]], writes=[b_in[i]])
            if gate_after:
                c.op("pool", lambda e: e.tensor_tensor(out=yf[i][:], in0=yf[i][:], in1=ng[:], op=ALU.mult), reads=[b_in[i], b_wo], writes=[b_in[i]])
                c.op("dve", lambda e: e.tensor_tensor(out=yn[i][:], in0=yf[i][:], in1=zt[i][:], op=ALU.mult), reads=[b_in[i]], writes=[b_yn[i]])
            else:
                c.op("pool", lambda e: e.tensor_tensor(out=yn[i][:], in0=yf[i][:], in1=ng[:], op=ALU.mult), reads=[b_in[i], b_wo], writes=[b_yn[i]])
            for hb in range(KC // 8):
                pT = self.ps[hb].ap().bitcast(BF16)
                c.tr_multi([(pT[:, k * 128:(k + 1) * 128], yn[i][:, (hb * 8 + k) * 128:(hb * 8 + k + 1) * 128], self.identb) for k in range(8)], reads=[b_yn[i], self.b_const], writes=[self.bps[hb]])
                c.op("act", lambda e: e.activation(out=yT[i][:, hb * 8:(hb + 1) * 8, :], in_=pT.rearrange("p (k n) -> p k n", k=8), func=AF.Copy), reads=[self.bps[hb]], writes=[b_yT[i]])
            G1, bG1 = (G1c, bG1c) if is_ctx else (G1x, bG1x)
            for nn in range(2):
                z, bz = self.ps[5 + nn], self.bps[5 + nn]
                c.mm(z[:, :], [(yT[i][:, k, :], Wo[:, k, nn * 512:(nn + 1) * 512]) for k in range(KC)], reads=[b_yT[i], b_wo], writes=[bz])
                c.op("dve", lambda e: e.tensor_tensor(out=tmps[i][:, nn * 512:(nn + 1) * 512], in0=z[:, :], in1=G1[:, nn * 512:(nn + 1) * 512], op=ALU.mult), reads=[bz, bG1], writes=[b_tmp[i]])
            c.op("pool", lambda e: e.tensor_tensor(out=xts[i][:], in0=xts[i][:], in1=tmps[i][:], op=ALU.add), reads=[b_tmp[i], b_xt[i]], writes=[b_xt[i]])
            c.dma("sp", self.xs.ap()[qb * 128:(qb + 1) * 128, :], xts[i][:], reads=[b_xt[i]], writes=[self.b_xs[qb]])

    def layer_mlstm(self, li, j, need_ctx):
        c, nc = self.c, self.nc
        T, NT, NCT = self.T, self.NT, self.NCT
        w_in = self.w["mlstm_w_in"].ap()[j]
        QK = self.scratch(f"ml_qk{li}", [2, 8, 64, T], BF16)
        Kt = self.scratch(f"ml_kt{li}", [NT, 128, 512], BF16)
        Va = self.scratch(f"ml_va{li}", [NT, 128, 8, 129], BF16)
        Os = self.scratch(f"ml_os{li}", [NT, 128, 1024], BF16)
        Gt = self.scratch(f"ml_gt{li}", [NT, 128, 32], F32)
        Yd = [self.scratch(f"ml_y{li}_{d}", [NT, 128, 1024], F32) for d in range(2)]
        b_QK = Buf(); b_Kt = Buf(); b_Va = Buf(); b_Os = Buf(); b_Gt = Buf(); b_Y = [Buf(), Buf()]
        b_w = Buf("ml_w")
        Win = c.sb([128, 8, 3104], BF16, "mlWin")
        for k in range(8):
            c.dma("pool", Win[:, k, :], w_in[k * 128:(k + 1) * 128, :], writes=[b_w])
        gb = c.sb([128, 32], F32, "mlgb")
        c.dma("sp", gb[:], self.w["mlstm_gate_b"].ap()[j:j + 1].rearrange("o a h -> o (a h)").partition_broadcast(128), writes=[b_w])
        A1x, bA1x = self.load_mod(li, 1, 0, "A1x")
        S1x, bS1x = self.load_mod(li, 0, 0, "S1x")
        A1c, bA1c = self.load_mod(li, 1, 1, "A1c")
        S1c, bS1c = self.load_mod(li, 0, 1, "S1c")
        self.alloc_norm_bufs(2)
        hTs = [c.sb([128, 8, 512], BF16, "hT") for _ in range(2)]
        b_hT = [Buf(), Buf()]
        stg = [c.sb([64, 512], BF16, "mlstg") for _ in range(3)]; b_stg = [Buf() for _ in range(3)]
        kst = [c.sb([128, 512], BF16, "mlkst") for _ in range(2)]; b_kst = [Buf(), Buf()]
        vst = [c.sb([128, 8, 129], BF16, "mlvst") for _ in range(2)]; b_vst = [Buf(), Buf()]
        ost = [c.sb([128, 1024], BF16, "mlost") for _ in range(2)]; b_ost = [Buf(), Buf()]
        gst = [c.sb([128, 32], F32, "mlgst") for _ in range(2)]; b_gst = [Buf(), Buf()]
        gtmp = [c.sb([128, 8], F32, "mlgtmp") for _ in range(2)]
        for i in range(2):
            c.op("pool", lambda e: e.memset(vst[i][:], 1.0), writes=[b_vst[i]])
        cnt = 0
        for gi, (t0, n, is_ctx) in enumerate(self.groups(4)):
            ncols = n * 128
            col0 = t0 * 128
            hT, bh = hTs[gi % 2], b_hT[gi % 2]
            for s in range(n):
                if is_ctx:
                    self.norm_tile(t0 + s, A1c, bA1c, S1c, bS1c, hT, bh, s * 128, (t0 + s) % 2)
                else:
                    self.norm_tile(t0 + s, A1x, bA1x, S1x, bS1x, hT, bh, s * 128, (t0 + s) % 2)
            for qk in range(2):
                for h in range(8):
                    i = cnt % 3
                    cnt += 1
                    P, bP = self.ps[2 + i], self.bps[2 + i]
                    col = qk * 512 + h * 64
                    c.mm(P[0:64, :ncols], [(Win[:, k, col:col + 64], hT[:, k, :ncols]) for k in range(8)], reads=[b_w, bh], writes=[bP])
                    c.op("act", lambda e: e.activation(out=stg[i][:, :ncols], in_=P[0:64, :ncols], func=AF.Copy, scale=(0.125 if qk == 1 else 1.0)), reads=[bP], writes=[b_stg[i]])
                    c.dma("sp", QK.ap()[qk, h, :, col0:col0 + ncols], stg[i][:, :ncols], reads=[b_stg[i]], writes=[b_QK])
            for s in range(n):
                sl = slice(s * 128, (s + 1) * 128)
                i2 = s % 2

                def tokproj(c0, w_):
                    nonlocal cnt
                    i = cnt % 3
                    cnt += 1
                    P, bP = self.ps[2 + i], self.bps[2 + i]
                    c.mm(P[:, :w_], [(hT[:, k, sl], Win[:, k, c0:c0 + w_]) for k in range(8)], reads=[b_w, bh], writes=[bP])
                    return P, bP
                P, bP = tokproj(512, 512)
                c.op("act", lambda e: e.activation(out=kst[i2][:], in_=P[:, :], func=AF.Copy, scale=0.125), reads=[bP], writes=[b_kst[i2]])
                c.dma("sp", Kt.ap()[t0 + s], kst[i2][:], reads=[b_kst[i2]], writes=[b_Kt])
                for vh in range(2):
                    P, bP = tokproj(1024 + vh * 512, 512)
                    c.op("act", lambda e: e.activation(out=vst[i2][:, vh * 4:(vh + 1) * 4, 0:128], in_=P[:, :].rearrange("p (h d) -> p h d", d=128), func=AF.Copy), reads=[bP], writes=[b_vst[i2]])
                c.dma("sp", Va.ap()[t0 + s], vst[i2][:], reads=[b_vst[i2]], writes=[b_Va])
                for oh in range(2):
                    P, bP = tokproj(2048 + oh * 512, 512)
                    c.op("act", lambda e: e.activation(out=ost[i2][:, oh * 512:(oh + 1) * 512], in_=P[:, :], func=AF.Sigmoid), reads=[bP], writes=[b_ost[i2]])
                c.dma("sp", Os.ap()[t0 + s], ost[i2][:], reads=[b_ost[i2]], writes=[b_Os])
                P, bP = tokproj(3072, 32)
                g_ = gst[i2]
                c.op("dve", lambda e: e.tensor_tensor(out=g_[:], in0=P[:, 0:32], in1=gb[:], op=ALU.add), reads=[bP, b_w], writes=[b_gst[i2]])
                for r in (1, 3):
                    cs_ = slice(r * 8, (r + 1) * 8)
                    c.op("act", lambda e: e.activation(out=g_[:, cs_], in_=g_[:, cs_], func=AF.Exp, scale=-1.0), reads=[b_gst[i2]], writes=[b_gst[i2]])
                    c.op("act", lambda e: e.activation(out=g_[:, cs_], in_=g_[:, cs_], func=AF.Ln, bias=1.0), reads=[b_gst[i2]], writes=[b_gst[i2]])
                    c.op("act", lambda e: e.activation(out=g_[:, cs_], in_=g_[:, cs_], func=AF.Copy, scale=-1.0), reads=[b_gst[i2]], writes=[b_gst[i2]])
                c.dma("sp", Gt.ap()[t0 + s], g_[:], reads=[b_gst[i2]], writes=[b_Gt])
        c.barrier()
        c.sb_release(self.mark0)
        sel = c.sb([8, 8, 128], F32, "sel8"); b_sel = Buf()
        c.dma("sp", sel[:], self.w["sel32"].ap()[0:8, 0:8, :], writes=[b_sel])
        qTs = [c.sb([64, 8, 128], BF16, "mqT") for _ in range(2)]; kTs = [c.sb([64, 8, 128], BF16, "mkT") for _ in range(2)]
        kts = [c.sb([128, 512], BF16, "mkt") for _ in range(2)]; vas = [c.sb([128, 8, 129], BF16, "mva") for _ in range(2)]
        gts = [c.sb([128, 32], F32, "mgt") for _ in range(2)]; b_ld = [Buf(), Buf()]
        GM = [c.sb([8, 12, 128], F32, "GM") for _ in range(2)]; b_GM = [Buf(), Buf()]
        sm8 = [c.sb([8, 8], F32, "sm8") for _ in range(2)]; b_sm8 = [Buf(), Buf()]
        ms = c.sb([8, 2], F32, "ms"); b_ms = Buf()
        dg = c.sb([8, 8], F32, "dg"); b_dg = Buf()
        tk = [c.sb([128, 40], F32, "tk") for _ in range(2)]; b_tk = [Buf(), Buf()]
        cwc = [c.sb([64, 8], F32, "cwc") for _ in range(2)]; b_cwc = [Buf(), Buf()]
        scm = [c.sb([128, 512], F32, "mscm") for _ in range(2)]; b_scm = [Buf() for _ in range(2)]
        aa = [c.sb([128, 512], F32, "maa") for _ in range(2)]; b_aa = [Buf() for _ in range(2)]
        EE = [c.sb([128, 512], F32, "mEE") for _ in range(2)]; b_EE = [Buf() for _ in range(2)]
        MT = [c.sb([128, 512], BF16, "mMT") for _ in range(2)]; b_MT = [Buf() for _ in range(2)]
        yi = [c.sb([128, 129], F32, "myi") for _ in range(4)]; b_yi = [Buf() for _ in range(4)]
        nd4 = [c.sb([128, 4, 132], F32, "mnd4") for _ in range(2)]; b_nd4 = [Buf() for _ in range(2)]
        Vw = [c.sb([128, 129], BF16, "mVw") for _ in range(4)]; b_Vw = [Buf() for _ in range(4)]
        Yt = [c.sb([128, 1024], F32, "mYt") for _ in range(2)]; b_Yt = [Buf(), Buf()]
        S32 = c.sb([64, 8, 129], F32, "mS32"); Sb = c.sb([64, 8, 129], BF16, "mSb"); b_S = [Buf() for _ in range(8)]
        ci = 0; hi = 0
        id8 = self.ident32[0:8, 0:8]
        for d in range(2):
            tri = self.trile32 if d == 0 else self.trige32
            order = list(range(NT)) if d == 0 else (list(range(NCT - 1, -1, -1)) + list(range(NT - 1, NCT - 1, -1)))
            c.op("dve", lambda e: e.memset(S32[:], 0.0), writes=b_S)
            c.op("pool", lambda e: e.memset(Sb[:], 0.0), writes=b_S)
            c.op("dve", lambda e: e.memset(ms[:], 0.0), writes=[b_ms])
            endc = 127 if d == 0 else 0
            def gate_load(ch, i):
                cols = slice(ch * 128, (ch + 1) * 128)
                bl = b_ld[i]
                c.dma("sp", qTs[i][:], QK.ap()[0, :, :, cols].rearrange("h d t -> d h t"), reads=[b_QK], writes=[bl])
                c.dma("sp", kTs[i][:], QK.ap()[1, :, :, cols].rearrange("h d t -> d h t"), reads=[b_QK], writes=[bl])
                c.dma("sp", kts[i][:], Kt.ap()[ch], reads=[b_Kt], writes=[bl])
                c.dma("sp", vas[i][:], Va.ap()[ch], reads=[b_Va], writes=[bl])
                c.dma("sp", gts[i][:], Gt.ap()[ch], reads=[b_Gt], writes=[bl])

            def gate(ch, i):
                bl = b_ld[i]
                ig = gts[i][:, d * 16:d * 16 + 8]
                lf = gts[i][:, d * 16 + 8:d * 16 + 16]
                G, bG = GM[i], b_GM[i]
                s8, bs8 = sm8[i], b_sm8[i]
                t_, bt = tk[i], b_tk[i]
                p0, bp0 = self.ps[0], self.bps[0]
                c.mm(p0[0:8, 0:128], [(ig, self.ident32)], reads=[bl, self.b_const], writes=[bp0])
                c.mm(p0[0:8, 128:256], [(lf, tri)], reads=[bl, self.b_const], writes=[bp0])
                c.mm(p0[:, 256:264], [(tri, lf)], reads=[bl, self.b_const], writes=[bp0])
                c.op("act", lambda e: e.activation(out=G[:, 0:2, :], in_=p0[0:8, 0:256].rearrange("p (a t) -> p a t", a=2), func=AF.Copy), reads=[bp0], writes=[bG])
                c.op("act", lambda e: e.activation(out=t_[:, 32:40], in_=p0[:, 256:264], func=AF.Copy), reads=[bp0], writes=[bt])
                c.op("dve", lambda e: e.tensor_tensor(out=t_[:, 0:8], in0=ig, in1=t_[:, 32:40], op=ALU.subtract), reads=[bl, bt], writes=[bt])
                c.op("dve", lambda e: e.tensor_tensor(out=G[:, 2, :], in0=G[:, 0, :], in1=G[:, 1, :], op=ALU.subtract), reads=[bG], writes=[bG])
                src, dst = 2, 3
                for k in range(7):
                    sft = 1 << k
                    c.op("dve", lambda e: e.tensor_copy(out=G[:, dst, :], in_=G[:, src, :]), reads=[bG], writes=[bG])
                    if d == 0:
                        c.op("dve", lambda e: e.tensor_tensor(out=G[:, dst, sft:128], in0=G[:, src, sft:128], in1=G[:, src, 0:128 - sft], op=ALU.max), reads=[bG], writes=[bG])
                    else:
                        c.op("dve", lambda e: e.tensor_tensor(out=G[:, dst, 0:128 - sft], in0=G[:, src, 0:128 - sft], in1=G[:, src, sft:128], op=ALU.max), reads=[bG], writes=[bG])
                    src, dst = dst, (3 if dst == 4 else 4)
                cmr = src
                c.op("dve", lambda e: e.tensor_scalar(out=G[:, cmr, :], in0=G[:, cmr, :], scalar1=ms[:, 0:1], scalar2=None, op0=ALU.max), reads=[bG, b_ms], writes=[bG])
                c.op("act", lambda e: e.activation(out=G[:, 5, :], in_=G[:, cmr, :], func=AF.Copy, scale=-1.0), reads=[bG], writes=[bG])
                c.op("act", lambda e: e.activation(out=G[:, 6, :], in_=G[:, cmr, :], func=AF.Exp, scale=-1.0, bias=ms[:, 0:1]), reads=[bG, b_ms], writes=[bG])
                c.op("dve", lambda e: e.tensor_tensor(out=G[:, 7, :], in0=G[:, 1, :], in1=G[:, cmr, :], op=ALU.add), reads=[bG], writes=[bG])
                c.op("act", lambda e: e.activation(out=G[:, 7, :], in_=G[:, 7, :], func=AF.Exp, scale=-1.0), reads=[bG], writes=[bG])
                c.op("dve", lambda e: e.tensor_copy(out=s8[:, 0:1], in_=G[:, cmr, endc:endc + 1]), reads=[bG], writes=[bs8])
                c.op("dve", lambda e: e.tensor_scalar(out=s8[:, 1:2], in0=s8[:, 0:1], scalar1=-1.0, scalar2=None, op0=ALU.mult), reads=[bs8], writes=[bs8])
                c.op("dve", lambda e: e.tensor_copy(out=s8[:, 2:3], in_=G[:, 1, endc:endc + 1]), reads=[bG], writes=[bs8])
                c.op("act", lambda e: e.activation(out=G[:, 8, :], in_=G[:, 2, :], func=AF.Exp, bias=s8[:, 1:2]), reads=[bG, bs8], writes=[bG])
                c.op("act", lambda e: e.activation(out=s8[:, 3:4], in_=ms[:, 0:1], func=AF.Exp, bias=s8[:, 1:2]), reads=[b_ms, bs8], writes=[bs8])
                c.op("dve", lambda e: e.tensor_tensor(out=ms[:, 0:1], in0=s8[:, 2:3], in1=s8[:, 0:1], op=ALU.add), reads=[bs8, bG], writes=[b_ms])
                p1, bp1 = self.ps[1], self.bps[1]
                c.mm_multi([(p1[:, (r - 6) * 8:(r - 5) * 8], [(G[:, r, :], id8)]) for r in (6, 7, 8)], reads=[bG, self.b_const], writes=[bp1])
                c.op("act", lambda e: e.activation(out=t_[:, 8:32], in_=p1[:, 0:24], func=AF.Copy), reads=[bp1], writes=[bt])
                c.op("dve", lambda e: e.tensor_scalar(out=dg[:], in0=id8, scalar1=s8[:, 3:4], scalar2=None, op0=ALU.mult), reads=[self.b_const, bs8], writes=[b_dg])
                c.mm(p1[0:64, 32:40], [(self.ones32[0:8, 0:64], dg[:])], reads=[self.b_const, b_dg], writes=[bp1])
                c.op("act", lambda e: e.activation(out=cwc[i][:], in_=p1[0:64, 32:40], func=AF.Copy), reads=[bp1], writes=[b_cwc[i]])
            def heads(ch, i, nxt):
                if nxt is not None:
                    gate_load(*nxt)
                bl = b_ld[i]; G, bG = GM[i], b_GM[i]; t_, bt = tk[i], b_tk[i]
                Y, bY = Yt[i], b_Yt[i]
                for hb0 in (0, 4):
                    if hb0 == 4 and nxt is not None:
                        gate(*nxt)
                    psc, bpsc = self.ps[2], self.bps[2]
                    pbc, bpbc = self.ps[6], self.bps[6]
                    for q4 in range(4):
                        h = hb0 + q4
                        cs4 = slice(q4 * 128, (q4 + 1) * 128)
                        c.mm(psc[:, cs4], [(kTs[i][:, h, :], qTs[i][:, h, :])], reads=[bl], writes=[bpsc])
                        c.mm(pbc[:, cs4], [(sel[:, h, :], G[:, 5, :]), (G[:, 2, :], sel[:, h, :])], reads=[b_sel, bG], writes=[bpbc])
                    pins = []
                    for q4 in range(4):
                        h = hb0 + q4
                        bk = 3 if q4 < 2 else 7
                        pin_ap = self.ps[bk][:, (q4 % 2) * 256:(q4 % 2) * 256 + 129]
                        c.mm(pin_ap, [(qTs[i][:, h, :], Sb[:, h, :])], reads=[bl, b_S[h]], writes=[self.bps[bk]])
                        pins.append((pin_ap, self.bps[bk]))
                    k4 = (hb0 // 4)
                    c.op("dve", lambda e: e.tensor_tensor(out=scm[k4][:].rearrange("p (h t) -> p h t", h=4), in0=psc[:, :].rearrange("p (h t) -> p h t", h=4),
                                                          in1=tri.unsqueeze(1).to_broadcast([128, 4, 128]), op=ALU.mult), reads=[bpsc, self.b_const], writes=[b_scm[k4]])
                    c.op("dve", lambda e: e.tensor_scalar(out=aa[k4][:], in0=pbc[:, :], scalar1=0.0, scalar2=None, op0=ALU.min), reads=[bpbc], writes=[b_aa[k4]])
                    c.op("act", lambda e: e.activation(out=EE[k4][:], in_=aa[k4][:], func=AF.Exp), reads=[b_aa[k4]], writes=[b_EE[k4]])
                    c.op("pool", lambda e: e.tensor_tensor(out=MT[k4][:], in0=scm[k4][:], in1=EE[k4][:], op=ALU.mult), reads=[b_scm[k4], b_EE[k4]], writes=[b_MT[k4]])
                    for q4 in range(4):
                        h = hb0 + q4
                        pin_ap, bpin = pins[q4]
                        c.op("act", lambda e: e.activation(out=yi[q4][:], in_=pin_ap, func=AF.Copy, scale=t_[:, 8 + h:9 + h]), reads=[bpin, bt], writes=[b_yi[q4]])
                        c.op("pool", lambda e: e.tensor_tensor(out=Vw[q4][:], in0=vas[i][:, h, :], in1=t_[:, 24 + h:25 + h].to_broadcast([128, 129]), op=ALU.mult), reads=[bl, bt], writes=[b_Vw[q4]])
                    pnds = []; psts = []
                    for q4 in range(4):
                        h = hb0 + q4
                        bk = 4 + q4 // 2
                        pnd_ap = self.ps[bk][:, (q4 % 2) * 256:(q4 % 2) * 256 + 129]
                        c.mm(pnd_ap, [(MT[k4][:, q4 * 128:(q4 + 1) * 128], vas[i][:, h, :])], reads=[b_MT[k4], bl], writes=[self.bps[bk]])
                        pnds.append((pnd_ap, self.bps[bk]))
                    for q4 in range(4):
                        h = hb0 + q4
                        if q4 < 3:
                            pst_ap, bpst = self.ps[1][0:64, q4 * 129:(q4 + 1) * 129], self.bps[1]
                        else:
                            pst_ap, bpst = self.ps[0][0:64, 264:393], self.bps[0]
                        c.mm(pst_ap, [(kts[i][:, h * 64:(h + 1) * 64], Vw[q4][:])], reads=[bl, b_Vw[q4]], writes=[bpst])
                        psts.append((pst_ap, bpst))
                    n4 = nd4[k4]; bn = b_nd4[k4]
                    for q4 in range(4):
                        pnd_ap, bpnd = pnds[q4]
                        c.op("dve", lambda e: e.tensor_tensor(out=n4[:, q4, 0:129], in0=pnd_ap, in1=yi[q4][:], op=ALU.add), reads=[bpnd, b_yi[q4]], writes=[bn])
                    c.op("dve", lambda e: e.tensor_scalar(out=n4[:, :, 129:130], in0=n4[:, :, 128:129], scalar1=-1.0, scalar2=None, op0=ALU.mult), reads=[bn], writes=[bn])
                    c.op("dve", lambda e: e.tensor_tensor(out=n4[:, :, 130:131], in0=n4[:, :, 128:129], in1=n4[:, :, 129:130], op=ALU.max), reads=[bn], writes=[bn])
                    c.op("dve", lambda e: e.tensor_tensor(out=n4[:, :, 130:131], in0=n4[:, :, 130:131], in1=t_[:, 16 + hb0:20 + hb0].unsqueeze(2), op=ALU.max), reads=[bn, bt], writes=[bn])
                    c.op("dve", lambda e: e.reciprocal(out=n4[:, :, 131:132], in_=n4[:, :, 130:131]), reads=[bn], writes=[bn])
                    c.op("dve", lambda e: e.tensor_tensor(out=Y[:, hb0 * 128:(hb0 + 4) * 128].rearrange("p (h d) -> p h d", h=4), in0=n4[:, :, 0:128],
                                                          in1=n4[:, :, 131:132].to_broadcast([128, 4, 128]), op=ALU.mult), reads=[bn], writes=[bY])
                    for q4 in range(4):
                        h = hb0 + q4
                        pst_ap, bpst = psts[q4]
                        c.op("dve", lambda e: e.scalar_tensor_tensor(out=S32[:, h, :], in0=S32[:, h, :], scalar=cwc[i][:, h:h + 1], in1=pst_ap, op0=ALU.mult, op1=ALU.add), reads=[b_cwc[i], bpst], writes=[b_S[h]])
                        c.op("act", lambda e: e.activation(out=Sb[:, h, :], in_=S32[:, h, :], func=AF.Copy), reads=[], writes=[b_S[h]])
                c.dma("sp", Yd[d].ap()[ch], Y[:], reads=[bY], writes=[b_Y[d]])
            gate_load(order[0], 0)
            gate(order[0], 0)
            for idx, ch in enumerate(order):
                heads(ch, idx % 2, (order[idx + 1], (idx + 1) % 2) if idx + 1 < len(order) else None)
        c.barrier()
        c.sb_release(self.mark0)
        Wo = c.sb([128, 8, 1024], BF16, "mlWo"); b_wo = Buf()
        c.dma("pool", Wo[:], self.w["mlstm_w_out"].ap()[j].rearrange("(k p) n -> p k n", p=128), writes=[b_wo])
        ng = c.sb([128, 1024], F32, "mlng")
        c.dma("sp", ng[:], self.w["mlstm_norm_g"].ap()[j:j + 1, :].partition_broadcast(128), writes=[b_wo])
        self.gated_out(li, need_ctx, Yd, b_Y, Os, b_Os, 1024, 8, ng, Wo, b_wo, True)
        self.phase_end()

    def build(self, wshapes):
        self.inp("x", [self.NL, D])
        self.inp("ctx", [self.NCX, D])
        self.inp("cT", [128, 8, 2])
        self.inp("cmat", [128, 4, 128])
        self.inp("sel32", [32, 32, 128])
        self.inp("rope64", [2, 64, self.NL])
        self.inp("rope32", [2, 32, self.NL])
        self.inp("rope96", [2, 96, self.NL])
        self.inp("ssm_conv_wT", [wshapes["ssm_conv_w"][0], 128, 24, 5])
        self.inp("ssm_conv_bT", [wshapes["ssm_conv_w"][0], 128, 24])
        for k, s in wshapes.items():
            self.inp(k, s)
        self.out = self.nc.dram_tensor("out", [self.NL, D], F32, kind="ExternalOutput")
        self.setup_consts()
        self.prologue()
        cnt = {0: 0, 1: 0, 2: 0, 3: 0, 9: 0}
        for li, kind in enumerate(self.kinds):
            need_ctx = li < self.depth - 1
            j = cnt[kind]
            cnt[kind] += 1
            self.want_precast = li
            if kind == 0:
                self.layer_gqa(li, j, need_ctx)
            elif kind == 1:
                self.layer_ssd(li, j, need_ctx)
            elif kind == 2:
                self.layer_mlstm(li, j, need_ctx)
            elif kind == 3:
                self.layer_mla(li, j, need_ctx)
            self.layer_moe(li, need_ctx)
        self.final()
        return self.nc


WEIGHT_KEYS = ["norm1_g", "norm2_g", "w_mod", "b_mod", "moe_w_group", "moe_b_group", "moe_w_expert", "moe_b_expert",
               "moe_w_gate", "moe_w_up", "moe_w_down", "attn_w_in", "attn_sink", "attn_w_out",
               "ssm_w_in", "ssm_conv_w", "ssm_conv_b", "ssm_dt_bias", "ssm_a_log", "ssm_d", "ssm_norm_g", "ssm_w_out",
               "mlstm_w_in", "mlstm_gate_b", "mlstm_norm_g", "mlstm_w_out",
               "mla_w_in", "mla_q_norm_g", "mla_w_q_up", "mla_kv_norm_g", "mla_w_kv_up", "mla_w_out", "final_norm_g"]


def run_model(inputs, kinds, n_cores=None):
    x = np.asarray(inputs["x"], np.float32)
    ctx = np.asarray(inputs["ctx"], np.float32)
    c = np.asarray(inputs["c"], np.float32)
    c_ctx = np.asarray(inputs["c_ctx"], np.float32)
    B, n_lat, _ = x.shape
    n_ctx = ctx.shape[1]
    weights = {k: np.ascontiguousarray(np.asarray(inputs[k], np.float32)) for k in WEIGHT_KEYS}
    m = Model(n_lat, n_ctx, kinds)
    nc = m.build({k: v.shape for k, v in weights.items()})
    consts = host_consts(n_lat)
    in_maps = []
    for b in range(B):
        cT = np.stack([c[b].reshape(8, 128).T, c_ctx.reshape(8, 128).T], axis=-1)
        d = {"x": np.ascontiguousarray(x[b]), "ctx": np.ascontiguousarray(ctx[b]), "cT": np.ascontiguousarray(cT.astype(np.float32))}
        d.update(consts)
        d.update(weights)
        d["ssm_conv_wT"] = np.ascontiguousarray(weights["ssm_conv_w"].reshape(-1, 5, 24, 128).transpose(0, 3, 2, 1))
        d["ssm_conv_bT"] = np.ascontiguousarray(weights["ssm_conv_b"].reshape(-1, 24, 128).transpose(0, 2, 1))
        in_maps.append(d)
    res = run_bass_kernel_spmd(nc, in_maps, core_ids=list(range(B)))
    return np.stack([np.asarray(r["out"], np.float32) for r in res.results], axis=0)


def kernel(**inputs):
    return run_model(inputs, [0, 1, 2, 3])
```

```python
import numpy as np
import concourse.bass as bass
import concourse.mybir as mybir

F32 = mybir.dt.float32
BF16 = mybir.dt.bfloat16
AF = mybir.ActivationFunctionType
ALU = mybir.AluOpType
AX = mybir.AxisListType


class Buf:
    __slots__ = ("w", "r", "name")

    def __init__(self, name=""):
        self.w = None
        self.r = []
        self.name = name


class Ctx:
    EPOCH = 30000

    def __init__(self, nc, n_dma=None):
        self.nc = nc
        self.E = {"pe": nc.tensor, "act": nc.scalar, "dve": nc.vector, "pool": nc.gpsimd, "sp": nc.sync}
        self.csem = {}
        self.seen = {e: {} for e in self.E}
        self.semid = {}
        n_dma = n_dma or {"sp": 48, "act": 2, "pool": 30}
        self.dslots = {}
        self.drr = {}
        for q, n in n_dma.items():
            self.dslots[q] = [[self._new_sem(f"d{q}{i}"), 0] for i in range(n)]
            self.drr[q] = 0
        for e in ("pe", "act", "dve", "pool"):
            self.csem[e] = [self._new_sem(f"c{e}0"), 0, 0]
        self.sb_off = 0
        self.sb_base = 16512
        self.sb_cap = 229344 - 16512
        self.n_alloc = 0
        self.n_ins = 0
        self.n_wait = 0

    def _new_sem(self, name):
        s = self.nc.alloc_semaphore(name)
        self.semid[id(s)] = s
        return s

    def sb_mark(self):
        return self.sb_off

    def sb_release(self, mark):
        self.sb_off = mark

    def sb(self, shape, dtype, name=None):
        esz = 4 if dtype == F32 else 2
        if dtype in (mybir.dt.int32, mybir.dt.uint32):
            esz = 4
        n = 1
        for s in shape[1:]:
            n *= s
        nbytes = (n * esz + 63) // 64 * 64
        off = self.sb_off
        if off + nbytes > self.sb_cap:
            raise RuntimeError(f"SBUF overflow: want {nbytes} at {off} cap {self.sb_cap} ({name})")
        self.sb_off += nbytes
        self.n_alloc += 1
        t = self.nc.alloc_sbuf_tensor_at(f"{name or 't'}_{self.n_alloc}", list(shape), dtype, offset=self._abs(off))
        return t

    def _abs(self, off):
        return self.sb_base + off

    def _wait(self, eng, ev):
        if ev is None:
            return
        sem, val = ev
        k = id(sem)
        if self.seen[eng].get(k, 0) >= val:
            return
        self.E[eng].wait_ge(sem, val)
        self.n_wait += 1
        self.seen[eng][k] = val

    def _deps(self, eng, reads, writes):
        for b in reads:
            if b.w is not None and not (eng == "pe" and b.w[2] == "pe"):
                self._wait(eng, b.w[:2])
        for b in writes:
            if b.w is not None and not (eng == "pe" and b.w[2] == "pe"):
                self._wait(eng, b.w[:2])
            for r in b.r:
                if not (eng == "pe" and r[2] == "pe"):
                    self._wait(eng, r[:2])

    def _record(self, ev, reads, writes):
        for b in reads:
            b.r.append(ev)
            if len(b.r) > 64:
                b.r = b.r[-64:] if False else b.r
        for b in writes:
            b.w = ev
            b.r = []

    def _signal(self, eng, ins):
        st = self.csem[eng]
        if st[1] >= self.EPOCH:
            st = self.csem[eng] = [self._new_sem(f"c{eng}{st[2] + 1}"), 0, st[2] + 1]
        st[1] += 1
        ins.then_inc(st[0], 1)
        return (st[0], st[1], eng)

    def op(self, eng, fn, reads=(), writes=()):
        self._deps(eng, reads, writes)
        ins = fn(self.E[eng])
        self.n_ins += 1
        ev = self._signal(eng, ins)
        self._record(ev, reads, writes)
        return ev

    def mm(self, out, pairs, reads=(), writes=(), start=True, stop=True):
        self._deps("pe", reads, writes)
        n = len(pairs)
        ins = None
        for i, (l, r) in enumerate(pairs):
            ins = self.nc.tensor.matmul(out, l, r, start=(start and i == 0), stop=(stop and i == n - 1))
            self.n_ins += 1
        ev = self._signal("pe", ins)
        self._record(ev, reads, writes)
        return ev

    def mm_multi(self, groups, reads=(), writes=()):
        self._deps("pe", reads, writes)
        ins = None
        for out, pairs in groups:
            n = len(pairs)
            for i, (l, r) in enumerate(pairs):
                ins = self.nc.tensor.matmul(out, l, r, start=(i == 0), stop=(i == n - 1))
                self.n_ins += 1
        ev = self._signal("pe", ins)
        self._record(ev, reads, writes)
        return ev

    def tr(self, out, in_, ident, reads=(), writes=()):
        self._deps("pe", reads, writes)
        ins = self.nc.tensor.transpose(out, in_, ident)
        self.n_ins += 1
        ev = self._signal("pe", ins)
        self._record(ev, reads, writes)
        return ev

    def tr_multi(self, items, reads=(), writes=()):
        self._deps("pe", reads, writes)
        ins = None
        for out, in_, ident in items:
            ins = self.nc.tensor.transpose(out, in_, ident)
            self.n_ins += 1
        ev = self._signal("pe", ins)
        self._record(ev, reads, writes)
        return ev

    def dma(self, q, out, in_, reads=(), writes=()):
        self._deps(q, reads, writes)
        slots = self.dslots[q]
        i = self.drr[q]
        self.drr[q] = (i + 1) % len(slots)
        sl = slots[i]
        if sl[1] > 0:
            self._wait(q, (sl[0], sl[1]))
        ins = self.E[q].dma_start(out=out, in_=in_)
        self.n_ins += 1
        sl[1] += 16
        ins.then_inc(sl[0], 16)
        ev = (sl[0], sl[1], "dma")
        self._record(ev, reads, writes)
        return ev

    def barrier(self):
        evs = []
        for e, st in self.csem.items():
            if st[1] > 0:
                evs.append((st[0], st[1]))
        for q, slots in self.dslots.items():
            for sl in slots:
                if sl[1] > 0:
                    evs.append((sl[0], sl[1]))
        for eng in self.E:
            for ev in evs:
                self._wait(eng, ev)

    def finish(self, eng="sp"):
        evs = []
        for e, st in self.csem.items():
            if st[1] > 0:
                evs.append((st[0], st[1]))
        for q, slots in self.dslots.items():
            for sl in slots:
                if sl[1] > 0:
                    evs.append((sl[0], sl[1]))
        for ev in evs:
            self._wait(eng, ev)
from concourse.bass_utils import run_bass_kernel_spmd
D = 1024
EPS = 1e-6


def host_consts(n_lat):
    ident = np.eye(128, dtype=np.float32)
    ones = np.ones((128, 128), np.float32)
    j = np.arange(128)[:, None]
    i = np.arange(128)[None, :]
    tri_le = (j <= i).astype(np.float32)
    tri_ge = (j >= i).astype(np.float32)
    cm = np.stack([ident, ones, tri_le, tri_ge], axis=1)
    sel = np.zeros((32, 32, 128), np.float32)
    for h in range(32):
        sel[h, h, :] = 1.0
    rows = n_lat // 64
    row = np.repeat(np.arange(rows), 64).astype(np.float32)
    col = np.tile(np.arange(64), rows).astype(np.float32)

    def tab(rot_dim, nrep):
        q = rot_dim // 4
        inv = (10000.0 ** (-np.arange(q, dtype=np.float32) / q)).astype(np.float32)
        ang = np.concatenate([row[:, None] * inv, col[:, None] * inv], axis=-1).astype(np.float32)
        cs = np.cos(ang).astype(np.float32).T
        sn = np.sin(ang).astype(np.float32).T
        cs = np.concatenate([cs] * (2 * nrep), axis=0)
        sn = np.concatenate([sn] * (2 * nrep), axis=0)
        return np.ascontiguousarray(np.stack([cs, sn], axis=0))

    r32 = tab(32, 1)
    L = r32.shape[2]
    r96 = np.ascontiguousarray(np.concatenate([np.stack([np.ones((64, L), np.float32), np.zeros((64, L), np.float32)], axis=0), r32], axis=1))
    return {"cmat": np.ascontiguousarray(cm), "sel32": sel, "rope64": tab(64, 1), "rope32": r32, "rope96": r96}


class Model:
    def __init__(self, n_lat, n_ctx, kinds, debug=False):
        self.NL, self.NCX = n_lat, n_ctx
        self.T = n_lat + n_ctx
        self.NT = self.T // 128
        self.NCT = n_ctx // 128
        self.NLT = n_lat // 128
        self.kinds = kinds
        self.depth = len(kinds)
        self.nc = bass.Bass("TRN2", target_bir_lowering=False)
        self.c = Ctx(self.nc)
        self.w = {}

    def inp(self, name, shape, dtype=F32):
        t = self.nc.dram_tensor(name, list(shape), dtype, kind="ExternalInput")
        self.w[name] = t
        return t

    def scratch(self, name, shape, dtype):
        return self.nc.dram_tensor(name, list(shape), dtype)

    def declare(self, shapes):
        for k, s in shapes.items():
            self.inp(k, s)

    def groups(self, gsz, with_ctx=True):
        g = []
        if with_ctx:
            g.append((0, self.NCT, True))
        t = self.NCT
        while t < self.NT:
            n = min(gsz, self.NT - t)
            g.append((t, n, False))
            t += n
        return g

    def setup_consts(self):
        c, nc = self.c, self.nc
        self.cm32 = c.sb([128, 4, 128], F32, "cm32")
        self.cmb = c.sb([128, 4, 128], BF16, "cmb")
        self.b_const = Buf("const")
        c.dma("sp", self.cm32[:], self.w["cmat"].ap(), writes=[self.b_const])
        c.op("dve", lambda e: e.tensor_copy(out=self.cmb[:], in_=self.cm32[:]), reads=[self.b_const], writes=[self.b_const])
        self.ident32 = self.cm32[:, 0, :]
        self.ones32 = self.cm32[:, 1, :]
        self.trile32 = self.cm32[:, 2, :]
        self.trige32 = self.cm32[:, 3, :]
        self.identb = self.cmb[:, 0, :]
        self.onesb = self.cmb[:, 1, :]
        self.trileb = self.cmb[:, 2, :]
        self.trigeb = self.cmb[:, 3, :]
        self.ps = [nc.alloc_psum_tensor(f"psb{i}", [128, 512], F32) for i in range(8)]
        self.bps = [Buf(f"ps{i}") for i in range(8)]
        self.mark0 = c.sb_mark()

    def phase_end(self):
        self.c.barrier()
        self.c.sb_release(self.mark0)

    def prologue(self):
        c, nc = self.c, self.nc
        L = self.depth
        self.modv = self.scratch("modv", [L, 2, 6 * D], F32)
        self.xs = self.scratch("xs", [self.T, D], F32)
        cs = c.sb([128, 8, 2], F32, "cs")
        b_cs = Buf()
        c.dma("sp", cs[:], self.w["cT"].ap(), writes=[b_cs])
        c.op("act", lambda e: e.activation(out=cs[:], in_=cs[:], func=AF.Silu), reads=[b_cs], writes=[b_cs])
        b_xs = self.b_xs = [Buf(f"xs{t}") for t in range(self.NT)]
        c.dma("sp", self.xs.ap()[0:self.NCX, :], self.w["ctx"].ap(), writes=b_xs[0:self.NCT])
        c.dma("sp", self.xs.ap()[self.NCX:self.T, :], self.w["x"].ap(), writes=b_xs[self.NCT:])
        wm = [c.sb([128, 8, 512], F32, f"wm{i}") for i in range(2)]
        b_wm = [Buf(), Buf()]
        modsb = c.sb([2, 6 * D], F32, "modsb")
        b_mod = Buf()
        bmb = c.sb([2, 6 * D], F32, "bmb")
        gb = c.sb([2, 2, D], F32, "gb")
        b_misc = Buf()
        self.b_modv = [Buf(f"modv{i}") for i in range(L)]
        it = 0
        for li in range(L):
            c.dma("sp", bmb[:], self.w["b_mod"].ap()[li:li + 1, :].partition_broadcast(2), writes=[b_misc])
            c.dma("sp", gb[:, 0, :], self.w["norm1_g"].ap()[li:li + 1, :].partition_broadcast(2), writes=[b_misc])
            c.dma("sp", gb[:, 1, :], self.w["norm2_g"].ap()[li:li + 1, :].partition_broadcast(2), writes=[b_misc])
            for j in range(12):
                s = it % 2
                it += 1
                c.dma("sp", wm[s][:], self.w["w_mod"].ap()[li, :, j * 512:(j + 1) * 512].rearrange("(k p) n -> p k n", p=128), writes=[b_wm[s]])
                pb = it % 2
                c.mm(self.ps[pb][0:2, :], [(cs[:, k, :], wm[s][:, k, :]) for k in range(8)], reads=[b_cs, b_wm[s]], writes=[self.bps[pb]])
                c.op("dve", lambda e: e.tensor_tensor(out=modsb[:, j * 512:(j + 1) * 512], in0=self.ps[pb][0:2, :], in1=bmb[:, j * 512:(j + 1) * 512], op=ALU.add),
                     reads=[self.bps[pb], b_misc], writes=[b_mod])
            for (ch, gi) in ((1, 0), (4, 1)):
                c.op("dve", lambda e: e.scalar_tensor_tensor(out=modsb[:, ch * D:(ch + 1) * D], in0=modsb[:, ch * D:(ch + 1) * D], scalar=1.0, in1=gb[:, gi, :], op0=ALU.add, op1=ALU.mult),
                     reads=[b_mod, b_misc], writes=[b_mod])
            c.dma("sp", self.modv.ap()[li], modsb[:], reads=[b_mod], writes=[self.b_modv[li]])
        self.phase_end()

    def load_mod(self, li, chunk, row, name):
        c = self.c
        t = c.sb([128, D], F32, name)
        b = Buf(name)
        c.dma("sp", t[:], self.modv.ap()[li, row:row + 1, chunk * D:(chunk + 1) * D].partition_broadcast(128), reads=[self.b_modv[li]], writes=[b])
        return t, b

    def set_norm_order(self, tiles):
        self.norm_next = {tiles[k]: tiles[k + 1] for k in range(len(tiles) - 1)}
        self.npref = None

    def alloc_norm_bufs(self, nbuf=2):
        c = self.c
        self.norm_next = {}
        self.npref = None
        if getattr(self, "want_precast", None) is not None:
            li_ = self.want_precast
            self.want_precast = None
            self.moe_precast(li_)
        self.nb = []
        for i in range(nbuf):
            d = dict(x=c.sb([128, D], F32, "nx"), bx=Buf(), junk=c.sb([128, D], BF16, "nj"), bj=Buf(),
                     st=c.sb([128, 2], F32, "nst"), bst=Buf(), tmp=c.sb([128, D], F32, "ntmp"), btmp=Buf(),
                     h=c.sb([128, D], BF16, "nh"), bh=Buf())
            self.nb.append(d)
        self.nbi = 0

    def norm_tile(self, tile, A, bA, B, bB, hT, b_hT, col0, psb, want_x=False):
        c = self.c
        d = self.nb[self.nbi % len(self.nb)]
        self.nbi += 1
        if getattr(self, "npref", None) == tile:
            self.npref = None
        else:
            c.dma("sp", d["x"][:], self.xs.ap()[tile * 128:(tile + 1) * 128, :], reads=[self.b_xs[tile]], writes=[d["bx"]])
        nxt = self.norm_next.get(tile) if getattr(self, "norm_next", None) else None
        if nxt is not None:
            d2 = self.nb[self.nbi % len(self.nb)]
            c.dma("sp", d2["x"][:], self.xs.ap()[nxt * 128:(nxt + 1) * 128, :], reads=[self.b_xs[nxt]], writes=[d2["bx"]])
            self.npref = nxt
        c.op("dve", lambda e: e.memset(d["st"][:], 0.0), writes=[d["bst"]])
        c.op("act", lambda e: e.activation(out=d["junk"][:], in_=d["x"][:], func=AF.Square, accum_out=d["st"][:, 0:1]), reads=[d["bx"]], writes=[d["bj"], d["bst"]])
        c.op("dve", lambda e: e.tensor_scalar(out=d["st"][:, 1:2], in0=d["st"][:, 0:1], scalar1=1.0 / D, scalar2=EPS, op0=ALU.mult, op1=ALU.add), reads=[d["bst"]], writes=[d["bst"]])
        c.op("act", lambda e: e.activation(out=d["st"][:, 1:2], in_=d["st"][:, 1:2], func=AF.Sqrt), reads=[d["bst"]], writes=[d["bst"]])
        c.op("dve", lambda e: e.reciprocal(out=d["st"][:, 1:2], in_=d["st"][:, 1:2]), reads=[d["bst"]], writes=[d["bst"]])
        c.op("dve", lambda e: e.scalar_tensor_tensor(out=d["tmp"][:], in0=d["x"][:], scalar=d["st"][:, 1:2], in1=A[:], op0=ALU.mult, op1=ALU.mult),
             reads=[d["bx"], d["bst"], bA], writes=[d["btmp"]])
        c.op("pool", lambda e: e.tensor_tensor(out=d["h"][:], in0=d["tmp"][:], in1=B[:], op=ALU.add), reads=[d["btmp"], bB], writes=[d["bh"]])
        pT = self.ps[psb].ap().bitcast(BF16)
        c.tr_multi([(pT[:, k * 128:(k + 1) * 128], d["h"][:, k * 128:(k + 1) * 128], self.identb) for k in range(8)],
                   reads=[d["bh"], self.b_const], writes=[self.bps[psb]])
        c.op("act", lambda e: e.activation(out=hT[:, :, col0:col0 + 128], in_=pT.rearrange("p (k n) -> p k n", k=8), func=AF.Copy),
             reads=[self.bps[psb]], writes=[b_hT])
        return d

    def layer_gqa(self, li, j, need_ctx):
        c, nc = self.c, self.nc
        T, NT, NCT = self.T, self.NT, self.NCT
        w_in = self.w["attn_w_in"].ap()[j]
        w_out = self.w["attn_w_out"].ap()[j]
        QT = self.scratch(f"gqa_qt{li}", [4, NT, 64, 4, 128], BF16)
        b_QT = [Buf() for _ in range(NT)]
        b_w = Buf("gqa_w")
        Wq = c.sb([128, 8, 1024], BF16, "Wq")
        Wqr = c.sb([128, 8, 1024], BF16, "Wqr")
        Wk = c.sb([128, 8, 256], BF16, "Wk")
        Wkr = c.sb([128, 8, 256], BF16, "Wkr")
        Wv = c.sb([128, 8, 256], BF16, "Wv")
        c.dma("pool", Wq[:], w_in[:, 0:1024].rearrange("(k p) n -> p k n", p=128), writes=[b_w])
        c.dma("pool", Wk[:], w_in[:, 1024:1280].rearrange("(k p) n -> p k n", p=128), writes=[b_w])
        c.dma("pool", Wv[:], w_in[:, 1280:1536].rearrange("(k p) n -> p k n", p=128), writes=[b_w])
        b_wr = Buf("gqa_wr")
        for (W, Wr, nh) in ((Wq, Wqr, 16), (Wk, Wkr, 4)):
            for k in range(8):
                src = W[:, k, :].rearrange("p (h two i) -> p h two i", two=2, i=32)
                dst = Wr[:, k, :].rearrange("p (h two i) -> p h two i", two=2, i=32)
                c.op("act", lambda e: e.activation(out=dst[:, :, 0, :], in_=src[:, :, 1, :], func=AF.Copy, scale=-1.0), reads=[b_w], writes=[b_wr])
                c.op("dve", lambda e: e.tensor_copy(out=dst[:, :, 1, :], in_=src[:, :, 0, :]), reads=[b_w], writes=[b_wr])
        KT = c.sb([64, 4, T], BF16, "KT")
        b_KT = Buf("KT")
        Vs = c.sb([128, NT, 4, 65], BF16, "Vs")
        b_V = Buf("Vs")
        c.op("pool", lambda e: e.memset(Vs[:], 1.0), writes=[b_V])
        mark = c.sb_mark()
        A1x, bA1x = self.load_mod(li, 1, 0, "A1x")
        S1x, bS1x = self.load_mod(li, 0, 0, "S1x")
        A1c, bA1c = self.load_mod(li, 1, 1, "A1c")
        S1c, bS1c = self.load_mod(li, 0, 1, "S1c")
        self.alloc_norm_bufs(2)
        self.set_norm_order([t0_ + s_ for (t0_, n_, _c) in self.groups(4) for s_ in range(n_)])
        hTs = [c.sb([128, 8, 512], BF16, "hT") for _ in range(2)]
        b_hT = [Buf(), Buf()]
        rts = [c.sb([64, 2, 512], F32, "rt") for _ in range(2)]
        b_rt = [Buf(), Buf()]
        qst = [c.sb([64, 4, 512], BF16, "qst") for _ in range(2)]
        b_qst = [Buf(), Buf()]
        t1s = [c.sb([64, 512], F32, "t1") for _ in range(2)]
        t2s = [c.sb([64, 512], F32, "t2") for _ in range(2)]
        b_t1 = [Buf(), Buf()]
        b_t2 = [Buf(), Buf()]
        cnt = 0
        qcnt = 0
        for gi, (t0, n, is_ctx) in enumerate(self.groups(4)):
            ncols = n * 128
            hT, bh = hTs[gi % 2], b_hT[gi % 2]
            rt, brt = rts[gi % 2], b_rt[gi % 2]
            for s in range(n):
                if is_ctx:
                    self.norm_tile(t0 + s, A1c, bA1c, S1c, bS1c, hT, bh, s * 128, (t0 + s) % 2)
                else:
                    self.norm_tile(t0 + s, A1x, bA1x, S1x, bS1x, hT, bh, s * 128, (t0 + s) % 2)
            if not is_ctx:
                l0 = (t0 - NCT) * 128
                c.dma("sp", rt[:, :, :ncols], self.w["rope64"].ap()[:, :, l0:l0 + ncols].rearrange("two d l -> d two l"), writes=[brt])

            def proj_head(W, Wr, col, dst_ap, dst_buf):
                nonlocal cnt
                i = cnt % 2
                cnt += 1
                P1, bP1 = self.ps[2 + i], self.bps[2 + i]
                P2, bP2 = self.ps[4 + i], self.bps[4 + i]
                c.mm(P1[0:64, :ncols], [(W[:, k, col:col + 64], hT[:, k, :ncols]) for k in range(8)], reads=[b_w, bh], writes=[bP1])
                if is_ctx:
                    c.op("act", lambda e: e.activation(out=dst_ap, in_=P1[0:64, :ncols], func=AF.Copy), reads=[bP1], writes=[dst_buf])
                else:
                    c.mm(P2[0:64, :ncols], [(Wr[:, k, col:col + 64], hT[:, k, :ncols]) for k in range(8)], reads=[b_wr, bh], writes=[bP2])
                    c.op("dve", lambda e: e.tensor_tensor(out=t1s[i][:, :ncols], in0=P1[0:64, :ncols], in1=rt[:, 0, :ncols], op=ALU.mult), reads=[bP1, brt], writes=[b_t1[i]])
                    c.op("dve", lambda e: e.tensor_tensor(out=t2s[i][:, :ncols], in0=P2[0:64, :ncols], in1=rt[:, 1, :ncols], op=ALU.mult), reads=[bP2, brt], writes=[b_t2[i]])
                    c.op("pool", lambda e: e.tensor_tensor(out=dst_ap, in0=t1s[i][:, :ncols], in1=t2s[i][:, :ncols], op=ALU.add), reads=[b_t1[i], b_t2[i]], writes=[dst_buf])

            for kvh in range(4):
                qs, bq = qst[qcnt % 2], b_qst[qcnt % 2]
                qcnt += 1
                for g in range(4):
                    proj_head(Wq, Wqr, (kvh * 4 + g) * 64, qs[:, g, :ncols], bq)
                for qb_ in range(n):
                    c.dma("sp", QT.ap()[kvh, t0 + qb_], qs[:, :, qb_ * 128:(qb_ + 1) * 128], reads=[bq], writes=[b_QT[t0 + qb_]])
                proj_head(Wk, Wkr, kvh * 64, KT[:, kvh, t0 * 128:t0 * 128 + ncols], b_KT)
            for s in range(n):
                i = cnt % 2
                cnt += 1
                Pv, bPv = self.ps[6 + i], self.bps[6 + i]
                c.mm(Pv[:, 0:256], [(hT[:, k, s * 128:(s + 1) * 128], Wv[:, k, :]) for k in range(8)], reads=[b_w, bh], writes=[bPv])
                c.op("act", lambda e: e.activation(out=Vs[:, t0 + s, :, 0:64], in_=Pv[:, 0:256].rearrange("p (h d) -> p h d", h=4), func=AF.Copy), reads=[bPv], writes=[b_V])
        c.barrier()
        c.sb_release(mark)
        Wo = c.sb([64, 16, 1024], BF16, "Wo")
        b_wo = Buf()
        c.dma("pool", Wo[:], w_out.rearrange("(h d) n -> d h n", d=64), writes=[b_wo])
        sk = c.sb([65, 16], F32, "sk")
        b_sk = Buf()
        sinkrow = c.sb([65, 16, 128], F32, "sinkrow")
        c.dma("sp", sk[64:65, :], self.w["attn_sink"].ap()[j:j + 1, :], writes=[b_sk])
        c.op("act", lambda e: e.activation(out=sk[64:65, :], in_=sk[64:65, :], func=AF.Exp), reads=[b_sk], writes=[b_sk])
        c.op("dve", lambda e: e.tensor_copy(out=sinkrow[64:65, :, :], in_=sk[64:65, :].unsqueeze(2).to_broadcast([1, 16, 128])), reads=[b_sk], writes=[b_sk])
        G1x, bG1x = self.load_mod(li, 2, 0, "G1x")
        G1c, bG1c = self.load_mod(li, 2, 1, "G1c")
        Qts = [c.sb([64, 512], BF16, "Qt") for _ in range(8)]
        b_Qt = [Buf() for _ in range(8)]
        PTs = [c.sb([128, 512], BF16, "PT") for _ in range(4)]
        b_PT = [Buf() for _ in range(4)]
        osb = [c.sb([65, 512], F32, "osb") for _ in range(2)]
        b_osb = [Buf(), Buf()]
        rden = [c.sb([65, 512], F32, "rden") for _ in range(2)]
        b_rden = [Buf(), Buf()]
        yTs = [c.sb([64, 16, 128], BF16, "yT") for _ in range(2)]
        b_yT = [Buf(), Buf()]
        xts = [c.sb([128, D], F32, "xres") for _ in range(2)]
        b_xt = [Buf(), Buf()]
        tmps = [c.sb([128, D], F32, "rtmp") for _ in range(2)]
        b_tmp = [Buf(), Buf()]
        scale = 64 ** -0.5
        qi = 0
        pi = 0
        si = 0
        qbs = list(range(0 if need_ctx else NCT, NT))

        def ga_load(bi_):
            qb_ = qbs[bi_]
            c.dma("sp", xts[bi_ % 2][:], self.xs.ap()[qb_ * 128:(qb_ + 1) * 128, :], reads=[self.b_xs[qb_]], writes=[b_xt[bi_ % 2]])
            for kvh_ in range(4):
                q8 = (bi_ % 2) * 4 + kvh_
                c.dma("sp", Qts[q8][:], QT.ap()[kvh_, qb_].rearrange("d g t -> d (g t)"), reads=[b_QT[qb_]], writes=[b_Qt[q8]])
        ga_load(0)
        for bi, qb in enumerate(qbs):
            is_ctx = qb < NCT
            if bi + 1 < len(qbs):
                ga_load(bi + 1)
            if is_ctx:
                keys = [(t, None) for t in range(NCT)]
            else:
                keys = []
                if qb - 1 >= NCT:
                    keys.append((qb - 1, self.trigeb))
                keys.append((qb, None))
                if qb + 1 < NT:
                    keys.append((qb + 1, self.trileb))
                keys += [(t, None) for t in range(NCT)]
            xt, bxt = xts[bi % 2], b_xt[bi % 2]
            yT, byT = yTs[bi % 2], b_yT[bi % 2]
            for kvh in range(4):
                Qt, bQt = Qts[(bi % 2) * 4 + kvh], b_Qt[(bi % 2) * 4 + kvh]
                oT, boT = self.ps[2 + kvh % 2], self.bps[2 + kvh % 2]
                SB = (0, 1, 7)
                LA = 2

                def issue_S(ki_):
                    kt_ = keys[ki_][0]
                    bk_ = SB[(si + ki_) % len(SB)]
                    c.mm(self.ps[bk_][:, :], [(KT[:, kvh, kt_ * 128:(kt_ + 1) * 128], Qt[:])], reads=[b_KT, bQt], writes=[self.bps[bk_]])
                for k0 in range(min(LA, len(keys))):
                    issue_S(k0)
                for ki, (kt, mask) in enumerate(keys):
                    bk = SB[(si + ki) % len(SB)]
                    sT, bsT = self.ps[bk], self.bps[bk]
                    if ki + LA < len(keys):
                        issue_S(ki + LA)
                    PT, bPT = PTs[pi % 4], b_PT[pi % 4]
                    pi += 1
                    c.op("act", lambda e: e.activation(out=PT[:], in_=sT[:, :], func=AF.Exp, scale=scale), reads=[bsT], writes=[bPT])
                    if mask is not None:
                        c.op("dve", lambda e: e.tensor_tensor(out=PT[:].rearrange("p (g t) -> p g t", g=4), in0=PT[:].rearrange("p (g t) -> p g t", g=4),
                                                              in1=mask.unsqueeze(1).to_broadcast([128, 4, 128]), op=ALU.mult), reads=[bPT, self.b_const], writes=[bPT])
                    c.mm(oT[0:65, :], [(Vs[:, kt, kvh, :], PT[:])], reads=[b_V, bPT], writes=[boT], start=(ki == 0), stop=(ki == len(keys) - 1))
                si += len(keys)
                o, bo = osb[kvh % 2], b_osb[kvh % 2]
                rd, brd = rden[kvh % 2], b_rden[kvh % 2]
                c.op("act", lambda e: e.activation(out=o[:], in_=oT[0:65, :], func=AF.Copy), reads=[boT], writes=[bo])
                c.op("dve", lambda e: e.tensor_tensor(out=rd[64:65, :], in0=o[64:65, :], in1=sinkrow[64:65, kvh * 4:(kvh + 1) * 4, :].rearrange("p g t -> p (g t)"), op=ALU.add),
                     reads=[bo, b_sk], writes=[brd])
                c.mm(self.ps[4][0:64, :], [(self.ones32[64:65, 0:64], rd[64:65, :])], reads=[self.b_const, brd], writes=[self.bps[4]])
                c.op("dve", lambda e: e.reciprocal(out=rd[0:64, :], in_=self.ps[4][0:64, :]), reads=[self.bps[4], brd], writes=[brd])
                c.op("dve", lambda e: e.tensor_tensor(out=yT[:, kvh * 4:(kvh + 1) * 4, :].rearrange("d g t -> d (g t)"), in0=o[0:64, :], in1=rd[0:64, :], op=ALU.mult),
                     reads=[bo, brd], writes=[byT])
            G1, bG1 = (G1c, bG1c) if is_ctx else (G1x, bG1x)
            tmp, btmp = tmps[bi % 2], b_tmp[bi % 2]
            for nn in range(2):
                z, bz = self.ps[5 + nn], self.bps[5 + nn]
                c.mm(z[:, :], [(yT[:, hq, :], Wo[:, hq, nn * 512:(nn + 1) * 512]) for hq in range(16)], reads=[byT, b_wo], writes=[bz])
                c.op("dve", lambda e: e.tensor_tensor(out=tmp[:, nn * 512:(nn + 1) * 512], in0=z[:, :], in1=G1[:, nn * 512:(nn + 1) * 512], op=ALU.mult), reads=[bz, bG1], writes=[btmp])
            c.op("pool", lambda e: e.tensor_tensor(out=xt[:], in0=xt[:], in1=tmp[:], op=ALU.add), reads=[btmp, bxt], writes=[bxt])
            c.dma("sp", self.xs.ap()[qb * 128:(qb + 1) * 128, :], xt[:], reads=[bxt], writes=[self.b_xs[qb]])
        self.phase_end()

    def moe_precast(self, li):
        c = self.c
        wg = self.w["moe_w_gate"].ap()[li]
        wu = self.w["moe_w_up"].ap()[li]
        wd = self.w["moe_w_down"].ap()[li]
        self.wgb = self.scratch(f"moe_wgb{li}", [16, 1024, 512], BF16)
        self.wdb = self.scratch(f"moe_wdb{li}", [16, 256, 1024], BF16)
        self.b_wgb = Buf(); self.b_wdb = Buf()
        for e2 in range(8):
            c.dma("pool", self.wgb.ap()[2 * e2:2 * e2 + 2, :, 0:256], wg[2 * e2:2 * e2 + 2], writes=[self.b_wgb])
            c.dma("pool", self.wgb.ap()[2 * e2:2 * e2 + 2, :, 256:512], wu[2 * e2:2 * e2 + 2], writes=[self.b_wgb])
            c.dma("pool", self.wdb.ap()[2 * e2:2 * e2 + 2], wd[2 * e2:2 * e2 + 2], writes=[self.b_wdb])
        self.precast_done = li

    def layer_moe(self, li, need_ctx):
        c, nc = self.c, self.nc
        NCT = self.NCT
        if getattr(self, "precast_done", -1) != li:
            self.moe_precast(li)
        wgb = self.wgb.ap()
        wdb = self.wdb.ap()
        b_w = Buf("moe_wr")
        Wr = c.sb([128, 8, 20], BF16, "Wr")
        c.dma("pool", Wr[:, :, 0:4], self.w["moe_w_group"].ap()[li].rearrange("(k p) n -> p k n", p=128), writes=[b_w])
        c.dma("pool", Wr[:, :, 4:20], self.w["moe_w_expert"].ap()[li].rearrange("(k p) n -> p k n", p=128), writes=[b_w])
        brow = c.sb([128, 20], F32, "brow")
        c.dma("sp", brow[:, 0:4], self.w["moe_b_group"].ap()[li:li + 1, :].partition_broadcast(128), writes=[b_w])
        c.dma("sp", brow[:, 4:20], self.w["moe_b_expert"].ap()[li:li + 1, :].partition_broadcast(128), writes=[b_w])
        sel16 = c.sb([16, 16, 128], BF16, "sel16")
        c.dma("pool", sel16[:], self.w["sel32"].ap()[0:16, 0:16, :], writes=[b_w])
        hT = c.sb([128, 8, 1024], BF16, "mhT")
        b_hT = Buf()
        act = c.sb([128, 16, 2, 1024], BF16, "mact")
        b_act = Buf()
        Wgu = [c.sb([128, 8, 512], BF16, "Wgu") for _ in range(2)]
        b_Wgu = [Buf(), Buf()]
        Wd = [c.sb([128, 16, 2, 256], BF16, "Wd") for _ in range(2)]
        b_Wd = [Buf(), Buf()]
        xq = [c.sb([128, 256], F32, "xq") for _ in range(4)]
        b_xq = [Buf() for _ in range(4)]
        xqi = 0
        self.alloc_norm_bufs(2)
        self.set_norm_order([t0_ + s_ for (t0_, n_, _c) in self.groups(8, with_ctx=need_ctx) for s_ in range(n_)])
        A2 = c.sb([128, D], F32, "A2"); S2 = c.sb([128, D], F32, "S2"); G2 = c.sb([128, D], F32, "G2")
        b_m = Buf()
        R = 8
        lg = c.sb([128, R, 20], F32, "lg"); le = c.sb([128, R, 16], F32, "le"); le2 = c.sb([128, R, 16], F32, "le2")
        gm = c.sb([128, R, 4], F32, "gm"); eg = c.sb([128, R, 4], F32, "eg"); pen = c.sb([128, R, 4], F32, "pen")
        mk1 = c.sb([128, R, 16], F32, "mk1"); mk2 = c.sb([128, R, 16], F32, "mk2"); cmb = c.sb([128, R, 16], F32, "cmb")
        sc = c.sb([128, 8, R], F32, "rsc")
        b_r = Buf("route")
        cmbT = c.sb([16, 1024], BF16, "cmbT")
        b_cT = Buf()
        s_sb = [c.sb([128, 512], F32, "msil") for _ in range(2)]
        b_s = [Buf(), Buf()]
        t_sb = [c.sb([128, 512], F32, "mt") for _ in range(2)]
        b_t = [Buf(), Buf()]
        tmpd = [c.sb([128, 256], F32, "mtd") for _ in range(2)]
        b_td = [Buf(), Buf()]
        wi = 0
        di = 0
        ui = 0
        for (t0, n, is_ctx) in self.groups(8, with_ctx=need_ctx):
            G = n * 128
            row = 1 if is_ctx else 0
            for (tl, ch) in ((S2, 3), (A2, 4), (G2, 5)):
                c.dma("sp", tl[:], self.modv.ap()[li, row:row + 1, ch * D:(ch + 1) * D].partition_broadcast(128), reads=[self.b_modv[li]], writes=[b_m])
            for s in range(n):
                self.norm_tile(t0 + s, A2, b_m, S2, b_m, hT, b_hT, s * 128, s % 2)
            lp, blp = self.ps[2], self.bps[2]
            c.mm_multi([(lp[:, s * 20:(s + 1) * 20], [(hT[:, k, s * 128:(s + 1) * 128], Wr[:, k, :]) for k in range(8)]) for s in range(n)],
                       reads=[b_hT, b_w], writes=[blp])
            V = lambda e: e
            lgn = lg[:, 0:n, :]
            c.op("dve", lambda e: e.tensor_tensor(out=lgn, in0=lp[:, 0:n * 20].rearrange("p (s j) -> p s j", j=20), in1=brow[:].unsqueeze(1).to_broadcast([128, n, 20]), op=ALU.add),
                 reads=[blp, b_w], writes=[b_r])
            R1 = [b_r]
            c.op("dve", lambda e: e.tensor_reduce(out=sc[:, 0, 0:n], in_=lgn[:, :, 0:4], axis=AX.X, op=ALU.max), reads=R1, writes=R1)
            c.op("dve", lambda e: e.tensor_tensor(out=gm[:, 0:n, :], in0=lgn[:, :, 0:4], in1=sc[:, 0, 0:n].unsqueeze(2).to_broadcast([128, n, 4]), op=ALU.is_equal), reads=R1, writes=R1)
            c.op("dve", lambda e: e.tensor_tensor(out=eg[:, 0:n, :], in0=lgn[:, :, 0:4], in1=sc[:, 0, 0:n].unsqueeze(2).to_broadcast([128, n, 4]), op=ALU.subtract), reads=R1, writes=R1)
            c.op("act", lambda e: e.activation(out=eg[:, 0:n, :], in_=eg[:, 0:n, :], func=AF.Exp), reads=R1, writes=R1)
            c.op("dve", lambda e: e.tensor_reduce(out=sc[:, 1, 0:n], in_=eg[:, 0:n, :], axis=AX.X, op=ALU.add), reads=R1, writes=R1)
            c.op("dve", lambda e: e.reciprocal(out=sc[:, 1, 0:n], in_=sc[:, 1, 0:n]), reads=R1, writes=R1)
            c.op("dve", lambda e: e.tensor_scalar(out=pen[:, 0:n, :], in0=gm[:, 0:n, :], scalar1=1.0, scalar2=1e30, op0=ALU.subtract, op1=ALU.mult), reads=R1, writes=R1)
            c.op("dve", lambda e: e.tensor_copy(out=le[:, 0:n, :], in_=lgn[:, :, 4:20]), reads=R1, writes=R1)
            lev = le[:, 0:n, :].rearrange("p s (g j) -> p (s g) j", g=4)
            c.op("dve", lambda e: e.tensor_tensor(out=lev, in0=lev, in1=pen[:, 0:n, :].rearrange("p s g -> p (s g)").unsqueeze(2).to_broadcast([128, n * 4, 4]), op=ALU.add), reads=R1, writes=R1)
            c.op("dve", lambda e: e.tensor_reduce(out=sc[:, 2, 0:n], in_=le[:, 0:n, :], axis=AX.X, op=ALU.max), reads=R1, writes=R1)
            c.op("dve", lambda e: e.tensor_tensor(out=mk1[:, 0:n, :], in0=le[:, 0:n, :], in1=sc[:, 2, 0:n].unsqueeze(2).to_broadcast([128, n, 16]), op=ALU.is_equal), reads=R1, writes=R1)
            c.op("dve", lambda e: e.scalar_tensor_tensor(out=le2[:, 0:n, :], in0=mk1[:, 0:n, :], scalar=-1e30, in1=le[:, 0:n, :], op0=ALU.mult, op1=ALU.add), reads=R1, writes=R1)
            c.op("dve", lambda e: e.tensor_reduce(out=sc[:, 3, 0:n], in_=le2[:, 0:n, :], axis=AX.X, op=ALU.max), reads=R1, writes=R1)
            c.op("dve", lambda e: e.tensor_tensor(out=mk2[:, 0:n, :], in0=le2[:, 0:n, :], in1=sc[:, 3, 0:n].unsqueeze(2).to_broadcast([128, n, 16]), op=ALU.is_equal), reads=R1, writes=R1)
            c.op("dve", lambda e: e.tensor_tensor(out=sc[:, 4, 0:n], in0=sc[:, 2, 0:n], in1=sc[:, 3, 0:n], op=ALU.subtract), reads=R1, writes=R1)
            c.op("act", lambda e: e.activation(out=sc[:, 4, 0:n], in_=sc[:, 4, 0:n], func=AF.Sigmoid), reads=R1, writes=R1)
            c.op("dve", lambda e: e.tensor_tensor(out=sc[:, 4, 0:n], in0=sc[:, 4, 0:n], in1=sc[:, 1, 0:n], op=ALU.mult), reads=R1, writes=R1)
            c.op("dve", lambda e: e.tensor_tensor(out=sc[:, 5, 0:n], in0=sc[:, 1, 0:n], in1=sc[:, 4, 0:n], op=ALU.subtract), reads=R1, writes=R1)
            c.op("dve", lambda e: e.tensor_tensor(out=mk1[:, 0:n, :], in0=mk1[:, 0:n, :], in1=sc[:, 4, 0:n].unsqueeze(2).to_broadcast([128, n, 16]), op=ALU.mult), reads=R1, writes=R1)
            c.op("dve", lambda e: e.tensor_tensor(out=mk2[:, 0:n, :], in0=mk2[:, 0:n, :], in1=sc[:, 5, 0:n].unsqueeze(2).to_broadcast([128, n, 16]), op=ALU.mult), reads=R1, writes=R1)
            c.op("dve", lambda e: e.tensor_tensor(out=cmb[:, 0:n, :], in0=mk1[:, 0:n, :], in1=mk2[:, 0:n, :], op=ALU.add), reads=R1, writes=R1)
            for hb in range((n + 3) // 4):
                s0, s1 = hb * 4, min(n, hb * 4 + 4)
                pc, bpc = self.ps[3 + hb], self.bps[3 + hb]
                c.tr_multi([(pc[0:16, (s - s0) * 128:(s - s0 + 1) * 128], cmb[:, s, :], self.ident32) for s in range(s0, s1)], reads=[b_r, self.b_const], writes=[bpc])
                c.op("act", lambda e: e.activation(out=cmbT[:, s0 * 128:s1 * 128], in_=pc[0:16, 0:(s1 - s0) * 128], func=AF.Copy), reads=[bpc], writes=[b_cT])
            ncb = (G + 511) // 512
            for ex in range(16):
                W, bW = Wgu[wi % 2], b_Wgu[wi % 2]
                wi += 1
                c.dma("sp", W[:], wgb[ex].rearrange("(k p) n -> p k n", p=128), reads=[self.b_wgb], writes=[bW])
                for cb in range(ncb):
                    c0 = cb * 512
                    cw = min(512, G - c0)
                    pbc, bpbc = self.ps[4 + (ui % 2)], self.bps[4 + (ui % 2)]
                    c.mm(pbc[:, :cw], [(sel16[:, ex, :], cmbT[:, c0:c0 + cw])], reads=[b_w, b_cT], writes=[bpbc])
                    for ffc in range(2):
                        i = ui % 2
                        ui += 1
                        pg_, bpg = self.ps[0 + i], self.bps[0 + i]
                        pu, bpu = self.ps[2 + i], self.bps[2 + i]
                        c.mm(pg_[:, :cw], [(W[:, k, ffc * 128:(ffc + 1) * 128], hT[:, k, c0:c0 + cw]) for k in range(8)], reads=[bW, b_hT], writes=[bpg])
                        c.mm(pu[:, :cw], [(W[:, k, 256 + ffc * 128:256 + (ffc + 1) * 128], hT[:, k, c0:c0 + cw]) for k in range(8)], reads=[bW, b_hT], writes=[bpu])
                        c.op("act", lambda e: e.activation(out=s_sb[i][:, :cw], in_=pg_[:, :cw], func=AF.Silu), reads=[bpg], writes=[b_s[i]])
                        c.op("dve", lambda e: e.tensor_tensor(out=t_sb[i][:, :cw], in0=s_sb[i][:, :cw], in1=pu[:, :cw], op=ALU.mult), reads=[b_s[i], bpu], writes=[b_t[i]])
                        c.op("dve", lambda e: e.tensor_tensor(out=act[:, ex, ffc, c0:c0 + cw], in0=t_sb[i][:, :cw], in1=pbc[:, :cw], op=ALU.mult), reads=[b_t[i], bpbc], writes=[b_act])
            for dq in range(4):
                Wdt, bWd = Wd[di % 2], b_Wd[di % 2]
                di += 1
                c.dma("sp", Wdt[:], wdb[:, :, dq * 256:(dq + 1) * 256].rearrange("e (f p) n -> p e f n", p=128), reads=[self.b_wdb], writes=[bWd])
                for s in range(n):
                    pa, bpa = self.ps[4 + s // 2], self.bps[4 + s // 2]
                    pav = pa[:, (s % 2) * 256:(s % 2 + 1) * 256]
                    c.mm(pav, [(act[:, ex, ffc, s * 128:(s + 1) * 128], Wdt[:, ex, ffc, :]) for ex in range(16) for ffc in range(2)], reads=[b_act, bWd], writes=[bpa])
                    i = s % 2
                    xq_, bxq = xq[xqi % 4], b_xq[xqi % 4]
                    xqi += 1
                    tl = t0 + s
                    c.dma("sp", xq_[:], self.xs.ap()[tl * 128:(tl + 1) * 128, dq * 256:(dq + 1) * 256], reads=[self.b_xs[tl]], writes=[bxq])
                    c.op("dve", lambda e: e.tensor_tensor(out=tmpd[i][:], in0=pav, in1=G2[:, dq * 256:(dq + 1) * 256], op=ALU.mult), reads=[bpa, b_m], writes=[b_td[i]])
                    c.op("pool", lambda e: e.tensor_tensor(out=xq_[:], in0=xq_[:], in1=tmpd[i][:], op=ALU.add), reads=[b_td[i], bxq], writes=[bxq])
                    c.dma("sp", self.xs.ap()[tl * 128:(tl + 1) * 128, dq * 256:(dq + 1) * 256], xq_[:], reads=[bxq], writes=[self.b_xs[tl]])
        self.phase_end()

    def final(self):
        c = self.c
        gf = c.sb([128, D], F32, "gfin")
        b_g = Buf()
        c.dma("sp", gf[:], self.w["final_norm_g"].ap().rearrange("(o n) -> o n", o=1).partition_broadcast(128), writes=[b_g])
        xs_ = [c.sb([128, D], F32, "fx") for _ in range(2)]
        js = [c.sb([128, D], BF16, "fj") for _ in range(2)]
        st = [c.sb([128, 2], F32, "fst") for _ in range(2)]
        ys = [c.sb([128, D], F32, "fy") for _ in range(2)]
        bx = [Buf(), Buf()]; bj = [Buf(), Buf()]; bs = [Buf(), Buf()]; by = [Buf(), Buf()]
        b_out = Buf()
        for t in range(self.NCT, self.NT):
            i = t % 2
            c.dma("sp", xs_[i][:], self.xs.ap()[t * 128:(t + 1) * 128, :], reads=[self.b_xs[t]], writes=[bx[i]])
            c.op("dve", lambda e: e.memset(st[i][:], 0.0), writes=[bs[i]])
            c.op("act", lambda e: e.activation(out=js[i][:], in_=xs_[i][:], func=AF.Square, accum_out=st[i][:, 0:1]), reads=[bx[i]], writes=[bj[i], bs[i]])
            c.op("dve", lambda e: e.tensor_scalar(out=st[i][:, 1:2], in0=st[i][:, 0:1], scalar1=1.0 / D, scalar2=EPS, op0=ALU.mult, op1=ALU.add), reads=[bs[i]], writes=[bs[i]])
            c.op("act", lambda e: e.activation(out=st[i][:, 1:2], in_=st[i][:, 1:2], func=AF.Sqrt), reads=[bs[i]], writes=[bs[i]])
            c.op("dve", lambda e: e.reciprocal(out=st[i][:, 1:2], in_=st[i][:, 1:2]), reads=[bs[i]], writes=[bs[i]])
            c.op("dve", lambda e: e.scalar_tensor_tensor(out=ys[i][:], in0=xs_[i][:], scalar=st[i][:, 1:2], in1=gf[:], op0=ALU.mult, op1=ALU.mult), reads=[bx[i], bs[i], b_g], writes=[by[i]])
            lt = t - self.NCT
            c.dma("sp", self.out.ap()[lt * 128:(lt + 1) * 128, :], ys[i][:], reads=[by[i]], writes=[b_out])
        self.c.finish("sp")

    def layer_mla(self, li, j, need_ctx):
        c, nc = self.c, self.nc
        T, NT, NCT = self.T, self.NT, self.NCT
        w_in = self.w["mla_w_in"].ap()[j]
        w_qup = self.w["mla_w_q_up"].ap()[j]
        w_kvup = self.w["mla_w_kv_up"].ap()[j]
        w_out = self.w["mla_w_out"].ap()[j]
        QT = self.scratch(f"mla_qt{li}", [16, 96, T], BF16)
        KTd = self.scratch(f"mla_kt{li}", [16, 96, T], BF16)
        Vd = self.scratch(f"mla_v{li}", [16, NT, 128, 65], BF16)
        YT = self.scratch(f"mla_yt{li}", [16, 64, T], BF16)
        b_QT = Buf(); b_KTd = Buf(); b_Vd = Buf(); b_YT = Buf()
        b_w = Buf("mla_w")
        Win = c.sb([128, 8, 544], BF16, "Win")
        c.dma("pool", Win[:], w_in.rearrange("(k p) n -> p k n", p=128), writes=[b_w])
        Wkr_rot = c.sb([128, 8, 32], BF16, "Wkrrot")
        Wq = c.sb([128, 2, 1536], BF16, "Wqup")
        c.dma("pool", Wq[:], w_qup.rearrange("(k p) n -> p k n", p=128), writes=[b_w])
        Wqr = c.sb([128, 2, 1536], BF16, "Wquprot")
        Wkn = c.sb([128, 2, 16, 64], BF16, "Wkn")
        Wv = c.sb([128, 2, 16, 64], BF16, "Wvv")
        kvv = w_kvup.rearrange("(k p) (h two d) -> p k h two d", p=128, two=2, d=64)
        for kc in range(2):
            c.dma("pool", Wkn[:, kc], kvv[:, kc, :, 0, :], writes=[b_w])
            c.dma("pool", Wv[:, kc], kvv[:, kc, :, 1, :], writes=[b_w])
        b_wr = Buf("mla_wr")
        c.op("pool", lambda e: e.memset(Wqr[:], 0.0), writes=[b_wr])
        for kc in range(2):
            src = Wq[:, kc, :].rearrange("p (h d) -> p h d", d=96)
            dst = Wqr[:, kc, :].rearrange("p (h d) -> p h d", d=96)
            c.op("act", lambda e: e.activation(out=dst[:, :, 64:80], in_=src[:, :, 80:96], func=AF.Copy, scale=-1.0), reads=[b_w], writes=[b_wr])
            c.op("dve", lambda e: e.tensor_copy(out=dst[:, :, 80:96], in_=src[:, :, 64:80]), reads=[b_w], writes=[b_wr])
        c.op("act", lambda e: e.activation(out=Wkr_rot[:, :, 0:16], in_=Win[:, :, 528:544], func=AF.Copy, scale=-1.0), reads=[b_w], writes=[b_wr])
        c.op("dve", lambda e: e.tensor_copy(out=Wkr_rot[:, :, 16:32], in_=Win[:, :, 512:528]), reads=[b_w], writes=[b_wr])
        gq = c.sb([128, 512], F32, "gqkv")
        c.dma("sp", gq[:, 0:256], self.w["mla_q_norm_g"].ap()[j:j + 1, :].partition_broadcast(128), writes=[b_w])
        c.dma("sp", gq[:, 256:512], self.w["mla_kv_norm_g"].ap()[j:j + 1, :].partition_broadcast(128), writes=[b_w])
        mark = c.sb_mark()
        A1x, bA1x = self.load_mod(li, 1, 0, "A1x")
        S1x, bS1x = self.load_mod(li, 0, 0, "S1x")
        A1c, bA1c = self.load_mod(li, 1, 1, "A1c")
        S1c, bS1c = self.load_mod(li, 0, 1, "S1c")
        self.alloc_norm_bufs(2)
        self.set_norm_order([t0_ + s_ for (t0_, n_, _c) in self.groups(4) for s_ in range(n_)])
        hTs = [c.sb([128, 8, 512], BF16, "hT") for _ in range(2)]
        b_hT = [Buf(), Buf()]
        cnT = [c.sb([128, 4, 512], BF16, "cnT") for _ in range(2)]
        b_cnT = [Buf(), Buf()]
        rts = [c.sb([96, 2, 512], F32, "rt96") for _ in range(2)]
        rks = [c.sb([32, 2, 512], F32, "rt32") for _ in range(2)]
        b_rt = [Buf(), Buf()]
        krT = [c.sb([32, 512], BF16, "krT") for _ in range(2)]
        b_krT = [Buf(), Buf()]
        st = [c.sb([128, 4], F32, "mst") for _ in range(2)]
        b_st = [Buf(), Buf()]
        jk = c.sb([128, 256], BF16, "mjunk"); b_jk = Buf()
        cn = [c.sb([128, 512], BF16, "cn") for _ in range(2)]
        b_cn = [Buf(), Buf()]
        qst = [c.sb([96, 512], BF16, "mqst") for _ in range(2)]
        b_qst = [Buf(), Buf()]
        kst = [c.sb([64, 512], BF16, "mkst") for _ in range(2)]
        b_kst = [Buf(), Buf()]
        t1s = [c.sb([96, 512], F32, "t1") for _ in range(2)]
        t2s = [c.sb([96, 512], F32, "t2") for _ in range(2)]
        b_t1 = [Buf(), Buf()]; b_t2 = [Buf(), Buf()]
        Vt = [c.sb([128, 16, 65], BF16, "Vt") for _ in range(2)]
        b_Vt = [Buf(), Buf()]
        for i in range(2):
            c.op("pool", lambda e: e.memset(Vt[i][:], 1.0), writes=[b_Vt[i]])
        cnt = 0
        ti = 0
        for gi, (t0, n, is_ctx) in enumerate(self.groups(4)):
            ncols = n * 128
            col0 = t0 * 128
            hT, bh = hTs[gi % 2], b_hT[gi % 2]
            cT_, bcT = cnT[gi % 2], b_cnT[gi % 2]
            rt, rk, brt = rts[gi % 2], rks[gi % 2], b_rt[gi % 2]
            for s in range(n):
                if is_ctx:
                    self.norm_tile(t0 + s, A1c, bA1c, S1c, bS1c, hT, bh, s * 128, (t0 + s) % 2)
                else:
                    self.norm_tile(t0 + s, A1x, bA1x, S1x, bS1x, hT, bh, s * 128, (t0 + s) % 2)
            if not is_ctx:
                l0 = (t0 - NCT) * 128
                c.dma("sp", rt[:, :, :ncols], self.w["rope96"].ap()[:, :, l0:l0 + ncols].rearrange("two d l -> d two l"), writes=[brt])
                c.dma("sp", rk[:, :, :ncols], self.w["rope32"].ap()[:, :, l0:l0 + ncols].rearrange("two d l -> d two l"), writes=[brt])
            for s in range(n):
                i = ti % 2
                ti += 1
                pA, bpA = self.ps[2 + i], self.bps[2 + i]
                c.mm(pA[:, :], [(hT[:, k, s * 128:(s + 1) * 128], Win[:, k, 0:512]) for k in range(8)], reads=[bh, b_w], writes=[bpA])
                c.op("dve", lambda e: e.memset(st[i][:], 0.0), writes=[b_st[i]])
                for u in range(2):
                    c.op("act", lambda e: e.activation(out=jk[:], in_=pA[:, u * 256:(u + 1) * 256], func=AF.Square, accum_out=st[i][:, u:u + 1]), reads=[bpA], writes=[b_jk, b_st[i]])
                c.op("dve", lambda e: e.tensor_scalar(out=st[i][:, 2:4], in0=st[i][:, 0:2], scalar1=1.0 / 256, scalar2=EPS, op0=ALU.mult, op1=ALU.add), reads=[b_st[i]], writes=[b_st[i]])
                c.op("act", lambda e: e.activation(out=st[i][:, 2:4], in_=st[i][:, 2:4], func=AF.Sqrt), reads=[b_st[i]], writes=[b_st[i]])
                c.op("dve", lambda e: e.reciprocal(out=st[i][:, 2:4], in_=st[i][:, 2:4]), reads=[b_st[i]], writes=[b_st[i]])
                for u in range(2):
                    c.op("dve", lambda e: e.scalar_tensor_tensor(out=cn[i][:, u * 256:(u + 1) * 256], in0=pA[:, u * 256:(u + 1) * 256], scalar=st[i][:, 2 + u:3 + u], in1=gq[:, u * 256:(u + 1) * 256], op0=ALU.mult, op1=ALU.mult),
                         reads=[bpA, b_st[i], b_w], writes=[b_cn[i]])
                pT = self.ps[4 + i].ap().bitcast(BF16)
                c.tr_multi([(pT[:, k * 128:(k + 1) * 128], cn[i][:, k * 128:(k + 1) * 128], self.identb) for k in range(4)], reads=[b_cn[i], self.b_const], writes=[self.bps[4 + i]])
                c.op("act", lambda e: e.activation(out=cT_[:, :, s * 128:(s + 1) * 128], in_=pT[:, 0:512].rearrange("p (k n) -> p k n", k=4), func=AF.Copy), reads=[self.bps[4 + i]], writes=[bcT])

            def rope_proj(pairs1, pairs2, M, rtab, dst_ap, dst_buf, rd):
                nonlocal cnt
                i = cnt % 2
                cnt += 1
                P1, bP1 = self.ps[2 + i], self.bps[2 + i]
                P2, bP2 = self.ps[6 + i], self.bps[6 + i]
                c.mm(P1[0:M, :ncols], pairs1, reads=rd, writes=[bP1])
                if is_ctx or pairs2 is None:
                    c.op("act", lambda e: e.activation(out=dst_ap, in_=P1[0:M, :ncols], func=AF.Copy), reads=[bP1], writes=[dst_buf])
                else:
                    c.mm(P2[0:M, :ncols], pairs2, reads=rd + [b_wr], writes=[bP2])
                    c.op("dve", lambda e: e.tensor_tensor(out=t1s[i][0:M, :ncols], in0=P1[0:M, :ncols], in1=rtab[0:M, 0, :ncols], op=ALU.mult), reads=[bP1, brt], writes=[b_t1[i]])
                    c.op("dve", lambda e: e.tensor_tensor(out=t2s[i][0:M, :ncols], in0=P2[0:M, :ncols], in1=rtab[0:M, 1, :ncols], op=ALU.mult), reads=[bP2, brt], writes=[b_t2[i]])
                    c.op("pool", lambda e: e.tensor_tensor(out=dst_ap, in0=t1s[i][0:M, :ncols], in1=t2s[i][0:M, :ncols], op=ALU.add), reads=[b_t1[i], b_t2[i]], writes=[dst_buf])

            kr_, bkr = krT[gi % 2], b_krT[gi % 2]
            rope_proj([(Win[:, k, 512:544], hT[:, k, :ncols]) for k in range(8)], [(Wkr_rot[:, k, :], hT[:, k, :ncols]) for k in range(8)], 32, rk, kr_[:, :ncols], bkr, [bh, b_w])
            for h in range(16):
                qs, bq = qst[h % 2], b_qst[h % 2]
                rope_proj([(Wq[:, kc, h * 96:(h + 1) * 96], cT_[:, kc, :ncols]) for kc in range(2)],
                          [(Wqr[:, kc, h * 96:(h + 1) * 96], cT_[:, kc, :ncols]) for kc in range(2)], 96, rt, qs[:, :ncols], bq, [bcT, b_w])
                c.dma("sp", QT.ap()[h, :, col0:col0 + ncols], qs[:, :ncols], reads=[bq], writes=[b_QT])
                ks, bk = kst[h % 2], b_kst[h % 2]
                rope_proj([(Wkn[:, kc, h, :], cT_[:, 2 + kc, :ncols]) for kc in range(2)], None, 64, None, ks[:, :ncols], bk, [bcT, b_w])
                c.dma("sp", KTd.ap()[h, 0:64, col0:col0 + ncols], ks[:, :ncols], reads=[bk], writes=[b_KTd])
                c.dma("sp", KTd.ap()[h, 64:96, col0:col0 + ncols], kr_[:, :ncols], reads=[bkr], writes=[b_KTd])
            for s in range(n):
                vt, bvt = Vt[s % 2], b_Vt[s % 2]
                for hh in range(2):
                    i = cnt % 2
                    cnt += 1
                    Pv, bPv = self.ps[2 + i], self.bps[2 + i]
                    c.mm(Pv[:, :], [(cT_[:, 2 + kc, s * 128:(s + 1) * 128], Wv[:, kc, hh * 8:(hh + 1) * 8, :].rearrange("p h d -> p (h d)")) for kc in range(2)], reads=[bcT, b_w], writes=[bPv])
                    c.op("act", lambda e: e.activation(out=vt[:, hh * 8:(hh + 1) * 8, 0:64], in_=Pv[:, :].rearrange("p (h d) -> p h d", d=64), func=AF.Copy), reads=[bPv], writes=[bvt])
                c.dma("sp", Vd.ap()[:, t0 + s].rearrange("h p d -> p h d"), vt[:], reads=[bvt], writes=[b_Vd])
        c.barrier()
        c.sb_release(mark)
        QTs = [c.sb([96, T], BF16, "QTh") for _ in range(2)]
        KTs = [c.sb([96, T], BF16, "KTh") for _ in range(2)]
        Vhs = [c.sb([128, NT, 65], BF16, "Vh") for _ in range(2)]
        b_hd = [Buf(), Buf()]
        PTs = [c.sb([128, 512], BF16, "PT") for _ in range(4)]
        b_PT = [Buf() for _ in range(4)]
        osb = [c.sb([65, 512], F32, "osb") for _ in range(2)]
        b_osb = [Buf(), Buf()]
        ysb = [c.sb([64, 512], BF16, "ysb") for _ in range(2)]
        b_ysb = [Buf(), Buf()]
        rbs = [c.sb([64, 512], F32, "rbs") for _ in range(2)]
        b_rbs = [Buf(), Buf()]
        scale = 96 ** -0.5
        pi = 0; si = 0; oi = 0

        def load_head(h_):
            c.dma("sp", QTs[h_ % 2][:], QT.ap()[h_], reads=[b_QT], writes=[b_hd[h_ % 2]])
            c.dma("sp", KTs[h_ % 2][:], KTd.ap()[h_], reads=[b_KTd], writes=[b_hd[h_ % 2]])
            c.dma("sp", Vhs[h_ % 2][:], Vd.ap()[h_].rearrange("t p d -> p t d"), reads=[b_Vd], writes=[b_hd[h_ % 2]])
        load_head(0)
        for h in range(16):
            Qh, Kh, Vh, bhd = QTs[h % 2], KTs[h % 2], Vhs[h % 2], b_hd[h % 2]
            if h + 1 < 16:
                load_head(h + 1)
            units = []
            for (t0, n, is_ctx) in self.groups(4, with_ctx=need_ctx):
                keys = list(range(NCT)) if is_ctx else list(range(NT))
                gslot = oi % 2
                oi += 1
                for ki, kt in enumerate(keys):
                    units.append((t0 * 128, n * 128, kt, ki == 0, ki == len(keys) - 1, gslot))
            SB = (0, 1, 5, 6, 7)
            LA = 3

            def issue_S(ui):
                col0_, ncols_, kt_, _, _, _ = units[ui]
                bk_ = SB[(si + ui) % len(SB)]
                c.mm(self.ps[bk_][:, :ncols_], [(Kh[:, kt_ * 128:(kt_ + 1) * 128], Qh[:, col0_:col0_ + ncols_])], reads=[bhd], writes=[self.bps[bk_]])

            def epilogue(col0_, ncols_, gslot):
                oT, boT = self.ps[2 + gslot], self.bps[2 + gslot]
                o, bo = osb[gslot], b_osb[gslot]
                ys, bys = ysb[gslot], b_ysb[gslot]
                c.op("act", lambda e: e.activation(out=o[:, :ncols_], in_=oT[0:65, :ncols_], func=AF.Copy), reads=[boT], writes=[bo])
                c.mm(self.ps[4][0:64, :ncols_], [(self.ones32[64:65, 0:64], o[64:65, :ncols_])], reads=[self.b_const, bo], writes=[self.bps[4]])
                rb, brb = rbs[gslot], b_rbs[gslot]
                c.op("dve", lambda e: e.reciprocal(out=rb[:, :ncols_], in_=self.ps[4][0:64, :ncols_]), reads=[self.bps[4]], writes=[brb])
                c.op("dve", lambda e: e.tensor_tensor(out=ys[:, :ncols_], in0=o[0:64, :ncols_], in1=rb[:, :ncols_], op=ALU.mult), reads=[bo, brb], writes=[bys])
                c.dma("sp", YT.ap()[h, :, col0_:col0_ + ncols_], ys[:, :ncols_], reads=[bys], writes=[b_YT])

            pending = []
            for k0 in range(min(LA, len(units))):
                issue_S(k0)
            for ui, (col0, ncols, kt, first, last, gslot) in enumerate(units):
                bk = SB[(si + ui) % len(SB)]
                sT, bsT = self.ps[bk], self.bps[bk]
                if ui + LA < len(units):
                    issue_S(ui + LA)
                PT, bPT = PTs[pi % 4], b_PT[pi % 4]
                pi += 1
                oT, boT = self.ps[2 + gslot], self.bps[2 + gslot]
                c.op("act", lambda e: e.activation(out=PT[:, :ncols], in_=sT[:, :ncols], func=AF.Exp, scale=scale), reads=[bsT], writes=[bPT])
                c.mm(oT[0:65, :ncols], [(Vh[:, kt, :], PT[:, :ncols])], reads=[bhd, bPT], writes=[boT], start=first, stop=last)
                if pending and pending[0][0] <= ui:
                    _, args = pending.pop(0)
                    epilogue(*args)
                if last:
                    pending.append((ui + 4, (col0, ncols, gslot)))
            for _, args in pending:
                epilogue(*args)
            si += len(units)
        c.barrier()
        c.sb_release(mark)
        Wo = c.sb([64, 16, 1024], BF16, "Wo")
        b_wo = Buf()
        c.dma("pool", Wo[:], w_out.rearrange("(h d) n -> d h n", d=64), writes=[b_wo])
        self.attn_out(li, need_ctx, Wo, b_wo, YT, b_YT)
        self.phase_end()

    def attn_out(self, li, need_ctx, Wo, b_wo, YT, b_YT):
        c = self.c
        NCT, NT = self.NCT, self.NT
        G1x, bG1x = self.load_mod(li, 2, 0, "G1x")
        G1c, bG1c = self.load_mod(li, 2, 1, "G1c")
        yTs = [c.sb([64, 16, 128], BF16, "yT") for _ in range(2)]
        b_yT = [Buf(), Buf()]
        xts = [c.sb([128, D], F32, "xres") for _ in range(2)]
        b_xt = [Buf(), Buf()]
        tmps = [c.sb([128, D], F32, "rtmp") for _ in range(2)]
        b_tmp = [Buf(), Buf()]
        qbs = list(range(0 if need_ctx else NCT, NT))

        def ao_load(bi_):
            qb_ = qbs[bi_]
            c.dma("sp", xts[bi_ % 2][:], self.xs.ap()[qb_ * 128:(qb_ + 1) * 128, :], reads=[self.b_xs[qb_]], writes=[b_xt[bi_ % 2]])
            c.dma("sp", yTs[bi_ % 2][:], YT.ap()[:, :, qb_ * 128:(qb_ + 1) * 128].rearrange("h d t -> d h t"), reads=[b_YT], writes=[b_yT[bi_ % 2]])
        ao_load(0)
        for bi, qb in enumerate(qbs):
            is_ctx = qb < NCT
            xt, bxt = xts[bi % 2], b_xt[bi % 2]
            yT, byT = yTs[bi % 2], b_yT[bi % 2]
            tmp, btmp = tmps[bi % 2], b_tmp[bi % 2]
            if bi + 1 < len(qbs):
                ao_load(bi + 1)
            G1, bG1 = (G1c, bG1c) if is_ctx else (G1x, bG1x)
            for nn in range(2):
                z, bz = self.ps[5 + nn], self.bps[5 + nn]
                c.mm(z[:, :], [(yT[:, hq, :], Wo[:, hq, nn * 512:(nn + 1) * 512]) for hq in range(16)], reads=[byT, b_wo], writes=[bz])
                c.op("dve", lambda e: e.tensor_tensor(out=tmp[:, nn * 512:(nn + 1) * 512], in0=z[:, :], in1=G1[:, nn * 512:(nn + 1) * 512], op=ALU.mult), reads=[bz, bG1], writes=[btmp])
            c.op("pool", lambda e: e.tensor_tensor(out=xt[:], in0=xt[:], in1=tmp[:], op=ALU.add), reads=[btmp, bxt], writes=[bxt])
            c.dma("sp", self.xs.ap()[qb * 128:(qb + 1) * 128, :], xt[:], reads=[bxt], writes=[self.b_xs[qb]])

    def layer_ssd(self, li, j, need_ctx):
        c, nc = self.c, self.nc
        T, NT, NCT, NL, NCX = self.T, self.NT, self.NCT, self.NL, self.NCX
        w_in = self.w["ssm_w_in"].ap()[j]
        XB = self.scratch(f"ssd_xb{li}", [24, 128, T], BF16)
        XC = self.scratch(f"ssd_xc{li}", [24, 128, T], BF16)
        Zs = self.scratch(f"ssd_z{li}", [NT, 128, 2048], BF16)
        DT = self.scratch(f"ssd_dt{li}", [NT, 128, 64], F32)
        Yd = [self.scratch(f"ssd_y{li}_{d}", [NT, 128, 2048], F32) for d in range(2)]
        b_XB = Buf(); b_XC = Buf(); b_Z = Buf(); b_DT = Buf(); b_Y = [Buf(), Buf()]
        b_w = Buf("ssd_w")
        Win = c.sb([128, 8, 5184], BF16, "ssdWin")
        for k in range(8):
            c.dma("pool", Win[:, k, :], w_in[k * 128:(k + 1) * 128, :], writes=[b_w])
        dtb = c.sb([128, 64], F32, "dtb")
        c.dma("sp", dtb[:], self.w["ssm_dt_bias"].ap()[j:j + 1].rearrange("o d h -> o (d h)").partition_broadcast(128), writes=[b_w])
        mark = c.sb_mark()
        A1x, bA1x = self.load_mod(li, 1, 0, "A1x")
        S1x, bS1x = self.load_mod(li, 0, 0, "S1x")
        A1c, bA1c = self.load_mod(li, 1, 1, "A1c")
        S1c, bS1c = self.load_mod(li, 0, 1, "S1c")
        self.alloc_norm_bufs(2)
        self.set_norm_order([t0_ + s_ for (t0_, n_, _c) in self.groups(4) for s_ in range(n_)])
        hTs = [c.sb([128, 8, 512], BF16, "hT") for _ in range(2)]
        b_hT = [Buf(), Buf()]
        stg = [c.sb([128, 512], BF16, "stg") for _ in range(3)]
        b_stg = [Buf() for _ in range(3)]
        zst = [c.sb([128, 2048], BF16, "zst") for _ in range(2)]
        b_zst = [Buf(), Buf()]
        dts = [c.sb([128, 64], F32, "dts") for _ in range(2)]
        b_dts = [Buf(), Buf()]
        cnt = 0
        for gi, (t0, n, is_ctx) in enumerate(self.groups(4)):
            ncols = n * 128
            col0 = t0 * 128
            hT, bh = hTs[gi % 2], b_hT[gi % 2]
            for s in range(n):
                if is_ctx:
                    self.norm_tile(t0 + s, A1c, bA1c, S1c, bS1c, hT, bh, s * 128, (t0 + s) % 2)
                else:
                    self.norm_tile(t0 + s, A1x, bA1x, S1x, bS1x, hT, bh, s * 128, (t0 + s) % 2)
            for fc in range(24):
                i = cnt % 3
                cnt += 1
                P, bP = self.ps[2 + i], self.bps[2 + i]
                c.mm(P[:, :ncols], [(Win[:, k, 2048 + fc * 128:2048 + (fc + 1) * 128], hT[:, k, :ncols]) for k in range(8)], reads=[b_w, bh], writes=[bP])
                c.op("act", lambda e: e.activation(out=stg[i][:, :ncols], in_=P[:, :ncols], func=AF.Copy), reads=[bP], writes=[b_stg[i]])
                c.dma("sp", XB.ap()[fc, :, col0:col0 + ncols], stg[i][:, :ncols], reads=[b_stg[i]], writes=[b_XB])
            for s in range(n):
                zs, bz = zst[s % 2], b_zst[s % 2]
                for zc in range(4):
                    i = cnt % 3
                    cnt += 1
                    P, bP = self.ps[2 + i], self.bps[2 + i]
                    c.mm(P[:, :], [(hT[:, k, s * 128:(s + 1) * 128], Win[:, k, zc * 512:(zc + 1) * 512]) for k in range(8)], reads=[b_w, bh], writes=[bP])
                    c.op("act", lambda e: e.activation(out=zs[:, zc * 512:(zc + 1) * 512], in_=P[:, :], func=AF.Silu), reads=[bP], writes=[bz])
                c.dma("sp", Zs.ap()[t0 + s], zs[:], reads=[bz], writes=[b_Z])
                i = cnt % 3
                cnt += 1
                P, bP = self.ps[2 + i], self.bps[2 + i]
                dt_, bdt = dts[s % 2], b_dts[s % 2]
                c.mm(P[:, 0:64], [(hT[:, k, s * 128:(s + 1) * 128], Win[:, k, 5120:5184]) for k in range(8)], reads=[b_w, bh], writes=[bP])
                c.op("dve", lambda e: e.tensor_tensor(out=dt_[:], in0=P[:, 0:64], in1=dtb[:], op=ALU.add), reads=[bP, b_w], writes=[bdt])
                c.op("act", lambda e: e.activation(out=dt_[:], in_=dt_[:], func=AF.Exp), reads=[bdt], writes=[bdt])
                c.op("act", lambda e: e.activation(out=dt_[:], in_=dt_[:], func=AF.Ln, bias=1.0), reads=[bdt], writes=[bdt])
                c.dma("sp", DT.ap()[t0 + s], dt_[:], reads=[bdt], writes=[b_DT])
        c.barrier()
        c.sb_release(self.mark0)
        cw = c.sb([128, 24, 5], F32, "convw"); cbias = c.sb([128, 24], F32, "convb")
        b_cw = Buf()
        c.dma("sp", cw[:], self.w["ssm_conv_wT"].ap()[j], writes=[b_cw])
        c.dma("sp", cbias[:], self.w["ssm_conv_bT"].ap()[j], writes=[b_cw])
        segs = [(0, NCX), (NCX, NL)]
        Lmax = max(NCX, NL)
        xp = [c.sb([128, Lmax + 4], BF16, "xp") for _ in range(2)]
        b_xp = [Buf(), Buf()]
        acc = [c.sb([128, Lmax], F32, "cacc") for _ in range(2)]
        b_acc = [Buf(), Buf()]
        cout = [c.sb([128, Lmax], BF16, "cout") for _ in range(2)]
        b_cout = [Buf(), Buf()]
        it = 0
        work = [(fc, o0, L) for fc in range(24) for (o0, L) in segs]

        def conv_load(k_):
            fc_, o0_, L_ = work[k_]
            i_ = k_ % 2
            c.op("pool", lambda e: e.memset(xp[i_][:], 0.0), writes=[b_xp[i_]])
            c.dma("sp", xp[i_][:, 2:2 + L_], XB.ap()[fc_, :, o0_:o0_ + L_], reads=[b_XB], writes=[b_xp[i_]])
        conv_load(0)
        for fc in range(24):
            for (o0, L) in segs:
                i = it % 2
                it += 1
                if it < len(work):
                    conv_load(it)
                c.op("dve", lambda e: e.tensor_scalar(out=acc[i][:, :L], in0=xp[i][:, 0:L], scalar1=cw[:, fc, 0:1], scalar2=None, op0=ALU.mult), reads=[b_xp[i], b_cw], writes=[b_acc[i]])
                for k in range(1, 5):
                    eng = "dve"
                    c.op(eng, lambda e: e.scalar_tensor_tensor(out=acc[i][:, :L], in0=xp[i][:, k:k + L], scalar=cw[:, fc, k:k + 1], in1=acc[i][:, :L], op0=ALU.mult, op1=ALU.add),
                         reads=[b_xp[i], b_cw, b_acc[i]], writes=[b_acc[i]])
                c.op("act", lambda e: e.activation(out=cout[i][:, :L], in_=acc[i][:, :L], func=AF.Silu, bias=cbias[:, fc:fc + 1]), reads=[b_acc[i], b_cw], writes=[b_cout[i]])
                c.dma("sp", XC.ap()[fc, :, o0:o0 + L], cout[i][:, :L], reads=[b_cout[i]], writes=[b_XC])
        c.barrier()
        c.sb_release(self.mark0)
        sel = c.sb([32, 32, 128], F32, "sel32"); b_sel = Buf()
        c.dma("sp", sel[:], self.w["sel32"].ap(), writes=[b_sel])
        aneg = c.sb([128, 64], F32, "aneg"); dsk = c.sb([128, 64], F32, "dsk"); b_an = Buf()
        c.dma("sp", aneg[:], self.w["ssm_a_log"].ap()[j:j + 1].rearrange("o d h -> o (d h)").partition_broadcast(128), writes=[b_an])
        c.op("act", lambda e: e.activation(out=aneg[:], in_=aneg[:], func=AF.Exp), reads=[b_an], writes=[b_an])
        c.op("act", lambda e: e.activation(out=aneg[:], in_=aneg[:], func=AF.Copy, scale=-1.0), reads=[b_an], writes=[b_an])
        c.dma("sp", dsk[:], self.w["ssm_d"].ap()[j:j + 1].rearrange("o d h -> o (d h)").partition_broadcast(128), writes=[b_an])
        c.op("dve", lambda e: e.tensor_tensor(out=dsk[:, 0:32], in0=dsk[:, 0:32], in1=dsk[:, 32:64], op=ALU.add), reads=[b_an], writes=[b_an])
        xcs = [c.sb([128, 24, 128], BF16, "xc") for _ in range(2)]; b_xc = [Buf(), Buf()]
        dtt = [c.sb([128, 64], F32, "dtt") for _ in range(2)]; b_dtt = [Buf(), Buf()]
        gt = [c.sb([128, 8, 32], F32, "gt") for _ in range(2)]; b_gt = [Buf(), Buf()]
        acT = [c.sb([32, 128], F32, "acT") for _ in range(2)]; b_acT = [Buf(), Buf()]
        nacT = [c.sb([32, 128], F32, "nacT") for _ in range(2)]
        xtok = [c.sb([128, 32, 64], F32, "xtok") for _ in range(2)]; b_xtok = [Buf(), Buf()]
        u = [c.sb([128, 32, 64], BF16, "u") for _ in range(2)]; b_u = [Buf(), Buf()]
        Vw = [c.sb([128, 32, 64], BF16, "Vw") for _ in range(2)]; b_Vw = [Buf(), Buf()]
        Btok = [c.sb([128, 4, 128], BF16, "Btok") for _ in range(2)]; b_Bt = [Buf(), Buf()]
        scm = [c.sb([128, 128], F32, "scm") for _ in range(2)]; b_scm = [Buf(), Buf()]
        aa = [c.sb([128, 512], F32, "aa") for _ in range(4)]; b_aa = [Buf() for _ in range(4)]
        EE = [c.sb([128, 512], F32, "EE") for _ in range(4)]; b_EE = [Buf() for _ in range(4)]
        MT = [c.sb([128, 512], BF16, "MT") for _ in range(4)]; b_MT = [Buf() for _ in range(4)]
        yi = [c.sb([128, 512], F32, "yi") for _ in range(2)]; b_yi = [Buf(), Buf()]
        Yt = [c.sb([128, 2048], F32, "Yt") for _ in range(2)]; b_Yt = [Buf(), Buf()]
        S32 = c.sb([128, 4, 512], F32, "S32"); Sb = c.sb([128, 4, 512], BF16, "Sb"); b_S = [Buf() for _ in range(4)]
        ci = 0; hi = 0
        for d in range(2):
            tri = self.trile32 if d == 0 else self.trige32
            order = list(range(NT)) if d == 0 else (list(range(NCT - 1, -1, -1)) + list(range(NT - 1, NCT - 1, -1)))
            c.op("dve", lambda e: e.memset(S32[:], 0.0), writes=b_S)
            c.op("pool", lambda e: e.memset(Sb[:], 0.0), writes=b_S)
            def prep_load(ch, i):
                xc, bxc = xcs[i], b_xc[i]
                c.dma("sp", xc[:], XC.ap()[:, :, ch * 128:(ch + 1) * 128].rearrange("f p t -> p f t"), reads=[b_XC], writes=[bxc])
                c.dma("sp", dtt[i][:], DT.ap()[ch], reads=[b_DT], writes=[b_dtt[i]])

            def prep(ch, i):
                xc, bxc = xcs[i], b_xc[i]
                g_, bg = gt[i], b_gt[i]
                dc = slice(d * 32, (d + 1) * 32)
                c.op("dve", lambda e: e.tensor_tensor(out=g_[:, 0, :], in0=dtt[i][:, dc], in1=aneg[:, dc], op=ALU.mult), reads=[b_dtt[i], b_an], writes=[bg])
                p0, bp0 = self.ps[0], self.bps[0]
                c.mm(p0[:, 0:32], [(tri, g_[:, 0, :])], reads=[self.b_const, bg], writes=[bp0])
                c.mm(p0[:, 32:64], [(self.ones32, g_[:, 0, :])], reads=[self.b_const, bg], writes=[bp0])
                c.mm(p0[0:32, 128:256], [(g_[:, 0, :], tri)], reads=[self.b_const, bg], writes=[bp0])
                c.op("act", lambda e: e.activation(out=g_[:, 1, :], in_=p0[:, 0:32], func=AF.Copy), reads=[bp0], writes=[bg])
                c.op("act", lambda e: e.activation(out=g_[:, 2, :], in_=p0[:, 0:32], func=AF.Copy, scale=-1.0), reads=[bp0], writes=[bg])
                c.op("act", lambda e: e.activation(out=g_[:, 3, :], in_=p0[:, 0:32], func=AF.Exp), reads=[bp0], writes=[bg])
                c.op("dve", lambda e: e.tensor_tensor(out=g_[:, 4, :], in0=p0[:, 32:64], in1=g_[:, 1, :], op=ALU.subtract), reads=[bp0, bg], writes=[bg])
                c.op("act", lambda e: e.activation(out=g_[:, 4, :], in_=g_[:, 4, :], func=AF.Exp), reads=[bg], writes=[bg])
                c.op("act", lambda e: e.activation(out=g_[:, 5, :], in_=p0[:, 32:64], func=AF.Exp), reads=[bp0], writes=[bg])
                c.op("act", lambda e: e.activation(out=acT[i][:], in_=p0[0:32, 128:256], func=AF.Copy), reads=[bp0], writes=[b_acT[i]])
                c.op("act", lambda e: e.activation(out=nacT[i][:], in_=p0[0:32, 128:256], func=AF.Copy, scale=-1.0), reads=[bp0], writes=[b_acT[i]])
                for hh in range(2):
                    pT = self.ps[1].ap().bitcast(BF16)
                    c.tr_multi([(pT[:, k * 128:(k + 1) * 128], xc[:, hh * 8 + k, :], self.identb) for k in range(8)], reads=[bxc, self.b_const], writes=[self.bps[1]])
                    c.op("act", lambda e: e.activation(out=xtok[i][:, hh * 16:(hh + 1) * 16, :].rearrange("p h d -> p (h d)"), in_=pT[:, :], func=AF.Copy), reads=[self.bps[1]], writes=[b_xtok[i]])
                pT = self.ps[1].ap().bitcast(BF16)
                c.tr_multi([(pT[:, k * 128:(k + 1) * 128], xc[:, 16 + k, :], self.identb) for k in range(4)], reads=[bxc, self.b_const], writes=[self.bps[1]])
                c.op("act", lambda e: e.activation(out=Btok[i][:].rearrange("p g n -> p (g n)"), in_=pT[:, 0:512], func=AF.Copy), reads=[self.bps[1]], writes=[b_Bt[i]])
                c.op("dve", lambda e: e.tensor_tensor(out=u[i][:], in0=xtok[i][:], in1=dtt[i][:, dc].unsqueeze(2).to_broadcast([128, 32, 64]), op=ALU.mult), reads=[b_xtok[i], b_dtt[i]], writes=[b_u[i]])
                c.op("pool", lambda e: e.tensor_tensor(out=Vw[i][:], in0=u[i][:], in1=g_[:, 4, :].unsqueeze(2).to_broadcast([128, 32, 64]), op=ALU.mult), reads=[b_u[i], bg], writes=[b_Vw[i]])
            def groups_(ch, i, nxt):
                if nxt is not None:
                    prep_load(*nxt)
                xc, bxc = xcs[i], b_xc[i]
                g_, bg = gt[i], b_gt[i]
                dc = slice(d * 32, (d + 1) * 32)
                Y, bY = Yt[i], b_Yt[i]
                for g in range(4):
                    if g == 2 and nxt is not None:
                        prep(*nxt)
                    pcb, bpcb = self.ps[2], self.bps[2]
                    c.mm(pcb[:, 0:128], [(xc[:, 16 + g, :], xc[:, 20 + g, :])], reads=[bxc], writes=[bpcb])
                    sm, bsm = scm[g % 2], b_scm[g % 2]
                    c.op("dve", lambda e: e.tensor_tensor(out=sm[:], in0=pcb[:, 0:128], in1=tri, op=ALU.mult), reads=[bpcb, self.b_const], writes=[bsm])
                    yps, byps = self.ps[4 + g % 2], self.bps[4 + g % 2]
                    for e8 in range(8):
                        h = g * 8 + e8
                        pbc, bpbc = self.ps[6 + e8 // 4], self.bps[6 + e8 // 4]
                        c.mm(pbc[:, (e8 % 4) * 128:(e8 % 4 + 1) * 128], [(sel[:, h, :], acT[i][:]), (nacT[i][:], sel[:, h, :])], reads=[b_sel, b_acT[i]], writes=[bpbc])
                    for hb in range(2):
                        k4 = (g % 2) * 2 + hb
                        pbc, bpbc = self.ps[6 + hb], self.bps[6 + hb]
                        c.op("dve", lambda e: e.tensor_scalar(out=aa[k4][:], in0=pbc[:, :], scalar1=0.0, scalar2=None, op0=ALU.min), reads=[bpbc], writes=[b_aa[k4]])
                        c.op("act", lambda e: e.activation(out=EE[k4][:], in_=aa[k4][:], func=AF.Exp), reads=[b_aa[k4]], writes=[b_EE[k4]])
                        c.op("pool", lambda e: e.tensor_tensor(out=MT[k4][:].rearrange("p (h t) -> p h t", h=4), in0=EE[k4][:].rearrange("p (h t) -> p h t", h=4),
                                                               in1=sm[:].unsqueeze(1).to_broadcast([128, 4, 128]), op=ALU.mult), reads=[bsm, b_EE[k4]], writes=[b_MT[k4]])
                    for e8 in range(8):
                        h = g * 8 + e8
                        k4 = (g % 2) * 2 + e8 // 4
                        c.mm(yps[:, e8 * 64:(e8 + 1) * 64], [(MT[k4][:, (e8 % 4) * 128:(e8 % 4 + 1) * 128], u[i][:, h, :])], reads=[b_MT[k4], b_u[i]], writes=[byps])
                    pin, bpin = self.ps[3], self.bps[3]
                    c.mm(pin[:, :], [(xc[:, 20 + g, :], Sb[:, g, :])], reads=[bxc, b_S[g]], writes=[bpin])
                    y_, byi = yi[g % 2], b_yi[g % 2]
                    c.op("dve", lambda e: e.tensor_tensor(out=y_[:].rearrange("p (h d) -> p h d", d=64), in0=pin[:, :].rearrange("p (h d) -> p h d", d=64),
                                                          in1=g_[:, 3, g * 8:(g + 1) * 8].unsqueeze(2).to_broadcast([128, 8, 64]), op=ALU.mult), reads=[bpin, bg], writes=[byi])
                    c.op("dve", lambda e: e.tensor_tensor(out=Y[:, g * 512:(g + 1) * 512], in0=yps[:, :], in1=y_[:], op=ALU.add), reads=[byps, byi], writes=[bY])
                    if d == 0:
                        c.op("pool", lambda e: e.tensor_tensor(out=y_[:].rearrange("p (h d) -> p h d", d=64), in0=xtok[i][:, g * 8:(g + 1) * 8, :],
                                                               in1=dsk[:, g * 8:(g + 1) * 8].unsqueeze(2).to_broadcast([128, 8, 64]), op=ALU.mult), reads=[b_xtok[i], b_an, bY], writes=[byi])
                        c.op("pool", lambda e: e.tensor_tensor(out=Y[:, g * 512:(g + 1) * 512], in0=Y[:, g * 512:(g + 1) * 512], in1=y_[:], op=ALU.add), reads=[byi], writes=[bY])
                    pst, bpst = self.ps[3], self.bps[3]
                    c.mm(pst[:, :], [(Btok[i][:, g, :], Vw[i][:, g * 8:(g + 1) * 8, :].rearrange("p h d -> p (h d)"))], reads=[b_Bt[i], b_Vw[i]], writes=[bpst])
                    c.op("dve", lambda e: e.tensor_tensor(out=S32[:, g, :].rearrange("p (h d) -> p h d", d=64), in0=S32[:, g, :].rearrange("p (h d) -> p h d", d=64),
                                                          in1=g_[:, 5, g * 8:(g + 1) * 8].unsqueeze(2).to_broadcast([128, 8, 64]), op=ALU.mult), reads=[bg], writes=[b_S[g]])
                    c.op("dve", lambda e: e.tensor_tensor(out=S32[:, g, :], in0=S32[:, g, :], in1=pst[:, :], op=ALU.add), reads=[bpst], writes=[b_S[g]])
                    c.op("act", lambda e: e.activation(out=Sb[:, g, :], in_=S32[:, g, :], func=AF.Copy), reads=[], writes=[b_S[g]])
                c.dma("sp", Yd[d].ap()[ch], Y[:], reads=[bY], writes=[b_Y[d]])
            prep_load(order[0], 0)
            prep(order[0], 0)
            for idx, ch in enumerate(order):
                groups_(ch, idx % 2, (order[idx + 1], (idx + 1) % 2) if idx + 1 < len(order) else None)
        c.barrier()
        c.sb_release(self.mark0)
        Wo = c.sb([128, 16, 1024], BF16, "ssdWo"); b_wo = Buf()
        c.dma("pool", Wo[:], self.w["ssm_w_out"].ap()[j].rearrange("(k p) n -> p k n", p=128), writes=[b_wo])
        ng = c.sb([128, 2048], F32, "ssdng")
        c.dma("sp", ng[:], self.w["ssm_norm_g"].ap()[j:j + 1, :].partition_broadcast(128), writes=[b_wo])
        self.gated_out(li, need_ctx, Yd, b_Y, Zs, b_Z, 2048, 4, ng, Wo, b_wo, False)
        self.phase_end()

    def gated_out(self, li, need_ctx, Yd, b_Y, Zs, b_Z, W, ngroups, ng, Wo, b_wo, gate_after):
        c = self.c
        NCT, NT = self.NCT, self.NT
        KC = W // 128
        gs = W // ngroups
        G1x, bG1x = self.load_mod(li, 2, 0, "G1x")
        G1c, bG1c = self.load_mod(li, 2, 1, "G1c")
        yf = [c.sb([128, W], F32, "yf") for _ in range(2)]; yb = [c.sb([128, W], F32, "yb") for _ in range(2)]
        zt = [c.sb([128, W], BF16, "zt") for _ in range(2)]
        b_in = [Buf(), Buf()]
        jk = c.sb([128, W], BF16, "gjunk"); b_jk = Buf()
        st = [c.sb([128, 2, 8], F32, "gst") for _ in range(2)]; b_st = [Buf(), Buf()]
        yn = [c.sb([128, W], BF16, "yn") for _ in range(2)]; b_yn = [Buf(), Buf()]
        yT = [c.sb([128, KC, 128], BF16, "gyT") for _ in range(2)]; b_yT = [Buf(), Buf()]
        xts = [c.sb([128, D], F32, "xres") for _ in range(2)]; b_xt = [Buf(), Buf()]
        tmps = [c.sb([128, D], F32, "rtmp") for _ in range(2)]; b_tmp = [Buf(), Buf()]
        qbs = list(range(0 if need_ctx else NCT, NT))

        def go_load(bi_):
            i_ = bi_ % 2
            qb_ = qbs[bi_]
            c.dma("sp", yf[i_][:], Yd[0].ap()[qb_], reads=[b_Y[0]], writes=[b_in[i_]])
            c.dma("sp", yb[i_][:], Yd[1].ap()[qb_], reads=[b_Y[1]], writes=[b_in[i_]])
            c.dma("sp", zt[i_][:], Zs.ap()[qb_], reads=[b_Z], writes=[b_in[i_]])
            c.dma("sp", xts[i_][:], self.xs.ap()[qb_ * 128:(qb_ + 1) * 128, :], reads=[self.b_xs[qb_]], writes=[b_xt[i_]])
        go_load(0)
        for bi, qb in enumerate(qbs):
            i = bi % 2
            is_ctx = qb < NCT
            if bi + 1 < len(qbs):
                go_load(bi + 1)
            c.op("pool", lambda e: e.tensor_tensor(out=yf[i][:], in0=yf[i][:], in1=yb[i][:], op=ALU.add), reads=[b_in[i]], writes=[b_in[i]])
            if not gate_after:
                c.op("dve", lambda e: e.tensor_tensor(out=yf[i][:], in0=yf[i][:], in1=zt[i][:], op=ALU.mult), reads=[b_in[i]], writes=[b_in[i]])
            c.op("dve", lambda e: e.memset(st[i][:], 0.0), writes=[b_st[i]])
            for g in range(ngroups):
                c.op("act", lambda e: e.activation(out=jk[:, g * gs:(g + 1) * gs], in_=yf[i][:, g * gs:(g + 1) * gs], func=AF.Square, accum_out=st[i][:, 0, g:g + 1]), reads=[b_in[i]], writes=[b_jk, b_st[i]])
            c.op("dve", lambda e: e.tensor_scalar(out=st[i][:, 1, :], in0=st[i][:, 0, :], scalar1=1.0 / gs, scalar2=EPS, op0=ALU.mult, op1=ALU.add), reads=[b_st[i]], writes=[b_st[i]])
            c.op("act", lambda e: e.activation(out=st[i][:, 1, :], in_=st[i][:, 1, :], func=AF.Sqrt), reads=[b_st[i]], writes=[b_st[i]])
            c.op("dve", lambda e: e.reciprocal(out=st[i][:, 1, :], in_=st[i][:, 1, :]), reads=[b_st[i]], writes=[b_st[i]])
            c.op("dve", lambda e: e.tensor_tensor(out=yf[i][:].rearrange("p (g d) -> p g d", g=ngroups), in0=yf[i][:].rearrange("p (g d) -> p g d", g=ngroups),
                                                  in1=st[i][:, 1, 0:ngroups].unsqueeze(2).to_broadcast([128, ngroups, gs]), op=ALU.mult), reads=[b_st[i], b_in[i]], writes=[b_in[i]])
            if gate_after:
                c.op("pool", lambda e: e.tensor_tensor(out=yf[i][:], in0=yf[i][:], in1=ng[:], op=ALU.mult), reads=[b_in[i], b_wo], writes=[b_in[i]])
                c.op("dve", lambda e: e.tensor_tensor(out=yn[i][:], in0=yf[i][:], in1=zt[i][:], op=ALU.mult), reads=[b_in[i]], writes=[b_yn[i]])
            else:
                c.op("pool", lambda e: e.tensor_tensor(out=yn[i][:], in0=yf[i][:], in1=ng[:], op=ALU.mult), reads=[b_in[i], b_wo], writes=[b_yn[i]])
            for hb in range(KC // 8):
                pT = self.ps[hb].ap().bitcast(BF16)
                c.tr_multi([(pT[:, k * 128:(k + 1) * 128], yn[i][:, (hb * 8 + k) * 128:(hb * 8 + k + 1) * 128], self.identb) for k in range(8)], reads=[b_yn[i], self.b_const], writes=[self.bps[hb]])
                c.op("act", lambda e: e.activation(out=yT[i][:, hb * 8:(hb + 1) * 8, :], in_=pT.rearrange("p (k n) -> p k n", k=8), func=AF.Copy), reads=[self.bps[hb]], writes=[b_yT[i]])
            G1, bG1 = (G1c, bG1c) if is_ctx else (G1x, bG1x)
            for nn in range(2):
                z, bz = self.ps[5 + nn], self.bps[5 + nn]
                c.mm(z[:, :], [(yT[i][:, k, :], Wo[:, k, nn * 512:(nn + 1) * 512]) for k in range(KC)], reads=[b_yT[i], b_wo], writes=[bz])
                c.op("dve", lambda e: e.tensor_tensor(out=tmps[i][:, nn * 512:(nn + 1) * 512], in0=z[:, :], in1=G1[:, nn * 512:(nn + 1) * 512], op=ALU.mult), reads=[bz, bG1], writes=[b_tmp[i]])
            c.op("pool", lambda e: e.tensor_tensor(out=xts[i][:], in0=xts[i][:], in1=tmps[i][:], op=ALU.add), reads=[b_tmp[i], b_xt[i]], writes=[b_xt[i]])
            c.dma("sp", self.xs.ap()[qb * 128:(qb + 1) * 128, :], xts[i][:], reads=[b_xt[i]], writes=[self.b_xs[qb]])

    def layer_mlstm(self, li, j, need_ctx):
        c, nc = self.c, self.nc
        T, NT, NCT = self.T, self.NT, self.NCT
        w_in = self.w["mlstm_w_in"].ap()[j]
        QK = self.scratch(f"ml_qk{li}", [2, 8, 64, T], BF16)
        Kt = self.scratch(f"ml_kt{li}", [NT, 128, 512], BF16)
        Va = self.scratch(f"ml_va{li}", [NT, 128, 8, 129], BF16)
        Os = self.scratch(f"ml_os{li}", [NT, 128, 1024], BF16)
        Gt = self.scratch(f"ml_gt{li}", [NT, 128, 32], F32)
        Yd = [self.scratch(f"ml_y{li}_{d}", [NT, 128, 1024], F32) for d in range(2)]
        b_QK = Buf(); b_Kt = Buf(); b_Va = Buf(); b_Os = Buf(); b_Gt = Buf(); b_Y = [Buf(), Buf()]
        b_w = Buf("ml_w")
        Win = c.sb([128, 8, 3104], BF16, "mlWin")
        for k in range(8):
            c.dma("pool", Win[:, k, :], w_in[k * 128:(k + 1) * 128, :], writes=[b_w])
        gb = c.sb([128, 32], F32, "mlgb")
        c.dma("sp", gb[:], self.w["mlstm_gate_b"].ap()[j:j + 1].rearrange("o a h -> o (a h)").partition_broadcast(128), writes=[b_w])
        A1x, bA1x = self.load_mod(li, 1, 0, "A1x")
        S1x, bS1x = self.load_mod(li, 0, 0, "S1x")
        A1c, bA1c = self.load_mod(li, 1, 1, "A1c")
        S1c, bS1c = self.load_mod(li, 0, 1, "S1c")
        self.alloc_norm_bufs(2)
        self.set_norm_order([t0_ + s_ for (t0_, n_, _c) in self.groups(4) for s_ in range(n_)])
        hTs = [c.sb([128, 8, 512], BF16, "hT") for _ in range(2)]
        b_hT = [Buf(), Buf()]
        stg = [c.sb([64, 512], BF16, "mlstg") for _ in range(3)]; b_stg = [Buf() for _ in range(3)]
        kst = [c.sb([128, 512], BF16, "mlkst") for _ in range(2)]; b_kst = [Buf(), Buf()]
        vst = [c.sb([128, 8, 129], BF16, "mlvst") for _ in range(2)]; b_vst = [Buf(), Buf()]
        ost = [c.sb([128, 1024], BF16, "mlost") for _ in range(2)]; b_ost = [Buf(), Buf()]
        gst = [c.sb([128, 32], F32, "mlgst") for _ in range(2)]; b_gst = [Buf(), Buf()]
        gtmp = [c.sb([128, 8], F32, "mlgtmp") for _ in range(2)]
        for i in range(2):
            c.op("pool", lambda e: e.memset(vst[i][:], 1.0), writes=[b_vst[i]])
        cnt = 0
        for gi, (t0, n, is_ctx) in enumerate(self.groups(4)):
            ncols = n * 128
            col0 = t0 * 128
            hT, bh = hTs[gi % 2], b_hT[gi % 2]
            for s in range(n):
                if is_ctx:
                    self.norm_tile(t0 + s, A1c, bA1c, S1c, bS1c, hT, bh, s * 128, (t0 + s) % 2)
                else:
                    self.norm_tile(t0 + s, A1x, bA1x, S1x, bS1x, hT, bh, s * 128, (t0 + s) % 2)
            for qk in range(2):
                for h in range(8):
                    i = cnt % 3
                    cnt += 1
                    P, bP = self.ps[2 + i], self.bps[2 + i]
                    col = qk * 512 + h * 64
                    c.mm(P[0:64, :ncols], [(Win[:, k, col:col + 64], hT[:, k, :ncols]) for k in range(8)], reads=[b_w, bh], writes=[bP])
                    c.op("act", lambda e: e.activation(out=stg[i][:, :ncols], in_=P[0:64, :ncols], func=AF.Copy, scale=(0.125 if qk == 1 else 1.0)), reads=[bP], writes=[b_stg[i]])
                    c.dma("sp", QK.ap()[qk, h, :, col0:col0 + ncols], stg[i][:, :ncols], reads=[b_stg[i]], writes=[b_QK])
            for s in range(n):
                sl = slice(s * 128, (s + 1) * 128)
                i2 = s % 2

                def tokproj(c0, w_):
                    nonlocal cnt
                    i = cnt % 3
                    cnt += 1
                    P, bP = self.ps[2 + i], self.bps[2 + i]
                    c.mm(P[:, :w_], [(hT[:, k, sl], Win[:, k, c0:c0 + w_]) for k in range(8)], reads=[b_w, bh], writes=[bP])
                    return P, bP
                P, bP = tokproj(512, 512)
                c.op("act", lambda e: e.activation(out=kst[i2][:], in_=P[:, :], func=AF.Copy, scale=0.125), reads=[bP], writes=[b_kst[i2]])
                c.dma("sp", Kt.ap()[t0 + s], kst[i2][:], reads=[b_kst[i2]], writes=[b_Kt])
                for vh in range(2):
                    P, bP = tokproj(1024 + vh * 512, 512)
                    c.op("act", lambda e: e.activation(out=vst[i2][:, vh * 4:(vh + 1) * 4, 0:128], in_=P[:, :].rearrange("p (h d) -> p h d", d=128), func=AF.Copy), reads=[bP], writes=[b_vst[i2]])
                c.dma("sp", Va.ap()[t0 + s], vst[i2][:], reads=[b_vst[i2]], writes=[b_Va])
                for oh in range(2):
                    P, bP = tokproj(2048 + oh * 512, 512)
                    c.op("act", lambda e: e.activation(out=ost[i2][:, oh * 512:(oh + 1) * 512], in_=P[:, :], func=AF.Sigmoid), reads=[bP], writes=[b_ost[i2]])
                c.dma("sp", Os.ap()[t0 + s], ost[i2][:], reads=[b_ost[i2]], writes=[b_Os])
                P, bP = tokproj(3072, 32)
                g_ = gst[i2]
                c.op("dve", lambda e: e.tensor_tensor(out=g_[:], in0=P[:, 0:32], in1=gb[:], op=ALU.add), reads=[bP, b_w], writes=[b_gst[i2]])
                for r in (1, 3):
                    cs_ = slice(r * 8, (r + 1) * 8)
                    c.op("act", lambda e: e.activation(out=g_[:, cs_], in_=g_[:, cs_], func=AF.Exp, scale=-1.0), reads=[b_gst[i2]], writes=[b_gst[i2]])
                    c.op("act", lambda e: e.activation(out=g_[:, cs_], in_=g_[:, cs_], func=AF.Ln, bias=1.0), reads=[b_gst[i2]], writes=[b_gst[i2]])
                    c.op("act", lambda e: e.activation(out=g_[:, cs_], in_=g_[:, cs_], func=AF.Copy, scale=-1.0), reads=[b_gst[i2]], writes=[b_gst[i2]])
                c.dma("sp", Gt.ap()[t0 + s], g_[:], reads=[b_gst[i2]], writes=[b_Gt])
        c.barrier()
        c.sb_release(self.mark0)
        sel = c.sb([8, 8, 128], F32, "sel8"); b_sel = Buf()
        c.dma("sp", sel[:], self.w["sel32"].ap()[0:8, 0:8, :], writes=[b_sel])
        qTs = [c.sb([64, 8, 128], BF16, "mqT") for _ in range(2)]; kTs = [c.sb([64, 8, 128], BF16, "mkT") for _ in range(2)]
        kts = [c.sb([128, 512], BF16, "mkt") for _ in range(2)]; vas = [c.sb([128, 8, 129], BF16, "mva") for _ in range(2)]
        gts = [c.sb([128, 32], F32, "mgt") for _ in range(2)]; b_ld = [Buf(), Buf()]
        GM = [c.sb([8, 12, 128], F32, "GM") for _ in range(2)]; b_GM = [Buf(), Buf()]
        sm8 = [c.sb([8, 8], F32, "sm8") for _ in range(2)]; b_sm8 = [Buf(), Buf()]
        ms = c.sb([8, 2], F32, "ms"); b_ms = Buf()
        dg = c.sb([8, 8], F32, "dg"); b_dg = Buf()
        tk = [c.sb([128, 40], F32, "tk") for _ in range(2)]; b_tk = [Buf(), Buf()]
        cwc = [c.sb([64, 8], F32, "cwc") for _ in range(2)]; b_cwc = [Buf(), Buf()]
        scm = [c.sb([128, 512], F32, "mscm") for _ in range(2)]; b_scm = [Buf() for _ in range(2)]
        aa = [c.sb([128, 512], F32, "maa") for _ in range(2)]; b_aa = [Buf() for _ in range(2)]
        EE = [c.sb([128, 512], F32, "mEE") for _ in range(2)]; b_EE = [Buf() for _ in range(2)]
        MT = [c.sb([128, 512], BF16, "mMT") for _ in range(2)]; b_MT = [Buf() for _ in range(2)]
        yi = [c.sb([128, 129], F32, "myi") for _ in range(4)]; b_yi = [Buf() for _ in range(4)]
        nd4 = [c.sb([128, 4, 132], F32, "mnd4") for _ in range(2)]; b_nd4 = [Buf() for _ in range(2)]
        Vw = [c.sb([128, 129], BF16, "mVw") for _ in range(4)]; b_Vw = [Buf() for _ in range(4)]
        Yt = [c.sb([128, 1024], F32, "mYt") for _ in range(2)]; b_Yt = [Buf(), Buf()]
        S32 = c.sb([64, 8, 129], F32, "mS32"); Sb = c.sb([64, 8, 129], BF16, "mSb"); b_S = [Buf() for _ in range(8)]
        ci = 0; hi = 0
        id8 = self.ident32[0:8, 0:8]
        for d in range(2):
            tri = self.trile32 if d == 0 else self.trige32
            order = list(range(NT)) if d == 0 else (list(range(NCT - 1, -1, -1)) + list(range(NT - 1, NCT - 1, -1)))
            c.op("dve", lambda e: e.memset(S32[:], 0.0), writes=b_S)
            c.op("pool", lambda e: e.memset(Sb[:], 0.0), writes=b_S)
            c.op("dve", lambda e: e.memset(ms[:], 0.0), writes=[b_ms])
            endc = 127 if d == 0 else 0
            def gate_load(ch, i):
                cols = slice(ch * 128, (ch + 1) * 128)
                bl = b_ld[i]
                c.dma("sp", qTs[i][:], QK.ap()[0, :, :, cols].rearrange("h d t -> d h t"), reads=[b_QK], writes=[bl])
                c.dma("sp", kTs[i][:], QK.ap()[1, :, :, cols].rearrange("h d t -> d h t"), reads=[b_QK], writes=[bl])
                c.dma("sp", kts[i][:], Kt.ap()[ch], reads=[b_Kt], writes=[bl])
                c.dma("sp", vas[i][:], Va.ap()[ch], reads=[b_Va], writes=[bl])
                c.dma("sp", gts[i][:], Gt.ap()[ch], reads=[b_Gt], writes=[bl])

            def gate(ch, i):
                bl = b_ld[i]
                ig = gts[i][:, d * 16:d * 16 + 8]
                lf = gts[i][:, d * 16 + 8:d * 16 + 16]
                G, bG = GM[i], b_GM[i]
                s8, bs8 = sm8[i], b_sm8[i]
                t_, bt = tk[i], b_tk[i]
                p0, bp0 = self.ps[0], self.bps[0]
                c.mm(p0[0:8, 0:128], [(ig, self.ident32)], reads=[bl, self.b_const], writes=[bp0])
                c.mm(p0[0:8, 128:256], [(lf, tri)], reads=[bl, self.b_const], writes=[bp0])
                c.mm(p0[:, 256:264], [(tri, lf)], reads=[bl, self.b_const], writes=[bp0])
                c.op("act", lambda e: e.activation(out=G[:, 0:2, :], in_=p0[0:8, 0:256].rearrange("p (a t) -> p a t", a=2), func=AF.Copy), reads=[bp0], writes=[bG])
                c.op("act", lambda e: e.activation(out=t_[:, 32:40], in_=p0[:, 256:264], func=AF.Copy), reads=[bp0], writes=[bt])
                c.op("dve", lambda e: e.tensor_tensor(out=t_[:, 0:8], in0=ig, in1=t_[:, 32:40], op=ALU.subtract), reads=[bl, bt], writes=[bt])
                c.op("dve", lambda e: e.tensor_tensor(out=G[:, 2, :], in0=G[:, 0, :], in1=G[:, 1, :], op=ALU.subtract), reads=[bG], writes=[bG])
                src, dst = 2, 3
                for k in range(7):
                    sft = 1 << k
                    c.op("dve", lambda e: e.tensor_copy(out=G[:, dst, :], in_=G[:, src, :]), reads=[bG], writes=[bG])
                    if d == 0:
                        c.op("dve", lambda e: e.tensor_tensor(out=G[:, dst, sft:128], in0=G[:, src, sft:128], in1=G[:, src, 0:128 - sft], op=ALU.max), reads=[bG], writes=[bG])
                    else:
                        c.op("dve", lambda e: e.tensor_tensor(out=G[:, dst, 0:128 - sft], in0=G[:, src, 0:128 - sft], in1=G[:, src, sft:128], op=ALU.max), reads=[bG], writes=[bG])
                    src, dst = dst, (3 if dst == 4 else 4)
                cmr = src
                c.op("dve", lambda e: e.tensor_scalar(out=G[:, cmr, :], in0=G[:, cmr, :], scalar1=ms[:, 0:1], scalar2=None, op0=ALU.max), reads=[bG, b_ms], writes=[bG])
                c.op("act", lambda e: e.activation(out=G[:, 5, :], in_=G[:, cmr, :], func=AF.Copy, scale=-1.0), reads=[bG], writes=[bG])
                c.op("act", lambda e: e.activation(out=G[:, 6, :], in_=G[:, cmr, :], func=AF.Exp, scale=-1.0, bias=ms[:, 0:1]), reads=[bG, b_ms], writes=[bG])
                c.op("dve", lambda e: e.tensor_tensor(out=G[:, 7, :], in0=G[:, 1, :], in1=G[:, cmr, :], op=ALU.add), reads=[bG], writes=[bG])
                c.op("act", lambda e: e.activation(out=G[:, 7, :], in_=G[:, 7, :], func=AF.Exp, scale=-1.0), reads=[bG], writes=[bG])
                c.op("dve", lambda e: e.tensor_copy(out=s8[:, 0:1], in_=G[:, cmr, endc:endc + 1]), reads=[bG], writes=[bs8])
                c.op("dve", lambda e: e.tensor_scalar(out=s8[:, 1:2], in0=s8[:, 0:1], scalar1=-1.0, scalar2=None, op0=ALU.mult), reads=[bs8], writes=[bs8])
                c.op("dve", lambda e: e.tensor_copy(out=s8[:, 2:3], in_=G[:, 1, endc:endc + 1]), reads=[bG], writes=[bs8])
                c.op("act", lambda e: e.activation(out=G[:, 8, :], in_=G[:, 2, :], func=AF.Exp, bias=s8[:, 1:2]), reads=[bG, bs8], writes=[bG])
                c.op("act", lambda e: e.activation(out=s8[:, 3:4], in_=ms[:, 0:1], func=AF.Exp, bias=s8[:, 1:2]), reads=[b_ms, bs8], writes=[bs8])
                c.op("dve", lambda e: e.tensor_tensor(out=ms[:, 0:1], in0=s8[:, 2:3], in1=s8[:, 0:1], op=ALU.add), reads=[bs8, bG], writes=[b_ms])
                p1, bp1 = self.ps[1], self.bps[1]
                c.mm_multi([(p1[:, (r - 6) * 8:(r - 5) * 8], [(G[:, r, :], id8)]) for r in (6, 7, 8)], reads=[bG, self.b_const], writes=[bp1])
                c.op("act", lambda e: e.activation(out=t_[:, 8:32], in_=p1[:, 0:24], func=AF.Copy), reads=[bp1], writes=[bt])
                c.op("dve", lambda e: e.tensor_scalar(out=dg[:], in0=id8, scalar1=s8[:, 3:4], scalar2=None, op0=ALU.mult), reads=[self.b_const, bs8], writes=[b_dg])
                c.mm(p1[0:64, 32:40], [(self.ones32[0:8, 0:64], dg[:])], reads=[self.b_const, b_dg], writes=[bp1])
                c.op("act", lambda e: e.activation(out=cwc[i][:], in_=p1[0:64, 32:40], func=AF.Copy), reads=[bp1], writes=[b_cwc[i]])
            def heads(ch, i, nxt):
                if nxt is not None:
                    gate_load(*nxt)
                bl = b_ld[i]; G, bG = GM[i], b_GM[i]; t_, bt = tk[i], b_tk[i]
                Y, bY = Yt[i], b_Yt[i]
                for hb0 in (0, 4):
                    if hb0 == 4 and nxt is not None:
                        gate(*nxt)
                    psc, bpsc = self.ps[2], self.bps[2]
                    pbc, bpbc = self.ps[6], self.bps[6]
                    for q4 in range(4):
                        h = hb0 + q4
                        cs4 = slice(q4 * 128, (q4 + 1) * 128)
                        c.mm(psc[:, cs4], [(kTs[i][:, h, :], qTs[i][:, h, :])], reads=[bl], writes=[bpsc])
                        c.mm(pbc[:, cs4], [(sel[:, h, :], G[:, 5, :]), (G[:, 2, :], sel[:, h, :])], reads=[b_sel, bG], writes=[bpbc])
                    pins = []
                    for q4 in range(4):
                        h = hb0 + q4
                        bk = 3 if q4 < 2 else 7
                        pin_ap = self.ps[bk][:, (q4 % 2) * 256:(q4 % 2) * 256 + 129]
                        c.mm(pin_ap, [(qTs[i][:, h, :], Sb[:, h, :])], reads=[bl, b_S[h]], writes=[self.bps[bk]])
                        pins.append((pin_ap, self.bps[bk]))
                    k4 = (hb0 // 4)
                    c.op("dve", lambda e: e.tensor_tensor(out=scm[k4][:].rearrange("p (h t) -> p h t", h=4), in0=psc[:, :].rearrange("p (h t) -> p h t", h=4),
                                                          in1=tri.unsqueeze(1).to_broadcast([128, 4, 128]), op=ALU.mult), reads=[bpsc, self.b_const], writes=[b_scm[k4]])
                    c.op("dve", lambda e: e.tensor_scalar(out=aa[k4][:], in0=pbc[:, :], scalar1=0.0, scalar2=None, op0=ALU.min), reads=[bpbc], writes=[b_aa[k4]])
                    c.op("act", lambda e: e.activation(out=EE[k4][:], in_=aa[k4][:], func=AF.Exp), reads=[b_aa[k4]], writes=[b_EE[k4]])
                    c.op("pool", lambda e: e.tensor_tensor(out=MT[k4][:], in0=scm[k4][:], in1=EE[k4][:], op=ALU.mult), reads=[b_scm[k4], b_EE[k4]], writes=[b_MT[k4]])
                    for q4 in range(4):
                        h = hb0 + q4
                        pin_ap, bpin = pins[q4]
                        c.op("act", lambda e: e.activation(out=yi[q4][:], in_=pin_ap, func=AF.Copy, scale=t_[:, 8 + h:9 + h]), reads=[bpin, bt], writes=[b_yi[q4]])
                        c.op("pool", lambda e: e.tensor_tensor(out=Vw[q4][:], in0=vas[i][:, h, :], in1=t_[:, 24 + h:25 + h].to_broadcast([128, 129]), op=ALU.mult), reads=[bl, bt], writes=[b_Vw[q4]])
                    pnds = []; psts = []
                    for q4 in range(4):
                        h = hb0 + q4
                        bk = 4 + q4 // 2
                        pnd_ap = self.ps[bk][:, (q4 % 2) * 256:(q4 % 2) * 256 + 129]
                        c.mm(pnd_ap, [(MT[k4][:, q4 * 128:(q4 + 1) * 128], vas[i][:, h, :])], reads=[b_MT[k4], bl], writes=[self.bps[bk]])
                        pnds.append((pnd_ap, self.bps[bk]))
                    for q4 in range(4):
                        h = hb0 + q4
                        if q4 < 3:
                            pst_ap, bpst = self.ps[1][0:64, q4 * 129:(q4 + 1) * 129], self.bps[1]
                        else:
                            pst_ap, bpst = self.ps[0][0:64, 264:393], self.bps[0]
                        c.mm(pst_ap, [(kts[i][:, h * 64:(h + 1) * 64], Vw[q4][:])], reads=[bl, b_Vw[q4]], writes=[bpst])
                        psts.append((pst_ap, bpst))
                    n4 = nd4[k4]; bn = b_nd4[k4]
                    for q4 in range(4):
                        pnd_ap, bpnd = pnds[q4]
                        c.op("dve", lambda e: e.tensor_tensor(out=n4[:, q4, 0:129], in0=pnd_ap, in1=yi[q4][:], op=ALU.add), reads=[bpnd, b_yi[q4]], writes=[bn])
                    c.op("dve", lambda e: e.tensor_scalar(out=n4[:, :, 129:130], in0=n4[:, :, 128:129], scalar1=-1.0, scalar2=None, op0=ALU.mult), reads=[bn], writes=[bn])
                    c.op("dve", lambda e: e.tensor_tensor(out=n4[:, :, 130:131], in0=n4[:, :, 128:129], in1=n4[:, :, 129:130], op=ALU.max), reads=[bn], writes=[bn])
                    c.op("dve", lambda e: e.tensor_tensor(out=n4[:, :, 130:131], in0=n4[:, :, 130:131], in1=t_[:, 16 + hb0:20 + hb0].unsqueeze(2), op=ALU.max), reads=[bn, bt], writes=[bn])
                    c.op("dve", lambda e: e.reciprocal(out=n4[:, :, 131:132], in_=n4[:, :, 130:131]), reads=[bn], writes=[bn])
                    c.op("dve", lambda e: e.tensor_tensor(out=Y[:, hb0 * 128:(hb0 + 4) * 128].rearrange("p (h d) -> p h d", h=4), in0=n4[:, :, 0:128],
                                                          in1=n4[:, :, 131:132].to_broadcast([128, 4, 128]), op=ALU.mult), reads=[bn], writes=[bY])
                    for q4 in range(4):
                        h = hb0 + q4
                        pst_ap, bpst = psts[q4]
                        c.op("dve", lambda e: e.scalar_tensor_tensor(out=S32[:, h, :], in0=S32[:, h, :], scalar=cwc[i][:, h:h + 1], in1=pst_ap, op0=ALU.mult, op1=ALU.add), reads=[b_cwc[i], bpst], writes=[b_S[h]])
                        c.op("act", lambda e: e.activation(out=Sb[:, h, :], in_=S32[:, h, :], func=AF.Copy), reads=[], writes=[b_S[h]])
                c.dma("sp", Yd[d].ap()[ch], Y[:], reads=[bY], writes=[b_Y[d]])
            gate_load(order[0], 0)
            gate(order[0], 0)
            for idx, ch in enumerate(order):
                heads(ch, idx % 2, (order[idx + 1], (idx + 1) % 2) if idx + 1 < len(order) else None)
        c.barrier()
        c.sb_release(self.mark0)
        Wo = c.sb([128, 8, 1024], BF16, "mlWo"); b_wo = Buf()
        c.dma("pool", Wo[:], self.w["mlstm_w_out"].ap()[j].rearrange("(k p) n -> p k n", p=128), writes=[b_wo])
        ng = c.sb([128, 1024], F32, "mlng")
        c.dma("sp", ng[:], self.w["mlstm_norm_g"].ap()[j:j + 1, :].partition_broadcast(128), writes=[b_wo])
        self.gated_out(li, need_ctx, Yd, b_Y, Os, b_Os, 1024, 8, ng, Wo, b_wo, True)
        self.phase_end()

    def build(self, wshapes):
        self.inp("x", [self.NL, D])
        self.inp("ctx", [self.NCX, D])
        self.inp("cT", [128, 8, 2])
        self.inp("cmat", [128, 4, 128])
        self.inp("sel32", [32, 32, 128])
        self.inp("rope64", [2, 64, self.NL])
        self.inp("rope32", [2, 32, self.NL])
        self.inp("rope96", [2, 96, self.NL])
        self.inp("ssm_conv_wT", [wshapes["ssm_conv_w"][0], 128, 24, 5])
        self.inp("ssm_conv_bT", [wshapes["ssm_conv_w"][0], 128, 24])
        for k, s in wshapes.items():
            self.inp(k, s)
        self.out = self.nc.dram_tensor("out", [self.NL, D], F32, kind="ExternalOutput")
        self.setup_consts()
        self.prologue()
        cnt = {0: 0, 1: 0, 2: 0, 3: 0, 9: 0}
        for li, kind in enumerate(self.kinds):
            need_ctx = li < self.depth - 1
            j = cnt[kind]
            cnt[kind] += 1
            self.want_precast = li
            if kind == 0:
                self.layer_gqa(li, j, need_ctx)
            elif kind == 1:
                self.layer_ssd(li, j, need_ctx)
            elif kind == 2:
                self.layer_mlstm(li, j, need_ctx)
            elif kind == 3:
                self.layer_mla(li, j, need_ctx)
            self.layer_moe(li, need_ctx)
        self.final()
        return self.nc


WEIGHT_KEYS = ["norm1_g", "norm2_g", "w_mod", "b_mod", "moe_w_group", "moe_b_group", "moe_w_expert", "moe_b_expert",
               "moe_w_gate", "moe_w_up", "moe_w_down", "attn_w_in", "attn_sink", "attn_w_out",
               "ssm_w_in", "ssm_conv_w", "ssm_conv_b", "ssm_dt_bias", "ssm_a_log", "ssm_d", "ssm_norm_g", "ssm_w_out",
               "mlstm_w_in", "mlstm_gate_b", "mlstm_norm_g", "mlstm_w_out",
               "mla_w_in", "mla_q_norm_g", "mla_w_q_up", "mla_kv_norm_g", "mla_w_kv_up", "mla_w_out", "final_norm_g"]


def run_model(inputs, kinds, n_cores=None):
    x = np.asarray(inputs["x"], np.float32)
    ctx = np.asarray(inputs["ctx"], np.float32)
    c = np.asarray(inputs["c"], np.float32)
    c_ctx = np.asarray(inputs["c_ctx"], np.float32)
    B, n_lat, _ = x.shape
    n_ctx = ctx.shape[1]
    weights = {k: np.ascontiguousarray(np.asarray(inputs[k], np.float32)) for k in WEIGHT_KEYS}
    m = Model(n_lat, n_ctx, kinds)
    nc = m.build({k: v.shape for k, v in weights.items()})
    consts = host_consts(n_lat)
    in_maps = []
    for b in range(B):
        cT = np.stack([c[b].reshape(8, 128).T, c_ctx.reshape(8, 128).T], axis=-1)
        d = {"x": np.ascontiguousarray(x[b]), "ctx": np.ascontiguousarray(ctx[b]), "cT": np.ascontiguousarray(cT.astype(np.float32))}
        d.update(consts)
        d.update(weights)
        d["ssm_conv_wT"] = np.ascontiguousarray(weights["ssm_conv_w"].reshape(-1, 5, 24, 128).transpose(0, 3, 2, 1))
        d["ssm_conv_bT"] = np.ascontiguousarray(weights["ssm_conv_b"].reshape(-1, 24, 128).transpose(0, 2, 1))
        in_maps.append(d)
    res = run_bass_kernel_spmd(nc, in_maps, core_ids=list(range(B)))
    return np.stack([np.asarray(r["out"], np.float32) for r in res.results], axis=0)


def kernel(**inputs):
    return run_model(inputs, [0, 1, 2, 3])
```

```python
import numpy as np
import concourse.bass as bass
import concourse.mybir as mybir

F32 = mybir.dt.float32
BF16 = mybir.dt.bfloat16
AF = mybir.ActivationFunctionType
ALU = mybir.AluOpType
AX = mybir.AxisListType


class Buf:
    __slots__ = ("w", "r", "name")

    def __init__(self, name=""):
        self.w = None
        self.r = []
        self.name = name


class Ctx:
    EPOCH = 30000

    def __init__(self, nc, n_dma=None):
        self.nc = nc
        self.E = {"pe": nc.tensor, "act": nc.scalar, "dve": nc.vector, "pool": nc.gpsimd, "sp": nc.sync}
        self.csem = {}
        self.seen = {e: {} for e in self.E}
        self.semid = {}
        n_dma = n_dma or {"sp": 48, "act": 2, "pool": 30}
        self.dslots = {}
        self.drr = {}
        for q, n in n_dma.items():
            self.dslots[q] = [[self._new_sem(f"d{q}{i}"), 0] for i in range(n)]
            self.drr[q] = 0
        for e in ("pe", "act", "dve", "pool"):
            self.csem[e] = [self._new_sem(f"c{e}0"), 0, 0]
        self.sb_off = 0
        self.sb_base = 16512
        self.sb_cap = 229344 - 16512
        self.n_alloc = 0
        self.n_ins = 0
        self.n_wait = 0

    def _new_sem(self, name):
        s = self.nc.alloc_semaphore(name)
        self.semid[id(s)] = s
        return s

    def sb_mark(self):
        return self.sb_off

    def sb_release(self, mark):
        self.sb_off = mark

    def sb(self, shape, dtype, name=None):
        esz = 4 if dtype == F32 else 2
        if dtype in (mybir.dt.int32, mybir.dt.uint32):
            esz = 4
        n = 1
        for s in shape[1:]:
            n *= s
        nbytes = (n * esz + 63) // 64 * 64
        off = self.sb_off
        if off + nbytes > self.sb_cap:
            raise RuntimeError(f"SBUF overflow: want {nbytes} at {off} cap {self.sb_cap} ({name})")
        self.sb_off += nbytes
        self.n_alloc += 1
        t = self.nc.alloc_sbuf_tensor_at(f"{name or 't'}_{self.n_alloc}", list(shape), dtype, offset=self._abs(off))
        return t

    def _abs(self, off):
        return self.sb_base + off

    def _wait(self, eng, ev):
        if ev is None:
            return
        sem, val = ev
        k = id(sem)
        if self.seen[eng].get(k, 0) >= val:
            return
        self.E[eng].wait_ge(sem, val)
        self.n_wait += 1
        self.seen[eng][k] = val

    def _deps(self, eng, reads, writes):
        for b in reads:
            if b.w is not None and not (eng == "pe" and b.w[2] == "pe"):
                self._wait(eng, b.w[:2])
        for b in writes:
            if b.w is not None and not (eng == "pe" and b.w[2] == "pe"):
                self._wait(eng, b.w[:2])
            for r in b.r:
                if not (eng == "pe" and r[2] == "pe"):
                    self._wait(eng, r[:2])

    def _record(self, ev, reads, writes):
        for b in reads:
            b.r.append(ev)
            if len(b.r) > 64:
                b.r = b.r[-64:] if False else b.r
        for b in writes:
            b.w = ev
            b.r = []

    def _signal(self, eng, ins):
        st = self.csem[eng]
        if st[1] >= self.EPOCH:
            st = self.csem[eng] = [self._new_sem(f"c{eng}{st[2] + 1}"), 0, st[2] + 1]
        st[1] += 1
        ins.then_inc(st[0], 1)
        return (st[0], st[1], eng)

    def op(self, eng, fn, reads=(), writes=()):
        self._deps(eng, reads, writes)
        ins = fn(self.E[eng])
        self.n_ins += 1
        ev = self._signal(eng, ins)
        self._record(ev, reads, writes)
        return ev

    def mm(self, out, pairs, reads=(), writes=(), start=True, stop=True):
        self._deps("pe", reads, writes)
        n = len(pairs)
        ins = None
        for i, (l, r) in enumerate(pairs):
            ins = self.nc.tensor.matmul(out, l, r, start=(start and i == 0), stop=(stop and i == n - 1))
            self.n_ins += 1
        ev = self._signal("pe", ins)
        self._record(ev, reads, writes)
        return ev

    def mm_multi(self, groups, reads=(), writes=()):
        self._deps("pe", reads, writes)
        ins = None
        for out, pairs in groups:
            n = len(pairs)
            for i, (l, r) in enumerate(pairs):
                ins = self.nc.tensor.matmul(out, l, r, start=(i == 0), stop=(i == n - 1))
                self.n_ins += 1
        ev = self._signal("pe", ins)
        self._record(ev, reads, writes)
        return ev

    def tr(self, out, in_, ident, reads=(), writes=()):
        self._deps("pe", reads, writes)
        ins = self.nc.tensor.transpose(out, in_, ident)
        self.n_ins += 1
        ev = self._signal("pe", ins)
        self._record(ev, reads, writes)
        return ev

    def tr_multi(self, items, reads=(), writes=()):
        self._deps("pe", reads, writes)
        ins = None
        for out, in_, ident in items:
            ins = self.nc.tensor.transpose(out, in_, ident)
            self.n_ins += 1
        ev = self._signal("pe", ins)
        self._record(ev, reads, writes)
        return ev

    def dma(self, q, out, in_, reads=(), writes=()):
        self._deps(q, reads, writes)
        slots = self.dslots[q]
        i = self.drr[q]
        self.drr[q] = (i + 1) % len(slots)
        sl = slots[i]
        if sl[1] > 0:
            self._wait(q, (sl[0], sl[1]))
        ins = self.E[q].dma_start(out=out, in_=in_)
        self.n_ins += 1
        sl[1] += 16
        ins.then_inc(sl[0], 16)
        ev = (sl[0], sl[1], "dma")
        self._record(ev, reads, writes)
        return ev

    def barrier(self):
        evs = []
        for e, st in self.csem.items():
            if st[1] > 0:
                evs.append((st[0], st[1]))
        for q, slots in self.dslots.items():
            for sl in slots:
                if sl[1] > 0:
                    evs.append((sl[0], sl[1]))
        for eng in self.E:
            for ev in evs:
                self._wait(eng, ev)

    def finish(self, eng="sp"):
        evs = []
        for e, st in self.csem.items():
            if st[1] > 0:
                evs.append((st[0], st[1]))
        for q, slots in self.dslots.items():
            for sl in slots:
                if sl[1] > 0:
                    evs.append((sl[0], sl[1]))
        for ev in evs:
            self._wait(eng, ev)
from concourse.bass_utils import run_bass_kernel_spmd
D = 1024
EPS = 1e-6


def host_consts(n_lat):
    ident = np.eye(128, dtype=np.float32)
    ones = np.ones((128, 128), np.float32)
    j = np.arange(128)[:, None]
    i = np.arange(128)[None, :]
    tri_le = (j <= i).astype(np.float32)
    tri_ge = (j >= i).astype(np.float32)
    cm = np.stack([ident, ones, tri_le, tri_ge], axis=1)
    sel = np.zeros((32, 32, 128), np.float32)
    for h in range(32):
        sel[h, h, :] = 1.0
    rows = n_lat // 64
    row = np.repeat(np.arange(rows), 64).astype(np.float32)
    col = np.tile(np.arange(64), rows).astype(np.float32)

    def tab(rot_dim, nrep):
        q = rot_dim // 4
        inv = (10000.0 ** (-np.arange(q, dtype=np.float32) / q)).astype(np.float32)
        ang = np.concatenate([row[:, None] * inv, col[:, None] * inv], axis=-1).astype(np.float32)
        cs = np.cos(ang).astype(np.float32).T
        sn = np.sin(ang).astype(np.float32).T
        cs = np.concatenate([cs] * (2 * nrep), axis=0)
        sn = np.concatenate([sn] * (2 * nrep), axis=0)
        return np.ascontiguousarray(np.stack([cs, sn], axis=0))

    r32 = tab(32, 1)
    L = r32.shape[2]
    r96 = np.ascontiguousarray(np.concatenate([np.stack([np.ones((64, L), np.float32), np.zeros((64, L), np.float32)], axis=0), r32], axis=1))
    return {"cmat": np.ascontiguousarray(cm), "sel32": sel, "rope64": tab(64, 1), "rope32": r32, "rope96": r96}


class Model:
    def __init__(self, n_lat, n_ctx, kinds, debug=False):
        self.NL, self.NCX = n_lat, n_ctx
        self.T = n_lat + n_ctx
        self.NT = self.T // 128
        self.NCT = n_ctx // 128
        self.NLT = n_lat // 128
        self.kinds = kinds
        self.depth = len(kinds)
        self.nc = bass.Bass("TRN2", target_bir_lowering=False)
        self.c = Ctx(self.nc)
        self.w = {}

    def inp(self, name, shape, dtype=F32):
        t = self.nc.dram_tensor(name, list(shape), dtype, kind="ExternalInput")
        self.w[name] = t
        return t

    def scratch(self, name, shape, dtype):
        return self.nc.dram_tensor(name, list(shape), dtype)

    def declare(self, shapes):
        for k, s in shapes.items():
            self.inp(k, s)

    def groups(self, gsz, with_ctx=True):
        g = []
        if with_ctx:
            g.append((0, self.NCT, True))
        t = self.NCT
        while t < self.NT:
            n = min(gsz, self.NT - t)
            g.append((t, n, False))
            t += n
        return g

    def setup_consts(self):
        c, nc = self.c, self.nc
        self.cm32 = c.sb([128, 4, 128], F32, "cm32")
        self.cmb = c.sb([128, 4, 128], BF16, "cmb")
        self.b_const = Buf("const")
        c.dma("sp", self.cm32[:], self.w["cmat"].ap(), writes=[self.b_const])
        c.op("dve", lambda e: e.tensor_copy(out=self.cmb[:], in_=self.cm32[:]), reads=[self.b_const], writes=[self.b_const])
        self.ident32 = self.cm32[:, 0, :]
        self.ones32 = self.cm32[:, 1, :]
        self.trile32 = self.cm32[:, 2, :]
        self.trige32 = self.cm32[:, 3, :]
        self.identb = self.cmb[:, 0, :]
        self.onesb = self.cmb[:, 1, :]
        self.trileb = self.cmb[:, 2, :]
        self.trigeb = self.cmb[:, 3, :]
        self.ps = [nc.alloc_psum_tensor(f"psb{i}", [128, 512], F32) for i in range(8)]
        self.bps = [Buf(f"ps{i}") for i in range(8)]
        self.mark0 = c.sb_mark()

    def phase_end(self):
        self.c.barrier()
        self.c.sb_release(self.mark0)

    def prologue(self):
        c, nc = self.c, self.nc
        L = self.depth
        self.modv = self.scratch("modv", [L, 2, 6 * D], F32)
        self.xs = self.scratch("xs", [self.T, D], F32)
        cs = c.sb([128, 8, 2], F32, "cs")
        b_cs = Buf()
        c.dma("sp", cs[:], self.w["cT"].ap(), writes=[b_cs])
        c.op("act", lambda e: e.activation(out=cs[:], in_=cs[:], func=AF.Silu), reads=[b_cs], writes=[b_cs])
        b_xs = self.b_xs = [Buf(f"xs{t}") for t in range(self.NT)]
        c.dma("sp", self.xs.ap()[0:self.NCX, :], self.w["ctx"].ap(), writes=b_xs[0:self.NCT])
        c.dma("sp", self.xs.ap()[self.NCX:self.T, :], self.w["x"].ap(), writes=b_xs[self.NCT:])
        wm = [c.sb([128, 8, 512], F32, f"wm{i}") for i in range(2)]
        b_wm = [Buf(), Buf()]
        modsb = c.sb([2, 6 * D], F32, "modsb")
        b_mod = Buf()
        bmb = c.sb([2, 6 * D], F32, "bmb")
        gb = c.sb([2, 2, D], F32, "gb")
        b_misc = Buf()
        self.b_modv = [Buf(f"modv{i}") for i in range(L)]
        it = 0
        for li in range(L):
            c.dma("sp", bmb[:], self.w["b_mod"].ap()[li:li + 1, :].partition_broadcast(2), writes=[b_misc])
            c.dma("sp", gb[:, 0, :], self.w["norm1_g"].ap()[li:li + 1, :].partition_broadcast(2), writes=[b_misc])
            c.dma("sp", gb[:, 1, :], self.w["norm2_g"].ap()[li:li + 1, :].partition_broadcast(2), writes=[b_misc])
            for j in range(12):
                s = it % 2
                it += 1
                c.dma("sp", wm[s][:], self.w["w_mod"].ap()[li, :, j * 512:(j + 1) * 512].rearrange("(k p) n -> p k n", p=128), writes=[b_wm[s]])
                pb = it % 2
                c.mm(self.ps[pb][0:2, :], [(cs[:, k, :], wm[s][:, k, :]) for k in range(8)], reads=[b_cs, b_wm[s]], writes=[self.bps[pb]])
                c.op("dve", lambda e: e.tensor_tensor(out=modsb[:, j * 512:(j + 1) * 512], in0=self.ps[pb][0:2, :], in1=bmb[:, j * 512:(j + 1) * 512], op=ALU.add),
                     reads=[self.bps[pb], b_misc], writes=[b_mod])
            for (ch, gi) in ((1, 0), (4, 1)):
                c.op("dve", lambda e: e.scalar_tensor_tensor(out=modsb[:, ch * D:(ch + 1) * D], in0=modsb[:, ch * D:(ch + 1) * D], scalar=1.0, in1=gb[:, gi, :], op0=ALU.add, op1=ALU.mult),
                     reads=[b_mod, b_misc], writes=[b_mod])
            c.dma("sp", self.modv.ap()[li], modsb[:], reads=[b_mod], writes=[self.b_modv[li]])
        self.phase_end()

    def load_mod(self, li, chunk, row, name):
        c = self.c
        t = c.sb([128, D], F32, name)
        b = Buf(name)
        c.dma("sp", t[:], self.modv.ap()[li, row:row + 1, chunk * D:(chunk + 1) * D].partition_broadcast(128), reads=[self.b_modv[li]], writes=[b])
        return t, b

    def set_norm_order(self, tiles):
        self.norm_next = {tiles[k]: tiles[k + 1] for k in range(len(tiles) - 1)}
        self.npref = None

    def alloc_norm_bufs(self, nbuf=2):
        c = self.c
        self.norm_next = {}
        self.npref = None
        if getattr(self, "want_precast", None) is not None:
            li_ = self.want_precast
            self.want_precast = None
            self.moe_precast(li_)
        self.nb = []
        for i in range(nbuf):
            d = dict(x=c.sb([128, D], F32, "nx"), bx=Buf(), junk=c.sb([128, D], BF16, "nj"), bj=Buf(),
                     st=c.sb([128, 2], F32, "nst"), bst=Buf(), tmp=c.sb([128, D], F32, "ntmp"), btmp=Buf(),
                     h=c.sb([128, D], BF16, "nh"), bh=Buf())
            self.nb.append(d)
        self.nbi = 0

    def norm_tile(self, tile, A, bA, B, bB, hT, b_hT, col0, psb, want_x=False):
        c = self.c
        d = self.nb[self.nbi % len(self.nb)]
        self.nbi += 1
        if getattr(self, "npref", None) == tile:
            self.npref = None
        else:
            c.dma("sp", d["x"][:], self.xs.ap()[tile * 128:(tile + 1) * 128, :], reads=[self.b_xs[tile]], writes=[d["bx"]])
        nxt = self.norm_next.get(tile) if getattr(self, "norm_next", None) else None
        if nxt is not None:
            d2 = self.nb[self.nbi % len(self.nb)]
            c.dma("sp", d2["x"][:], self.xs.ap()[nxt * 128:(nxt + 1) * 128, :], reads=[self.b_xs[nxt]], writes=[d2["bx"]])
            self.npref = nxt
        c.op("dve", lambda e: e.memset(d["st"][:], 0.0), writes=[d["bst"]])
        c.op("act", lambda e: e.activation(out=d["junk"][:], in_=d["x"][:], func=AF.Square, accum_out=d["st"][:, 0:1]), reads=[d["bx"]], writes=[d["bj"], d["bst"]])
        c.op("dve", lambda e: e.tensor_scalar(out=d["st"][:, 1:2], in0=d["st"][:, 0:1], scalar1=1.0 / D, scalar2=EPS, op0=ALU.mult, op1=ALU.add), reads=[d["bst"]], writes=[d["bst"]])
        c.op("act", lambda e: e.activation(out=d["st"][:, 1:2], in_=d["st"][:, 1:2], func=AF.Sqrt), reads=[d["bst"]], writes=[d["bst"]])
        c.op("dve", lambda e: e.reciprocal(out=d["st"][:, 1:2], in_=d["st"][:, 1:2]), reads=[d["bst"]], writes=[d["bst"]])
        c.op("dve", lambda e: e.scalar_tensor_tensor(out=d["tmp"][:], in0=d["x"][:], scalar=d["st"][:, 1:2], in1=A[:], op0=ALU.mult, op1=ALU.mult),
             reads=[d["bx"], d["bst"], bA], writes=[d["btmp"]])
        c.op("pool", lambda e: e.tensor_tensor(out=d["h"][:], in0=d["tmp"][:], in1=B[:], op=ALU.add), reads=[d["btmp"], bB], writes=[d["bh"]])
        pT = self.ps[psb].ap().bitcast(BF16)
        c.tr_multi([(pT[:, k * 128:(k + 1) * 128], d["h"][:, k * 128:(k + 1) * 128], self.identb) for k in range(8)],
                   reads=[d["bh"], self.b_const], writes=[self.bps[psb]])
        c.op("act", lambda e: e.activation(out=hT[:, :, col0:col0 + 128], in_=pT.rearrange("p (k n) -> p k n", k=8), func=AF.Copy),
             reads=[self.bps[psb]], writes=[b_hT])
        return d

    def layer_gqa(self, li, j, need_ctx):
        c, nc = self.c, self.nc
        T, NT, NCT = self.T, self.NT, self.NCT
        w_in = self.w["attn_w_in"].ap()[j]
        w_out = self.w["attn_w_out"].ap()[j]
        QT = self.scratch(f"gqa_qt{li}", [4, NT, 64, 4, 128], BF16)
        b_QT = [Buf() for _ in range(NT)]
        b_w = Buf("gqa_w")
        Wq = c.sb([128, 8, 1024], BF16, "Wq")
        Wqr = c.sb([128, 8, 1024], BF16, "Wqr")
        Wk = c.sb([128, 8, 256], BF16, "Wk")
        Wkr = c.sb([128, 8, 256], BF16, "Wkr")
        Wv = c.sb([128, 8, 256], BF16, "Wv")
        c.dma("pool", Wq[:], w_in[:, 0:1024].rearrange("(k p) n -> p k n", p=128), writes=[b_w])
        c.dma("pool", Wk[:], w_in[:, 1024:1280].rearrange("(k p) n -> p k n", p=128), writes=[b_w])
        c.dma("pool", Wv[:], w_in[:, 1280:1536].rearrange("(k p) n -> p k n", p=128), writes=[b_w])
        b_wr = Buf("gqa_wr")
        for (W, Wr, nh) in ((Wq, Wqr, 16), (Wk, Wkr, 4)):
            for k in range(8):
                src = W[:, k, :].rearrange("p (h two i) -> p h two i", two=2, i=32)
                dst = Wr[:, k, :].rearrange("p (h two i) -> p h two i", two=2, i=32)
                c.op("act", lambda e: e.activation(out=dst[:, :, 0, :], in_=src[:, :, 1, :], func=AF.Copy, scale=-1.0), reads=[b_w], writes=[b_wr])
                c.op("dve", lambda e: e.tensor_copy(out=dst[:, :, 1, :], in_=src[:, :, 0, :]), reads=[b_w], writes=[b_wr])
        KT = c.sb([64, 4, T], BF16, "KT")
        b_KT = Buf("KT")
        Vs = c.sb([128, NT, 4, 65], BF16, "Vs")
        b_V = Buf("Vs")
        c.op("pool", lambda e: e.memset(Vs[:], 1.0), writes=[b_V])
        mark = c.sb_mark()
        A1x, bA1x = self.load_mod(li, 1, 0, "A1x")
        S1x, bS1x = self.load_mod(li, 0, 0, "S1x")
        A1c, bA1c = self.load_mod(li, 1, 1, "A1c")
        S1c, bS1c = self.load_mod(li, 0, 1, "S1c")
        self.alloc_norm_bufs(2)
        self.set_norm_order([t0_ + s_ for (t0_, n_, _c) in self.groups(4) for s_ in range(n_)])
        hTs = [c.sb([128, 8, 512], BF16, "hT") for _ in range(2)]
        b_hT = [Buf(), Buf()]
        rts = [c.sb([64, 2, 512], F32, "rt") for _ in range(2)]
        b_rt = [Buf(), Buf()]
        qst = [c.sb([64, 4, 512], BF16, "qst") for _ in range(2)]
        b_qst = [Buf(), Buf()]
        t1s = [c.sb([64, 512], F32, "t1") for _ in range(2)]
        t2s = [c.sb([64, 512], F32, "t2") for _ in range(2)]
        b_t1 = [Buf(), Buf()]
        b_t2 = [Buf(), Buf()]
        cnt = 0
        qcnt = 0
        for gi, (t0, n, is_ctx) in enumerate(self.groups(4)):
            ncols = n * 128
            hT, bh = hTs[gi % 2], b_hT[gi % 2]
            rt, brt = rts[gi % 2], b_rt[gi % 2]
            for s in range(n):
                if is_ctx:
                    self.norm_tile(t0 + s, A1c, bA1c, S1c, bS1c, hT, bh, s * 128, (t0 + s) % 2)
                else:
                    self.norm_tile(t0 + s, A1x, bA1x, S1x, bS1x, hT, bh, s * 128, (t0 + s) % 2)
            if not is_ctx:
                l0 = (t0 - NCT) * 128
                c.dma("sp", rt[:, :, :ncols], self.w["rope64"].ap()[:, :, l0:l0 + ncols].rearrange("two d l -> d two l"), writes=[brt])

            def proj_head(W, Wr, col, dst_ap, dst_buf):
                nonlocal cnt
                i = cnt % 2
                cnt += 1
                P1, bP1 = self.ps[2 + i], self.bps[2 + i]
                P2, bP2 = self.ps[4 + i], self.bps[4 + i]
                c.mm(P1[0:64, :ncols], [(W[:, k, col:col + 64], hT[:, k, :ncols]) for k in range(8)], reads=[b_w, bh], writes=[bP1])
                if is_ctx:
                    c.op("act", lambda e: e.activation(out=dst_ap, in_=P1[0:64, :ncols], func=AF.Copy), reads=[bP1], writes=[dst_buf])
                else:
                    c.mm(P2[0:64, :ncols], [(Wr[:, k, col:col + 64], hT[:, k, :ncols]) for k in range(8)], reads=[b_wr, bh], writes=[bP2])
                    c.op("dve", lambda e: e.tensor_tensor(out=t1s[i][:, :ncols], in0=P1[0:64, :ncols], in1=rt[:, 0, :ncols], op=ALU.mult), reads=[bP1, brt], writes=[b_t1[i]])
                    c.op("dve", lambda e: e.tensor_tensor(out=t2s[i][:, :ncols], in0=P2[0:64, :ncols], in1=rt[:, 1, :ncols], op=ALU.mult), reads=[bP2, brt], writes=[b_t2[i]])
                    c.op("pool", lambda e: e.tensor_tensor(out=dst_ap, in0=t1s[i][:, :ncols], in1=t2s[i][:, :ncols], op=ALU.add), reads=[b_t1[i], b_t2[i]], writes=[dst_buf])

            for kvh in range(4):
                qs, bq = qst[qcnt % 2], b_qst[qcnt % 2]
                qcnt += 1
                for g in range(4):
                    proj_head(Wq, Wqr, (kvh * 4 + g) * 64, qs[:, g, :ncols], bq)
                for qb_ in range(n):
                    c.dma("sp", QT.ap()[kvh, t0 + qb_], qs[:, :, qb_ * 128:(qb_ + 1) * 128], reads=[bq], writes=[b_QT[t0 + qb_]])
                proj_head(Wk, Wkr, kvh * 64, KT[:, kvh, t0 * 128:t0 * 128 + ncols], b_KT)
            for s in range(n):
                i = cnt % 2
                cnt += 1
                Pv, bPv = self.ps[6 + i], self.bps[6 + i]
                c.mm(Pv[:, 0:256], [(hT[:, k, s * 128:(s + 1) * 128], Wv[:, k, :]) for k in range(8)], reads=[b_w, bh], writes=[bPv])
                c.op("act", lambda e: e.activation(out=Vs[:, t0 + s, :, 0:64], in_=Pv[:, 0:256].rearrange("p (h d) -> p h d", h=4), func=AF.Copy), reads=[bPv], writes=[b_V])
        c.barrier()
        c.sb_release(mark)
        Wo = c.sb([64, 16, 1024], BF16, "Wo")
        b_wo = Buf()
        c.dma("pool", Wo[:], w_out.rearrange("(h d) n -> d h n", d=64), writes=[b_wo])
        sk = c.sb([65, 16], F32, "sk")
        b_sk = Buf()
        sinkrow = c.sb([65, 16, 128], F32, "sinkrow")
        c.dma("sp", sk[64:65, :], self.w["attn_sink"].ap()[j:j + 1, :], writes=[b_sk])
        c.op("act", lambda e: e.activation(out=sk[64:65, :], in_=sk[64:65, :], func=AF.Exp), reads=[b_sk], writes=[b_sk])
        c.op("dve", lambda e: e.tensor_copy(out=sinkrow[64:65, :, :], in_=sk[64:65, :].unsqueeze(2).to_broadcast([1, 16, 128])), reads=[b_sk], writes=[b_sk])
        G1x, bG1x = self.load_mod(li, 2, 0, "G1x")
        G1c, bG1c = self.load_mod(li, 2, 1, "G1c")
        Qts = [c.sb([64, 512], BF16, "Qt") for _ in range(8)]
        b_Qt = [Buf() for _ in range(8)]
        PTs = [c.sb([128, 512], BF16, "PT") for _ in range(4)]
        b_PT = [Buf() for _ in range(4)]
        osb = [c.sb([65, 512], F32, "osb") for _ in range(2)]
        b_osb = [Buf(), Buf()]
        rden = [c.sb([65, 512], F32, "rden") for _ in range(2)]
        b_rden = [Buf(), Buf()]
        yTs = [c.sb([64, 16, 128], BF16, "yT") for _ in range(2)]
        b_yT = [Buf(), Buf()]
        xts = [c.sb([128, D], F32, "xres") for _ in range(2)]
        b_xt = [Buf(), Buf()]
        tmps = [c.sb([128, D], F32, "rtmp") for _ in range(2)]
        b_tmp = [Buf(), Buf()]
        scale = 64 ** -0.5
        qi = 0
        pi = 0
        si = 0
        qbs = list(range(0 if need_ctx else NCT, NT))

        def ga_load(bi_):
            qb_ = qbs[bi_]
            c.dma("sp", xts[bi_ % 2][:], self.xs.ap()[qb_ * 128:(qb_ + 1) * 128, :], reads=[self.b_xs[qb_]], writes=[b_xt[bi_ % 2]])
            for kvh_ in range(4):
                q8 = (bi_ % 2) * 4 + kvh_
                c.dma("sp", Qts[q8][:], QT.ap()[kvh_, qb_].rearrange("d g t -> d (g t)"), reads=[b_QT[qb_]], writes=[b_Qt[q8]])
        ga_load(0)
        for bi, qb in enumerate(qbs):
            is_ctx = qb < NCT
            if bi + 1 < len(qbs):
                ga_load(bi + 1)
            if is_ctx:
                keys = [(t, None) for t in range(NCT)]
            else:
                keys = []
                if qb - 1 >= NCT:
                    keys.append((qb - 1, self.trigeb))
                keys.append((qb, None))
                if qb + 1 < NT:
                    keys.append((qb + 1, self.trileb))
                keys += [(t, None) for t in range(NCT)]
            xt, bxt = xts[bi % 2], b_xt[bi % 2]
            yT, byT = yTs[bi % 2], b_yT[bi % 2]
            for kvh in range(4):
                Qt, bQt = Qts[(bi % 2) * 4 + kvh], b_Qt[(bi % 2) * 4 + kvh]
                oT, boT = self.ps[2 + kvh % 2], self.bps[2 + kvh % 2]
                SB = (0, 1, 7)
                LA = 2

                def issue_S(ki_):
                    kt_ = keys[ki_][0]
                    bk_ = SB[(si + ki_) % len(SB)]
                    c.mm(self.ps[bk_][:, :], [(KT[:, kvh, kt_ * 128:(kt_ + 1) * 128], Qt[:])], reads=[b_KT, bQt], writes=[self.bps[bk_]])
                for k0 in range(min(LA, len(keys))):
                    issue_S(k0)
                for ki, (kt, mask) in enumerate(keys):
                    bk = SB[(si + ki) % len(SB)]
                    sT, bsT = self.ps[bk], self.bps[bk]
                    if ki + LA < len(keys):
                        issue_S(ki + LA)
                    PT, bPT = PTs[pi % 4], b_PT[pi % 4]
                    pi += 1
                    c.op("act", lambda e: e.activation(out=PT[:], in_=sT[:, :], func=AF.Exp, scale=scale), reads=[bsT], writes=[bPT])
                    if mask is not None:
                        c.op("dve", lambda e: e.tensor_tensor(out=PT[:].rearrange("p (g t) -> p g t", g=4), in0=PT[:].rearrange("p (g t) -> p g t", g=4),
                                                              in1=mask.unsqueeze(1).to_broadcast([128, 4, 128]), op=ALU.mult), reads=[bPT, self.b_const], writes=[bPT])
                    c.mm(oT[0:65, :], [(Vs[:, kt, kvh, :], PT[:])], reads=[b_V, bPT], writes=[boT], start=(ki == 0), stop=(ki == len(keys) - 1))
                si += len(keys)
                o, bo = osb[kvh % 2], b_osb[kvh % 2]
                rd, brd = rden[kvh % 2], b_rden[kvh % 2]
                c.op("act", lambda e: e.activation(out=o[:], in_=oT[0:65, :], func=AF.Copy), reads=[boT], writes=[bo])
                c.op("dve", lambda e: e.tensor_tensor(out=rd[64:65, :], in0=o[64:65, :], in1=sinkrow[64:65, kvh * 4:(kvh + 1) * 4, :].rearrange("p g t -> p (g t)"), op=ALU.add),
                     reads=[bo, b_sk], writes=[brd])
                c.mm(self.ps[4][0:64, :], [(self.ones32[64:65, 0:64], rd[64:65, :])], reads=[self.b_const, brd], writes=[self.bps[4]])
                c.op("dve", lambda e: e.reciprocal(out=rd[0:64, :], in_=self.ps[4][0:64, :]), reads=[self.bps[4], brd], writes=[brd])
                c.op("dve", lambda e: e.tensor_tensor(out=yT[:, kvh * 4:(kvh + 1) * 4, :].rearrange("d g t -> d (g t)"), in0=o[0:64, :], in1=rd[0:64, :], op=ALU.mult),
                     reads=[bo, brd], writes=[byT])
            G1, bG1 = (G1c, bG1c) if is_ctx else (G1x, bG1x)
            tmp, btmp = tmps[bi % 2], b_tmp[bi % 2]
            for nn in range(2):
                z, bz = self.ps[5 + nn], self.bps[5 + nn]
                c.mm(z[:, :], [(yT[:, hq, :], Wo[:, hq, nn * 512:(nn + 1) * 512]) for hq in range(16)], reads=[byT, b_wo], writes=[bz])
                c.op("dve", lambda e: e.tensor_tensor(out=tmp[:, nn * 512:(nn + 1) * 512], in0=z[:, :], in1=G1[:, nn * 512:(nn + 1) * 512], op=ALU.mult), reads=[bz, bG1], writes=[btmp])
            c.op("pool", lambda e: e.tensor_tensor(out=xt[:], in0=xt[:], in1=tmp[:], op=ALU.add), reads=[btmp, bxt], writes=[bxt])
            c.dma("sp", self.xs.ap()[qb * 128:(qb + 1) * 128, :], xt[:], reads=[bxt], writes=[self.b_xs[qb]])
        self.phase_end()

    def moe_precast(self, li):
        c = self.c
        wg = self.w["moe_w_gate"].ap()[li]
        wu = self.w["moe_w_up"].ap()[li]
        wd = self.w["moe_w_down"].ap()[li]
        self.wgb = self.scratch(f"moe_wgb{li}", [16, 1024, 512], BF16)
        self.wdb = self.scratch(f"moe_wdb{li}", [16, 256, 1024], BF16)
        self.b_wgb = Buf(); self.b_wdb = Buf()
        for e2 in range(8):
            c.dma("pool", self.wgb.ap()[2 * e2:2 * e2 + 2, :, 0:256], wg[2 * e2:2 * e2 + 2], writes=[self.b_wgb])
            c.dma("pool", self.wgb.ap()[2 * e2:2 * e2 + 2, :, 256:512], wu[2 * e2:2 * e2 + 2], writes=[self.b_wgb])
            c.dma("pool", self.wdb.ap()[2 * e2:2 * e2 + 2], wd[2 * e2:2 * e2 + 2], writes=[self.b_wdb])
        self.precast_done = li

    def layer_moe(self, li, need_ctx):
        c, nc = self.c, self.nc
        NCT = self.NCT
        if getattr(self, "precast_done", -1) != li:
            self.moe_precast(li)
        wgb = self.wgb.ap()
        wdb = self.wdb.ap()
        b_w = Buf("moe_wr")
        Wr = c.sb([128, 8, 20], BF16, "Wr")
        c.dma("pool", Wr[:, :, 0:4], self.w["moe_w_group"].ap()[li].rearrange("(k p) n -> p k n", p=128), writes=[b_w])
        c.dma("pool", Wr[:, :, 4:20], self.w["moe_w_expert"].ap()[li].rearrange("(k p) n -> p k n", p=128), writes=[b_w])
        brow = c.sb([128, 20], F32, "brow")
        c.dma("sp", brow[:, 0:4], self.w["moe_b_group"].ap()[li:li + 1, :].partition_broadcast(128), writes=[b_w])
        c.dma("sp", brow[:, 4:20], self.w["moe_b_expert"].ap()[li:li + 1, :].partition_broadcast(128), writes=[b_w])
        sel16 = c.sb([16, 16, 128], BF16, "sel16")
        c.dma("pool", sel16[:], self.w["sel32"].ap()[0:16, 0:16, :], writes=[b_w])
        hT = c.sb([128, 8, 1024], BF16, "mhT")
        b_hT = Buf()
        act = c.sb([128, 16, 2, 1024], BF16, "mact")
        b_act = Buf()
        Wgu = [c.sb([128, 8, 512], BF16, "Wgu") for _ in range(2)]
        b_Wgu = [Buf(), Buf()]
        Wd = [c.sb([128, 16, 2, 256], BF16, "Wd") for _ in range(2)]
        b_Wd = [Buf(), Buf()]
        xq = [c.sb([128, 256], F32, "xq") for _ in range(4)]
        b_xq = [Buf() for _ in range(4)]
        xqi = 0
        self.alloc_norm_bufs(2)
        self.set_norm_order([t0_ + s_ for (t0_, n_, _c) in self.groups(8, with_ctx=need_ctx) for s_ in range(n_)])
        A2 = c.sb([128, D], F32, "A2"); S2 = c.sb([128, D], F32, "S2"); G2 = c.sb([128, D], F32, "G2")
        b_m = Buf()
        R = 8
        lg = c.sb([128, R, 20], F32, "lg"); le = c.sb([128, R, 16], F32, "le"); le2 = c.sb([128, R, 16], F32, "le2")
        gm = c.sb([128, R, 4], F32, "gm"); eg = c.sb([128, R, 4], F32, "eg"); pen = c.sb([128, R, 4], F32, "pen")
        mk1 = c.sb([128, R, 16], F32, "mk1"); mk2 = c.sb([128, R, 16], F32, "mk2"); cmb = c.sb([128, R, 16], F32, "cmb")
        sc = c.sb([128, 8, R], F32, "rsc")
        b_r = Buf("route")
        cmbT = c.sb([16, 1024], BF16, "cmbT")
        b_cT = Buf()
        s_sb = [c.sb([128, 512], F32, "msil") for _ in range(2)]
        b_s = [Buf(), Buf()]
        t_sb = [c.sb([128, 512], F32, "mt") for _ in range(2)]
        b_t = [Buf(), Buf()]
        tmpd = [c.sb([128, 256], F32, "mtd") for _ in range(2)]
        b_td = [Buf(), Buf()]
        wi = 0
        di = 0
        ui = 0
        for (t0, n, is_ctx) in self.groups(8, with_ctx=need_ctx):
            G = n * 128
            row = 1 if is_ctx else 0
            for (tl, ch) in ((S2, 3), (A2, 4), (G2, 5)):
                c.dma("sp", tl[:], self.modv.ap()[li, row:row + 1, ch * D:(ch + 1) * D].partition_broadcast(128), reads=[self.b_modv[li]], writes=[b_m])
            for s in range(n):
                self.norm_tile(t0 + s, A2, b_m, S2, b_m, hT, b_hT, s * 128, s % 2)
            lp, blp = self.ps[2], self.bps[2]
            c.mm_multi([(lp[:, s * 20:(s + 1) * 20], [(hT[:, k, s * 128:(s + 1) * 128], Wr[:, k, :]) for k in range(8)]) for s in range(n)],
                       reads=[b_hT, b_w], writes=[blp])
            V = lambda e: e
            lgn = lg[:, 0:n, :]
            c.op("dve", lambda e: e.tensor_tensor(out=lgn, in0=lp[:, 0:n * 20].rearrange("p (s j) -> p s j", j=20), in1=brow[:].unsqueeze(1).to_broadcast([128, n, 20]), op=ALU.add),
                 reads=[blp, b_w], writes=[b_r])
            R1 = [b_r]
            c.op("dve", lambda e: e.tensor_reduce(out=sc[:, 0, 0:n], in_=lgn[:, :, 0:4], axis=AX.X, op=ALU.max), reads=R1, writes=R1)
            c.op("dve", lambda e: e.tensor_tensor(out=gm[:, 0:n, :], in0=lgn[:, :, 0:4], in1=sc[:, 0, 0:n].unsqueeze(2).to_broadcast([128, n, 4]), op=ALU.is_equal), reads=R1, writes=R1)
            c.op("dve", lambda e: e.tensor_tensor(out=eg[:, 0:n, :], in0=lgn[:, :, 0:4], in1=sc[:, 0, 0:n].unsqueeze(2).to_broadcast([128, n, 4]), op=ALU.subtract), reads=R1, writes=R1)
            c.op("act", lambda e: e.activation(out=eg[:, 0:n, :], in_=eg[:, 0:n, :], func=AF.Exp), reads=R1, writes=R1)
            c.op("dve", lambda e: e.tensor_reduce(out=sc[:, 1, 0:n], in_=eg[:, 0:n, :], axis=AX.X, op=ALU.add), reads=R1, writes=R1)
            c.op("dve", lambda e: e.reciprocal(out=sc[:, 1, 0:n], in_=sc[:, 1, 0:n]), reads=R1, writes=R1)
            c.op("dve", lambda e: e.tensor_scalar(out=pen[:, 0:n, :], in0=gm[:, 0:n, :], scalar1=1.0, scalar2=1e30, op0=ALU.subtract, op1=ALU.mult), reads=R1, writes=R1)
            c.op("dve", lambda e: e.tensor_copy(out=le[:, 0:n, :], in_=lgn[:, :, 4:20]), reads=R1, writes=R1)
            lev = le[:, 0:n, :].rearrange("p s (g j) -> p (s g) j", g=4)
            c.op("dve", lambda e: e.tensor_tensor(out=lev, in0=lev, in1=pen[:, 0:n, :].rearrange("p s g -> p (s g)").unsqueeze(2).to_broadcast([128, n * 4, 4]), op=ALU.add), reads=R1, writes=R1)
            c.op("dve", lambda e: e.tensor_reduce(out=sc[:, 2, 0:n], in_=le[:, 0:n, :], axis=AX.X, op=ALU.max), reads=R1, writes=R1)
            c.op("dve", lambda e: e.tensor_tensor(out=mk1[:, 0:n, :], in0=le[:, 0:n, :], in1=sc[:, 2, 0:n].unsqueeze(2).to_broadcast([128, n, 16]), op=ALU.is_equal), reads=R1, writes=R1)
            c.op("dve", lambda e: e.scalar_tensor_tensor(out=le2[:, 0:n, :], in0=mk1[:, 0:n, :], scalar=-1e30, in1=le[:, 0:n, :], op0=ALU.mult, op1=ALU.add), reads=R1, writes=R1)
            c.op("dve", lambda e: e.tensor_reduce(out=sc[:, 3, 0:n], in_=le2[:, 0:n, :], axis=AX.X, op=ALU.max), reads=R1, writes=R1)
            c.op("dve", lambda e: e.tensor_tensor(out=mk2[:, 0:n, :], in0=le2[:, 0:n, :], in1=sc[:, 3, 0:n].unsqueeze(2).to_broadcast([128, n, 16]), op=ALU.is_equal), reads=R1, writes=R1)
            c.op("dve", lambda e: e.tensor_tensor(out=sc[:, 4, 0:n], in0=sc[:, 2, 0:n], in1=sc[:, 3, 0:n], op=ALU.subtract), reads=R1, writes=R1)
            c.op("act", lambda e: e.activation(out=sc[:, 4, 0:n], in_=sc[:, 4, 0:n], func=AF.Sigmoid), reads=R1, writes=R1)
            c.op("dve", lambda e: e.tensor_tensor(out=sc[:, 4, 0:n], in0=sc[:, 4, 0:n], in1=sc[:, 1, 0:n], op=ALU.mult), reads=R1, writes=R1)
            c.op("dve", lambda e: e.tensor_tensor(out=sc[:, 5, 0:n], in0=sc[:, 1, 0:n], in1=sc[:, 4, 0:n], op=ALU.subtract), reads=R1, writes=R1)
            c.op("dve", lambda e: e.tensor_tensor(out=mk1[:, 0:n, :], in0=mk1[:, 0:n, :], in1=sc[:, 4, 0:n].unsqueeze(2).to_broadcast([128, n, 16]), op=ALU.mult), reads=R1, writes=R1)
            c.op("dve", lambda e: e.tensor_tensor(out=mk2[:, 0:n, :], in0=mk2[:, 0:n, :], in1=sc[:, 5, 0:n].unsqueeze(2).to_broadcast([128, n, 16]), op=ALU.mult), reads=R1, writes=R1)
            c.op("dve", lambda e: e.tensor_tensor(out=cmb[:, 0:n, :], in0=mk1[:, 0:n, :], in1=mk2[:, 0:n, :], op=ALU.add), reads=R1, writes=R1)
            for hb in range((n + 3) // 4):
                s0, s1 = hb * 4, min(n, hb * 4 + 4)
                pc, bpc = self.ps[3 + hb], self.bps[3 + hb]
                c.tr_multi([(pc[0:16, (s - s0) * 128:(s - s0 + 1) * 128], cmb[:, s, :], self.ident32) for s in range(s0, s1)], reads=[b_r, self.b_const], writes=[bpc])
                c.op("act", lambda e: e.activation(out=cmbT[:, s0 * 128:s1 * 128], in_=pc[0:16, 0:(s1 - s0) * 128], func=AF.Copy), reads=[bpc], writes=[b_cT])
            ncb = (G + 511) // 512
            for ex in range(16):
                W, bW = Wgu[wi % 2], b_Wgu[wi % 2]
                wi += 1
                c.dma("sp", W[:], wgb[ex].rearrange("(k p) n -> p k n", p=128), reads=[self.b_wgb], writes=[bW])
                for cb in range(ncb):
                    c0 = cb * 512
                    cw = min(512, G - c0)
                    pbc, bpbc = self.ps[4 + (ui % 2)], self.bps[4 + (ui % 2)]
                    c.mm(pbc[:, :cw], [(sel16[:, ex, :], cmbT[:, c0:c0 + cw])], reads=[b_w, b_cT], writes=[bpbc])
                    for ffc in range(2):
                        i = ui % 2
                        ui += 1
                        pg_, bpg = self.ps[0 + i], self.bps[0 + i]
                        pu, bpu = self.ps[2 + i], self.bps[2 + i]
                        c.mm(pg_[:, :cw], [(W[:, k, ffc * 128:(ffc + 1) * 128], hT[:, k, c0:c0 + cw]) for k in range(8)], reads=[bW, b_hT], writes=[bpg])
                        c.mm(pu[:, :cw], [(W[:, k, 256 + ffc * 128:256 + (ffc + 1) * 128], hT[:, k, c0:c0 + cw]) for k in range(8)], reads=[bW, b_hT], writes=[bpu])
                        c.op("act", lambda e: e.activation(out=s_sb[i][:, :cw], in_=pg_[:, :cw], func=AF.Silu), reads=[bpg], writes=[b_s[i]])
                        c.op("dve", lambda e: e.tensor_tensor(out=t_sb[i][:, :cw], in0=s_sb[i][:, :cw], in1=pu[:, :cw], op=ALU.mult), reads=[b_s[i], bpu], writes=[b_t[i]])
                        c.op("dve", lambda e: e.tensor_tensor(out=act[:, ex, ffc, c0:c0 + cw], in0=t_sb[i][:, :cw], in1=pbc[:, :cw], op=ALU.mult), reads=[b_t[i], bpbc], writes=[b_act])
            for dq in range(4):
                Wdt, bWd = Wd[di % 2], b_Wd[di % 2]
                di += 1
                c.dma("sp", Wdt[:], wdb[:, :, dq * 256:(dq + 1) * 256].rearrange("e (f p) n -> p e f n", p=128), reads=[self.b_wdb], writes=[bWd])
                for s in range(n):
                    pa, bpa = self.ps[4 + s // 2], self.bps[4 + s // 2]
                    pav = pa[:, (s % 2) * 256:(s % 2 + 1) * 256]
                    c.mm(pav, [(act[:, ex, ffc, s * 128:(s + 1) * 128], Wdt[:, ex, ffc, :]) for ex in range(16) for ffc in range(2)], reads=[b_act, bWd], writes=[bpa])
                    i = s % 2
                    xq_, bxq = xq[xqi % 4], b_xq[xqi % 4]
                    xqi += 1
                    tl = t0 + s
                    c.dma("sp", xq_[:], self.xs.ap()[tl * 128:(tl + 1) * 128, dq * 256:(dq + 1) * 256], reads=[self.b_xs[tl]], writes=[bxq])
                    c.op("dve", lambda e: e.tensor_tensor(out=tmpd[i][:], in0=pav, in1=G2[:, dq * 256:(dq + 1) * 256], op=ALU.mult), reads=[bpa, b_m], writes=[b_td[i]])
                    c.op("pool", lambda e: e.tensor_tensor(out=xq_[:], in0=xq_[:], in1=tmpd[i][:], op=ALU.add), reads=[b_td[i], bxq], writes=[bxq])
                    c.dma("sp", self.xs.ap()[tl * 128:(tl + 1) * 128, dq * 256:(dq + 1) * 256], xq_[:], reads=[bxq], writes=[self.b_xs[tl]])
        self.phase_end()

    def final(self):
        c = self.c
        gf = c.sb([128, D], F32, "gfin")
        b_g = Buf()
        c.dma("sp", gf[:], self.w["final_norm_g"].ap().rearrange("(o n) -> o n", o=1).partition_broadcast(128), writes=[b_g])
        xs_ = [c.sb([128, D], F32, "fx") for _ in range(2)]
        js = [c.sb([128, D], BF16, "fj") for _ in range(2)]
        st = [c.sb([128, 2], F32, "fst") for _ in range(2)]
        ys = [c.sb([128, D], F32, "fy") for _ in range(2)]
        bx = [Buf(), Buf()]; bj = [Buf(), Buf()]; bs = [Buf(), Buf()]; by = [Buf(), Buf()]
        b_out = Buf()
        for t in range(self.NCT, self.NT):
            i = t % 2
            c.dma("sp", xs_[i][:], self.xs.ap()[t * 128:(t + 1) * 128, :], reads=[self.b_xs[t]], writes=[bx[i]])
            c.op("dve", lambda e: e.memset(st[i][:], 0.0), writes=[bs[i]])
            c.op("act", lambda e: e.activation(out=js[i][:], in_=xs_[i][:], func=AF.Square, accum_out=st[i][:, 0:1]), reads=[bx[i]], writes=[bj[i], bs[i]])
            c.op("dve", lambda e: e.tensor_scalar(out=st[i][:, 1:2], in0=st[i][:, 0:1], scalar1=1.0 / D, scalar2=EPS, op0=ALU.mult, op1=ALU.add), reads=[bs[i]], writes=[bs[i]])
            c.op("act", lambda e: e.activation(out=st[i][:, 1:2], in_=st[i][:, 1:2], func=AF.Sqrt), reads=[bs[i]], writes=[bs[i]])
            c.op("dve", lambda e: e.reciprocal(out=st[i][:, 1:2], in_=st[i][:, 1:2]), reads=[bs[i]], writes=[bs[i]])
            c.op("dve", lambda e: e.scalar_tensor_tensor(out=ys[i][:], in0=xs_[i][:], scalar=st[i][:, 1:2], in1=gf[:], op0=ALU.mult, op1=ALU.mult), reads=[bx[i], bs[i], b_g], writes=[by[i]])
            lt = t - self.NCT
            c.dma("sp", self.out.ap()[lt * 128:(lt + 1) * 128, :], ys[i][:], reads=[by[i]], writes=[b_out])
        self.c.finish("sp")

    def layer_mla(self, li, j, need_ctx):
        c, nc = self.c, self.nc
        T, NT, NCT = self.T, self.NT, self.NCT
        w_in = self.w["mla_w_in"].ap()[j]
        w_qup = self.w["mla_w_q_up"].ap()[j]
        w_kvup = self.w["mla_w_kv_up"].ap()[j]
        w_out = self.w["mla_w_out"].ap()[j]
        QT = self.scratch(f"mla_qt{li}", [16, 96, T], BF16)
        KTd = self.scratch(f"mla_kt{li}", [16, 96, T], BF16)
        Vd = self.scratch(f"mla_v{li}", [16, NT, 128, 65], BF16)
        YT = self.scratch(f"mla_yt{li}", [16, 64, T], BF16)
        b_QT = Buf(); b_KTd = Buf(); b_Vd = Buf(); b_YT = Buf()
        b_w = Buf("mla_w")
        Win = c.sb([128, 8, 544], BF16, "Win")
        c.dma("pool", Win[:], w_in.rearrange("(k p) n -> p k n", p=128), writes=[b_w])
        Wkr_rot = c.sb([128, 8, 32], BF16, "Wkrrot")
        Wq = c.sb([128, 2, 1536], BF16, "Wqup")
        c.dma("pool", Wq[:], w_qup.rearrange("(k p) n -> p k n", p=128), writes=[b_w])
        Wqr = c.sb([128, 2, 1536], BF16, "Wquprot")
        Wkn = c.sb([128, 2, 16, 64], BF16, "Wkn")
        Wv = c.sb([128, 2, 16, 64], BF16, "Wvv")
        kvv = w_kvup.rearrange("(k p) (h two d) -> p k h two d", p=128, two=2, d=64)
        for kc in range(2):
            c.dma("pool", Wkn[:, kc], kvv[:, kc, :, 0, :], writes=[b_w])
            c.dma("pool", Wv[:, kc], kvv[:, kc, :, 1, :], writes=[b_w])
        b_wr = Buf("mla_wr")
        c.op("pool", lambda e: e.memset(Wqr[:], 0.0), writes=[b_wr])
        for kc in range(2):
            src = Wq[:, kc, :].rearrange("p (h d) -> p h d", d=96)
            dst = Wqr[:, kc, :].rearrange("p (h d) -> p h d", d=96)
            c.op("act", lambda e: e.activation(out=dst[:, :, 64:80], in_=src[:, :, 80:96], func=AF.Copy, scale=-1.0), reads=[b_w], writes=[b_wr])
            c.op("dve", lambda e: e.tensor_copy(out=dst[:, :, 80:96], in_=src[:, :, 64:80]), reads=[b_w], writes=[b_wr])
        c.op("act", lambda e: e.activation(out=Wkr_rot[:, :, 0:16], in_=Win[:, :, 528:544], func=AF.Copy, scale=-1.0), reads=[b_w], writes=[b_wr])
        c.op("dve", lambda e: e.tensor_copy(out=Wkr_rot[:, :, 16:32], in_=Win[:, :, 512:528]), reads=[b_w], writes=[b_wr])
        gq = c.sb([128, 512], F32, "gqkv")
        c.dma("sp", gq[:, 0:256], self.w["mla_q_norm_g"].ap()[j:j + 1, :].partition_broadcast(128), writes=[b_w])
        c.dma("sp", gq[:, 256:512], self.w["mla_kv_norm_g"].ap()[j:j + 1, :].partition_broadcast(128), writes=[b_w])
        mark = c.sb_mark()
        A1x, bA1x = self.load_mod(li, 1, 0, "A1x")
        S1x, bS1x = self.load_mod(li, 0, 0, "S1x")
        A1c, bA1c = self.load_mod(li, 1, 1, "A1c")
        S1c, bS1c = self.load_mod(li, 0, 1, "S1c")
        self.alloc_norm_bufs(2)
        self.set_norm_order([t0_ + s_ for (t0_, n_, _c) in self.groups(4) for s_ in range(n_)])
        hTs = [c.sb([128, 8, 512], BF16, "hT") for _ in range(2)]
        b_hT = [Buf(), Buf()]
        cnT = [c.sb([128, 4, 512], BF16, "cnT") for _ in range(2)]
        b_cnT = [Buf(), Buf()]
        rts = [c.sb([96, 2, 512], F32, "rt96") for _ in range(2)]
        rks = [c.sb([32, 2, 512], F32, "rt32") for _ in range(2)]
        b_rt = [Buf(), Buf()]
        krT = [c.sb([32, 512], BF16, "krT") for _ in range(2)]
        b_krT = [Buf(), Buf()]
        st = [c.sb([128, 4], F32, "mst") for _ in range(2)]
        b_st = [Buf(), Buf()]
        jk = c.sb([128, 256], BF16, "mjunk"); b_jk = Buf()
        cn = [c.sb([128, 512], BF16, "cn") for _ in range(2)]
        b_cn = [Buf(), Buf()]
        qst = [c.sb([96, 512], BF16, "mqst") for _ in range(2)]
        b_qst = [Buf(), Buf()]
        kst = [c.sb([64, 512], BF16, "mkst") for _ in range(2)]
        b_kst = [Buf(), Buf()]
        t1s = [c.sb([96, 512], F32, "t1") for _ in range(2)]
        t2s = [c.sb([96, 512], F32, "t2") for _ in range(2)]
        b_t1 = [Buf(), Buf()]; b_t2 = [Buf(), Buf()]
        Vt = [c.sb([128, 16, 65], BF16, "Vt") for _ in range(2)]
        b_Vt = [Buf(), Buf()]
        for i in range(2):
            c.op("pool", lambda e: e.memset(Vt[i][:], 1.0), writes=[b_Vt[i]])
        cnt = 0
        ti = 0
        for gi, (t0, n, is_ctx) in enumerate(self.groups(4)):
            ncols = n * 128
            col0 = t0 * 128
            hT, bh = hTs[gi % 2], b_hT[gi % 2]
            cT_, bcT = cnT[gi % 2], b_cnT[gi % 2]
            rt, rk, brt = rts[gi % 2], rks[gi % 2], b_rt[gi % 2]
            for s in range(n):
                if is_ctx:
                    self.norm_tile(t0 + s, A1c, bA1c, S1c, bS1c, hT, bh, s * 128, (t0 + s) % 2)
                else:
                    self.norm_tile(t0 + s, A1x, bA1x, S1x, bS1x, hT, bh, s * 128, (t0 + s) % 2)
            if not is_ctx:
                l0 = (t0 - NCT) * 128
                c.dma("sp", rt[:, :, :ncols], self.w["rope96"].ap()[:, :, l0:l0 + ncols].rearrange("two d l -> d two l"), writes=[brt])
                c.dma("sp", rk[:, :, :ncols], self.w["rope32"].ap()[:, :, l0:l0 + ncols].rearrange("two d l -> d two l"), writes=[brt])
            for s in range(n):
                i = ti % 2
                ti += 1
                pA, bpA = self.ps[2 + i], self.bps[2 + i]
                c.mm(pA[:, :], [(hT[:, k, s * 128:(s + 1) * 128], Win[:, k, 0:512]) for k in range(8)], reads=[bh, b_w], writes=[bpA])
                c.op("dve", lambda e: e.memset(st[i][:], 0.0), writes=[b_st[i]])
                for u in range(2):
                    c.op("act", lambda e: e.activation(out=jk[:], in_=pA[:, u * 256:(u + 1) * 256], func=AF.Square, accum_out=st[i][:, u:u + 1]), reads=[bpA], writes=[b_jk, b_st[i]])
                c.op("dve", lambda e: e.tensor_scalar(out=st[i][:, 2:4], in0=st[i][:, 0:2], scalar1=1.0 / 256, scalar2=EPS, op0=ALU.mult, op1=ALU.add), reads=[b_st[i]], writes=[b_st[i]])
                c.op("act", lambda e: e.activation(out=st[i][:, 2:4], in_=st[i][:, 2:4], func=AF.Sqrt), reads=[b_st[i]], writes=[b_st[i]])
                c.op("dve", lambda e: e.reciprocal(out=st[i][:, 2:4], in_=st[i][:, 2:4]), reads=[b_st[i]], writes=[b_st[i]])
                for u in range(2):
                    c.op("dve", lambda e: e.scalar_tensor_tensor(out=cn[i][:, u * 256:(u + 1) * 256], in0=pA[:, u * 256:(u + 1) * 256], scalar=st[i][:, 2 + u:3 + u], in1=gq[:, u * 256:(u + 1) * 256], op0=ALU.mult, op1=ALU.mult),
                         reads=[bpA, b_st[i], b_w], writes=[b_cn[i]])
                pT = self.ps[4 + i].ap().bitcast(BF16)
                c.tr_multi([(pT[:, k * 128:(k + 1) * 128], cn[i][:, k * 128:(k + 1) * 128], self.identb) for k in range(4)], reads=[b_cn[i], self.b_const], writes=[self.bps[4 + i]])
                c.op("act", lambda e: e.activation(out=cT_[:, :, s * 128:(s + 1) * 128], in_=pT[:, 0:512].rearrange("p (k n) -> p k n", k=4), func=AF.Copy), reads=[self.bps[4 + i]], writes=[bcT])

            def rope_proj(pairs1, pairs2, M, rtab, dst_ap, dst_buf, rd):
                nonlocal cnt
                i = cnt % 2
                cnt += 1
                P1, bP1 = self.ps[2 + i], self.bps[2 + i]
                P2, bP2 = self.ps[6 + i], self.bps[6 + i]
                c.mm(P1[0:M, :ncols], pairs1, reads=rd, writes=[bP1])
                if is_ctx or pairs2 is None:
                    c.op("act", lambda e: e.activation(out=dst_ap, in_=P1[0:M, :ncols], func=AF.Copy), reads=[bP1], writes=[dst_buf])
                else:
                    c.mm(P2[0:M, :ncols], pairs2, reads=rd + [b_wr], writes=[bP2])
                    c.op("dve", lambda e: e.tensor_tensor(out=t1s[i][0:M, :ncols], in0=P1[0:M, :ncols], in1=rtab[0:M, 0, :ncols], op=ALU.mult), reads=[bP1, brt], writes=[b_t1[i]])
                    c.op("dve", lambda e: e.tensor_tensor(out=t2s[i][0:M, :ncols], in0=P2[0:M, :ncols], in1=rtab[0:M, 1, :ncols], op=ALU.mult), reads=[bP2, brt], writes=[b_t2[i]])
                    c.op("pool", lambda e: e.tensor_tensor(out=dst_ap, in0=t1s[i][0:M, :ncols], in1=t2s[i][0:M, :ncols], op=ALU.add), reads=[b_t1[i], b_t2[i]], writes=[dst_buf])

            kr_, bkr = krT[gi % 2], b_krT[gi % 2]
            rope_proj([(Win[:, k, 512:544], hT[:, k, :ncols]) for k in range(8)], [(Wkr_rot[:, k, :], hT[:, k, :ncols]) for k in range(8)], 32, rk, kr_[:, :ncols], bkr, [bh, b_w])
            for h in range(16):
                qs, bq = qst[h % 2], b_qst[h % 2]
                rope_proj([(Wq[:, kc, h * 96:(h + 1) * 96], cT_[:, kc, :ncols]) for kc in range(2)],
                          [(Wqr[:, kc, h * 96:(h + 1) * 96], cT_[:, kc, :ncols]) for kc in range(2)], 96, rt, qs[:, :ncols], bq, [bcT, b_w])
                c.dma("sp", QT.ap()[h, :, col0:col0 + ncols], qs[:, :ncols], reads=[bq], writes=[b_QT])
                ks, bk = kst[h % 2], b_kst[h % 2]
                rope_proj([(Wkn[:, kc, h, :], cT_[:, 2 + kc, :ncols]) for kc in range(2)], None, 64, None, ks[:, :ncols], bk, [bcT, b_w])
                c.dma("sp", KTd.ap()[h, 0:64, col0:col0 + ncols], ks[:, :ncols], reads=[bk], writes=[b_KTd])
                c.dma("sp", KTd.ap()[h, 64:96, col0:col0 + ncols], kr_[:, :ncols], reads=[bkr], writes=[b_KTd])
            for s in range(n):
                vt, bvt = Vt[s % 2], b_Vt[s % 2]
                for hh in range(2):
                    i = cnt % 2
                    cnt += 1
                    Pv, bPv = self.ps[2 + i], self.bps[2 + i]
                    c.mm(Pv[:, :], [(cT_[:, 2 + kc, s * 128:(s + 1) * 128], Wv[:, kc, hh * 8:(hh + 1) * 8, :].rearrange("p h d -> p (h d)")) for kc in range(2)], reads=[bcT, b_w], writes=[bPv])
                    c.op("act", lambda e: e.activation(out=vt[:, hh * 8:(hh + 1) * 8, 0:64], in_=Pv[:, :].rearrange("p (h d) -> p h d", d=64), func=AF.Copy), reads=[bPv], writes=[bvt])
                c.dma("sp", Vd.ap()[:, t0 + s].rearrange("h p d -> p h d"), vt[:], reads=[bvt], writes=[b_Vd])
        c.barrier()
        c.sb_release(mark)
        QTs = [c.sb([96, T], BF16, "QTh") for _ in range(2)]
        KTs = [c.sb([96, T], BF16, "KTh") for _ in range(2)]
        Vhs = [c.sb([128, NT, 65], BF16, "Vh") for _ in range(2)]
        b_hd = [Buf(), Buf()]
        PTs = [c.sb([128, 512], BF16, "PT") for _ in range(4)]
        b_PT = [Buf() for _ in range(4)]
        osb = [c.sb([65, 512], F32, "osb") for _ in range(2)]
        b_osb = [Buf(), Buf()]
        ysb = [c.sb([64, 512], BF16, "ysb") for _ in range(2)]
        b_ysb = [Buf(), Buf()]
        rbs = [c.sb([64, 512], F32, "rbs") for _ in range(2)]
        b_rbs = [Buf(), Buf()]
        scale = 96 ** -0.5
        pi = 0; si = 0; oi = 0

        def load_head(h_):
            c.dma("sp", QTs[h_ % 2][:], QT.ap()[h_], reads=[b_QT], writes=[b_hd[h_ % 2]])
            c.dma("sp", KTs[h_ % 2][:], KTd.ap()[h_], reads=[b_KTd], writes=[b_hd[h_ % 2]])
            c.dma("sp", Vhs[h_ % 2][:], Vd.ap()[h_].rearrange("t p d -> p t d"), reads=[b_Vd], writes=[b_hd[h_ % 2]])
        load_head(0)
        for h in range(16):
            Qh, Kh, Vh, bhd = QTs[h % 2], KTs[h % 2], Vhs[h % 2], b_hd[h % 2]
            if h + 1 < 16:
                load_head(h + 1)
            units = []
            for (t0, n, is_ctx) in self.groups(4, with_ctx=need_ctx):
                keys = list(range(NCT)) if is_ctx else list(range(NT))
                gslot = oi % 2
                oi += 1
                for ki, kt in enumerate(keys):
                    units.append((t0 * 128, n * 128, kt, ki == 0, ki == len(keys) - 1, gslot))
            SB = (0, 1, 5, 6, 7)
            LA = 3

            def issue_S(ui):
                col0_, ncols_, kt_, _, _, _ = units[ui]
                bk_ = SB[(si + ui) % len(SB)]
                c.mm(self.ps[bk_][:, :ncols_], [(Kh[:, kt_ * 128:(kt_ + 1) * 128], Qh[:, col0_:col0_ + ncols_])], reads=[bhd], writes=[self.bps[bk_]])

            def epilogue(col0_, ncols_, gslot):
                oT, boT = self.ps[2 + gslot], self.bps[2 + gslot]
                o, bo = osb[gslot], b_osb[gslot]
                ys, bys = ysb[gslot], b_ysb[gslot]
                c.op("act", lambda e: e.activation(out=o[:, :ncols_], in_=oT[0:65, :ncols_], func=AF.Copy), reads=[boT], writes=[bo])
                c.mm(self.ps[4][0:64, :ncols_], [(self.ones32[64:65, 0:64], o[64:65, :ncols_])], reads=[self.b_const, bo], writes=[self.bps[4]])
                rb, brb = rbs[gslot], b_rbs[gslot]
                c.op("dve", lambda e: e.reciprocal(out=rb[:, :ncols_], in_=self.ps[4][0:64, :ncols_]), reads=[self.bps[4]], writes=[brb])
                c.op("dve", lambda e: e.tensor_tensor(out=ys[:, :ncols_], in0=o[0:64, :ncols_], in1=rb[:, :ncols_], op=ALU.mult), reads=[bo, brb], writes=[bys])
                c.dma("sp", YT.ap()[h, :, col0_:col0_ + ncols_], ys[:, :ncols_], reads=[bys], writes=[b_YT])

            pending = []
            for k0 in range(min(LA, len(units))):
                issue_S(k0)
            for ui, (col0, ncols, kt, first, last, gslot) in enumerate(units):
                bk = SB[(si + ui) % len(SB)]
                sT, bsT = self.ps[bk], self.bps[bk]
                if ui + LA < len(units):
                    issue_S(ui + LA)
                PT, bPT = PTs[pi % 4], b_PT[pi % 4]
                pi += 1
                oT, boT = self.ps[2 + gslot], self.bps[2 + gslot]
                c.op("act", lambda e: e.activation(out=PT[:, :ncols], in_=sT[:, :ncols], func=AF.Exp, scale=scale), reads=[bsT], writes=[bPT])
                c.mm(oT[0:65, :ncols], [(Vh[:, kt, :], PT[:, :ncols])], reads=[bhd, bPT], writes=[boT], start=first, stop=last)
                if pending and pending[0][0] <= ui:
                    _, args = pending.pop(0)
                    epilogue(*args)
                if last:
                    pending.append((ui + 4, (col0, ncols, gslot)))
            for _, args in pending:
                epilogue(*args)
            si += len(units)
        c.barrier()
        c.sb_release(mark)
        Wo = c.sb([64, 16, 1024], BF16, "Wo")
        b_wo = Buf()
        c.dma("pool", Wo[:], w_out.rearrange("(h d) n -> d h n", d=64), writes=[b_wo])
        self.attn_out(li, need_ctx, Wo, b_wo, YT, b_YT)
        self.phase_end()

    def attn_out(self, li, need_ctx, Wo, b_wo, YT, b_YT):
        c = self.c
        NCT, NT = self.NCT, self.NT
        G1x, bG1x = self.load_mod(li, 2, 0, "G1x")
        G1c, bG1c = self.load_mod(li, 2, 1, "G1c")
        yTs = [c.sb([64, 16, 128], BF16, "yT") for _ in range(2)]
        b_yT = [Buf(), Buf()]
        xts = [c.sb([128, D], F32, "xres") for _ in range(2)]
        b_xt = [Buf(), Buf()]
        tmps = [c.sb([128, D], F32, "rtmp") for _ in range(2)]
        b_tmp = [Buf(), Buf()]
        qbs = list(range(0 if need_ctx else NCT, NT))

        def ao_load(bi_):
            qb_ = qbs[bi_]
            c.dma("sp", xts[bi_ % 2][:], self.xs.ap()[qb_ * 128:(qb_ + 1) * 128, :], reads=[self.b_xs[qb_]], writes=[b_xt[bi_ % 2]])
            c.dma("sp", yTs[bi_ % 2][:], YT.ap()[:, :, qb_ * 128:(qb_ + 1) * 128].rearrange("h d t -> d h t"), reads=[b_YT], writes=[b_yT[bi_ % 2]])
        ao_load(0)
        for bi, qb in enumerate(qbs):
            is_ctx = qb < NCT
            xt, bxt = xts[bi % 2], b_xt[bi % 2]
            yT, byT = yTs[bi % 2], b_yT[bi % 2]
            tmp, btmp = tmps[bi % 2], b_tmp[bi % 2]
            if bi + 1 < len(qbs):
                ao_load(bi + 1)
            G1, bG1 = (G1c, bG1c) if is_ctx else (G1x, bG1x)
            for nn in range(2):
                z, bz = self.ps[5 + nn], self.bps[5 + nn]
                c.mm(z[:, :], [(yT[:, hq, :], Wo[:, hq, nn * 512:(nn + 1) * 512]) for hq in range(16)], reads=[byT, b_wo], writes=[bz])
                c.op("dve", lambda e: e.tensor_tensor(out=tmp[:, nn * 512:(nn + 1) * 512], in0=z[:, :], in1=G1[:, nn * 512:(nn + 1) * 512], op=ALU.mult), reads=[bz, bG1], writes=[btmp])
            c.op("pool", lambda e: e.tensor_tensor(out=xt[:], in0=xt[:], in1=tmp[:], op=ALU.add), reads=[btmp, bxt], writes=[bxt])
            c.dma("sp", self.xs.ap()[qb * 128:(qb + 1) * 128, :], xt[:], reads=[bxt], writes=[self.b_xs[qb]])

    def layer_ssd(self, li, j, need_ctx):
        c, nc = self.c, self.nc
        T, NT, NCT, NL, NCX = self.T, self.NT, self.NCT, self.NL, self.NCX
        w_in = self.w["ssm_w_in"].ap()[j]
        XB = self.scratch(f"ssd_xb{li}", [24, 128, T], BF16)
        XC = self.scratch(f"ssd_xc{li}", [24, 128, T], BF16)
        Zs = self.scratch(f"ssd_z{li}", [NT, 128, 2048], BF16)
        DT = self.scratch(f"ssd_dt{li}", [NT, 128, 64], F32)
        Yd = [self.scratch(f"ssd_y{li}_{d}", [NT, 128, 2048], F32) for d in range(2)]
        b_XB = Buf(); b_XC = Buf(); b_Z = Buf(); b_DT = Buf(); b_Y = [Buf(), Buf()]
        b_w = Buf("ssd_w")
        Win = c.sb([128, 8, 5184], BF16, "ssdWin")
        for k in range(8):
            c.dma("pool", Win[:, k, :], w_in[k * 128:(k + 1) * 128, :], writes=[b_w])
        dtb = c.sb([128, 64], F32, "dtb")
        c.dma("sp", dtb[:], self.w["ssm_dt_bias"].ap()[j:j + 1].rearrange("o d h -> o (d h)").partition_broadcast(128), writes=[b_w])
        mark = c.sb_mark()
        A1x, bA1x = self.load_mod(li, 1, 0, "A1x")
        S1x, bS1x = self.load_mod(li, 0, 0, "S1x")
        A1c, bA1c = self.load_mod(li, 1, 1, "A1c")
        S1c, bS1c = self.load_mod(li, 0, 1, "S1c")
        self.alloc_norm_bufs(2)
        self.set_norm_order([t0_ + s_ for (t0_, n_, _c) in self.groups(4) for s_ in range(n_)])
        hTs = [c.sb([128, 8, 512], BF16, "hT") for _ in range(2)]
        b_hT = [Buf(), Buf()]
        stg = [c.sb([128, 512], BF16, "stg") for _ in range(3)]
        b_stg = [Buf() for _ in range(3)]
        zst = [c.sb([128, 2048], BF16, "zst") for _ in range(2)]
        b_zst = [Buf(), Buf()]
        dts = [c.sb([128, 64], F32, "dts") for _ in range(2)]
        b_dts = [Buf(), Buf()]
        cnt = 0
        for gi, (t0, n, is_ctx) in enumerate(self.groups(4)):
            ncols = n * 128
            col0 = t0 * 128
            hT, bh = hTs[gi % 2], b_hT[gi % 2]
            for s in range(n):
                if is_ctx:
                    self.norm_tile(t0 + s, A1c, bA1c, S1c, bS1c, hT, bh, s * 128, (t0 + s) % 2)
                else:
                    self.norm_tile(t0 + s, A1x, bA1x, S1x, bS1x, hT, bh, s * 128, (t0 + s) % 2)
            for fc in range(24):
                i = cnt % 3
                cnt += 1
                P, bP = self.ps[2 + i], self.bps[2 + i]
                c.mm(P[:, :ncols], [(Win[:, k, 2048 + fc * 128:2048 + (fc + 1) * 128], hT[:, k, :ncols]) for k in range(8)], reads=[b_w, bh], writes=[bP])
                c.op("act", lambda e: e.activation(out=stg[i][:, :ncols], in_=P[:, :ncols], func=AF.Copy), reads=[bP], writes=[b_stg[i]])
                c.dma("sp", XB.ap()[fc, :, col0:col0 + ncols], stg[i][:, :ncols], reads=[b_stg[i]], writes=[b_XB])
            for s in range(n):
                zs, bz = zst[s % 2], b_zst[s % 2]
                for zc in range(4):
                    i = cnt % 3
                    cnt += 1
                    P, bP = self.ps[2 + i], self.bps[2 + i]
                    c.mm(P[:, :], [(hT[:, k, s * 128:(s + 1) * 128], Win[:, k, zc * 512:(zc + 1) * 512]) for k in range(8)], reads=[b_w, bh], writes=[bP])
                    c.op("act", lambda e: e.activation(out=zs[:, zc * 512:(zc + 1) * 512], in_=P[:, :], func=AF.Silu), reads=[bP], writes=[bz])
                c.dma("sp", Zs.ap()[t0 + s], zs[:], reads=[bz], writes=[b_Z])
                i = cnt % 3
                cnt += 1
                P, bP = self.ps[2 + i], self.bps[2 + i]
                dt_, bdt = dts[s % 2], b_dts[s % 2]
                c.mm(P[:, 0:64], [(hT[:, k, s * 128:(s + 1) * 128], Win[:, k, 5120:5184]) for k in range(8)], reads=[b_w, bh], writes=[bP])
                c.op("dve", lambda e: e.tensor_tensor(out=dt_[:], in0=P[:, 0:64], in1=dtb[:], op=ALU.add), reads=[bP, b_w], writes=[bdt])
                c.op("act", lambda e: e.activation(out=dt_[:], in_=dt_[:], func=AF.Exp), reads=[bdt], writes=[bdt])
                c.op("act", lambda e: e.activation(out=dt_[:], in_=dt_[:], func=AF.Ln, bias=1.0), reads=[bdt], writes=[bdt])
                c.dma("sp", DT.ap()[t0 + s], dt_[:], reads=[bdt], writes=[b_DT])
        c.barrier()
        c.sb_release(self.mark0)
        cw = c.sb([128, 24, 5], F32, "convw"); cbias = c.sb([128, 24], F32, "convb")
        b_cw = Buf()
        c.dma("sp", cw[:], self.w["ssm_conv_wT"].ap()[j], writes=[b_cw])
        c.dma("sp", cbias[:], self.w["ssm_conv_bT"].ap()[j], writes=[b_cw])
        segs = [(0, NCX), (NCX, NL)]
        Lmax = max(NCX, NL)
        xp = [c.sb([128, Lmax + 4], BF16, "xp") for _ in range(2)]
        b_xp = [Buf(), Buf()]
        acc = [c.sb([128, Lmax], F32, "cacc") for _ in range(2)]
        b_acc = [Buf(), Buf()]
        cout = [c.sb([128, Lmax], BF16, "cout") for _ in range(2)]
        b_cout = [Buf(), Buf()]
        it = 0
        work = [(fc, o0, L) for fc in range(24) for (o0, L) in segs]

        def conv_load(k_):
            fc_, o0_, L_ = work[k_]
            i_ = k_ % 2
            c.op("pool", lambda e: e.memset(xp[i_][:], 0.0), writes=[b_xp[i_]])
            c.dma("sp", xp[i_][:, 2:2 + L_], XB.ap()[fc_, :, o0_:o0_ + L_], reads=[b_XB], writes=[b_xp[i_]])
        conv_load(0)
        for fc in range(24):
            for (o0, L) in segs:
                i = it % 2
                it += 1
                if it < len(work):
                    conv_load(it)
                c.op("dve", lambda e: e.tensor_scalar(out=acc[i][:, :L], in0=xp[i][:, 0:L], scalar1=cw[:, fc, 0:1], scalar2=None, op0=ALU.mult), reads=[b_xp[i], b_cw], writes=[b_acc[i]])
                for k in range(1, 5):
                    eng = "dve"
                    c.op(eng, lambda e: e.scalar_tensor_tensor(out=acc[i][:, :L], in0=xp[i][:, k:k + L], scalar=cw[:, fc, k:k + 1], in1=acc[i][:, :L], op0=ALU.mult, op1=ALU.add),
                         reads=[b_xp[i], b_cw, b_acc[i]], writes=[b_acc[i]])
                c.op("act", lambda e: e.activation(out=cout[i][:, :L], in_=acc[i][:, :L], func=AF.Silu, bias=cbias[:, fc:fc + 1]), reads=[b_acc[i], b_cw], writes=[b_cout[i]])
                c.dma("sp", XC.ap()[fc, :, o0:o0 + L], cout[i][:, :L], reads=[b_cout[i]], writes=[b_XC])
        c.barrier()
        c.sb_release(self.mark0)
        ACS = self.scratch(f"ssd_acs{li}", [2 * NT, 32 * 128], F32)
        b_ACS = [Buf() for _ in range(2 * NT)]
        bcs = [c.sb([128, 32, 128], F32, "bcs") for _ in range(2)]; b_bcs = [Buf(), Buf()]
        aneg = c.sb([128, 64], F32, "aneg"); dsk = c.sb([128, 64], F32, "dsk"); b_an = Buf()
        c.dma("sp", aneg[:], self.w["ssm_a_log"].ap()[j:j + 1].rearrange("o d h -> o (d h)").partition_broadcast(128), writes=[b_an])
        c.op("act", lambda e: e.activation(out=aneg[:], in_=aneg[:], func=AF.Exp), reads=[b_an], writes=[b_an])
        c.op("act", lambda e: e.activation(out=aneg[:], in_=aneg[:], func=AF.Copy, scale=-1.0), reads=[b_an], writes=[b_an])
        c.dma("sp", dsk[:], self.w["ssm_d"].ap()[j:j + 1].rearrange("o d h -> o (d h)").partition_broadcast(128), writes=[b_an])
        c.op("dve", lambda e: e.tensor_tensor(out=dsk[:, 0:32], in0=dsk[:, 0:32], in1=dsk[:, 32:64], op=ALU.add), reads=[b_an], writes=[b_an])
        xcs = [c.sb([128, 24, 128], BF16, "xc") for _ in range(2)]; b_xc = [Buf(), Buf()]
        dtt = [c.sb([128, 64], F32, "dtt") for _ in range(2)]; b_dtt = [Buf(), Buf()]
        gt = [c.sb([128, 8, 32], F32, "gt") for _ in range(2)]; b_gt = [Buf(), Buf()]
        acT = [c.sb([32, 128], F32, "acT") for _ in range(2)]; b_acT = [Buf(), Buf()]
        nacT = [c.sb([32, 128], F32, "nacT") for _ in range(2)]
        xtok = [c.sb([128, 32, 64], F32, "xtok") for _ in range(2)]; b_xtok = [Buf(), Buf()]
        u = [c.sb([128, 32, 64], BF16, "u") for _ in range(2)]; b_u = [Buf(), Buf()]
        Vw = [c.sb([128, 32, 64], BF16, "Vw") for _ in range(2)]; b_Vw = [Buf(), Buf()]
        Btok = [c.sb([128, 4, 128], BF16, "Btok") for _ in range(2)]; b_Bt = [Buf(), Buf()]
        scm = [c.sb([128, 128], F32, "scm") for _ in range(2)]; b_scm = [Buf(), Buf()]
        aa = [c.sb([128, 512], F32, "aa") for _ in range(4)]; b_aa = [Buf() for _ in range(4)]
        EE = [c.sb([128, 512], F32, "EE") for _ in range(4)]; b_EE = [Buf() for _ in range(4)]
        MT = [c.sb([128, 512], BF16, "MT") for _ in range(4)]; b_MT = [Buf() for _ in range(4)]
        yi = [c.sb([128, 512], F32, "yi") for _ in range(2)]; b_yi = [Buf(), Buf()]
        Yt = [c.sb([128, 2048], F32, "Yt") for _ in range(2)]; b_Yt = [Buf(), Buf()]
        S32 = c.sb([128, 4, 512], F32, "S32"); Sb = c.sb([128, 4, 512], BF16, "Sb"); b_S = [Buf() for _ in range(4)]
        ci = 0; hi = 0
        for d in range(2):
            tri = self.trile32 if d == 0 else self.trige32
            order = list(range(NT)) if d == 0 else (list(range(NCT - 1, -1, -1)) + list(range(NT - 1, NCT - 1, -1)))
            c.op("dve", lambda e: e.memset(S32[:], 0.0), writes=b_S)
            c.op("pool", lambda e: e.memset(Sb[:], 0.0), writes=b_S)
            def prep_load(ch, i):
                xc, bxc = xcs[i], b_xc[i]
                c.dma("sp", xc[:], XC.ap()[:, :, ch * 128:(ch + 1) * 128].rearrange("f p t -> p f t"), reads=[b_XC], writes=[bxc])
                c.dma("sp", dtt[i][:], DT.ap()[ch], reads=[b_DT], writes=[b_dtt[i]])

            def prep(ch, i):
                xc, bxc = xcs[i], b_xc[i]
                g_, bg = gt[i], b_gt[i]
                dc = slice(d * 32, (d + 1) * 32)
                c.op("dve", lambda e: e.tensor_tensor(out=g_[:, 0, :], in0=dtt[i][:, dc], in1=aneg[:, dc], op=ALU.mult), reads=[b_dtt[i], b_an], writes=[bg])
                p0, bp0 = self.ps[0], self.bps[0]
                c.mm(p0[:, 0:32], [(tri, g_[:, 0, :])], reads=[self.b_const, bg], writes=[bp0])
                c.mm(p0[:, 32:64], [(self.ones32, g_[:, 0, :])], reads=[self.b_const, bg], writes=[bp0])
                c.mm(p0[0:32, 128:256], [(g_[:, 0, :], tri)], reads=[self.b_const, bg], writes=[bp0])
                c.op("act", lambda e: e.activation(out=g_[:, 1, :], in_=p0[:, 0:32], func=AF.Copy), reads=[bp0], writes=[bg])
                c.op("act", lambda e: e.activation(out=g_[:, 2, :], in_=p0[:, 0:32], func=AF.Copy, scale=-1.0), reads=[bp0], writes=[bg])
                c.op("act", lambda e: e.activation(out=g_[:, 3, :], in_=p0[:, 0:32], func=AF.Exp), reads=[bp0], writes=[bg])
                c.op("dve", lambda e: e.tensor_tensor(out=g_[:, 4, :], in0=p0[:, 32:64], in1=g_[:, 1, :], op=ALU.subtract), reads=[bp0, bg], writes=[bg])
                c.op("act", lambda e: e.activation(out=g_[:, 4, :], in_=g_[:, 4, :], func=AF.Exp), reads=[bg], writes=[bg])
                c.op("act", lambda e: e.activation(out=g_[:, 5, :], in_=p0[:, 32:64], func=AF.Exp), reads=[bp0], writes=[bg])
                c.op("act", lambda e: e.activation(out=acT[i][:], in_=p0[0:32, 128:256], func=AF.Copy), reads=[bp0], writes=[b_acT[i]])
                slot = d * NT + ch
                c.dma("sp", ACS.ap()[slot:slot + 1, :].rearrange("o (h t) -> (o h) t", h=32), acT[i][:], reads=[b_acT[i]], writes=[b_ACS[slot]])
                c.dma("sp", bcs[i][:].rearrange("p h t -> p (h t)"), ACS.ap()[slot:slot + 1, :].partition_broadcast(128), reads=[b_ACS[slot]], writes=[b_bcs[i]])
                for hh in range(2):
                    pT = self.ps[1].ap().bitcast(BF16)
                    c.tr_multi([(pT[:, k * 128:(k + 1) * 128], xc[:, hh * 8 + k, :], self.identb) for k in range(8)], reads=[bxc, self.b_const], writes=[self.bps[1]])
                    c.op("act", lambda e: e.activation(out=xtok[i][:, hh * 16:(hh + 1) * 16, :].rearrange("p h d -> p (h d)"), in_=pT[:, :], func=AF.Copy), reads=[self.bps[1]], writes=[b_xtok[i]])
                pT = self.ps[1].ap().bitcast(BF16)
                c.tr_multi([(pT[:, k * 128:(k + 1) * 128], xc[:, 16 + k, :], self.identb) for k in range(4)], reads=[bxc, self.b_const], writes=[self.bps[1]])
                c.op("act", lambda e: e.activation(out=Btok[i][:].rearrange("p g n -> p (g n)"), in_=pT[:, 0:512], func=AF.Copy), reads=[self.bps[1]], writes=[b_Bt[i]])
                c.op("dve", lambda e: e.tensor_tensor(out=u[i][:], in0=xtok[i][:], in1=dtt[i][:, dc].unsqueeze(2).to_broadcast([128, 32, 64]), op=ALU.mult), reads=[b_xtok[i], b_dtt[i]], writes=[b_u[i]])
                c.op("pool", lambda e: e.tensor_tensor(out=Vw[i][:], in0=u[i][:], in1=g_[:, 4, :].unsqueeze(2).to_broadcast([128, 32, 64]), op=ALU.mult), reads=[b_u[i], bg], writes=[b_Vw[i]])
            def groups_(ch, i, nxt):
                if nxt is not None:
                    prep_load(*nxt)
                xc, bxc = xcs[i], b_xc[i]
                g_, bg = gt[i], b_gt[i]
                dc = slice(d * 32, (d + 1) * 32)
                Y, bY = Yt[i], b_Yt[i]
                for g in range(4):
                    if g == 1 and nxt is not None:
                        prep(*nxt)
                    pcb, bpcb = self.ps[2], self.bps[2]
                    c.mm(pcb[:, 0:128], [(xc[:, 16 + g, :], xc[:, 20 + g, :])], reads=[bxc], writes=[bpcb])
                    sm, bsm = scm[g % 2], b_scm[g % 2]
                    c.op("dve", lambda e: e.tensor_tensor(out=sm[:], in0=pcb[:, 0:128], in1=tri, op=ALU.mult), reads=[bpcb, self.b_const], writes=[bsm])
                    yps, byps = self.ps[4 + g % 2], self.bps[4 + g % 2]
                    for hb in range(2):
                        k4 = (g % 2) * 2 + hb
                        for q4 in range(4):
                            h = g * 8 + hb * 4 + q4
                            c.op("dve", lambda e: e.tensor_scalar(out=aa[k4][:, q4 * 128:(q4 + 1) * 128], in0=bcs[i][:, h, :], scalar1=g_[:, 2, h:h + 1], scalar2=0.0, op0=ALU.add, op1=ALU.min),
                                 reads=[b_bcs[i], bg], writes=[b_aa[k4]])
                        c.op("act", lambda e: e.activation(out=EE[k4][:], in_=aa[k4][:], func=AF.Exp), reads=[b_aa[k4]], writes=[b_EE[k4]])
                        c.op("pool", lambda e: e.tensor_tensor(out=MT[k4][:].rearrange("p (h t) -> p h t", h=4), in0=EE[k4][:].rearrange("p (h t) -> p h t", h=4),
                                                               in1=sm[:].unsqueeze(1).to_broadcast([128, 4, 128]), op=ALU.mult), reads=[bsm, b_EE[k4]], writes=[b_MT[k4]])
                    for e8 in range(8):
                        h = g * 8 + e8
                        k4 = (g % 2) * 2 + e8 // 4
                        c.mm(yps[:, e8 * 64:(e8 + 1) * 64], [(MT[k4][:, (e8 % 4) * 128:(e8 % 4 + 1) * 128], u[i][:, h, :])], reads=[b_MT[k4], b_u[i]], writes=[byps])
                    pin, bpin = self.ps[3], self.bps[3]
                    c.mm(pin[:, :], [(xc[:, 20 + g, :], Sb[:, g, :])], reads=[bxc, b_S[g]], writes=[bpin])
                    y_, byi = yi[g % 2], b_yi[g % 2]
                    c.op("dve", lambda e: e.tensor_tensor(out=y_[:].rearrange("p (h d) -> p h d", d=64), in0=pin[:, :].rearrange("p (h d) -> p h d", d=64),
                                                          in1=g_[:, 3, g * 8:(g + 1) * 8].unsqueeze(2).to_broadcast([128, 8, 64]), op=ALU.mult), reads=[bpin, bg], writes=[byi])
                    c.op("dve", lambda e: e.tensor_tensor(out=Y[:, g * 512:(g + 1) * 512], in0=yps[:, :], in1=y_[:], op=ALU.add), reads=[byps, byi], writes=[bY])
                    if d == 0:
                        c.op("pool", lambda e: e.tensor_tensor(out=y_[:].rearrange("p (h d) -> p h d", d=64), in0=xtok[i][:, g * 8:(g + 1) * 8, :],
                                                               in1=dsk[:, g * 8:(g + 1) * 8].unsqueeze(2).to_broadcast([128, 8, 64]), op=ALU.mult), reads=[b_xtok[i], b_an, bY], writes=[byi])
                        c.op("pool", lambda e: e.tensor_tensor(out=Y[:, g * 512:(g + 1) * 512], in0=Y[:, g * 512:(g + 1) * 512], in1=y_[:], op=ALU.add), reads=[byi], writes=[bY])
                    pst, bpst = self.ps[3], self.bps[3]
                    c.mm(pst[:, :], [(Btok[i][:, g, :], Vw[i][:, g * 8:(g + 1) * 8, :].rearrange("p h d -> p (h d)"))], reads=[b_Bt[i], b_Vw[i]], writes=[bpst])
                    c.op("dve", lambda e: e.tensor_tensor(out=S32[:, g, :].rearrange("p (h d) -> p h d", d=64), in0=S32[:, g, :].rearrange("p (h d) -> p h d", d=64),
                                                          in1=g_[:, 5, g * 8:(g + 1) * 8].unsqueeze(2).to_broadcast([128, 8, 64]), op=ALU.mult), reads=[bg], writes=[b_S[g]])
                    c.op("dve", lambda e: e.tensor_tensor(out=S32[:, g, :], in0=S32[:, g, :], in1=pst[:, :], op=ALU.add), reads=[bpst], writes=[b_S[g]])
                    c.op("act", lambda e: e.activation(out=Sb[:, g, :], in_=S32[:, g, :], func=AF.Copy), reads=[], writes=[b_S[g]])
                c.dma("sp", Yd[d].ap()[ch], Y[:], reads=[bY], writes=[b_Y[d]])
            prep_load(order[0], 0)
            prep(order[0], 0)
            for idx, ch in enumerate(order):
                groups_(ch, idx % 2, (order[idx + 1], (idx + 1) % 2) if idx + 1 < len(order) else None)
        c.barrier()
        c.sb_release(self.mark0)
        Wo = c.sb([128, 16, 1024], BF16, "ssdWo"); b_wo = Buf()
        c.dma("pool", Wo[:], self.w["ssm_w_out"].ap()[j].rearrange("(k p) n -> p k n", p=128), writes=[b_wo])
        ng = c.sb([128, 2048], F32, "ssdng")
        c.dma("sp", ng[:], self.w["ssm_norm_g"].ap()[j:j + 1, :].partition_broadcast(128), writes=[b_wo])
        self.gated_out(li, need_ctx, Yd, b_Y, Zs, b_Z, 2048, 4, ng, Wo, b_wo, False)
        self.phase_end()

    def gated_out(self, li, need_ctx, Yd, b_Y, Zs, b_Z, W, ngroups, ng, Wo, b_wo, gate_after):
        c = self.c
        NCT, NT = self.NCT, self.NT
        KC = W // 128
        gs = W // ngroups
        G1x, bG1x = self.load_mod(li, 2, 0, "G1x")
        G1c, bG1c = self.load_mod(li, 2, 1, "G1c")
        yf = [c.sb([128, W], F32, "yf") for _ in range(2)]; yb = [c.sb([128, W], F32, "yb") for _ in range(2)]
        zt = [c.sb([128, W], BF16, "zt") for _ in range(2)]
        b_in = [Buf(), Buf()]
        jk = c.sb([128, W], BF16, "gjunk"); b_jk = Buf()
        st = [c.sb([128, 2, 8], F32, "gst") for _ in range(2)]; b_st = [Buf(), Buf()]
        yn = [c.sb([128, W], BF16, "yn") for _ in range(2)]; b_yn = [Buf(), Buf()]
        yT = [c.sb([128, KC, 128], BF16, "gyT") for _ in range(2)]; b_yT = [Buf(), Buf()]
        xts = [c.sb([128, D], F32, "xres") for _ in range(2)]; b_xt = [Buf(), Buf()]
        tmps = [c.sb([128, D], F32, "rtmp") for _ in range(2)]; b_tmp = [Buf(), Buf()]
        qbs = list(range(0 if need_ctx else NCT, NT))

        def go_load(bi_):
            i_ = bi_ % 2
            qb_ = qbs[bi_]
            c.dma("sp", yf[i_][:], Yd[0].ap()[qb_], reads=[b_Y[0]], writes=[b_in[i_]])
            c.dma("sp", yb[i_][:], Yd[1].ap()[qb_], reads=[b_Y[1]], writes=[b_in[i_]])
            c.dma("sp", zt[i_][:], Zs.ap()[qb_], reads=[b_Z], writes=[b_in[i_]])
            c.dma("sp", xts[i_][:], self.xs.ap()[qb_ * 128:(qb_ + 1) * 128, :], reads=[self.b_xs[qb_]], writes=[b_xt[i_]])
        go_load(0)
        for bi, qb in enumerate(qbs):
            i = bi % 2
            is_ctx = qb < NCT
            if bi + 1 < len(qbs):
                go_load(bi + 1)
            c.op("pool", lambda e: e.tensor_tensor(out=yf[i][:], in0=yf[i][:], in1=yb[i][:], op=ALU.add), reads=[b_in[i]], writes=[b_in[i]])
            if not gate_after:
                c.op("dve", lambda e: e.tensor_tensor(out=yf[i][:], in0=yf[i][:], in1=zt[i][:], op=ALU.mult), reads=[b_in[i]], writes=[b_in[i]])
            c.op("dve", lambda e: e.memset(st[i][:], 0.0), writes=[b_st[i]])
            for g in range(ngroups):
                c.op("act", lambda e: e.activation(out=jk[:, g * gs:(g + 1) * gs], in_=yf[i][:, g * gs:(g + 1) * gs], func=AF.Square, accum_out=st[i][:, 0, g:g + 1]), reads=[b_in[i]], writes=[b_jk, b_st[i]])
            c.op("dve", lambda e: e.tensor_scalar(out=st[i][:, 1, :], in0=st[i][:, 0, :], scalar1=1.0 / gs, scalar2=EPS, op0=ALU.mult, op1=ALU.add), reads=[b_st[i]], writes=[b_st[i]])
            c.op("act", lambda e: e.activation(out=st[i][:, 1, :], in_=st[i][:, 1, :], func=AF.Sqrt), reads=[b_st[i]], writes=[b_st[i]])
            c.op("dve", lambda e: e.reciprocal(out=st[i][:, 1, :], in_=st[i][:, 1, :]), reads=[b_st[i]], writes=[b_st[i]])
            c.op("dve", lambda e: e.tensor_tensor(out=yf[i][:].rearrange("p (g d) -> p g d", g=ngroups), in0=yf[i][:].rearrange("p (g d) -> p g d", g=ngroups),
                                                  in1=st[i][:, 1, 0:ngroups].unsqueeze(2).to_broadcast([128, ngroups, gs]), op=ALU.mult), reads=[b_st[i], b_in[i]], writes=[b_in[i]])
            if gate_after:
                c.op("pool", lambda e: e.tensor_tensor(out=yf[i][:], in0=yf[i][:], in1=ng[:], op=ALU.mult), reads=[b_in[i], b_wo], writes=[b_in[i]])
                c.op("dve", lambda e: e.tensor_tensor(out=yn[i][:], in0=yf[i][:], in1=zt[i][:], op=ALU.mult), reads=[b_in[i]], writes=[b_yn[i]])
            else:
                c.op("pool", lambda e: e.tensor_tensor(out=yn[i][:], in0=yf[i][:], in1=ng[:], op=ALU.mult), reads=[b_in[i], b_wo], writes=[b_yn[i]])
            for hb in range(KC // 8):
                pT = self.ps[hb].ap().bitcast(BF16)
                c.tr_multi([(pT[:, k * 128:(k + 1) * 128], yn[i][:, (hb * 8 + k) * 128:(hb * 8 + k + 1) * 128], self.identb) for k in range(8)], reads=[b_yn[i], self.b_const], writes=[self.bps[hb]])
                c.op("act", lambda e: e.activation(out=yT[i][:, hb * 8:(hb + 1) * 8, :], in_=pT.rearrange("p (k n) -> p k n", k=8), func=AF.Copy), reads=[self.bps[hb]], writes=[b_yT[i]])
            G1, bG1 = (G1c, bG1c) if is_ctx else (G1x, bG1x)
            for nn in range(2):
                z, bz = self.ps[5 + nn], self.bps[5 + nn]
                c.mm(z[:, :], [(yT[i][:, k, :], Wo[:, k, nn * 512:(nn + 1) * 512]) for k in range(KC)], reads=[b_yT[i], b_wo], writes=[bz])
                c.op("dve", lambda e: e.tensor_tensor(out=tmps[i][:, nn * 512:(nn + 1) * 512], in0=z[:, :], in1=G1[:, nn * 512:(nn + 1) * 512], op=ALU.mult), reads=[bz, bG1], writes=[b_tmp[i]])
            c.op("pool", lambda e: e.tensor_tensor(out=xts[i][:], in0=xts[i][:], in1=tmps[i][:], op=ALU.add), reads=[b_tmp[i], b_xt[i]], writes=[b_xt[i]])
            c.dma("sp", self.xs.ap()[qb * 128:(qb + 1) * 128, :], xts[i][:], reads=[b_xt[i]], writes=[self.b_xs[qb]])

    def layer_mlstm(self, li, j, need_ctx):
        c, nc = self.c, self.nc
        T, NT, NCT = self.T, self.NT, self.NCT
        w_in = self.w["mlstm_w_in"].ap()[j]
        QK = self.scratch(f"ml_qk{li}", [2, 8, 64, T], BF16)
        Kt = self.scratch(f"ml_kt{li}", [NT, 128, 512], BF16)
        Va = self.scratch(f"ml_va{li}", [NT, 128, 8, 129], BF16)
        Os = self.scratch(f"ml_os{li}", [NT, 128, 1024], BF16)
        Gt = self.scratch(f"ml_gt{li}", [NT, 128, 32], F32)
        Yd = [self.scratch(f"ml_y{li}_{d}", [NT, 128, 1024], F32) for d in range(2)]
        b_QK = Buf(); b_Kt = Buf(); b_Va = Buf(); b_Os = Buf(); b_Gt = Buf(); b_Y = [Buf(), Buf()]
        b_w = Buf("ml_w")
        Win = c.sb([128, 8, 3104], BF16, "mlWin")
        for k in range(8):
            c.dma("pool", Win[:, k, :], w_in[k * 128:(k + 1) * 128, :], writes=[b_w])
        gb = c.sb([128, 32], F32, "mlgb")
        c.dma("sp", gb[:], self.w["mlstm_gate_b"].ap()[j:j + 1].rearrange("o a h -> o (a h)").partition_broadcast(128), writes=[b_w])
        A1x, bA1x = self.load_mod(li, 1, 0, "A1x")
        S1x, bS1x = self.load_mod(li, 0, 0, "S1x")
        A1c, bA1c = self.load_mod(li, 1, 1, "A1c")
        S1c, bS1c = self.load_mod(li, 0, 1, "S1c")
        self.alloc_norm_bufs(2)
        self.set_norm_order([t0_ + s_ for (t0_, n_, _c) in self.groups(4) for s_ in range(n_)])
        hTs = [c.sb([128, 8, 512], BF16, "hT") for _ in range(2)]
        b_hT = [Buf(), Buf()]
        stg = [c.sb([64, 512], BF16, "mlstg") for _ in range(3)]; b_stg = [Buf() for _ in range(3)]
        kst = [c.sb([128, 512], BF16, "mlkst") for _ in range(2)]; b_kst = [Buf(), Buf()]
        vst = [c.sb([128, 8, 129], BF16, "mlvst") for _ in range(2)]; b_vst = [Buf(), Buf()]
        ost = [c.sb([128, 1024], BF16, "mlost") for _ in range(2)]; b_ost = [Buf(), Buf()]
        gst = [c.sb([128, 32], F32, "mlgst") for _ in range(2)]; b_gst = [Buf(), Buf()]
        gtmp = [c.sb([128, 8], F32, "mlgtmp") for _ in range(2)]
        for i in range(2):
            c.op("pool", lambda e: e.memset(vst[i][:], 1.0), writes=[b_vst[i]])
        cnt = 0
        for gi, (t0, n, is_ctx) in enumerate(self.groups(4)):
            ncols = n * 128
            col0 = t0 * 128
            hT, bh = hTs[gi % 2], b_hT[gi % 2]
            for s in range(n):
                if is_ctx:
                    self.norm_tile(t0 + s, A1c, bA1c, S1c, bS1c, hT, bh, s * 128, (t0 + s) % 2)
                else:
                    self.norm_tile(t0 + s, A1x, bA1x, S1x, bS1x, hT, bh, s * 128, (t0 + s) % 2)
            for qk in range(2):
                for h in range(8):
                    i = cnt % 3
                    cnt += 1
                    P, bP = self.ps[2 + i], self.bps[2 + i]
                    col = qk * 512 + h * 64
                    c.mm(P[0:64, :ncols], [(Win[:, k, col:col + 64], hT[:, k, :ncols]) for k in range(8)], reads=[b_w, bh], writes=[bP])
                    c.op("act", lambda e: e.activation(out=stg[i][:, :ncols], in_=P[0:64, :ncols], func=AF.Copy, scale=(0.125 if qk == 1 else 1.0)), reads=[bP], writes=[b_stg[i]])
                    c.dma("sp", QK.ap()[qk, h, :, col0:col0 + ncols], stg[i][:, :ncols], reads=[b_stg[i]], writes=[b_QK])
            for s in range(n):
                sl = slice(s * 128, (s + 1) * 128)
                i2 = s % 2

                def tokproj(c0, w_):
                    nonlocal cnt
                    i = cnt % 3
                    cnt += 1
                    P, bP = self.ps[2 + i], self.bps[2 + i]
                    c.mm(P[:, :w_], [(hT[:, k, sl], Win[:, k, c0:c0 + w_]) for k in range(8)], reads=[b_w, bh], writes=[bP])
                    return P, bP
                P, bP = tokproj(512, 512)
                c.op("act", lambda e: e.activation(out=kst[i2][:], in_=P[:, :], func=AF.Copy, scale=0.125), reads=[bP], writes=[b_kst[i2]])
                c.dma("sp", Kt.ap()[t0 + s], kst[i2][:], reads=[b_kst[i2]], writes=[b_Kt])
                for vh in range(2):
                    P, bP = tokproj(1024 + vh * 512, 512)
                    c.op("act", lambda e: e.activation(out=vst[i2][:, vh * 4:(vh + 1) * 4, 0:128], in_=P[:, :].rearrange("p (h d) -> p h d", d=128), func=AF.Copy), reads=[bP], writes=[b_vst[i2]])
                c.dma("sp", Va.ap()[t0 + s], vst[i2][:], reads=[b_vst[i2]], writes=[b_Va])
                for oh in range(2):
                    P, bP = tokproj(2048 + oh * 512, 512)
                    c.op("act", lambda e: e.activation(out=ost[i2][:, oh * 512:(oh + 1) * 512], in_=P[:, :], func=AF.Sigmoid), reads=[bP], writes=[b_ost[i2]])
                c.dma("sp", Os.ap()[t0 + s], ost[i2][:], reads=[b_ost[i2]], writes=[b_Os])
                P, bP = tokproj(3072, 32)
                g_ = gst[i2]
                c.op("dve", lambda e: e.tensor_tensor(out=g_[:], in0=P[:, 0:32], in1=gb[:], op=ALU.add), reads=[bP, b_w], writes=[b_gst[i2]])
                for r in (1, 3):
                    cs_ = slice(r * 8, (r + 1) * 8)
                    c.op("act", lambda e: e.activation(out=g_[:, cs_], in_=g_[:, cs_], func=AF.Exp, scale=-1.0), reads=[b_gst[i2]], writes=[b_gst[i2]])
                    c.op("act", lambda e: e.activation(out=g_[:, cs_], in_=g_[:, cs_], func=AF.Ln, bias=1.0), reads=[b_gst[i2]], writes=[b_gst[i2]])
                    c.op("act", lambda e: e.activation(out=g_[:, cs_], in_=g_[:, cs_], func=AF.Copy, scale=-1.0), reads=[b_gst[i2]], writes=[b_gst[i2]])
                c.dma("sp", Gt.ap()[t0 + s], g_[:], reads=[b_gst[i2]], writes=[b_Gt])
        c.barrier()
        c.sb_release(self.mark0)
        sel = c.sb([8, 8, 128], F32, "sel8"); b_sel = Buf()
        c.dma("sp", sel[:], self.w["sel32"].ap()[0:8, 0:8, :], writes=[b_sel])
        qTs = [c.sb([64, 8, 128], BF16, "mqT") for _ in range(2)]; kTs = [c.sb([64, 8, 128], BF16, "mkT") for _ in range(2)]
        kts = [c.sb([128, 512], BF16, "mkt") for _ in range(2)]; vas = [c.sb([128, 8, 129], BF16, "mva") for _ in range(2)]
        gts = [c.sb([128, 32], F32, "mgt") for _ in range(2)]; b_ld = [Buf(), Buf()]
        GM = [c.sb([8, 12, 128], F32, "GM") for _ in range(2)]; b_GM = [Buf(), Buf()]
        sm8 = [c.sb([8, 8], F32, "sm8") for _ in range(2)]; b_sm8 = [Buf(), Buf()]
        ms = c.sb([8, 2], F32, "ms"); b_ms = Buf()
        dg = c.sb([8, 8], F32, "dg"); b_dg = Buf()
        tk = [c.sb([128, 40], F32, "tk") for _ in range(2)]; b_tk = [Buf(), Buf()]
        cwc = [c.sb([64, 8], F32, "cwc") for _ in range(2)]; b_cwc = [Buf(), Buf()]
        scm = [c.sb([128, 512], F32, "mscm") for _ in range(2)]; b_scm = [Buf() for _ in range(2)]
        aa = [c.sb([128, 512], F32, "maa") for _ in range(2)]; b_aa = [Buf() for _ in range(2)]
        EE = [c.sb([128, 512], F32, "mEE") for _ in range(2)]; b_EE = [Buf() for _ in range(2)]
        MT = [c.sb([128, 512], BF16, "mMT") for _ in range(2)]; b_MT = [Buf() for _ in range(2)]
        yi = [c.sb([128, 129], F32, "myi") for _ in range(4)]; b_yi = [Buf() for _ in range(4)]
        nd4 = [c.sb([128, 4, 132], F32, "mnd4") for _ in range(2)]; b_nd4 = [Buf() for _ in range(2)]
        Vw = [c.sb([128, 129], BF16, "mVw") for _ in range(4)]; b_Vw = [Buf() for _ in range(4)]
        Yt = [c.sb([128, 1024], F32, "mYt") for _ in range(2)]; b_Yt = [Buf(), Buf()]
        S32 = c.sb([64, 8, 129], F32, "mS32"); Sb = c.sb([64, 8, 129], BF16, "mSb"); b_S = [Buf() for _ in range(8)]
        ci = 0; hi = 0
        id8 = self.ident32[0:8, 0:8]
        for d in range(2):
            tri = self.trile32 if d == 0 else self.trige32
            order = list(range(NT)) if d == 0 else (list(range(NCT - 1, -1, -1)) + list(range(NT - 1, NCT - 1, -1)))
            c.op("dve", lambda e: e.memset(S32[:], 0.0), writes=b_S)
            c.op("pool", lambda e: e.memset(Sb[:], 0.0), writes=b_S)
            c.op("dve", lambda e: e.memset(ms[:], 0.0), writes=[b_ms])
            endc = 127 if d == 0 else 0
            def gate_load(ch, i):
                cols = slice(ch * 128, (ch + 1) * 128)
                bl = b_ld[i]
                c.dma("sp", qTs[i][:], QK.ap()[0, :, :, cols].rearrange("h d t -> d h t"), reads=[b_QK], writes=[bl])
                c.dma("sp", kTs[i][:], QK.ap()[1, :, :, cols].rearrange("h d t -> d h t"), reads=[b_QK], writes=[bl])
                c.dma("sp", kts[i][:], Kt.ap()[ch], reads=[b_Kt], writes=[bl])
                c.dma("sp", vas[i][:], Va.ap()[ch], reads=[b_Va], writes=[bl])
                c.dma("sp", gts[i][:], Gt.ap()[ch], reads=[b_Gt], writes=[bl])

            def gate(ch, i):
                bl = b_ld[i]
                ig = gts[i][:, d * 16:d * 16 + 8]
                lf = gts[i][:, d * 16 + 8:d * 16 + 16]
                G, bG = GM[i], b_GM[i]
                s8, bs8 = sm8[i], b_sm8[i]
                t_, bt = tk[i], b_tk[i]
                p0, bp0 = self.ps[0], self.bps[0]
                c.mm(p0[0:8, 0:128], [(ig, self.ident32)], reads=[bl, self.b_const], writes=[bp0])
                c.mm(p0[0:8, 128:256], [(lf, tri)], reads=[bl, self.b_const], writes=[bp0])
                c.mm(p0[:, 256:264], [(tri, lf)], reads=[bl, self.b_const], writes=[bp0])
                c.op("act", lambda e: e.activation(out=G[:, 0:2, :], in_=p0[0:8, 0:256].rearrange("p (a t) -> p a t", a=2), func=AF.Copy), reads=[bp0], writes=[bG])
                c.op("act", lambda e: e.activation(out=t_[:, 32:40], in_=p0[:, 256:264], func=AF.Copy), reads=[bp0], writes=[bt])
                c.op("dve", lambda e: e.tensor_tensor(out=t_[:, 0:8], in0=ig, in1=t_[:, 32:40], op=ALU.subtract), reads=[bl, bt], writes=[bt])
                c.op("dve", lambda e: e.tensor_tensor(out=G[:, 2, :], in0=G[:, 0, :], in1=G[:, 1, :], op=ALU.subtract), reads=[bG], writes=[bG])
                src, dst = 2, 3
                for k in range(7):
                    sft = 1 << k
                    c.op("dve", lambda e: e.tensor_copy(out=G[:, dst, :], in_=G[:, src, :]), reads=[bG], writes=[bG])
                    if d == 0:
                        c.op("dve", lambda e: e.tensor_tensor(out=G[:, dst, sft:128], in0=G[:, src, sft:128], in1=G[:, src, 0:128 - sft], op=ALU.max), reads=[bG], writes=[bG])
                    else:
                        c.op("dve", lambda e: e.tensor_tensor(out=G[:, dst, 0:128 - sft], in0=G[:, src, 0:128 - sft], in1=G[:, src, sft:128], op=ALU.max), reads=[bG], writes=[bG])
                    src, dst = dst, (3 if dst == 4 else 4)
                cmr = src
                c.op("dve", lambda e: e.tensor_scalar(out=G[:, cmr, :], in0=G[:, cmr, :], scalar1=ms[:, 0:1], scalar2=None, op0=ALU.max), reads=[bG, b_ms], writes=[bG])
                c.op("act", lambda e: e.activation(out=G[:, 5, :], in_=G[:, cmr, :], func=AF.Copy, scale=-1.0), reads=[bG], writes=[bG])
                c.op("act", lambda e: e.activation(out=G[:, 6, :], in_=G[:, cmr, :], func=AF.Exp, scale=-1.0, bias=ms[:, 0:1]), reads=[bG, b_ms], writes=[bG])
                c.op("dve", lambda e: e.tensor_tensor(out=G[:, 7, :], in0=G[:, 1, :], in1=G[:, cmr, :], op=ALU.add), reads=[bG], writes=[bG])
                c.op("act", lambda e: e.activation(out=G[:, 7, :], in_=G[:, 7, :], func=AF.Exp, scale=-1.0), reads=[bG], writes=[bG])
                c.op("dve", lambda e: e.tensor_copy(out=s8[:, 0:1], in_=G[:, cmr, endc:endc + 1]), reads=[bG], writes=[bs8])
                c.op("dve", lambda e: e.tensor_scalar(out=s8[:, 1:2], in0=s8[:, 0:1], scalar1=-1.0, scalar2=None, op0=ALU.mult), reads=[bs8], writes=[bs8])
                c.op("dve", lambda e: e.tensor_copy(out=s8[:, 2:3], in_=G[:, 1, endc:endc + 1]), reads=[bG], writes=[bs8])
                c.op("act", lambda e: e.activation(out=G[:, 8, :], in_=G[:, 2, :], func=AF.Exp, bias=s8[:, 1:2]), reads=[bG, bs8], writes=[bG])
                c.op("act", lambda e: e.activation(out=s8[:, 3:4], in_=ms[:, 0:1], func=AF.Exp, bias=s8[:, 1:2]), reads=[b_ms, bs8], writes=[bs8])
                c.op("dve", lambda e: e.tensor_tensor(out=ms[:, 0:1], in0=s8[:, 2:3], in1=s8[:, 0:1], op=ALU.add), reads=[bs8, bG], writes=[b_ms])
                p1, bp1 = self.ps[1], self.bps[1]
                c.mm_multi([(p1[:, (r - 6) * 8:(r - 5) * 8], [(G[:, r, :], id8)]) for r in (6, 7, 8)], reads=[bG, self.b_const], writes=[bp1])
                c.op("act", lambda e: e.activation(out=t_[:, 8:32], in_=p1[:, 0:24], func=AF.Copy), reads=[bp1], writes=[bt])
                c.op("dve", lambda e: e.tensor_scalar(out=dg[:], in0=id8, scalar1=s8[:, 3:4], scalar2=None, op0=ALU.mult), reads=[self.b_const, bs8], writes=[b_dg])
                c.mm(p1[0:64, 32:40], [(self.ones32[0:8, 0:64], dg[:])], reads=[self.b_const, b_dg], writes=[bp1])
                c.op("act", lambda e: e.activation(out=cwc[i][:], in_=p1[0:64, 32:40], func=AF.Copy), reads=[bp1], writes=[b_cwc[i]])
            def heads(ch, i, nxt):
                if nxt is not None:
                    gate_load(*nxt)
                bl = b_ld[i]; G, bG = GM[i], b_GM[i]; t_, bt = tk[i], b_tk[i]
                Y, bY = Yt[i], b_Yt[i]
                for hb0 in (0, 4):
                    if hb0 == 4 and nxt is not None:
                        gate(*nxt)
                    psc, bpsc = self.ps[2], self.bps[2]
                    pbc, bpbc = self.ps[6], self.bps[6]
                    for q4 in range(4):
                        h = hb0 + q4
                        cs4 = slice(q4 * 128, (q4 + 1) * 128)
                        c.mm(psc[:, cs4], [(kTs[i][:, h, :], qTs[i][:, h, :])], reads=[bl], writes=[bpsc])
                        c.mm(pbc[:, cs4], [(sel[:, h, :], G[:, 5, :]), (G[:, 2, :], sel[:, h, :])], reads=[b_sel, bG], writes=[bpbc])
                    pins = []
                    for q4 in range(4):
                        h = hb0 + q4
                        bk = 3 if q4 < 2 else 7
                        pin_ap = self.ps[bk][:, (q4 % 2) * 256:(q4 % 2) * 256 + 129]
                        c.mm(pin_ap, [(qTs[i][:, h, :], Sb[:, h, :])], reads=[bl, b_S[h]], writes=[self.bps[bk]])
                        pins.append((pin_ap, self.bps[bk]))
                    k4 = (hb0 // 4)
                    c.op("dve", lambda e: e.tensor_tensor(out=scm[k4][:].rearrange("p (h t) -> p h t", h=4), in0=psc[:, :].rearrange("p (h t) -> p h t", h=4),
                                                          in1=tri.unsqueeze(1).to_broadcast([128, 4, 128]), op=ALU.mult), reads=[bpsc, self.b_const], writes=[b_scm[k4]])
                    c.op("dve", lambda e: e.tensor_scalar(out=aa[k4][:], in0=pbc[:, :], scalar1=0.0, scalar2=None, op0=ALU.min), reads=[bpbc], writes=[b_aa[k4]])
                    c.op("act", lambda e: e.activation(out=EE[k4][:], in_=aa[k4][:], func=AF.Exp), reads=[b_aa[k4]], writes=[b_EE[k4]])
                    c.op("pool", lambda e: e.tensor_tensor(out=MT[k4][:], in0=scm[k4][:], in1=EE[k4][:], op=ALU.mult), reads=[b_scm[k4], b_EE[k4]], writes=[b_MT[k4]])
                    for q4 in range(4):
                        h = hb0 + q4
                        pin_ap, bpin = pins[q4]
                        c.op("act", lambda e: e.activation(out=yi[q4][:], in_=pin_ap, func=AF.Copy, scale=t_[:, 8 + h:9 + h]), reads=[bpin, bt], writes=[b_yi[q4]])
                        c.op("pool", lambda e: e.tensor_tensor(out=Vw[q4][:], in0=vas[i][:, h, :], in1=t_[:, 24 + h:25 + h].to_broadcast([128, 129]), op=ALU.mult), reads=[bl, bt], writes=[b_Vw[q4]])
                    pnds = []; psts = []
                    for q4 in range(4):
                        h = hb0 + q4
                        bk = 4 + q4 // 2
                        pnd_ap = self.ps[bk][:, (q4 % 2) * 256:(q4 % 2) * 256 + 129]
                        c.mm(pnd_ap, [(MT[k4][:, q4 * 128:(q4 + 1) * 128], vas[i][:, h, :])], reads=[b_MT[k4], bl], writes=[self.bps[bk]])
                        pnds.append((pnd_ap, self.bps[bk]))
                    for q4 in range(4):
                        h = hb0 + q4
                        if q4 < 3:
                            pst_ap, bpst = self.ps[1][0:64, q4 * 129:(q4 + 1) * 129], self.bps[1]
                        else:
                            pst_ap, bpst = self.ps[0][0:64, 264:393], self.bps[0]
                        c.mm(pst_ap, [(kts[i][:, h * 64:(h + 1) * 64], Vw[q4][:])], reads=[bl, b_Vw[q4]], writes=[bpst])
                        psts.append((pst_ap, bpst))
                    n4 = nd4[k4]; bn = b_nd4[k4]
                    for q4 in range(4):
                        pnd_ap, bpnd = pnds[q4]
                        c.op("dve", lambda e: e.tensor_tensor(out=n4[:, q4, 0:129], in0=pnd_ap, in1=yi[q4][:], op=ALU.add), reads=[bpnd, b_yi[q4]], writes=[bn])
                    c.op("dve", lambda e: e.tensor_scalar(out=n4[:, :, 129:130], in0=n4[:, :, 128:129], scalar1=-1.0, scalar2=None, op0=ALU.mult), reads=[bn], writes=[bn])
                    c.op("dve", lambda e: e.tensor_tensor(out=n4[:, :, 130:131], in0=n4[:, :, 128:129], in1=n4[:, :, 129:130], op=ALU.max), reads=[bn], writes=[bn])
                    c.op("dve", lambda e: e.tensor_tensor(out=n4[:, :, 130:131], in0=n4[:, :, 130:131], in1=t_[:, 16 + hb0:20 + hb0].unsqueeze(2), op=ALU.max), reads=[bn, bt], writes=[bn])
                    c.op("dve", lambda e: e.reciprocal(out=n4[:, :, 131:132], in_=n4[:, :, 130:131]), reads=[bn], writes=[bn])
                    c.op("dve", lambda e: e.tensor_tensor(out=Y[:, hb0 * 128:(hb0 + 4) * 128].rearrange("p (h d) -> p h d", h=4), in0=n4[:, :, 0:128],
                                                          in1=n4[:, :, 131:132].to_broadcast([128, 4, 128]), op=ALU.mult), reads=[bn], writes=[bY])
                    for q4 in range(4):
                        h = hb0 + q4
                        pst_ap, bpst = psts[q4]
                        c.op("dve", lambda e: e.scalar_tensor_tensor(out=S32[:, h, :], in0=S32[:, h, :], scalar=cwc[i][:, h:h + 1], in1=pst_ap, op0=ALU.mult, op1=ALU.add), reads=[b_cwc[i], bpst], writes=[b_S[h]])
                        c.op("act", lambda e: e.activation(out=Sb[:, h, :], in_=S32[:, h, :], func=AF.Copy), reads=[], writes=[b_S[h]])
                c.dma("sp", Yd[d].ap()[ch], Y[:], reads=[bY], writes=[b_Y[d]])
            gate_load(order[0], 0)
            gate(order[0], 0)
            for idx, ch in enumerate(order):
                heads(ch, idx % 2, (order[idx + 1], (idx + 1) % 2) if idx + 1 < len(order) else None)
        c.barrier()
        c.sb_release(self.mark0)
        Wo = c.sb([128, 8, 1024], BF16, "mlWo"); b_wo = Buf()
        c.dma("pool", Wo[:], self.w["mlstm_w_out"].ap()[j].rearrange("(k p) n -> p k n", p=128), writes=[b_wo])
        ng = c.sb([128, 1024], F32, "mlng")
        c.dma("sp", ng[:], self.w["mlstm_norm_g"].ap()[j:j + 1, :].partition_broadcast(128), writes=[b_wo])
        self.gated_out(li, need_ctx, Yd, b_Y, Os, b_Os, 1024, 8, ng, Wo, b_wo, True)
        self.phase_end()

    def build(self, wshapes):
        self.inp("x", [self.NL, D])
        self.inp("ctx", [self.NCX, D])
        self.inp("cT", [128, 8, 2])
        self.inp("cmat", [128, 4, 128])
        self.inp("sel32", [32, 32, 128])
        self.inp("rope64", [2, 64, self.NL])
        self.inp("rope32", [2, 32, self.NL])
        self.inp("rope96", [2, 96, self.NL])
        self.inp("ssm_conv_wT", [wshapes["ssm_conv_w"][0], 128, 24, 5])
        self.inp("ssm_conv_bT", [wshapes["ssm_conv_w"][0], 128, 24])
        for k, s in wshapes.items():
            self.inp(k, s)
        self.out = self.nc.dram_tensor("out", [self.NL, D], F32, kind="ExternalOutput")
        self.setup_consts()
        self.prologue()
        cnt = {0: 0, 1: 0, 2: 0, 3: 0, 9: 0}
        for li, kind in enumerate(self.kinds):
            need_ctx = li < self.depth - 1
            j = cnt[kind]
            cnt[kind] += 1
            self.want_precast = li
            if kind == 0:
                self.layer_gqa(li, j, need_ctx)
            elif kind == 1:
                self.layer_ssd(li, j, need_ctx)
            elif kind == 2:
                self.layer_mlstm(li, j, need_ctx)
            elif kind == 3:
                self.layer_mla(li, j, need_ctx)
            self.layer_moe(li, need_ctx)
        self.final()
        return self.nc


WEIGHT_KEYS = ["norm1_g", "norm2_g", "w_mod", "b_mod", "moe_w_group", "moe_b_group", "moe_w_expert", "moe_b_expert",
               "moe_w_gate", "moe_w_up", "moe_w_down", "attn_w_in", "attn_sink", "attn_w_out",
               "ssm_w_in", "ssm_conv_w", "ssm_conv_b", "ssm_dt_bias", "ssm_a_log", "ssm_d", "ssm_norm_g", "ssm_w_out",
               "mlstm_w_in", "mlstm_gate_b", "mlstm_norm_g", "mlstm_w_out",
               "mla_w_in", "mla_q_norm_g", "mla_w_q_up", "mla_kv_norm_g", "mla_w_kv_up", "mla_w_out", "final_norm_g"]


def run_model(inputs, kinds, n_cores=None):
    x = np.asarray(inputs["x"], np.float32)
    ctx = np.asarray(inputs["ctx"], np.float32)
    c = np.asarray(inputs["c"], np.float32)
    c_ctx = np.asarray(inputs["c_ctx"], np.float32)
    B, n_lat, _ = x.shape
    n_ctx = ctx.shape[1]
    weights = {k: np.ascontiguousarray(np.asarray(inputs[k], np.float32)) for k in WEIGHT_KEYS}
    m = Model(n_lat, n_ctx, kinds)
    nc = m.build({k: v.shape for k, v in weights.items()})
    consts = host_consts(n_lat)
    in_maps = []
    for b in range(B):
        cT = np.stack([c[b].reshape(8, 128).T, c_ctx.reshape(8, 128).T], axis=-1)
        d = {"x": np.ascontiguousarray(x[b]), "ctx": np.ascontiguousarray(ctx[b]), "cT": np.ascontiguousarray(cT.astype(np.float32))}
        d.update(consts)
        d.update(weights)
        d["ssm_conv_wT"] = np.ascontiguousarray(weights["ssm_conv_w"].reshape(-1, 5, 24, 128).transpose(0, 3, 2, 1))
        d["ssm_conv_bT"] = np.ascontiguousarray(weights["ssm_conv_b"].reshape(-1, 24, 128).transpose(0, 2, 1))
        in_maps.append(d)
    res = run_bass_kernel_spmd(nc, in_maps, core_ids=list(range(B)))
    return np.stack([np.asarray(r["out"], np.float32) for r in res.results], axis=0)


def kernel(**inputs):
    return run_model(inputs, [0, 1, 2, 3])
```
